# Optimizing a Trainium2 kernel written in Bass

```python
import math
import jax
import jax.numpy as jnp
from jax import lax
import numpy as np

D_MODEL = 2048
BATCH = 4
SEQ = 4096
DEPTH = 2

GRID_W = 64
CTX_LEN = 256
N_MOD = 6
EPS = 1e-6
F32 = jnp.float32

ATTN_HEADS = 8
ATTN_KV_HEADS = 2
HEAD_DIM = 128
ATTN_GROUP = ATTN_HEADS // ATTN_KV_HEADS
ATTN_SCALE = HEAD_DIM ** -0.5
WINDOW = 128
BLOCK = 128
ROPE_THETA = 10000.0

S5_WIDTH = D_MODEL // 2
S5_CH = 16
S5_GROUPS = S5_WIDTH // S5_CH
S5_STATE = 64

AB_Q = ATTN_HEADS * HEAD_DIM
AB_KV = ATTN_KV_HEADS * HEAD_DIM
AB_IN = AB_Q + 2 * AB_KV + S5_WIDTH
AB_OUT = AB_Q + S5_WIDTH

SSD_INNER = 2 * D_MODEL
SSD_HEAD_DIM = 64
SSD_HEADS = SSD_INNER // SSD_HEAD_DIM
SSD_GROUPS = 8
SSD_HPG = SSD_HEADS // SSD_GROUPS
SSD_STATE = 128
SSD_CONV = 5
SSD_CHUNK = 128
SSD_XBC = SSD_INNER + 2 * SSD_GROUPS * SSD_STATE
SSD_IN = SSD_INNER + SSD_XBC + 2 * SSD_HEADS

PEER_HEADS = 8
PEER_NKEYS = 128
PEER_EXPERTS = PEER_NKEYS * PEER_NKEYS
PEER_QDIM = 128
PEER_HALF = PEER_QDIM // 2
PEER_TOPK = 16
PEER_TOKENS = 128

kernel_name = "hybrid_swa_s5_ssd_peer_dit"


def rmsnorm(x, g):
    x32 = x.astype(F32)
    y = x32 * lax.rsqrt(jnp.mean(x32 * x32, axis=-1, keepdims=True) + EPS)
    return (y * g.astype(F32)).astype(x.dtype)


def modulate(h, shift, scale):
    return h * (1 + scale) + shift


def axial_rope(x, length):
    t = jnp.arange(length)
    row = (t // GRID_W).astype(F32)
    col = (t % GRID_W).astype(F32)
    n_freq = HEAD_DIM // 4
    inv = ROPE_THETA ** (-jnp.arange(n_freq, dtype=F32) / n_freq)
    ang = jnp.concatenate([row[:, None] * inv, col[:, None] * inv], axis=-1)
    ang = ang.reshape((1, length) + (1,) * (x.ndim - 3) + (HEAD_DIM // 2,))
    cos = jnp.cos(ang).astype(x.dtype)
    sin = jnp.sin(ang).astype(x.dtype)
    x1, x2 = jnp.split(x, 2, axis=-1)
    return jnp.concatenate([x1 * cos - x2 * sin, x2 * cos + x1 * sin], axis=-1)


def window_attention_latent(q, k, v, k_ctx, v_ctx, sink):
    bsz, seq = q.shape[:2]
    nb = seq // BLOCK
    qb = q.reshape(bsz, nb, BLOCK, ATTN_KV_HEADS, ATTN_GROUP, HEAD_DIM)

    def bands(t):
        tp = jnp.pad(t, ((0, 0), (BLOCK, BLOCK), (0, 0), (0, 0)))
        tp = tp.reshape(bsz, nb + 2, BLOCK, ATTN_KV_HEADS, HEAD_DIM)
        return jnp.concatenate([tp[:, :-2], tp[:, 1:-1], tp[:, 2:]], axis=2)

    kb, vb = bands(k), bands(v)
    s_band = jnp.einsum('bnqhgd,bnkhd->bnhgqk', qb, kb).astype(F32) * ATTN_SCALE
    start = jnp.arange(nb)[:, None, None] * BLOCK
    qpos = start + jnp.arange(BLOCK)[None, :, None]
    kpos = start - BLOCK + jnp.arange(3 * BLOCK)[None, None, :]
    ok = (jnp.abs(qpos - kpos) <= WINDOW) & (kpos >= 0) & (kpos < seq)
    s_band = jnp.where(ok[None, :, None, None], s_band, -jnp.inf)
    s_ctx = jnp.einsum('bnqhgd,bchd->bnhgqc', qb, k_ctx).astype(F32) * ATTN_SCALE
    s_sink = jnp.broadcast_to(sink.astype(F32)[None, None, :, :, None, None], s_ctx.shape[:-1] + (1,))
    p = jax.nn.softmax(jnp.concatenate([s_sink, s_ctx, s_band], axis=-1), axis=-1).astype(v.dtype)
    n_ctx = k_ctx.shape[1]
    o = (jnp.einsum('bnhgqc,bchd->bnqhgd', p[..., 1:1 + n_ctx], v_ctx)
         + jnp.einsum('bnhgqk,bnkhd->bnqhgd', p[..., 1 + n_ctx:], vb))
    return o.reshape(bsz, seq, AB_Q)


def context_attention(q, k, v, sink):
    bsz, length = q.shape[:2]
    s = jnp.einsum('bqhgd,bkhd->bhgqk', q, k).astype(F32) * ATTN_SCALE
    s_sink = jnp.broadcast_to(sink.astype(F32)[None, :, :, None, None], s.shape[:-1] + (1,))
    p = jax.nn.softmax(jnp.concatenate([s_sink, s], axis=-1), axis=-1).astype(v.dtype)
    o = jnp.einsum('bhgqk,bkhd->bqhgd', p[..., 1:], v)
    return o.reshape(bsz, length, AB_Q)


def s5_discretise(a_re, a_im, log_dt, b_re, b_im):
    dt = jnp.exp(log_dt.astype(F32))
    ar, ai = a_re.astype(F32), a_im.astype(F32)
    mag = jnp.exp(dt * ar)
    abar_re, abar_im = mag * jnp.cos(dt * ai), mag * jnp.sin(dt * ai)
    den = ar * ar + ai * ai
    f_re = ((abar_re - 1) * ar + abar_im * ai) / den
    f_im = (abar_im * ar - (abar_re - 1) * ai) / den
    br, bi = b_re.astype(F32), b_im.astype(F32)
    bbar_re = f_re[..., None] * br - f_im[..., None] * bi
    bbar_im = f_re[..., None] * bi + f_im[..., None] * br
    return abar_re, abar_im, bbar_re, bbar_im


def _complex_affine_combine(e1, e2):
    a1r, a1i, b1r, b1i = e1
    a2r, a2i, b2r, b2i = e2
    return (a2r * a1r - a2i * a1i, a2r * a1i + a2i * a1r,
            a2r * b1r - a2i * b1i + b2r, a2r * b1i + a2i * b1r + b2i)


def complex_diag_scan(abar_re, abar_im, bu_re, bu_im, reverse, init=None):
    if init is not None:
        s_re, s_im = init
        first = -1 if reverse else 0
        bu_re = bu_re.at[:, first].add(abar_re * s_re - abar_im * s_im)
        bu_im = bu_im.at[:, first].add(abar_re * s_im + abar_im * s_re)
    length = bu_re.shape[1]
    ar = jnp.broadcast_to(abar_re, (1, length) + abar_re.shape)
    ai = jnp.broadcast_to(abar_im, (1, length) + abar_im.shape)
    _, _, st_re, st_im = lax.associative_scan(_complex_affine_combine, (ar, ai, bu_re, bu_im),
                                              reverse=reverse, axis=1)
    return st_re, st_im


def s5_mixer(u_lat, u_ctx, a_re, a_im, log_dt, b_re, b_im, c_re, c_im, d, glu_w, glu_b, with_ctx):
    def grp(u):
        return u.reshape(u.shape[0], u.shape[1], S5_GROUPS, S5_CH).astype(F32)

    ul, uc = grp(u_lat), grp(u_ctx)
    dd = d.astype(F32)
    y_lat = ul * dd
    y_ctx = uc * dd if with_ctx else None
    for direction in range(2):
        reverse = direction == 1
        abr, abi, bbr, bbi = s5_discretise(a_re[direction], a_im[direction], log_dt[direction],
                                           b_re[direction], b_im[direction])
        cr, ci = c_re[direction].astype(F32), c_im[direction].astype(F32)

        def drive(u):
            return (jnp.einsum('blgc,gpc->blgp', u, bbr), jnp.einsum('blgc,gpc->blgp', u, bbi))

        def read(sr, si):
            return jnp.einsum('blgp,gcp->blgc', sr, cr) - jnp.einsum('blgp,gcp->blgc', si, ci)

        cs_re, cs_im = complex_diag_scan(abr, abi, *drive(uc), reverse)
        end = 0 if reverse else -1
        ls_re, ls_im = complex_diag_scan(abr, abi, *drive(ul), reverse,
                                         init=(cs_re[:, end], cs_im[:, end]))
        y_lat = y_lat + read(ls_re, ls_im)
        if with_ctx:
            y_ctx = y_ctx + read(cs_re, cs_im)

    def glu(y, dtype):
        g = jax.nn.gelu(y.reshape(y.shape[0], y.shape[1], S5_WIDTH), approximate=False)
        return (g * jax.nn.sigmoid(g @ glu_w.astype(F32) + glu_b.astype(F32))).astype(dtype)

    return glu(y_lat, u_lat.dtype), (glu(y_ctx, u_ctx.dtype) if with_ctx else None)


def attn_s5_mixer(h_lat, h_ctx, w_in, sink, a_re, a_im, log_dt, b_re, b_im, c_re, c_im, d,
                  glu_w, glu_b, w_out, with_ctx):
    def split(p):
        bsz, length = p.shape[:2]
        q, k, v, u = jnp.split(p, [AB_Q, AB_Q + AB_KV, AB_Q + 2 * AB_KV], axis=-1)
        return (q.reshape(bsz, length, ATTN_KV_HEADS, ATTN_GROUP, HEAD_DIM),
                k.reshape(bsz, length, ATTN_KV_HEADS, HEAD_DIM),
                v.reshape(bsz, length, ATTN_KV_HEADS, HEAD_DIM), u)

    ql, kl, vl, ul = split(h_lat @ w_in)
    qc, kc, vc, uc = split(h_ctx @ w_in)
    seq = h_lat.shape[1]
    ql, kl = axial_rope(ql, seq), axial_rope(kl, seq)
    sink_g = sink.reshape(ATTN_KV_HEADS, ATTN_GROUP)
    attn_lat = window_attention_latent(ql, kl, vl, kc, vc, sink_g)
    s5_lat, s5_ctx = s5_mixer(ul, uc, a_re, a_im, log_dt, b_re, b_im, c_re, c_im, d, glu_w, glu_b, with_ctx)
    out_lat = jnp.concatenate([attn_lat, s5_lat], axis=-1) @ w_out
    out_ctx = None
    if with_ctx:
        attn_ctx = context_attention(qc, kc, vc, sink_g)
        out_ctx = jnp.concatenate([attn_ctx, s5_ctx], axis=-1) @ w_out
    return out_lat, out_ctx


def centred_depthwise_conv(x, w, b):
    y = lax.conv_general_dilated(x, w[:, None, :].astype(x.dtype), window_strides=(1,),
                                 padding=[(SSD_CONV // 2, SSD_CONV // 2)],
                                 dimension_numbers=('NWC', 'WIO', 'NWC'),
                                 feature_group_count=x.shape[-1])
    return y + b.astype(x.dtype)


def ssd_inputs(h, w_in, conv_w, conv_b, dt_bias):
    bsz, length = h.shape[:2]
    p = h @ w_in
    z, xbc, dt_raw = jnp.split(p, [SSD_INNER, SSD_INNER + SSD_XBC], axis=-1)
    xbc = jax.nn.silu(centred_depthwise_conv(xbc, conv_w, conv_b))
    xs, bm, cm = jnp.split(xbc, [SSD_INNER, SSD_INNER + SSD_GROUPS * SSD_STATE], axis=-1)
    dt = jax.nn.softplus((dt_raw.reshape(bsz, length, 2, SSD_HEADS) + dt_bias).astype(F32))
    return (z, xs.reshape(bsz, length, SSD_HEADS, SSD_HEAD_DIM),
            bm.reshape(bsz, length, SSD_GROUPS, SSD_STATE),
            cm.reshape(bsz, length, SSD_GROUPS, SSD_STATE), dt)


def ssd_chunked(x, dt, a, bm, cm, init):
    bsz, length = x.shape[:2]
    nc = length // SSD_CHUNK
    xd = (x.astype(F32) * dt[..., None]).reshape(bsz, nc, SSD_CHUNK, SSD_GROUPS, SSD_HPG, SSD_HEAD_DIM)
    la = (dt * a).reshape(bsz, nc, SSD_CHUNK, SSD_GROUPS, SSD_HPG).transpose(0, 3, 4, 1, 2)
    cs = jnp.cumsum(la, axis=-1)
    bc = bm.reshape(bsz, nc, SSD_CHUNK, SSD_GROUPS, SSD_STATE).astype(F32)
    cc = cm.reshape(bsz, nc, SSD_CHUNK, SSD_GROUPS, SSD_STATE).astype(F32)
    tril = jnp.tril(jnp.ones((SSD_CHUNK, SSD_CHUNK), dtype=bool))
    decay_in = jnp.exp(jnp.where(tril, cs[..., :, None] - cs[..., None, :], -jnp.inf))
    cb = jnp.einsum('bclgn,bcsgn->bcgls', cc, bc)
    y_diag = jnp.einsum('bcgls,bgjcls,bcsgjp->bclgjp', cb, decay_in, xd)
    decay_to_end = jnp.exp(cs[..., -1:] - cs)
    states = jnp.einsum('bclgn,bgjcl,bclgjp->bcgjpn', bc, decay_to_end, xd)
    init_g = init.reshape(bsz, 1, SSD_GROUPS, SSD_HPG, SSD_HEAD_DIM, SSD_STATE).astype(F32)
    states = jnp.concatenate([init_g, states], axis=1)
    tot = jnp.cumsum(jnp.pad(cs[..., -1], ((0, 0), (0, 0), (0, 0), (1, 0))), axis=-1)
    tril_c = jnp.tril(jnp.ones((nc + 1, nc + 1), dtype=bool))
    decay_chunk = jnp.exp(jnp.where(tril_c, tot[..., :, None] - tot[..., None, :], -jnp.inf))
    carried = jnp.einsum('bgjzc,bcgjpn->bzgjpn', decay_chunk, states)
    y_off = jnp.einsum('bclgn,bcgjpn,bgjcl->bclgjp', cc, carried[:, :-1], jnp.exp(cs))
    y = (y_diag + y_off).reshape(bsz, length, SSD_HEADS, SSD_HEAD_DIM)
    return y, carried[:, -1].reshape(bsz, SSD_HEADS, SSD_HEAD_DIM, SSD_STATE)


def ssd_final_state(x, dt, a, bm):
    bsz, length = x.shape[:2]
    cs = jnp.cumsum(dt * a, axis=1)
    w = (jnp.exp(cs[:, -1:] - cs) * dt).reshape(bsz, length, SSD_GROUPS, SSD_HPG)
    xg = x.astype(F32).reshape(bsz, length, SSD_GROUPS, SSD_HPG, SSD_HEAD_DIM)
    st = jnp.einsum('blgn,blgj,blgjp->bgjpn', bm.astype(F32), w, xg)
    return st.reshape(bsz, SSD_HEADS, SSD_HEAD_DIM, SSD_STATE)


def ssd_mixer(h_lat, h_ctx, w_in, conv_w, conv_b, dt_bias, a_log, d, norm_g, w_out, with_ctx):
    zl, xl, bl, cl, dtl = ssd_inputs(h_lat, w_in, conv_w, conv_b, dt_bias)
    zc, xc, bcx, ccx, dtc = ssd_inputs(h_ctx, w_in, conv_w, conv_b, dt_bias)
    a = -jnp.exp(a_log.astype(F32))
    dskip = d.astype(F32)[:, None]
    y_lat = xl.astype(F32) * dskip
    y_ctx = xc.astype(F32) * dskip if with_ctx else None
    for direction in range(2):
        if direction == 0:
            prep = lambda t: t
        else:
            prep = lambda t: jnp.flip(t, axis=1)
        xc_d, bc_d, cc_d, dtc_d = prep(xc), prep(bcx), prep(ccx), prep(dtc[:, :, direction])
        if with_ctx:
            zero = jnp.zeros((xc.shape[0], SSD_HEADS, SSD_HEAD_DIM, SSD_STATE), F32)
            yc_d, ctx_state = ssd_chunked(xc_d, dtc_d, a[direction], bc_d, cc_d, zero)
            y_ctx = y_ctx + prep(yc_d)
        else:
            ctx_state = ssd_final_state(xc_d, dtc_d, a[direction], bc_d)
        yl_d, _ = ssd_chunked(prep(xl), prep(dtl[:, :, direction]), a[direction], prep(bl), prep(cl), ctx_state)
        y_lat = y_lat + prep(yl_d)

    def finish(y, z, dtype):
        bsz, length = y.shape[:2]
        g = y.reshape(bsz, length, SSD_INNER) * jax.nn.silu(z.astype(F32))
        g = rmsnorm(g.reshape(bsz, length, SSD_GROUPS, SSD_INNER // SSD_GROUPS),
                    norm_g.reshape(SSD_GROUPS, SSD_INNER // SSD_GROUPS))
        return g.reshape(bsz, length, SSD_INNER).astype(dtype) @ w_out

    return finish(y_lat, zl, h_lat.dtype), (finish(y_ctx, zc, h_ctx.dtype) if with_ctx else None)


def peer_ffn(h, wq, keys, u, v):
    shape = h.shape
    hs = h.reshape(-1, PEER_TOKENS, shape[-1])

    def one_block(hb):
        q = (hb @ wq).reshape(hb.shape[0], PEER_HEADS, 2, PEER_HALF).astype(F32)
        s1 = jnp.einsum('thd,hkd->thk', q[:, :, 0], keys[:, 0].astype(F32))
        s2 = jnp.einsum('thd,hkd->thk', q[:, :, 1], keys[:, 1].astype(F32))
        v1, i1 = lax.top_k(s1, PEER_TOPK)
        v2, i2 = lax.top_k(s2, PEER_TOPK)
        cand = (v1[..., :, None] + v2[..., None, :]).reshape(hb.shape[0], PEER_HEADS, PEER_TOPK * PEER_TOPK)
        score, flat = lax.top_k(cand, PEER_TOPK)
        e1 = jnp.take_along_axis(i1, flat // PEER_TOPK, axis=-1)
        e2 = jnp.take_along_axis(i2, flat % PEER_TOPK, axis=-1)
        expert = e1 * PEER_NKEYS + e2
        gate = jax.nn.softmax(score, axis=-1)
        act = jax.nn.gelu(jnp.einsum('thkd,td->thk', u[expert], hb).astype(F32), approximate=False)
        return jnp.einsum('thk,thkd->td', (gate * act).astype(hb.dtype), v[expert])

    return lax.map(one_block, hs).reshape(shape)


def setup_inputs(seed: int = 0) -> dict:
    key = jax.random.key(seed)
    ks = iter(jax.random.split(key, 48))

    def nrm(shape, s):
        return jax.random.normal(next(ks), shape, F32) * s

    n_even = (DEPTH + 1) // 2
    n_odd = DEPTH // 2
    x = nrm((BATCH, SEQ, D_MODEL), 1.0)
    c = nrm((BATCH, D_MODEL), 1.0)
    ctx = nrm((BATCH, CTX_LEN, D_MODEL), 1.0)
    c_ctx = nrm((D_MODEL,), 1.0)
    mod_w = nrm((DEPTH, D_MODEL, N_MOD * D_MODEL), 0.5 * D_MODEL ** -0.5)
    mod_b = nrm((DEPTH, N_MOD * D_MODEL), 0.02)
    norm1_g = 1.0 + nrm((DEPTH, D_MODEL), 0.02)
    norm2_g = 1.0 + nrm((DEPTH, D_MODEL), 0.02)
    ab_w_in = nrm((n_even, D_MODEL, AB_IN), D_MODEL ** -0.5)
    attn_sink = nrm((n_even, ATTN_HEADS), 0.5)
    s5_shape = (n_even, 2, S5_GROUPS, S5_STATE)
    s5_a_re = -0.5 + nrm(s5_shape, 0.01)
    s5_a_im = jnp.pi * jnp.arange(S5_STATE, dtype=F32) + nrm(s5_shape, 0.01)
    s5_log_dt = jax.random.uniform(next(ks), s5_shape, F32, math.log(1e-3), math.log(1e-1))
    s5_b_re = nrm(s5_shape + (S5_CH,), (2 * S5_CH) ** -0.5)
    s5_b_im = nrm(s5_shape + (S5_CH,), (2 * S5_CH) ** -0.5)
    s5_c_re = nrm((n_even, 2, S5_GROUPS, S5_CH, S5_STATE), S5_STATE ** -0.5)
    s5_c_im = nrm((n_even, 2, S5_GROUPS, S5_CH, S5_STATE), S5_STATE ** -0.5)
    s5_d = nrm((n_even, S5_GROUPS, S5_CH), 0.5)
    s5_glu_w = nrm((n_even, S5_WIDTH, S5_WIDTH), S5_WIDTH ** -0.5)
    s5_glu_b = nrm((n_even, S5_WIDTH), 0.02)
    ab_w_out = nrm((n_even, AB_OUT, D_MODEL), AB_OUT ** -0.5)
    ssd_w_in = nrm((n_odd, D_MODEL, SSD_IN), D_MODEL ** -0.5)
    ssd_conv_w = nrm((n_odd, SSD_CONV, SSD_XBC), SSD_CONV ** -0.5)
    ssd_conv_b = nrm((n_odd, SSD_XBC), 0.02)
    dt0 = jnp.exp(jax.random.uniform(next(ks), (n_odd, 2, SSD_HEADS), F32, math.log(1e-3), math.log(1e-1)))
    ssd_dt_bias = dt0 + jnp.log(-jnp.expm1(-dt0))
    ssd_a_log = jnp.log(jax.random.uniform(next(ks), (n_odd, 2, SSD_HEADS), F32, 1.0, 16.0))
    ssd_d = 1.0 + nrm((n_odd, SSD_HEADS), 0.02)
    ssd_norm_g = 1.0 + nrm((n_odd, SSD_INNER), 0.02)
    ssd_w_out = nrm((n_odd, SSD_INNER, D_MODEL), SSD_INNER ** -0.5)
    peer_wq = nrm((DEPTH, D_MODEL, PEER_HEADS * PEER_QDIM), D_MODEL ** -0.5)
    peer_keys = nrm((DEPTH, PEER_HEADS, 2, PEER_NKEYS, PEER_HALF), PEER_HALF ** -0.5)
    peer_u = nrm((DEPTH, PEER_EXPERTS, D_MODEL), D_MODEL ** -0.5)
    peer_v = nrm((DEPTH, PEER_EXPERTS, D_MODEL), PEER_HEADS ** -0.5)
    final_norm_g = 1.0 + nrm((D_MODEL,), 0.02)
    return {"x": x, "c": c, "ctx": ctx, "c_ctx": c_ctx, "mod_w": mod_w, "mod_b": mod_b,
            "norm1_g": norm1_g, "norm2_g": norm2_g, "ab_w_in": ab_w_in, "attn_sink": attn_sink,
            "s5_a_re": s5_a_re, "s5_a_im": s5_a_im, "s5_log_dt": s5_log_dt, "s5_b_re": s5_b_re,
            "s5_b_im": s5_b_im, "s5_c_re": s5_c_re, "s5_c_im": s5_c_im, "s5_d": s5_d,
            "s5_glu_w": s5_glu_w, "s5_glu_b": s5_glu_b, "ab_w_out": ab_w_out,
            "ssd_w_in": ssd_w_in, "ssd_conv_w": ssd_conv_w, "ssd_conv_b": ssd_conv_b,
            "ssd_dt_bias": ssd_dt_bias, "ssd_a_log": ssd_a_log, "ssd_d": ssd_d, "ssd_norm_g": ssd_norm_g,
            "ssd_w_out": ssd_w_out, "peer_wq": peer_wq, "peer_keys": peer_keys, "peer_u": peer_u,
            "peer_v": peer_v, "final_norm_g": final_norm_g}


def reference(x, c, ctx, c_ctx, mod_w, mod_b, norm1_g, norm2_g, ab_w_in, attn_sink,
              s5_a_re, s5_a_im, s5_log_dt, s5_b_re, s5_b_im, s5_c_re, s5_c_im, s5_d,
              s5_glu_w, s5_glu_b, ab_w_out, ssd_w_in, ssd_conv_w, ssd_conv_b, ssd_dt_bias,
              ssd_a_log, ssd_d, ssd_norm_g, ssd_w_out, peer_wq, peer_keys, peer_u, peer_v,
              final_norm_g):
    h, hc = x, ctx
    for i in range(DEPTH):
        last = i == DEPTH - 1
        mod_l = jax.nn.silu(c) @ mod_w[i] + mod_b[i]
        mod_c = jax.nn.silu(c_ctx) @ mod_w[i] + mod_b[i]
        sh1, sc1, g1, sh2, sc2, g2 = [m[:, None] for m in jnp.split(mod_l, N_MOD, axis=-1)]
        csh1, csc1, cg1, csh2, csc2, cg2 = jnp.split(mod_c, N_MOD, axis=-1)
        a_lat = modulate(rmsnorm(h, norm1_g[i]), sh1, sc1)
        a_ctx = modulate(rmsnorm(hc, norm1_g[i]), csh1, csc1)
        j = i // 2
        if i % 2 == 0:
            o_lat, o_ctx = attn_s5_mixer(a_lat, a_ctx, ab_w_in[j], attn_sink[j], s5_a_re[j], s5_a_im[j],
                                         s5_log_dt[j], s5_b_re[j], s5_b_im[j], s5_c_re[j], s5_c_im[j],
                                         s5_d[j], s5_glu_w[j], s5_glu_b[j], ab_w_out[j], not last)
        else:
            o_lat, o_ctx = ssd_mixer(a_lat, a_ctx, ssd_w_in[j], ssd_conv_w[j], ssd_conv_b[j], ssd_dt_bias[j],
                                     ssd_a_log[j], ssd_d[j], ssd_norm_g[j], ssd_w_out[j], not last)
        h = h + (g1 * o_lat).astype(h.dtype)
        f_lat = modulate(rmsnorm(h, norm2_g[i]), sh2, sc2)
        h = h + (g2 * peer_ffn(f_lat, peer_wq[i], peer_keys[i], peer_u[i], peer_v[i])).astype(h.dtype)
        if not last:
            hc = hc + (cg1 * o_ctx).astype(hc.dtype)
            f_ctx = modulate(rmsnorm(hc, norm2_g[i]), csh2, csc2)
            hc = hc + (cg2 * peer_ffn(f_ctx, peer_wq[i], peer_keys[i], peer_u[i], peer_v[i])).astype(hc.dtype)
    return rmsnorm(h, final_norm_g)
```

```python
from contextlib import ExitStack
import numpy as np
import concourse.bass as bass
import concourse.mybir as mybir
from concourse.bass_utils import run_bass_kernel_spmd

F32 = mybir.dt.float32
BF16 = mybir.dt.bfloat16
I32 = mybir.dt.int32
U32 = mybir.dt.uint32
AF = mybir.ActivationFunctionType
ALU = mybir.AluOpType
AX = mybir.AxisListType

EPOCH = 30000


class Res:
    __slots__ = ("name", "lw", "rd", "t")

    def __init__(self, name, t=None):
        self.name = name
        self.lw = None
        self.rd = []
        self.t = t

    def __getitem__(self, k):
        return self.t[k]


class Prog:
    ENGS = ("pe", "act", "dve", "pool", "sp")
    NSLOT = {"sp": 24, "act": 6, "pool": 12}

    def __init__(self):
        self.nc = bass.Bass("TRN2", target_bir_lowering=False)
        self.es = ExitStack()
        self.ops = []
        self.psbanks = None
        self.psn = 0
        self.reserved = set()
        self.scopes = [self.es]
        self.last_c = {}
        self.slot_next = {q: 0 for q in self.NSLOT}
        self.slot_last = {q: [None] * k for q, k in self.NSLOT.items()}
        self.pending = {e: set() for e in self.ENGS}

    def sb(self, name, shape, dt=F32):
        self.uid = getattr(self, "uid", 0) + 1
        name = f"{name}_{self.uid}"
        t = self.scopes[-1].enter_context(self.nc.sbuf_tensor(name, list(shape), dt))
        return Res(name, t)

    def scope(self):
        prog = self

        class _S:
            def __enter__(s2):
                prog.scopes.append(ExitStack())

            def __exit__(s2, *a):
                prog.scopes.pop().close()
                prog.barrier()
        return _S()

    def barrier(self):
        s = set(self.last_c.values())
        for q in self.NSLOT:
            s.update(x for x in self.slot_last[q] if x is not None)
        for e in self.ENGS:
            self.pending[e] = set(s)

    def ps(self, name, shape, dt=F32):
        t = self.es.enter_context(self.nc.psum_tensor(name, list(shape), dt))
        return Res(name, t)

    def bank(self):
        if self.psbanks is None:
            self.psall = self.ps("psall", [128, 8, 512], F32)
            self.psbanks = [Res(f"psb{i}", self.psall.t[:, i, :]) for i in range(8)]
        while self.psn % 8 in self.reserved:
            self.psn += 1
        b = self.psbanks[self.psn % 8]
        self.psn += 1
        return b

    def bank2(self):
        self.bank()
        self.psn -= 1
        while self.psn % 2 or (self.psn % 8) in self.reserved or (self.psn % 8 + 1) in self.reserved:
            self.psn += 1
        i = self.psn % 8
        self.psn += 2
        self.last_pair = (i, i + 1)
        return self.psall.t[:, i:i + 2, :].rearrange("p a b -> p (a b)"), self.psbanks[i], self.psbanks[i + 1]

    def dram(self, name, shape, dt=F32, kind="Internal"):
        t = self.nc.dram_tensor(name, list(shape), dt, kind=kind)
        return Res(name, t.ap())

    def op(self, eng, fn, reads=(), writes=(), dma=False):
        idx = len(self.ops)
        deps = set()
        for r in reads:
            if r.lw is not None:
                deps.add(r.lw)
        for w in writes:
            if w.lw is not None:
                deps.add(w.lw)
            deps.update(w.rd)
        for r in reads:
            r.rd.append(idx)
        for w in writes:
            w.lw = idx
            w.rd = []
        if self.pending[eng]:
            deps.update(self.pending[eng])
            self.pending[eng] = set()
        slot = None
        if dma:
            slot = self.slot_next[eng]
            self.slot_next[eng] = (slot + 1) % self.NSLOT[eng]
            prev = self.slot_last[eng][slot]
            if prev is not None:
                deps.add(prev)
            self.slot_last[eng][slot] = idx
        else:
            self.last_c[eng] = idx
        self.ops.append([eng, fn, deps, dma, slot])
        return idx

    def pe(self, fn, reads=(), writes=()):
        return self.op("pe", fn, reads, writes)

    def act(self, fn, reads=(), writes=()):
        return self.op("act", fn, reads, writes)

    def dve(self, fn, reads=(), writes=()):
        return self.op("dve", fn, reads, writes)

    def pool(self, fn, reads=(), writes=()):
        return self.op("pool", fn, reads, writes)

    def dma(self, out, in_, reads=(), writes=(), q="sp", **kw):
        nc = self.nc
        e = {"sp": nc.sync, "act": nc.scalar, "pool": nc.gpsimd}[q]
        return self.op(q, lambda: e.dma_start(out=out, in_=in_, **kw), reads, writes, dma=True)

    def dump(self, name, ap, res, shape):
        if not getattr(self, "debug", False):
            return
        d = self.dram("DBG_" + name, shape, F32, "ExternalOutput")
        self.dma(d[tuple(slice(None) for _ in shape)], ap, reads=[res], writes=[d])
        self.dbg = getattr(self, "dbg", []) + [d]

    def final(self, resources):
        nc = self.nc
        self.op("sp", lambda: nc.sync.nop(), reads=list(resources), writes=[])

    def finish(self):
        nc = self.nc
        ops = self.ops
        n = len(ops)

        def skip(d, i):
            return ops[d][0] == "pe" and ops[i][0] == "pe" and not ops[d][3] and not ops[i][3]

        needed = [False] * n
        for i, (eng, fn, deps, dma, slot) in enumerate(ops):
            for d in deps:
                if not skip(d, i):
                    needed[d] = True
        cnt = {e: 0 for e in self.ENGS}
        slot_cnt = {q: [0] * k for q, k in self.NSLOT.items()}
        slot_ep = {q: [0] * k for q, k in self.NSLOT.items()}
        ticket = [None] * n
        for i, (eng, fn, deps, dma, s) in enumerate(ops):
            if dma:
                if slot_cnt[eng][s] + 16 > EPOCH:
                    slot_cnt[eng][s] = 0
                    slot_ep[eng][s] += 1
                slot_cnt[eng][s] += 16
                ticket[i] = (("d", eng, s, slot_ep[eng][s]), slot_cnt[eng][s])
            elif needed[i]:
                cnt[eng] += 1
                ep = (cnt[eng] - 1) // EPOCH
                ticket[i] = (("c", eng, ep), cnt[eng] - ep * EPOCH)
        semkeys = sorted({t[0] for t in ticket if t is not None}, key=str)
        sems = {}
        for k in semkeys:
            sems[k] = self.es.enter_context(nc.semaphore("s_" + "_".join(str(x) for x in k)))
        self.nsem = len(sems)
        per_eng = {e: [] for e in self.ENGS}
        for i, o in enumerate(ops):
            per_eng[o[0]].append(i)
        engobj = {"pe": nc.tensor, "act": nc.scalar, "dve": nc.vector, "pool": nc.gpsimd, "sp": nc.sync}

        def emit(ename):
            e = engobj[ename]
            waited = {}
            for i in per_eng[ename]:
                eng, fn, deps, dma, slot = ops[i]
                dl = deps
                need = {}
                for d in dl:
                    if skip(d, i):
                        continue
                    k, v = ticket[d]
                    if need.get(k, 0) < v:
                        need[k] = v
                for k, v in sorted(need.items(), key=str):
                    if waited.get(k, 0) >= v:
                        continue
                    if any(kk[0] == k[0] and kk[1:-1] == k[1:-1] and kk[-1] > k[-1] for kk in waited):
                        continue
                    e.wait_ge(sems[k], v)
                    waited[k] = v
                ins = fn()
                if ticket[i] is not None:
                    k, v = ticket[i]
                    ins.then_inc(sems[k], 16 if dma else 1)

        with nc.Block() as block:
            @block.tensor
            def _(eng):
                emit("pe")

            @block.scalar
            def _(eng):
                emit("act")

            @block.vector
            def _(eng):
                emit("dve")

            @block.gpsimd
            def _(eng):
                emit("pool")

            @block.sync
            def _(eng):
                emit("sp")
        self.es.close()
        return nc


def phase_mod(P, depth, c_col, cc_col, mod_w, mod_b, MOD):
    nc = P.nc
    cs = P.sb("mod_cs", [128, 32])
    cs2 = P.sb("mod_cs2", [128, 32])
    rep = P.sb("mod_rep", [128, 32, 128])
    P.dma(cs[:, 0:16], c_col[:, :], reads=[c_col], writes=[cs])
    P.dma(cs[:, 16:32], cc_col[:, :], reads=[cc_col], writes=[cs])
    P.act(lambda: nc.scalar.activation(out=cs2[:, :], in_=cs[:, :], func=AF.Silu), [cs], [cs2])
    for kc in range(32):
        P.dve(lambda kc=kc: nc.vector.tensor_copy(out=rep[:, kc, :], in_=cs2[:, kc:kc + 1].to_broadcast([128, 128])),
              [cs2], [rep])
    wb = [P.sb(f"mod_w{i}", [128, 16, 512]) for i in range(2)]
    bb = [P.sb(f"mod_b{i}", [128, 512]) for i in range(2)]
    ob = [P.sb(f"mod_o{i}", [128, 512]) for i in range(4)]
    it = 0
    for l in range(depth):
        for n in range(24):
            wt, bt = wb[it % 2], bb[it % 2]
            P.dma(wt[:, :, :], mod_w[l, :, n * 512:(n + 1) * 512].rearrange("(kc p) n -> p kc n", p=128),
                  reads=[mod_w], writes=[wt])
            P.dma(bt[:, :], mod_b[l, n * 512:(n + 1) * 512].partition_broadcast(128), reads=[mod_b], writes=[bt])
            for kind in range(2):
                pb = P.bank()
                for kc in range(16):
                    P.pe(lambda kc=kc, kind=kind, pb=pb, wt=wt: nc.tensor.matmul(
                        pb[:, :], lhsT=rep[:, kind * 16 + kc, :], rhs=wt[:, kc, :], start=(kc == 0), stop=(kc == 15)),
                        [rep, wt], [pb])
                ot = ob[(it * 2 + kind) % 4]
                P.dve(lambda pb=pb, ot=ot, bt=bt: nc.vector.tensor_tensor(out=ot[:, :], in0=pb[:, :], in1=bt[:, :], op=ALU.add),
                      [pb, bt], [ot])
                P.dma(MOD[l, kind, :, n * 512:(n + 1) * 512], ot[:, :], reads=[ot], writes=[MOD])
            it += 1


D = 2048
EPS = 1e-6


class Ctx:
    pass


def tr_chunks(P, C, src, n, dst, col0=0, rows=128, cw=128):
    nc = P.nc
    per = 512 // rows if rows >= 128 else 4
    j0 = 0
    while j0 < n:
        nb = min(per, n - j0)
        pb = P.bank()
        for j in range(nb):
            P.pe(lambda j=j, j0=j0, pb=pb: nc.tensor.transpose(
                out=pb[0:cw, j * rows:(j + 1) * rows], in_=src[0:rows, col0 + (j0 + j) * cw: col0 + (j0 + j + 1) * cw],
                identity=C.ident[0:rows, 0:rows]), [src, C.ident], [pb])
        C.ev ^= 1
        o = dst[0:cw, j0:j0 + nb, 0:rows]
        i = pb[0:cw, 0:nb * rows].rearrange("p (j t) -> p j t", t=rows)
        if C.ev:
            P.act(lambda o=o, i=i: nc.scalar.copy(out=o, in_=i), [pb], [dst])
        else:
            P.dve(lambda o=o, i=i: nc.vector.tensor_copy(out=o, in_=i), [pb], [dst])
        j0 += nb


def linear(P, C, xT, nk, W, N, consume, cw=512, wtag="lw"):
    nc = P.nc
    wb = C.wbuf
    nch = (N + cw - 1) // cw
    for n in range(nch):
        w = min(cw, N - n * cw)
        wt = wb[C.wi % len(wb)]
        C.wi += 1
        P.dma(wt[:, 0:nk, 0:w], W[:, n * cw:n * cw + w].rearrange("(kc p) n -> p kc n", p=128),
              reads=[C.wsrc], writes=[wt])
        for t, xt in enumerate(xT):
            pb = P.bank()
            for kc in range(nk):
                P.pe(lambda kc=kc, pb=pb, xt=xt, wt=wt, w=w: nc.tensor.matmul(
                    pb[:, 0:w], lhsT=xt[:, kc, :], rhs=wt[:, kc, 0:w], start=(kc == 0), stop=(kc == nk - 1)),
                    [xt, wt], [pb])
            consume(n, t, pb, w)


def load_bc(P, dst, src_ap, srcres):
    P.dma(dst[:, :], src_ap.partition_broadcast(128), reads=[srcres], writes=[dst])


def prep_mod(P, C, l, j_shift, j_scale, g_ap, gres, Gp, Sh):
    nc = P.nc
    with P.scope():
        gt = P.sb("pm_g", [128, D])
        load_bc(P, gt, g_ap, gres)
        for kind in range(2):
            P.dma(Sh[kind][:, :], C.MOD[l, kind, :, j_shift * D:(j_shift + 1) * D], reads=[C.MOD], writes=[Sh[kind]])
            P.dma(Gp[kind][:, :], C.MOD[l, kind, :, j_scale * D:(j_scale + 1) * D], reads=[C.MOD], writes=[Gp[kind]])
            P.dve(lambda kind=kind: nc.vector.scalar_tensor_tensor(
                out=Gp[kind][:, :], in0=Gp[kind][:, :], scalar=1.0, in1=gt[:, :], op0=ALU.add, op1=ALU.mult),
                [Gp[kind], gt], [Gp[kind]])


def norm_mod(P, C, x, out, Gp, Sh, junk, st):
    nc = P.nc
    P.act(lambda: (nc.scalar.activation(out=junk[:, :], in_=x, func=AF.Square, accum_out=st[:, 0:1]),
                   nc.scalar.copy(out=P.scr[:, 0:1], in_=st[:, 0:1]))[1], [C.xres], [junk, st])
    P.dve(lambda: nc.vector.tensor_scalar(out=st[:, 1:2], in0=st[:, 0:1], scalar1=1.0 / D, scalar2=EPS,
                                          op0=ALU.mult, op1=ALU.add), [st], [st])
    P.act(lambda: nc.scalar.activation(out=st[:, 3:4], in_=st[:, 1:2], func=AF.Sqrt), [st], [st])
    P.dve(lambda: nc.vector.reciprocal(out=st[:, 2:3], in_=st[:, 3:4]), [st], [st])
    P.dve(lambda: nc.vector.scalar_tensor_tensor(out=out, in0=x, scalar=st[:, 2:3], in1=Gp[:, :],
                                                 op0=ALU.mult, op1=ALU.mult), [C.xres, st, Gp], [C.ores])
    P.dve(lambda: nc.vector.tensor_tensor(out=out, in0=out, in1=Sh[:, :], op=ALU.add), [C.ores, Sh], [C.ores])


def tile_groups(C, G=2):
    gs = []
    for t0 in range(0, C.NCT, G):
        gs.append((1, t0, min(G, C.NCT - t0)))
    for t0 in range(0, C.NL, G):
        gs.append((0, C.NCT + t0, min(G, C.NL - t0)))
    return gs


def phase_inproj0(P, C, l, hsrc):
    nc = P.nc
    with P.scope():
        Gp = [P.sb(f"ipGp{k}", [128, D]) for k in range(2)]
        Sh = [P.sb(f"ipSh{k}", [128, D]) for k in range(2)]
        prep_mod(P, C, l, 0, 1, C.norm1_g[l, :], C.norm1_g, Gp, Sh)
        G = 2
        xs = [P.sb(f"ipx{i}", [128, D]) for i in range(G)]
        xT = [P.sb(f"ipxT{i}", [128, 16, 128]) for i in range(G)]
        os_ = [P.sb(f"ipo{i}", [128, 2560]) for i in range(G)]
        junk = P.sb("ipjunk", [128, D])
        st = P.sb("ipst", [128, 4])
        cs = P.sb("ipcs", [128, 2, 64])
        tmp = P.sb("iptmp", [128, 4, 10, 64])
        C.wbuf = [P.sb(f"ipw{i}", [128, 16, 512]) for i in range(2)]
        for kind, t0, nt in tile_groups(C, G):
            for i in range(nt):
                t = t0 + i
                P.dma(xs[i][:, :], hsrc[t * 128:(t + 1) * 128, :], reads=[hsrc], writes=[xs[i]])
                C.xres, C.ores = xs[i], xs[i]
                norm_mod(P, C, xs[i][:, :], xs[i][:, :], Gp[kind], Sh[kind], junk, st)
                tr_chunks(P, C, xs[i], 16, xT[i])

            def consume(n, ti, pb, w):
                o = os_[ti]
                C.ev ^= 1
                if C.ev:
                    P.act(lambda: nc.scalar.copy(out=o[:, n * 512:n * 512 + w], in_=pb[:, 0:w]), [pb], [o])
                else:
                    P.dve(lambda: nc.vector.tensor_copy(out=o[:, n * 512:n * 512 + w], in_=pb[:, 0:w]), [pb], [o])
            C.wsrc = C.ab_w_in
            linear(P, C, xT[0:nt], 16, C.ab_w_in[0], 2560, consume)
            for i in range(nt):
                t = t0 + i
                o = os_[i]
                if kind == 0:
                    lt = t - C.NCT
                    P.dma(cs[:, :, :], C.rope[lt * 128:(lt + 1) * 128, :, :], reads=[C.rope], writes=[cs])
                    qk = o[:, 0:1280].rearrange("p (h two d) -> p h two d", two=2, d=64)
                    x1, x2 = qk[:, :, 0, :], qk[:, :, 1, :]
                    cosb = cs[:, 0:1, :].to_broadcast([128, 10, 64])
                    sinb = cs[:, 1:2, :].to_broadcast([128, 10, 64])
                    for j, (a, b) in enumerate([(x1, cosb), (x2, sinb), (x2, cosb), (x1, sinb)]):
                        P.dve(lambda j=j, a=a, b=b: nc.vector.tensor_tensor(out=tmp[:, j, :, :], in0=a, in1=b, op=ALU.mult),
                              [o, cs], [tmp])
                    P.dve(lambda x1=x1: nc.vector.tensor_tensor(out=x1, in0=tmp[:, 0, :, :], in1=tmp[:, 1, :, :], op=ALU.subtract),
                          [tmp], [o])
                    P.dve(lambda x2=x2: nc.vector.tensor_tensor(out=x2, in0=tmp[:, 2, :, :], in1=tmp[:, 3, :, :], op=ALU.add),
                          [tmp], [o])
                P.dma(C.QKVU[t * 128:(t + 1) * 128, :], o[:, :], reads=[o], writes=[C.QKVU])


def sub(res, name):
    return Res(name, res.t)


ATTN_SCALE = 128 ** -0.5


def phase_attn(P, C):
    nc = P.nc
    NCT, NL = C.NCT, C.NL
    T = NCT + NL
    with P.scope():
        KT = [P.sb(f"atKT{h}", [128, T * 128]) for h in range(2)]
        KTr = [sub(KT[0], f"KTr{t}") for t in range(T)]
        V = P.sb("atV", [128, T, 256])
        Vr = [sub(V, f"Vr{t}") for t in range(T)]
        kin = [P.sb(f"atk{i}", [128, 256]) for i in range(2)]
        sink = P.sb("atsink", [128, 8])
        load_bc(P, sink, C.attn_sink[0, :], C.attn_sink)
        mask = P.sb("atmask", [128, 384])
        P.dma(mask[:, :], C.bandmask[:, :], reads=[C.bandmask], writes=[mask])
        for t in range(T):
            ki = kin[t % 2]
            P.dma(ki[:, :], C.QKVU[t * 128:(t + 1) * 128, 1024:1280], reads=[C.QKVU], writes=[ki])
            P.dma(V[:, t, :], C.QKVU[t * 128:(t + 1) * 128, 1280:1536], reads=[C.QKVU], writes=[Vr[t]])
            pb = P.bank()
            for h in range(2):
                P.pe(lambda h=h, pb=pb, ki=ki: nc.tensor.transpose(
                    out=pb[:, h * 128:(h + 1) * 128], in_=ki[:, h * 128:(h + 1) * 128], identity=C.ident[:, :]),
                    [ki, C.ident], [pb])
            for h in range(2):
                P.act(lambda h=h, pb=pb, t=t: nc.scalar.copy(out=KT[h][:, t * 128:(t + 1) * 128],
                                                           in_=pb[:, h * 128:(h + 1) * 128]), [pb], [KTr[t]])
        qb = [P.sb(f"atq{i}", [128, 1024]) for i in range(2)]
        qT = [P.sb(f"atqT{i}", [128, 8, 128]) for i in range(2)]
        ob = [P.sb(f"ato{i}", [128, 1024]) for i in range(2)]
        scb = [P.sb(f"atsc{i}", [128, 640]) for i in range(2)]
        prb = [P.sb(f"atpr{i}", [128, 640]) for i in range(2)]
        ptb = [P.sb(f"atpt{i}", [128, 5, 128]) for i in range(2)]
        mst = [P.sb(f"atm{i}", [128, 8, 8]) for i in range(2)]
        it = 0
        for t in range(T):
            kind = 1 if t < NCT else 0
            q, QT, o_t, m = qb[t % 2], qT[t % 2], ob[t % 2], mst[t % 2]
            P.dma(q[:, :], C.QKVU[t * 128:(t + 1) * 128, 0:1024], reads=[C.QKVU], writes=[q])
            tr_chunks(P, C, q, 8, QT)
            ktiles = list(range(NCT))
            if kind == 0:
                n = t - NCT
                lo, hi = max(0, n - 1), min(NL - 1, n + 1)
                moff = (lo - (n - 1)) * 128
                ktiles += list(range(NCT + lo, NCT + hi + 1))
                nb = hi - lo + 1
            nk = len(ktiles) * 128
            for hq in range(8):
                h = hq // 4
                sc, pr, PT = scb[it % 2], prb[it % 2], ptb[it % 2]
                it += 1
                pbA = P.bank()
                P.pe(lambda pbA=pbA, QT=QT, hq=hq, h=h: nc.tensor.matmul(
                    pbA[:, 0:NCT * 128], lhsT=QT[:, hq, :], rhs=KT[h][:, 0:NCT * 128], start=True, stop=True),
                    [QT] + KTr[0:NCT], [pbA])
                P.act(lambda pbA=pbA, sc=sc: nc.scalar.activation(
                    out=sc[:, 0:NCT * 128], in_=pbA[:, 0:NCT * 128], func=AF.Copy, scale=ATTN_SCALE), [pbA], [sc])
                if kind == 0:
                    pbB = P.bank()
                    c0, c1 = (NCT + lo) * 128, (NCT + hi + 1) * 128
                    P.pe(lambda pbB=pbB, QT=QT, hq=hq, h=h, c0=c0, c1=c1, nb=nb: nc.tensor.matmul(
                        pbB[:, 0:nb * 128], lhsT=QT[:, hq, :], rhs=KT[h][:, c0:c1], start=True, stop=True),
                        [QT] + KTr[NCT + lo:NCT + hi + 1], [pbB])
                    P.dve(lambda pbB=pbB, sc=sc, nb=nb, moff=moff: nc.vector.scalar_tensor_tensor(
                        out=sc[:, NCT * 128:NCT * 128 + nb * 128], in0=pbB[:, 0:nb * 128], scalar=ATTN_SCALE,
                        in1=mask[:, moff:moff + nb * 128], op0=ALU.mult, op1=ALU.add), [pbB, mask], [sc])
                P.dve(lambda sc=sc, m=m, hq=hq, nk=nk: nc.vector.reduce_max(out=m[:, hq, 0:1], in_=sc[:, 0:nk], axis=AX.X),
                      [sc], [m])
                P.dve(lambda m=m, hq=hq: nc.vector.tensor_tensor(out=m[:, hq, 0:1], in0=m[:, hq, 0:1],
                                                                 in1=sink[:, hq:hq + 1], op=ALU.max), [m, sink], [m])
                P.dve(lambda m=m, hq=hq: nc.vector.tensor_scalar(out=m[:, hq, 1:2], in0=m[:, hq, 0:1], scalar1=-1.0,
                                                                 scalar2=None, op0=ALU.mult), [m], [m])
                P.act(lambda sc=sc, pr=pr, m=m, hq=hq, nk=nk: (nc.scalar.activation(
                    out=pr[:, 0:nk], in_=sc[:, 0:nk], func=AF.Exp, bias=m[:, hq, 1:2], scale=1.0,
                    accum_out=m[:, hq, 2:3]), nc.scalar.copy(out=P.scr[:, 0:1], in_=m[:, hq, 2:3]))[1], [sc, m], [pr, m])
                P.act(lambda m=m, hq=hq: nc.scalar.activation(
                    out=m[:, hq, 3:4], in_=sink[:, hq:hq + 1], func=AF.Exp, bias=m[:, hq, 1:2], scale=1.0), [sink, m], [m])
                P.dve(lambda m=m, hq=hq: nc.vector.tensor_tensor(out=m[:, hq, 4:5], in0=m[:, hq, 2:3],
                                                                 in1=m[:, hq, 3:4], op=ALU.add), [m], [m])
                P.dve(lambda m=m, hq=hq: nc.vector.reciprocal(out=m[:, hq, 5:6], in_=m[:, hq, 4:5]), [m], [m])
                tr_chunks(P, C, pr, nk // 128, PT)
                pbO = P.bank()
                for j, kt in enumerate(ktiles):
                    P.pe(lambda j=j, kt=kt, pbO=pbO, PT=PT, h=h, last=(j == len(ktiles) - 1): nc.tensor.matmul(
                        pbO[:, 0:128], lhsT=PT[:, j, :], rhs=V[:, kt, h * 128:(h + 1) * 128], start=(j == 0), stop=last),
                        [PT, Vr[kt]], [pbO])
                P.act(lambda pbO=pbO, o_t=o_t, m=m, hq=hq: nc.scalar.activation(
                    out=o_t[:, hq * 128:(hq + 1) * 128], in_=pbO[:, 0:128], func=AF.Copy, scale=m[:, hq, 5:6]),
                    [pbO, m], [o_t])
            P.dma(C.ATS5[t * 128:(t + 1) * 128, 0:1024], o_t[:, :], reads=[o_t], writes=[C.ATS5])


SPLIT = False


class Ctx:
    def __init__(self, P, NL, NCT, probes=()):
        T = NL + NCT
        self.NO = NL // 2 if SPLIT else NL
        self.P, self.NL, self.NCT, self.T = P, NL, NCT, T
        self.probes = set(probes)
        self.used_inputs = []
        self.ev = 0
        self.wi = 0
        R = T * 128
        self.spec = {
            "h0": ([R, 2048], "in"), "c_col": ([128, 16], "in"), "cc_col": ([128, 16], "in"),
            "mod_w": ([2, 2048, 12288], "in"), "mod_b": ([2, 12288], "in"),
            "norm1_g": ([2, 2048], "in"), "norm2_g": ([2, 2048], "in"),
            "ab_w_in": ([1, 2048, 2560], "in"), "attn_sink": ([1, 8], "in"),
            "s5_a_re": ([1, 2, 64, 64], "in"), "s5_a_im": ([1, 2, 64, 64], "in"), "s5_log_dt": ([1, 2, 64, 64], "in"),
            "s5_b_re": ([1, 2, 64, 64, 16], "in"), "s5_b_im": ([1, 2, 64, 64, 16], "in"),
            "s5_c_re": ([1, 2, 64, 16, 64], "in"), "s5_c_im": ([1, 2, 64, 16, 64], "in"),
            "s5_d": ([1, 1024], "in"), "s5_glu_w": ([1, 1024, 1024], "in"), "s5_glu_b": ([1, 1024], "in"),
            "ab_w_out": ([1, 2048, 2048], "in"),
            "ssd_w_in": ([1, 2048, 10368], "in"), "ssd_conv_w": ([1, 5, 6144], "in"), "ssd_conv_b": ([1, 6144], "in"),
            "ssd_dt_bias": ([1, 128], "in"), "ssd_a_log": ([1, 128], "in"), "ssd_d": ([1, 64], "in"),
            "ssd_norm_g": ([1, 4096], "in"), "ssd_w_out": ([1, 4096, 2048], "in"),
            "peer_wq": ([2, 2048, 1024], "in"), "peer_keys": ([2, 8, 2, 128, 64], "in"),
            "peer_u": ([2, 16384, 2048], "in"), "peer_v": ([2, 16384, 2048], "in"),
            "final_norm_g": ([2048], "in"),
            "ident": ([128, 128], "in"), "rope": ([NL * 128, 2, 64], "in"), "bandmask": ([128, 384], "in"),
            "tri": ([4, 128, 128], "in"), "ncol": ([128, 4], "in"), "iota256": ([128, 256], "in"),
            "MOD": ([2, 2, 128, 12288], "tmp"), "QKVU": ([R, 2560], "tmp"), "ATS5": ([R, 2048], "tmp"),
            "H": ([R, 2048], "tmp"), "Y5": ([R, 1024], "tmp"), "BB": ([2, 64, 16, 128], "tmp"),
            "SP": ([R, 10368], "tmp"), "XBC": ([R, 6144], "tmp"), "YS": ([NL * 128, 4096], "tmp"),
            "ssdmask": ([2, 128, 128], "in"),
            "UVB": ([2 * 16384, 4096], "tmp", BF16),
            "rowidx": ([128, NL // 2], "in", I32), "H2": ([NL // 2 * 128, 2048], "tmp"),
            "OUT": ([self.NO * 128, 2048], "out"),
        }

    def __getattr__(self, name):
        spec = self.__dict__.get("spec", {})
        if name not in spec:
            raise AttributeError(name)
        shape, kind = spec[name][0], spec[name][1]
        dt = spec[name][2] if len(spec[name]) > 2 else F32
        k = {"in": "ExternalInput", "out": "ExternalOutput"}.get(kind)
        if k is None:
            k = "ExternalOutput" if name in self.probes else "Internal"
        r = self.P.dram(name, shape, dt, k)
        if kind == "in":
            self.used_inputs.append(name)
        setattr(self, name, r)
        return r


STAGES = ["mod", "inproj0", "attn", "s5", "outproj0", "peer0", "ssd", "peer1", "final"]


def build_program(P, NL, NCT, stage="final", probes=()):
    C = Ctx(P, NL, NCT, probes)
    nc = P.nc
    si = STAGES.index(stage)
    C.ident_d = C.ident
    idt = P.es.enter_context(nc.sbuf_tensor("ident_sb", [128, 128], F32))
    ident_sb = Res("ident_sb", idt)
    P.dma(ident_sb[:, :], C.ident_d[:, :], reads=[C.ident_d], writes=[ident_sb])
    C.ident = ident_sb
    P.scr = Res("scr", P.es.enter_context(nc.sbuf_tensor("scr_tail", [128, 4], F32)))
    io = Res("iota_sb", P.es.enter_context(nc.sbuf_tensor("iota_sb", [128, 256], F32)))
    P.dma(io[:, :], C.iota256[:, :], reads=[C.iota256], writes=[io])
    C.iota = io
    with P.scope():
        phase_mod(P, 2, C.c_col, C.cc_col, C.mod_w, C.mod_b, C.MOD)
    outs = [C.MOD]
    if si >= 1:
        phase_inproj0(P, C, 0, C.h0)
        outs.append(C.QKVU)
    if si >= 2:
        phase_attn(P, C)
        outs.append(C.ATS5)
    if si >= 3:
        phase_s5(P, C)
        outs.append(C.Y5)
    if si >= 4:
        phase_outproj0(P, C, 0, C.h0)
        outs.append(C.H)
    if si >= 5:
        phase_peer(P, C, 0, list(range(C.T)))
    if si >= 6:
        phase_ssd_in(P, C, 1)
        phase_ssd_conv(P, C)
        for d in range(2):
            ssd_pass(P, C, d)
        phase_ssd_out(P, C, 1)
    if si >= 7:
        if SPLIT:
            phase_peer(P, C, 1, list(range(C.NL // 2)), split=True)
        else:
            phase_peer(P, C, 1, list(range(C.NCT, C.T)))
    if si >= 8:
        phase_final(P, C)
        outs.append(C.OUT)
    P.final(outs + getattr(P, "dbg", []))
    return C


def host_constants(NL):
    t = np.arange(NL * 128)
    row = (t // 64).astype(np.float32)
    col = (t % 64).astype(np.float32)
    inv = (10000.0 ** (-np.arange(32, dtype=np.float32) / 32)).astype(np.float32)
    ang = np.concatenate([row[:, None] * inv, col[:, None] * inv], axis=-1).astype(np.float32)
    rope = np.stack([np.cos(ang), np.sin(ang)], axis=1).astype(np.float32)
    i = np.arange(128)[:, None]
    j = np.arange(128)[None, :]
    neg = np.float32(-30000.0)
    bandmask = np.concatenate([np.where(j >= i, 0, neg), np.zeros((128, 128)), np.where(j <= i, 0, neg)],
                              axis=1).astype(np.float32)
    iota = np.broadcast_to(np.arange(256, dtype=np.float32)[None, :], (128, 256)).copy()
    return {"ident": np.eye(128, dtype=np.float32), "rope": rope, "bandmask": bandmask, "iota256": iota}


def make_inputs(inp, b, NL, NCT, used=None):
    m = {}
    m["h0"] = np.ascontiguousarray(np.concatenate([inp["ctx"][b][:NCT * 128], inp["x"][b][:NL * 128]], axis=0))
    m["c_col"] = np.ascontiguousarray(inp["c"][b].reshape(16, 128).T)
    m["cc_col"] = np.ascontiguousarray(inp["c_ctx"].reshape(16, 128).T)
    for k in ["mod_w", "mod_b", "norm1_g", "norm2_g", "ab_w_in", "attn_sink", "s5_a_re", "s5_a_im", "s5_log_dt",
              "s5_b_re", "s5_b_im", "s5_c_re", "s5_c_im", "s5_glu_w", "s5_glu_b", "ab_w_out", "ssd_w_in",
              "ssd_conv_w", "ssd_conv_b", "ssd_d", "ssd_norm_g", "ssd_w_out", "peer_wq", "peer_keys", "peer_u",
              "peer_v", "final_norm_g"]:
        m[k] = inp[k]
    m["s5_d"] = inp["s5_d"].reshape(1, 1024)
    m["ssd_dt_bias"] = inp["ssd_dt_bias"].reshape(1, 128)
    m["ssd_a_log"] = inp["ssd_a_log"].reshape(1, 128)
    m.update(host_constants(NL))
    m.update(host_s5_constants())
    if used is not None:
        m = {k: np.ascontiguousarray(v, dtype=np.float32) for k, v in m.items() if k in used}
    return m


PI = float(np.pi)


def sin_of(P, out, ang, shift, tmps, rin, rtmp, rout):
    nc = P.nc
    z, kf, ki = tmps
    P.dve(lambda: nc.vector.tensor_scalar(out=z, in0=ang, scalar1=float(shift), scalar2=None, op0=ALU.add), rin, rtmp)
    P.dve(lambda: nc.vector.tensor_scalar(out=ki, in0=z, scalar1=1.0 / (2 * PI), scalar2=None, op0=ALU.mult), rtmp, rtmp)
    P.dve(lambda: nc.vector.tensor_copy(out=kf, in_=ki), rtmp, rtmp)
    P.dve(lambda: nc.vector.scalar_tensor_tensor(out=z, in0=kf, scalar=-2 * PI, in1=z, op0=ALU.mult, op1=ALU.add), rtmp, rtmp)
    P.dve(lambda: nc.vector.tensor_scalar(out=kf, in0=z, scalar1=PI, scalar2=-2 * PI, op0=ALU.is_gt, op1=ALU.mult), rtmp, rtmp)
    P.dve(lambda: nc.vector.tensor_tensor(out=z, in0=z, in1=kf, op=ALU.add), rtmp, rtmp)
    P.dve(lambda: nc.vector.tensor_scalar(out=kf, in0=z, scalar1=-PI, scalar2=2 * PI, op0=ALU.is_lt, op1=ALU.mult), rtmp, rtmp)
    P.dve(lambda: nc.vector.tensor_tensor(out=z, in0=z, in1=kf, op=ALU.add), rtmp, rtmp)
    P.act(lambda: nc.scalar.activation(out=out, in_=z, func=AF.Sin), rtmp, rout)


def cmul(P, a_re, a_im, t_re, t_im, o_re, o_im, tmps, ra, rt, ro, conj=False):
    nc = P.nc
    tA, tB, tC, tD = tmps
    P.dve(lambda: nc.vector.tensor_tensor(out=tA[0], in0=a_re, in1=t_re, op=ALU.mult), ra + rt, [tA[1]])
    P.pool(lambda: nc.gpsimd.tensor_tensor(out=tB[0], in0=a_im, in1=t_im, op=ALU.mult), ra + rt, [tB[1]])
    P.pool(lambda: nc.gpsimd.tensor_tensor(out=tC[0], in0=a_re, in1=t_im, op=ALU.mult), ra + rt, [tC[1]])
    P.dve(lambda: nc.vector.tensor_tensor(out=tD[0], in0=a_im, in1=t_re, op=ALU.mult), ra + rt, [tD[1]])
    P.dve(lambda: nc.vector.tensor_tensor(out=o_re, in0=tA[0], in1=tB[0], op=ALU.subtract), [tA[1], tB[1]], ro)
    P.pool(lambda: nc.gpsimd.tensor_tensor(out=o_im, in0=tC[0], in1=tD[0], op=ALU.add), [tC[1], tD[1]], ro)


def phase_s5(P, C):
    with P.scope():
        for d in range(2):
            s5_prep(P, C, d)
    for d in range(2):
        s5_pass(P, C, d)


def s5_prep(P, C, d):
    nc = P.nc
    if True:
        if True:
            ar = P.sb(f"s5ar{d}", [64, 64]); ai = P.sb(f"s5ai{d}", [64, 64]); ld = P.sb(f"s5ld{d}", [64, 64])
            br = P.sb(f"s5br{d}", [64, 64, 16]); bi = P.sb(f"s5bi{d}", [64, 64, 16])
            P.dma(ar[:, :], C.s5_a_re[0, d], reads=[C.s5_a_re], writes=[ar])
            P.dma(ai[:, :], C.s5_a_im[0, d], reads=[C.s5_a_im], writes=[ai])
            P.dma(ld[:, :], C.s5_log_dt[0, d], reads=[C.s5_log_dt], writes=[ld])
            P.dma(br[:, :, :], C.s5_b_re[0, d], reads=[C.s5_b_re], writes=[br])
            P.dma(bi[:, :, :], C.s5_b_im[0, d], reads=[C.s5_b_im], writes=[bi])
            w = P.sb(f"s5w{d}", [64, 16, 64])
            W = lambda k: w[:, k, :]
            P.act(lambda: nc.scalar.activation(out=W(0), in_=ld[:, :], func=AF.Exp), [ld], [w])
            P.dve(lambda: nc.vector.tensor_tensor(out=W(1), in0=W(0), in1=ar[:, :], op=ALU.mult), [w, ar], [w])
            P.dve(lambda: nc.vector.tensor_tensor(out=W(2), in0=W(0), in1=ai[:, :], op=ALU.mult), [w, ai], [w])
            P.act(lambda: nc.scalar.activation(out=W(3), in_=W(1), func=AF.Exp), [w], [w])
            ki_s = P.sb(f"s5kis{d}", [64, 64], I32)
            tm3 = (W(4), W(14), ki_s[:, :])
            sin_of(P, W(5), W(2), 0.0, tm3, [w], [w, ki_s], [w])
            sin_of(P, W(6), W(2), PI / 2, tm3, [w], [w, ki_s], [w])
            P.dve(lambda: nc.vector.tensor_tensor(out=W(7), in0=W(3), in1=W(6), op=ALU.mult), [w], [w])
            P.dve(lambda: nc.vector.tensor_scalar(out=W(7), in0=W(7), scalar1=-1.0, scalar2=None, op0=ALU.add), [w], [w])
            P.dve(lambda: nc.vector.tensor_tensor(out=W(8), in0=W(3), in1=W(5), op=ALU.mult), [w], [w])
            P.dve(lambda: nc.vector.tensor_tensor(out=W(9), in0=ar[:, :], in1=ar[:, :], op=ALU.mult), [ar], [w])
            P.dve(lambda: nc.vector.tensor_tensor(out=W(12), in0=ai[:, :], in1=ai[:, :], op=ALU.mult), [ai], [w])
            P.dve(lambda: nc.vector.tensor_tensor(out=W(9), in0=W(9), in1=W(12), op=ALU.add), [w], [w])
            P.dve(lambda: nc.vector.reciprocal(out=W(9), in_=W(9)), [w], [w])
            P.dve(lambda: nc.vector.tensor_tensor(out=W(12), in0=W(7), in1=ar[:, :], op=ALU.mult), [w, ar], [w])
            P.dve(lambda: nc.vector.tensor_tensor(out=W(13), in0=W(8), in1=ai[:, :], op=ALU.mult), [w, ai], [w])
            P.dve(lambda: nc.vector.tensor_tensor(out=W(12), in0=W(12), in1=W(13), op=ALU.add), [w], [w])
            P.dve(lambda: nc.vector.tensor_tensor(out=W(10), in0=W(12), in1=W(9), op=ALU.mult), [w], [w])
            P.dve(lambda: nc.vector.tensor_tensor(out=W(12), in0=W(8), in1=ar[:, :], op=ALU.mult), [w, ar], [w])
            P.dve(lambda: nc.vector.tensor_tensor(out=W(13), in0=W(7), in1=ai[:, :], op=ALU.mult), [w, ai], [w])
            P.dve(lambda: nc.vector.tensor_tensor(out=W(12), in0=W(12), in1=W(13), op=ALU.subtract), [w], [w])
            P.dve(lambda: nc.vector.tensor_tensor(out=W(11), in0=W(12), in1=W(9), op=ALU.mult), [w], [w])
            bbT = P.sb(f"s5bbT{d}", [64, 16, 2, 64])
            t1 = P.sb(f"s5t1{d}", [64, 64, 16]); t2 = P.sb(f"s5t2{d}", [64, 64, 16])
            fre = w[:, 10, :].unsqueeze(2).to_broadcast([64, 64, 16])
            fim = w[:, 11, :].unsqueeze(2).to_broadcast([64, 64, 16])
            o_re = bbT[:, :, 0, :].rearrange("g c p -> g p c")
            o_im = bbT[:, :, 1, :].rearrange("g c p -> g p c")
            P.dve(lambda: nc.vector.tensor_tensor(out=t1[:, :, :], in0=br[:, :, :], in1=fre, op=ALU.mult), [br, w], [t1])
            P.dve(lambda: nc.vector.tensor_tensor(out=t2[:, :, :], in0=bi[:, :, :], in1=fim, op=ALU.mult), [bi, w], [t2])
            P.dve(lambda: nc.vector.tensor_tensor(out=o_re, in0=t1[:, :, :], in1=t2[:, :, :], op=ALU.subtract), [t1, t2], [bbT])
            P.dve(lambda: nc.vector.tensor_tensor(out=t1[:, :, :], in0=bi[:, :, :], in1=fre, op=ALU.mult), [bi, w, bbT], [t1])
            P.dve(lambda: nc.vector.tensor_tensor(out=t2[:, :, :], in0=br[:, :, :], in1=fim, op=ALU.mult), [br, w, bbT], [t2])
            P.dve(lambda: nc.vector.tensor_tensor(out=o_im, in0=t1[:, :, :], in1=t2[:, :, :], op=ALU.add), [t1, t2], [bbT])
            P.dma(C.BB[d], bbT[:, :, :, :].rearrange("g c r p -> g c (r p)"), reads=[bbT], writes=[C.BB])


def s5_pass(P, C, d):
    nc = P.nc
    NCT, NL, T = C.NCT, C.NL, C.T
    if True:
        with P.scope():
            tabs = [P.sb(f"s5tab{k}", [128, 4096]) for k in range(4)]
            with P.scope():
                rho = P.sb("s5rho", [128, 4096]); th = P.sb("s5th", [128, 4096]); dtb = P.sb("s5dtb", [128, 4096])
                tmp = P.sb("s5tmp", [128, 4096])
                ncol = P.sb("s5ncol", [128, 4])
                P.dma(ncol[:, :], C.ncol[:, :], reads=[C.ncol], writes=[ncol])
                load_bc(P, dtb, C.s5_log_dt[0, d].rearrange("g p -> (g p)"), C.s5_log_dt)
                load_bc(P, rho, C.s5_a_re[0, d].rearrange("g p -> (g p)"), C.s5_a_re)
                load_bc(P, th, C.s5_a_im[0, d].rearrange("g p -> (g p)"), C.s5_a_im)
                P.act(lambda: nc.scalar.activation(out=dtb[:, :], in_=dtb[:, :], func=AF.Exp), [dtb], [dtb])
                P.dve(lambda: nc.vector.tensor_tensor(out=rho[:, :], in0=rho[:, :], in1=dtb[:, :], op=ALU.mult), [rho, dtb], [rho])
                P.dve(lambda: nc.vector.tensor_tensor(out=th[:, :], in0=th[:, :], in1=dtb[:, :], op=ALU.mult), [th, dtb], [th])
                P.dve(lambda: nc.vector.tensor_scalar(out=th[:, :], in0=th[:, :], scalar1=ncol[:, d:d + 1], scalar2=None,
                                                      op0=ALU.mult), [th, ncol], [th])
                E = dtb
                tmp2 = P.sb("s5tmp2", [128, 4096])
                ki_b = P.sb("s5kib", [128, 4096], I32)
                tm3 = (tmp[:, :], tmp2[:, :], ki_b[:, :])
                sin_of(P, tabs[1][:, :], th[:, :], 0.0, tm3, [th], [tmp, tmp2, ki_b], [tabs[1]])
                sin_of(P, tabs[0][:, :], th[:, :], PI / 2, tm3, [th], [tmp, tmp2, ki_b], [tabs[0]])
                P.act(lambda: nc.scalar.activation(out=E[:, :], in_=rho[:, :], func=AF.Exp, scale=ncol[:, d:d + 1]),
                      [rho, ncol], [E])
                P.dve(lambda: nc.vector.tensor_tensor(out=tabs[2][:, :], in0=tabs[0][:, :], in1=E[:, :], op=ALU.mult),
                      [tabs[0], E], [tabs[2]])
                P.dve(lambda: nc.vector.tensor_tensor(out=tabs[3][:, :], in0=tabs[1][:, :], in1=E[:, :], op=ALU.mult),
                      [tabs[1], E], [tabs[3]])
                P.act(lambda: nc.scalar.activation(out=E[:, :], in_=rho[:, :], func=AF.Exp, scale=ncol[:, 2 + d:3 + d]),
                      [rho, ncol, tabs[2], tabs[3]], [E])
                P.dve(lambda: nc.vector.tensor_tensor(out=tabs[0][:, :], in0=tabs[0][:, :], in1=E[:, :], op=ALU.mult),
                      [tabs[0], E, tabs[2]], [tabs[0]])
                P.dve(lambda: nc.vector.scalar_tensor_tensor(out=tabs[1][:, :], in0=tabs[1][:, :], scalar=-1.0, in1=E[:, :],
                                                             op0=ALU.mult, op1=ALU.mult), [tabs[1], E, tabs[3]], [tabs[1]])
            for k in range(4):
                P.dump(f"tab{d}_{k}", tabs[k][:, :], tabs[k], [128, 4096])
            RB = P.sb("s5RB", [128, 8, 1024])
            P.pool(lambda: nc.gpsimd.memset(RB[:, :, :], 0.0), [], [RB])
            for g in range(64):
                gb, gl = g // 8, g % 8
                P.dma(RB[gl * 16:(gl + 1) * 16, gb, gl * 128:(gl + 1) * 128], C.BB[d, g], reads=[C.BB], writes=[RB])
            CM = P.sb("s5CM", [128, 1024])
            cin = [P.sb(f"s5cin{i}", [128, 128]) for i in range(2)]
            for gb in range(8):
                ci_ = cin[gb % 2]
                P.dma(ci_[:, 0:64], C.s5_c_re[0, d].rearrange("g c p -> (g c) p")[gb * 128:(gb + 1) * 128, :],
                      reads=[C.s5_c_re], writes=[ci_])
                P.dma(ci_[:, 64:128], C.s5_c_im[0, d].rearrange("g c p -> (g c) p")[gb * 128:(gb + 1) * 128, :],
                      reads=[C.s5_c_im], writes=[ci_])
                P.dve(lambda ci_=ci_: nc.vector.tensor_scalar(out=ci_[:, 64:128], in0=ci_[:, 64:128], scalar1=-1.0,
                                                              scalar2=None, op0=ALU.mult), [ci_], [ci_])
                pb = P.bank()
                P.pe(lambda ci_=ci_, pb=pb: nc.tensor.transpose(out=pb[:, 0:128], in_=ci_[:, :], identity=C.ident[:, :]),
                     [ci_, C.ident], [pb])
                P.act(lambda pb=pb, gb=gb: nc.scalar.copy(out=CM[:, gb * 128:(gb + 1) * 128], in_=pb[:, 0:128]), [pb], [CM])
            tri = P.sb("s5tri", [128, 2, 128])
            P.dma(tri[:, 0, :], C.tri[d], reads=[C.tri], writes=[tri])
            P.dma(tri[:, 1, :], C.tri[2 + d], reads=[C.tri], writes=[tri])
            dbc = P.sb("s5dbc", [128, 1024])
            load_bc(P, dbc, C.s5_d[0, :], C.s5_d)
            sbuf = [P.sb(f"s5s{i}", [128, 8192]) for i in range(2)]
            ub = [P.sb(f"s5u{i}", [128, 1024]) for i in range(1)]
            uTb = [P.sb(f"s5uT{i}", [128, 8, 128]) for i in range(1)]
            bsb = [P.sb(f"s5b{i}", [128, 1024]) for i in range(2)]
            sTb = [P.sb(f"s5sT{i}", [128, 4, 128]) for i in range(2)]
            yb = [P.sb(f"s5y{i}", [128, 1024]) for i in range(1)]
            tm = [[P.sb(f"s5tm{i}_{k}", [128, 8, 64]) for k in range(4)] for i in range(1)]
            order = list(range(T)) if d == 0 else list(range(NCT - 1, -1, -1)) + list(range(T - 1, NCT - 1, -1))
            for ci, t in enumerate(order):
                u, uT, snew, sprev, yv = ub[0], uTb[0], sbuf[ci % 2], sbuf[(ci + 1) % 2], yb[0]
                rows = slice(t * 128, (t + 1) * 128)
                P.dma(u[:, :], C.QKVU[rows, 1536:2560], reads=[C.QKVU], writes=[u])
                tr_chunks(P, C, u, 8, uT)
                it = 0
                for gb in range(8):
                    ap2, bA, bB = P.bank2()
                    for half, bk in enumerate((bA, bB)):
                        P.pe(lambda gb=gb, half=half, bk=bk, uT=uT: nc.tensor.matmul(
                            bk[:, :], lhsT=uT[:, gb, :], rhs=RB[:, gb, half * 512:(half + 1) * 512], start=True, stop=True),
                            [uT, RB], [bk])
                    bs = bsb[gb % 2]
                    P.act(lambda ap2=ap2, bs=bs: nc.scalar.copy(out=bs[:, :], in_=ap2), [bA, bB], [bs])
                    v = bs[:, :].rearrange("t (g r p) -> t g r p", g=8, r=2)
                    wv = snew[:, gb * 1024:(gb + 1) * 1024].rearrange("t (g r p) -> t g r p", g=8, r=2)
                    tr_ = tabs[0][:, gb * 512:(gb + 1) * 512].rearrange("t (g p) -> t g p", p=64)
                    ti_ = tabs[1][:, gb * 512:(gb + 1) * 512].rearrange("t (g p) -> t g p", p=64)
                    tmps = [(x[:, :, :], x) for x in tm[0]]
                    cmul(P, v[:, :, 0, :], v[:, :, 1, :], tr_, ti_, wv[:, :, 0, :], wv[:, :, 1, :], tmps,
                         [bs], [tabs[0], tabs[1]], [snew])
                for gb in range(8):
                    ap2, bA, bB = P.bank2()
                    for half, bk in enumerate((bA, bB)):
                        c0 = gb * 1024 + half * 512
                        P.pe(lambda bk=bk, c0=c0, snew=snew, first=(ci == 0): nc.tensor.matmul(
                            bk[:, :], lhsT=tri[:, 0, :], rhs=snew[:, c0:c0 + 512], start=True, stop=first),
                            [tri, snew], [bk])
                        if ci > 0:
                            P.pe(lambda bk=bk, c0=c0, sprev=sprev: nc.tensor.matmul(
                                bk[:, :], lhsT=tri[:, 1, :], rhs=sprev[:, c0:c0 + 512], start=False, stop=True),
                                [tri, sprev], [bk])
                    bs = bsb[gb % 2]
                    P.act(lambda ap2=ap2, bs=bs: nc.scalar.copy(out=bs[:, :], in_=ap2), [bA, bB], [bs])
                    v = bs[:, :].rearrange("t (g r p) -> t g r p", g=8, r=2)
                    wv = snew[:, gb * 1024:(gb + 1) * 1024].rearrange("t (g r p) -> t g r p", g=8, r=2)
                    tr_ = tabs[2][:, gb * 512:(gb + 1) * 512].rearrange("t (g p) -> t g p", p=64)
                    ti_ = tabs[3][:, gb * 512:(gb + 1) * 512].rearrange("t (g p) -> t g p", p=64)
                    tmps = [(x[:, :, :], x) for x in tm[0]]
                    cmul(P, v[:, :, 0, :], v[:, :, 1, :], tr_, ti_, wv[:, :, 0, :], wv[:, :, 1, :], tmps,
                         [bs], [tabs[2], tabs[3]], [snew])
                if ci == 0:
                    P.dump(f"s{d}", snew[:, :], snew, [128, 8192])
                    P.dump(f"CM{d}", CM[:, :], CM, [128, 1024])
                    P.dump(f"RB{d}", RB[:, :, :], RB, [128, 8, 1024])
                yap, yA, yB = P.bank2()
                P.reserved = set(P.last_pair)
                for g4 in range(16):
                    sT = sTb[g4 % 2]
                    tr_chunks(P, C, snew, 4, sT, col0=g4 * 512)
                    for j in range(4):
                        g = g4 * 4 + j
                        yk = yA if g < 32 else yB
                        P.pe(lambda sT=sT, j=j, g=g, yk=yk: nc.tensor.matmul(
                            yk[:, (g % 32) * 16:(g % 32) * 16 + 16], lhsT=sT[:, j, :], rhs=CM[:, g * 16:(g + 1) * 16],
                            start=True, stop=True), [sT, CM], [yk])
                if d == 0:
                    P.dve(lambda yv=yv, u=u: nc.vector.tensor_tensor(out=yv[:, :], in0=u[:, :], in1=dbc[:, :], op=ALU.mult),
                          [u, dbc], [yv])
                else:
                    P.dma(yv[:, :], C.Y5[rows, :], reads=[C.Y5], writes=[yv])
                P.dve(lambda yv=yv, yap=yap: nc.vector.tensor_tensor(out=yv[:, :], in0=yv[:, :], in1=yap, op=ALU.add),
                      [yv, yA, yB], [yv])
                P.reserved = set()
                P.dma(C.Y5[rows, :], yv[:, :], reads=[yv], writes=[C.Y5])


def host_s5_constants():
    i = np.arange(128)[:, None]
    j = np.arange(128)[None, :]
    tri = np.stack([(i <= j), (i >= j), np.broadcast_to(i == 127, (128, 128)), np.broadcast_to(i == 0, (128, 128))]
                   ).astype(np.float32)
    t = np.arange(128, dtype=np.float32)
    ncol = np.stack([t + 1, 128 - t, -(t + 1), -(128 - t)], axis=1).astype(np.float32)
    neg = np.float32(-30000.0)
    ssdmask = np.stack([np.where(i <= j, 0, neg), np.where(i >= j, 0, neg)]).astype(np.float32)
    return {"tri": tri, "ncol": ncol, "ssdmask": ssdmask}


def phase_outproj0(P, C, l, hsrc):
    nc = P.nc
    with P.scope():
        G = 2
        g1 = [P.sb(f"opg1{k}", [128, D]) for k in range(2)]
        for kind in range(2):
            P.dma(g1[kind][:, :], C.MOD[l, kind, :, 2 * D:3 * D], reads=[C.MOD], writes=[g1[kind]])
        gb = P.sb("opglub", [128, 1024])
        load_bc(P, gb, C.s5_glu_b[0, :], C.s5_glu_b)
        cat = [P.sb(f"opcat{i}", [128, D]) for i in range(G)]
        catT = [P.sb(f"opcatT{i}", [128, 16, 128]) for i in range(G)]
        gg = [P.sb(f"opg{i}", [128, 1024]) for i in range(G)]
        gT = [P.sb(f"opgT{i}", [128, 8, 128]) for i in range(G)]
        ht = [P.sb(f"oph{i}", [128, D]) for i in range(G)]
        zt = [P.sb(f"opz{i}", [128, 512]) for i in range(2)]
        C.wbuf = [P.sb(f"opw{i}", [128, 16, 512]) for i in range(2)]
        for kind, t0, nt in tile_groups(C, G):
            for i in range(nt):
                rows = slice((t0 + i) * 128, (t0 + i + 1) * 128)
                P.dma(gg[i][:, :], C.Y5[rows, :], reads=[C.Y5], writes=[gg[i]])
                P.dma(cat[i][:, 0:1024], C.ATS5[rows, 0:1024], reads=[C.ATS5], writes=[cat[i]])
                P.dma(ht[i][:, :], hsrc[rows, :], reads=[hsrc], writes=[ht[i]])
                P.act(lambda i=i: nc.scalar.activation(out=gg[i][:, :], in_=gg[i][:, :], func=AF.Gelu), [gg[i]], [gg[i]])
                tr_chunks(P, C, gg[i], 8, gT[i])

            def consume_glu(n, ti, pb, w):
                z = zt[(n + ti) % 2]
                P.dve(lambda: nc.vector.tensor_tensor(out=z[:, 0:w], in0=pb[:, 0:w], in1=gb[:, n * 512:n * 512 + w], op=ALU.add),
                      [pb, gb], [z])
                P.act(lambda: nc.scalar.activation(out=z[:, 0:w], in_=z[:, 0:w], func=AF.Sigmoid), [z], [z])
                P.dve(lambda: nc.vector.tensor_tensor(out=cat[ti][:, 1024 + n * 512:1024 + n * 512 + w],
                                                      in0=gg[ti][:, n * 512:n * 512 + w], in1=z[:, 0:w], op=ALU.mult),
                      [gg[ti], z], [cat[ti]])
            C.wsrc = C.s5_glu_w
            linear(P, C, gT[0:nt], 8, C.s5_glu_w[0], 1024, consume_glu)
            for i in range(nt):
                tr_chunks(P, C, cat[i], 16, catT[i])

            def consume_out(n, ti, pb, w, kind=kind):
                z = zt[(n + ti) % 2]
                P.dve(lambda: nc.vector.tensor_tensor(out=z[:, 0:w], in0=pb[:, 0:w], in1=g1[kind][:, n * 512:n * 512 + w],
                                                      op=ALU.mult), [pb, g1[kind]], [z])
                P.dve(lambda: nc.vector.tensor_tensor(out=ht[ti][:, n * 512:n * 512 + w], in0=ht[ti][:, n * 512:n * 512 + w],
                                                      in1=z[:, 0:w], op=ALU.add), [ht[ti], z], [ht[ti]])
            C.wsrc = C.ab_w_out
            linear(P, C, catT[0:nt], 16, C.ab_w_out[0], 2048, consume_out)
            for i in range(nt):
                rows = slice((t0 + i) * 128, (t0 + i + 1) * 128)
                P.dma(C.H[rows, :], ht[i][:, :], reads=[ht[i]], writes=[C.H])


def topk16(P, src_ap, srcres, seg2, vals, idx, vres, ires):
    nc = P.nc
    P.dve(lambda: nc.vector.max(out=vals[:, 0:8], in_=src_ap), [srcres], [vres])
    if idx is not None:
        P.dve(lambda: nc.vector.max_index(out=idx[:, 0:8], in_max=vals[:, 0:8], in_values=src_ap), [srcres, vres], [ires])
    P.dve(lambda: nc.vector.match_replace(out=seg2[:, :], in_to_replace=vals[:, 0:8], in_values=src_ap, imm_value=-1e30),
          [srcres, vres], [seg2])
    P.dve(lambda: nc.vector.max(out=vals[:, 8:16], in_=seg2[:, :]), [seg2], [vres])
    if idx is not None:
        P.dve(lambda: nc.vector.max_index(out=idx[:, 8:16], in_max=vals[:, 8:16], in_values=seg2[:, :]), [seg2, vres], [ires])


def phase_peer_convert(P, C, l):
    nc = P.nc
    with P.scope():
        fu = [P.sb(f"pcfu{i}", [128, 4, 2048]) for i in range(2)]
        fv = [P.sb(f"pcfv{i}", [128, 4, 2048]) for i in range(2)]
        bout = [P.sb(f"pcb{i}", [128, 4, 4096], BF16) for i in range(2)]
        for n in range(32):
            a, b, bo = fu[n % 2], fv[n % 2], bout[n % 2]
            P.dma(a[:, :, :], C.peer_u[l, n * 512:(n + 1) * 512, :].rearrange("(p r) d -> p r d", r=4),
                  reads=[C.peer_u], writes=[a])
            P.dma(b[:, :, :], C.peer_v[l, n * 512:(n + 1) * 512, :].rearrange("(p r) d -> p r d", r=4),
                  reads=[C.peer_v], writes=[b])
            P.dve(lambda a=a, bo=bo: nc.vector.tensor_copy(out=bo[:, :, 0:2048], in_=a[:, :, :]), [a], [bo])
            P.act(lambda b=b, bo=bo: nc.scalar.copy(out=bo[:, :, 2048:4096], in_=b[:, :, :]), [b], [bo])
            r0 = l * 16384 + n * 512
            P.dma(C.UVB[r0:r0 + 512, :].rearrange("(p r) d -> p r d", r=4), bo[:, :, :], reads=[bo], writes=[C.UVB])


def phase_peer(P, C, l, tiles, split=False):
    nc = P.nc
    phase_peer_convert(P, C, l)
    with P.scope():
        Gp = [P.sb(f"peGp{k}", [128, D]) for k in range(2)]
        Sh = [P.sb(f"peSh{k}", [128, D]) for k in range(2)]
        prep_mod(P, C, l, 3, 4, C.norm2_g[l, :], C.norm2_g, Gp, Sh)
        g2 = [P.sb(f"peg2{k}", [128, D]) for k in range(2)]
        for kind in range(2):
            P.dma(g2[kind][:, :], C.MOD[l, kind, :, 5 * D:6 * D], reads=[C.MOD], writes=[g2[kind]])
        KM = P.sb("peKM", [128, 8, 256])
        P.pool(lambda: nc.gpsimd.memset(KM[:, :, :], 0.0), [], [KM])
        kin = [P.sb(f"pekin{i}", [128, 2, 64]) for i in range(2)]
        for h in range(8):
            ki = kin[h % 2]
            P.dma(ki[:, :, :], C.peer_keys[l, h].rearrange("half k d -> k half d"), reads=[C.peer_keys], writes=[ki])
            pb = P.bank()
            P.pe(lambda ki=ki, pb=pb: nc.tensor.transpose(out=pb[:, 0:128], in_=ki[:, :, :].rearrange("k a d -> k (a d)"),
                                                         identity=C.ident[:, :]), [ki, C.ident], [pb])
            P.act(lambda pb=pb, h=h: nc.scalar.copy(out=KM[0:64, h, 0:128], in_=pb[0:64, 0:128]), [pb], [KM])
            P.act(lambda pb=pb, h=h: nc.scalar.copy(out=KM[64:128, h, 128:256], in_=pb[64:128, 0:128]), [pb], [KM])
        hb = [P.sb(f"peh{i}", [128, D]) for i in range(1)]
        fb = [P.sb(f"pef{i}", [128, D]) for i in range(1)]
        fT = P.sb("pefT", [128, 16, 128])
        q = P.sb("peq", [128, 1024])
        qT = P.sb("peqT", [128, 8, 128])
        sc = P.sb("pesc", [128, 8, 256])
        seg2 = P.sb("peseg2", [128, 256])
        s2h = P.sb("peseg2h", [128, 128])
        v12 = P.sb("pev12", [128, 8, 2, 16])
        i12 = P.sb("pei12", [128, 8, 2, 16], U32)
        i12f = P.sb("pei12f", [128, 8, 2, 16])
        cand = P.sb("pecand", [128, 8, 256])
        cidx = sc
        score = P.sb("pescore", [128, 8, 16])
        pos = P.sb("pepos", [128, 8, 16], U32)
        posf = P.sb("peposf", [128, 8, 16])
        ef = P.sb("peef", [128, 128])
        ei = P.sb("peei", [128, 128], I32)
        gate = P.sb("pegate", [128, 8, 16])
        gs = P.sb("pegs", [128, 16])
        araw = P.sb("pearaw", [128, 128])
        wgt = P.sb("pewgt", [128, 128])
        junk = P.sb("pejunk", [128, D], BF16)
        gl = P.sb("pegl", [128, 128])
        st = P.sb("pest", [128, 4])
        acc = P.sb("peacc", [128, D])
        NG = 8
        gbuf = [P.sb(f"peg{i}", [128, 2 * D], BF16) for i in range(NG)]
        if split:
            ridx = P.sb("peridx", [128, C.NL // 2], I32)
            P.dma(ridx[:, :], C.rowidx[:, :], reads=[C.rowidx], writes=[ridx])
        C.wbuf = [P.sb(f"pew{i}", [128, 16, 128]) for i in range(2)]
        gi = 0
        for it, t in enumerate(tiles):
            kind = 1 if (t < C.NCT and not split) else 0
            rows = slice(t * 128, (t + 1) * 128)
            hT, f = hb[0], fb[0]
            if split:
                P.op("pool", lambda t=t: nc.gpsimd.indirect_dma_start(
                    out=hT[:, :], out_offset=None, in_=C.H[:, :],
                    in_offset=bass.IndirectOffsetOnAxis(ap=ridx[:, t:t + 1], axis=0)), [ridx, C.H], [hT], dma=True)
            else:
                P.dma(hT[:, :], C.H[rows, :], reads=[C.H], writes=[hT])
            C.xres, C.ores = hT, f
            norm_mod(P, C, hT[:, :], f[:, :], Gp[kind], Sh[kind], junk, st)
            tr_chunks(P, C, f, 16, fT)

            def consume_q(n, ti, pb, w):
                P.act(lambda: nc.scalar.copy(out=q[:, n * 128:n * 128 + w], in_=pb[:, 0:w]), [pb], [q])
            C.wsrc = C.peer_wq
            linear(P, C, [fT], 16, C.peer_wq[l], 1024, consume_q, cw=128)
            tr_chunks(P, C, q, 8, qT)
            for h2 in range(4):
                pb = P.bank()
                for j in range(2):
                    h = h2 * 2 + j
                    P.pe(lambda pb=pb, j=j, h=h: nc.tensor.matmul(pb[:, j * 256:(j + 1) * 256], lhsT=qT[:, h, :], rhs=KM[:, h, :],
                                                                  start=True, stop=True), [qT, KM], [pb])
                P.act(lambda pb=pb, h2=h2: nc.scalar.copy(out=sc[:, 2 * h2:2 * h2 + 2, :],
                                                         in_=pb[:, :].rearrange("p (a b) -> p a b", b=256)), [pb], [sc])
            for h in range(8):
                for half in range(2):
                    topk16(P, sc[:, h, half * 128:(half + 1) * 128], sc, s2h,
                           v12[:, h, half, :], i12[:, h, half, :], v12, i12)
            P.dve(lambda: nc.vector.tensor_copy(out=i12f[:, :, :, :], in_=i12[:, :, :, :]), [i12], [i12f])
            P.dve(lambda: nc.vector.tensor_tensor(
                out=cand[:, :, :].rearrange("p h (a b) -> p h a b", b=16),
                in0=v12[:, :, 0, :].unsqueeze(3).to_broadcast([128, 8, 16, 16]),
                in1=v12[:, :, 1, :].unsqueeze(2).to_broadcast([128, 8, 16, 16]), op=ALU.add), [v12], [cand])
            P.dve(lambda: nc.vector.tensor_scalar(out=i12f[:, :, 0, :], in0=i12f[:, :, 0, :], scalar1=128.0, scalar2=None,
                                                  op0=ALU.mult), [i12f], [i12f])
            P.dve(lambda: nc.vector.tensor_tensor(
                out=cidx[:, :, :].rearrange("p h (a b) -> p h a b", b=16),
                in0=i12f[:, :, 0, :].unsqueeze(3).to_broadcast([128, 8, 16, 16]),
                in1=i12f[:, :, 1, :].unsqueeze(2).to_broadcast([128, 8, 16, 16]), op=ALU.add), [i12f], [cidx])
            for h in range(8):
                topk16(P, cand[:, h, :], cand, seg2, score[:, h, :], pos[:, h, :], score, pos)
            P.dve(lambda: nc.vector.tensor_copy(out=posf[:, :, :], in_=pos[:, :, :]), [pos], [posf])
            for h in range(8):
                for k in range(16):
                    P.dve(lambda h=h, k=k: (nc.vector.scalar_tensor_tensor(
                        out=seg2[:, :], in0=C.iota[:, :], scalar=posf[:, h, k:k + 1], in1=cidx[:, h, :],
                        op0=ALU.is_equal, op1=ALU.mult, accum_out=ef[:, h * 16 + k:h * 16 + k + 1]),
                        nc.vector.tensor_copy(out=P.scr[:, 1:2], in_=ef[:, h * 16 + k:h * 16 + k + 1]))[1],
                        [C.iota, posf, cidx], [seg2, ef])
            P.dve(lambda: nc.vector.tensor_copy(out=ei[:, :], in_=ef[:, :]), [ef], [ei])
            P.dve(lambda: nc.vector.tensor_tensor(out=gate[:, :, :], in0=score[:, :, :],
                                                  in1=score[:, :, 0:1].to_broadcast([128, 8, 16]), op=ALU.subtract), [score], [gate])
            P.act(lambda: nc.scalar.activation(out=gate[:, :, :], in_=gate[:, :, :], func=AF.Exp), [gate], [gate])
            P.dve(lambda: nc.vector.reduce_sum(out=gs[:, 0:8], in_=gate[:, :, :], axis=AX.X), [gate], [gs])
            P.dve(lambda: nc.vector.reciprocal(out=gs[:, 8:16], in_=gs[:, 0:8]), [gs], [gs])
            P.dve(lambda: nc.vector.tensor_tensor(out=gate[:, :, :], in0=gate[:, :, :],
                                                  in1=gs[:, 8:16].unsqueeze(2).to_broadcast([128, 8, 16]), op=ALU.mult), [gate, gs], [gate])
            gate2 = gate[:, :, :].rearrange("p h k -> p (h k)")
            for blk in range(32):
                gs_ = []
                for jj in range(4):
                    j = blk * 4 + jj
                    g = gbuf[gi % NG]
                    gi += 1
                    gs_.append(g)
                    P.op("pool", lambda g=g, j=j: nc.gpsimd.indirect_dma_start(
                        out=g[:, :], out_offset=None, in_=C.UVB[:, :],
                        in_offset=bass.IndirectOffsetOnAxis(ap=ei[:, j:j + 1], axis=0), element_offset=l * 16384 * 4096),
                        [ei, C.UVB], [g], dma=True)
                    P.dve(lambda g=g, j=j, f=f: (nc.vector.scalar_tensor_tensor(
                        out=junk[:, :], in0=g[:, 0:D], scalar=1.0, in1=f[:, :], op0=ALU.mult, op1=ALU.mult,
                        accum_out=araw[:, j:j + 1]), nc.vector.tensor_copy(out=P.scr[:, 1:2], in_=araw[:, j:j + 1]))[1],
                        [g, f], [junk, araw])
                c0, c1 = blk * 4, blk * 4 + 4
                P.act(lambda c0=c0, c1=c1: nc.scalar.activation(out=gl[:, c0:c1], in_=araw[:, c0:c1], func=AF.Gelu), [araw], [gl])
                P.dve(lambda c0=c0, c1=c1: nc.vector.tensor_tensor(out=wgt[:, c0:c1], in0=gl[:, c0:c1], in1=gate2[:, c0:c1],
                                                                   op=ALU.mult), [gl, gate], [wgt])
                for jj in range(4):
                    j = blk * 4 + jj
                    g = gs_[jj]
                    if j == 0:
                        P.dve(lambda g=g: nc.vector.tensor_scalar(out=acc[:, :], in0=g[:, D:2 * D], scalar1=wgt[:, 0:1], scalar2=None,
                                                                  op0=ALU.mult), [g, wgt], [acc])
                    else:
                        P.dve(lambda g=g, j=j: nc.vector.scalar_tensor_tensor(
                            out=acc[:, :], in0=g[:, D:2 * D], scalar=wgt[:, j:j + 1], in1=acc[:, :], op0=ALU.mult, op1=ALU.add),
                            [g, wgt, acc], [acc])
            P.dve(lambda kind=kind: nc.vector.tensor_tensor(out=acc[:, :], in0=acc[:, :], in1=g2[kind][:, :], op=ALU.mult),
                  [acc, g2[kind]], [acc])
            P.dve(lambda hT=hT: nc.vector.tensor_tensor(out=hT[:, :], in0=hT[:, :], in1=acc[:, :], op=ALU.add), [hT, acc], [hT])
            if split:
                P.dma(C.H2[rows, :], hT[:, :], reads=[hT], writes=[C.H2])
            else:
                P.dma(C.H[rows, :], hT[:, :], reads=[hT], writes=[C.H])


def seg2_half(seg2):
    return Res("seg2h", seg2.t[:, 0:128])


def phase_ssd_in(P, C, l):
    nc = P.nc
    with P.scope():
        Gp = [P.sb(f"siGp{k}", [128, D]) for k in range(2)]
        Sh = [P.sb(f"siSh{k}", [128, D]) for k in range(2)]
        prep_mod(P, C, l, 0, 1, C.norm1_g[l, :], C.norm1_g, Gp, Sh)
        G = 2
        xs = [P.sb(f"six{i}", [128, D]) for i in range(G)]
        xT = [P.sb(f"sixT{i}", [128, 16, 128]) for i in range(G)]
        ob = [P.sb(f"sio{i}", [128, 512]) for i in range(4)]
        junk = P.sb("sijunk", [128, D])
        st = P.sb("sist", [128, 4])
        C.wbuf = [P.sb(f"siw{i}", [128, 16, 512]) for i in range(2)]
        cnt = [0]
        for kind, t0, nt in tile_groups(C, G):
            for i in range(nt):
                t = t0 + i
                P.dma(xs[i][:, :], C.H[t * 128:(t + 1) * 128, :], reads=[C.H], writes=[xs[i]])
                C.xres, C.ores = xs[i], xs[i]
                norm_mod(P, C, xs[i][:, :], xs[i][:, :], Gp[kind], Sh[kind], junk, st)
                tr_chunks(P, C, xs[i], 16, xT[i])

            def consume(n, ti, pb, w, t0=t0):
                o = ob[cnt[0] % 4]
                cnt[0] += 1
                t = t0 + ti
                if cnt[0] % 2:
                    P.act(lambda: nc.scalar.copy(out=o[:, 0:w], in_=pb[:, 0:w]), [pb], [o])
                else:
                    P.dve(lambda: nc.vector.tensor_copy(out=o[:, 0:w], in_=pb[:, 0:w]), [pb], [o])
                P.dma(C.SP[t * 128:(t + 1) * 128, n * 512:n * 512 + w], o[:, 0:w], reads=[o], writes=[C.SP])
            C.wsrc = C.ssd_w_in
            linear(P, C, xT[0:nt], 16, C.ssd_w_in[0], 10368, consume)


def phase_ssd_conv(P, C):
    nc = P.nc
    CW = 2048
    with P.scope():
        wk = P.sb("scw", [128, 5, CW])
        bias = P.sb("scb", [128, CW])
        xk = [P.sb(f"scx{k}", [128, CW]) for k in range(5)]
        for c in range(3):
            cols = slice(4096 + c * CW, 4096 + (c + 1) * CW)
            for k in range(5):
                P.dma(wk[:, k, :], C.ssd_conv_w[0, k, c * CW:(c + 1) * CW].partition_broadcast(128),
                      reads=[C.ssd_conv_w], writes=[wk])
            P.dma(bias[:, :], C.ssd_conv_b[0, c * CW:(c + 1) * CW].partition_broadcast(128), reads=[C.ssd_conv_b], writes=[bias])
            for t in range(C.T):
                s0, s1 = (0, C.NCT) if t < C.NCT else (C.NCT, C.T)
                r0 = t * 128
                for k in range(5):
                    lo, hi = r0 + k - 2, r0 + k - 2 + 128
                    vlo, vhi = max(lo, s0 * 128), min(hi, s1 * 128)
                    if vlo > lo or vhi < hi:
                        P.pool(lambda k=k: nc.gpsimd.memset(xk[k][:, :], 0.0), [], [xk[k]])
                    P.dma(xk[k][vlo - lo:vhi - lo, :], C.SP[vlo:vhi, cols], reads=[C.SP], writes=[xk[k]])
                for k in range(5):
                    if k % 2 == 0:
                        P.dve(lambda k=k: nc.vector.tensor_tensor(out=xk[k][:, :], in0=xk[k][:, :], in1=wk[:, k, :], op=ALU.mult),
                              [xk[k], wk], [xk[k]])
                    else:
                        P.pool(lambda k=k: nc.gpsimd.tensor_tensor(out=xk[k][:, :], in0=xk[k][:, :], in1=wk[:, k, :], op=ALU.mult),
                               [xk[k], wk], [xk[k]])
                P.pool(lambda: nc.gpsimd.tensor_tensor(out=xk[1][:, :], in0=xk[1][:, :], in1=xk[3][:, :], op=ALU.add),
                       [xk[1], xk[3]], [xk[1]])
                P.dve(lambda: nc.vector.tensor_tensor(out=xk[0][:, :], in0=xk[0][:, :], in1=xk[2][:, :], op=ALU.add),
                      [xk[0], xk[2]], [xk[0]])
                P.pool(lambda: nc.gpsimd.tensor_tensor(out=xk[4][:, :], in0=xk[4][:, :], in1=bias[:, :], op=ALU.add),
                       [xk[4], bias], [xk[4]])
                P.dve(lambda: nc.vector.tensor_tensor(out=xk[0][:, :], in0=xk[0][:, :], in1=xk[1][:, :], op=ALU.add),
                      [xk[0], xk[1]], [xk[0]])
                P.dve(lambda: nc.vector.tensor_tensor(out=xk[0][:, :], in0=xk[0][:, :], in1=xk[4][:, :], op=ALU.add),
                      [xk[0], xk[4]], [xk[0]])
                P.act(lambda: nc.scalar.activation(out=xk[0][:, :], in_=xk[0][:, :], func=AF.Silu), [xk[0]], [xk[0]])
                P.dma(C.XBC[r0:r0 + 128, c * CW:(c + 1) * CW], xk[0][:, :], reads=[xk[0]], writes=[C.XBC])


def ssd_pass(P, C, d):
    nc = P.nc
    NCT, NL, T = C.NCT, C.NL, C.T
    with P.scope():
        tri = P.sb("sstri", [128, 128])
        P.dma(tri[:, :], C.tri[d], reads=[C.tri], writes=[tri])
        nmask = P.sb("ssnm", [128, 128])
        P.dma(nmask[:, :], C.ssdmask[d], reads=[C.ssdmask], writes=[nmask])
        ones = P.sb("ssones", [128, 128])
        P.pool(lambda: nc.gpsimd.memset(ones[:, :], 1.0), [], [ones])
        abc = P.sb("ssa", [128, 64])
        load_bc(P, abc, C.ssd_a_log[0, d * 64:(d + 1) * 64], C.ssd_a_log)
        P.act(lambda: nc.scalar.activation(out=abc[:, :], in_=abc[:, :], func=AF.Exp), [abc], [abc])
        P.dve(lambda: nc.vector.tensor_scalar(out=abc[:, :], in0=abc[:, :], scalar1=-1.0, scalar2=None, op0=ALU.mult), [abc], [abc])
        dtb = P.sb("ssdtb", [128, 64])
        load_bc(P, dtb, C.ssd_dt_bias[0, d * 64:(d + 1) * 64], C.ssd_dt_bias)
        dsk = P.sb("ssdsk", [128, 64])
        load_bc(P, dsk, C.ssd_d[0, :], C.ssd_d)
        ST = P.sb("ssST", [128, 4096])
        P.pool(lambda: nc.gpsimd.memset(ST[:, :], 0.0), [], [ST])
        xs = P.sb("ssx", [128, 4096]); xdd = P.sb("ssxdd", [128, 4096]); yt = P.sb("ssy", [128, 4096])
        bc = P.sb("ssbc", [128, 2048])
        BT = P.sb("ssBT", [128, 8, 128]); CT = P.sb("ssCT", [128, 8, 128]); cbT = P.sb("sscbT", [128, 8, 128])
        sm = P.sb("sssm", [128, 16, 64])
        LT = [P.sb(f"ssLT{i}", [128, 128]) for i in range(4)]
        dec = [P.sb(f"ssdec{i}", [128, 128]) for i in range(4)]
        yo = [P.sb(f"ssyo{i}", [128, 512]) for i in range(2)]
        yd = [P.sb(f"ssyd{i}", [128, 512]) for i in range(2)]
        if d == 0:
            order = list(range(T))
        else:
            order = list(range(NCT - 1, -1, -1)) + list(range(T - 1, NCT - 1, -1))
        DTR, X_, LA, CS, TOT, DTE, ECS, NCS, TMP, DT, ETOT = range(11)
        S = lambda k: sm[:, k, :]
        for t in order:
            lat = t >= NCT
            rows = slice(t * 128, (t + 1) * 128)
            lrows = slice((t - NCT) * 128, (t - NCT + 1) * 128)
            P.dma(xs[:, :], C.XBC[rows, 0:4096], reads=[C.XBC], writes=[xs])
            P.dma(bc[:, :], C.XBC[rows, 4096:6144], reads=[C.XBC], writes=[bc])
            P.dma(sm[:, DTR, :], C.SP[rows, 10240 + d * 64:10240 + (d + 1) * 64], reads=[C.SP], writes=[sm])
            P.dve(lambda: nc.vector.tensor_tensor(out=S(X_), in0=S(DTR), in1=dtb[:, :], op=ALU.add), [sm, dtb], [sm])
            P.act(lambda: nc.scalar.activation(out=S(TMP), in_=S(X_), func=AF.Abs), [sm], [sm])
            P.act(lambda: nc.scalar.activation(out=S(TMP), in_=S(TMP), func=AF.Exp, scale=-1.0), [sm], [sm])
            P.act(lambda: nc.scalar.activation(out=S(TMP), in_=S(TMP), func=AF.Ln, bias=1.0), [sm], [sm])
            P.dve(lambda: nc.vector.tensor_scalar(out=S(DT), in0=S(X_), scalar1=0.0, scalar2=None, op0=ALU.max), [sm], [sm])
            P.dve(lambda: nc.vector.tensor_tensor(out=S(DT), in0=S(DT), in1=S(TMP), op=ALU.add), [sm], [sm])
            P.dve(lambda: nc.vector.tensor_tensor(out=S(LA), in0=S(DT), in1=abc[:, :], op=ALU.mult), [sm, abc], [sm])
            pb = P.bank()
            P.pe(lambda pb=pb: nc.tensor.matmul(pb[:, 0:64], lhsT=tri[:, :], rhs=S(LA), start=True, stop=True), [tri, sm], [pb])
            P.pe(lambda pb=pb: nc.tensor.matmul(pb[:, 64:128], lhsT=ones[:, :], rhs=S(LA), start=True, stop=True), [ones, sm], [pb])
            P.act(lambda pb=pb: nc.scalar.copy(out=sm[:, CS:CS + 2, :], in_=pb[:, 0:128].rearrange("p (a b) -> p a b", b=64)),
                  [pb], [sm])
            P.dve(lambda: nc.vector.tensor_tensor(out=S(DTE), in0=S(TOT), in1=S(CS), op=ALU.subtract), [sm], [sm])
            P.act(lambda: nc.scalar.activation(out=S(DTE), in_=S(DTE), func=AF.Exp), [sm], [sm])
            P.act(lambda: nc.scalar.activation(out=S(ECS), in_=S(CS), func=AF.Exp), [sm], [sm])
            P.act(lambda: nc.scalar.activation(out=S(ETOT), in_=S(TOT), func=AF.Exp), [sm], [sm])
            P.dve(lambda: nc.vector.tensor_scalar(out=S(NCS), in0=S(CS), scalar1=-1.0, scalar2=None, op0=ALU.mult), [sm], [sm])
            x3 = xs[:, :].rearrange("p (h q) -> p h q", q=64)
            if lat:
                if d == 0:
                    P.pool(lambda: nc.gpsimd.tensor_tensor(out=yt[:, :].rearrange("p (h q) -> p h q", q=64), in0=x3,
                                                           in1=dsk[:, :].unsqueeze(2).to_broadcast([128, 64, 64]), op=ALU.mult),
                           [xs, dsk], [yt])
                else:
                    P.dma(yt[:, :], C.YS[lrows, :], reads=[C.YS], writes=[yt])
            P.dve(lambda: nc.vector.tensor_tensor(out=x3, in0=x3, in1=S(DT).unsqueeze(2).to_broadcast([128, 64, 64]), op=ALU.mult),
                  [xs, sm], [xs])
            P.dve(lambda: nc.vector.tensor_tensor(out=xdd[:, :].rearrange("p (h q) -> p h q", q=64), in0=x3,
                                                  in1=S(DTE).unsqueeze(2).to_broadcast([128, 64, 64]), op=ALU.mult),
                  [xs, sm], [xdd])
            if lat:
                tr_chunks(P, C, bc, 8, BT, col0=0)
                tr_chunks(P, C, bc, 8, CT, col0=1024)
                for g in range(8):
                    pb = P.bank()
                    P.pe(lambda pb=pb, g=g: nc.tensor.matmul(pb[:, 0:128], lhsT=BT[:, g, :], rhs=CT[:, g, :], start=True, stop=True),
                         [BT, CT], [pb])
                    P.act(lambda pb=pb, g=g: nc.scalar.copy(out=cbT[:, g, :], in_=pb[:, 0:128]), [pb], [cbT])
                for g in range(8):
                    pbo = P.bank()
                    P.pe(lambda pbo=pbo, g=g: nc.tensor.matmul(pbo[:, :], lhsT=CT[:, g, :], rhs=ST[:, g * 512:(g + 1) * 512],
                                                               start=True, stop=True), [CT, ST], [pbo])
                    yo_ = yo[g % 2]
                    P.dve(lambda pbo=pbo, yo_=yo_, g=g: nc.vector.tensor_tensor(
                        out=yo_[:, :].rearrange("p (h q) -> p h q", q=64), in0=pbo[:, :].rearrange("p (h q) -> p h q", q=64),
                        in1=sm[:, ECS, g * 8:(g + 1) * 8].unsqueeze(2).to_broadcast([128, 8, 64]), op=ALU.mult), [pbo, sm], [yo_])
                    pby = P.bank()
                    P.reserved = {(P.psn - 1) % 8}
                    for j in range(8):
                        h = g * 8 + j
                        lt, dc = LT[h % 4], dec[h % 4]
                        P.pool(lambda lt=lt, h=h: nc.gpsimd.tensor_scalar(out=lt[:, :], in0=tri[:, :], scalar1=sm[:, LA, h:h + 1],
                                                                         scalar2=None, op0=ALU.mult), [tri, sm], [lt])
                        pbd = P.bank()
                        P.pe(lambda pbd=pbd, lt=lt: nc.tensor.matmul(pbd[:, 0:128], lhsT=ones[:, :], rhs=lt[:, :], start=True, stop=False),
                             [ones, lt], [pbd])
                        P.pe(lambda pbd=pbd: nc.tensor.matmul(pbd[:, 0:128], lhsT=C.ident[:, :], rhs=nmask[:, :], start=False, stop=True),
                             [C.ident, nmask], [pbd])
                        P.act(lambda pbd=pbd, dc=dc, h=h: nc.scalar.activation(out=dc[:, :], in_=pbd[:, 0:128], func=AF.Exp,
                                                                                bias=sm[:, NCS, h:h + 1], scale=1.0), [pbd, sm], [dc])
                        P.dve(lambda dc=dc, g=g: nc.vector.tensor_tensor(out=dc[:, :], in0=dc[:, :], in1=cbT[:, g, :], op=ALU.mult),
                              [dc, cbT], [dc])
                        P.pe(lambda pby=pby, dc=dc, j=j, h=h: nc.tensor.matmul(pby[:, j * 64:(j + 1) * 64], lhsT=dc[:, :],
                                                                               rhs=xs[:, h * 64:(h + 1) * 64], start=True, stop=True),
                             [dc, xs], [pby])
                    P.reserved = set()
                    yd_ = yd[g % 2]
                    P.act(lambda pby=pby, yd_=yd_: nc.scalar.copy(out=yd_[:, :], in_=pby[:, :]), [pby], [yd_])
                    P.pool(lambda yo_=yo_, yd_=yd_: nc.gpsimd.tensor_tensor(out=yo_[:, :], in0=yo_[:, :], in1=yd_[:, :], op=ALU.add),
                           [yo_, yd_], [yo_])
                    P.pool(lambda yo_=yo_, g=g: nc.gpsimd.tensor_tensor(out=yt[:, g * 512:(g + 1) * 512], in0=yt[:, g * 512:(g + 1) * 512],
                                                                      in1=yo_[:, :], op=ALU.add), [yo_, yt], [yt])
                P.dma(C.YS[lrows, :], yt[:, :], reads=[yt], writes=[C.YS])
                if t == NCT:
                    P.dump(f"ssm{d}", sm[:, :, :], sm, [128, 16, 64])
                    P.dump(f"scbT{d}", cbT[:, :, :], cbT, [128, 8, 128])
                    P.dump(f"sdec{d}", dec[3][:, :], dec[3], [128, 128])
                    P.dump(f"sST{d}", ST[:, :], ST, [128, 4096])
                    P.dump(f"sxs{d}", xs[:, :], xs, [128, 4096])
                    P.dump(f"syd{d}", yd[1][:, :], yd[1], [128, 512])
                    P.dump(f"syo{d}", yo[1][:, :], yo[1], [128, 512])
            P.dve(lambda: nc.vector.tensor_tensor(out=ST[:, :].rearrange("p (h q) -> p h q", q=64),
                                                  in0=ST[:, :].rearrange("p (h q) -> p h q", q=64),
                                                  in1=S(ETOT).unsqueeze(2).to_broadcast([128, 64, 64]), op=ALU.mult), [ST, sm], [ST])
            for g in range(8):
                pbs = P.bank()
                P.pe(lambda pbs=pbs, g=g: nc.tensor.matmul(pbs[:, :], lhsT=bc[:, g * 128:(g + 1) * 128], rhs=xdd[:, g * 512:(g + 1) * 512],
                                                           start=True, stop=True), [bc, xdd], [pbs])
                P.dve(lambda pbs=pbs, g=g: nc.vector.tensor_tensor(out=ST[:, g * 512:(g + 1) * 512], in0=ST[:, g * 512:(g + 1) * 512],
                                                                   in1=pbs[:, :], op=ALU.add), [pbs, ST], [ST])


def phase_ssd_out(P, C, l):
    nc = P.nc
    with P.scope():
        g1 = P.sb("sog1", [128, D])
        P.dma(g1[:, :], C.MOD[l, 0, :, 2 * D:3 * D], reads=[C.MOD], writes=[g1])
        ng = P.sb("song", [128, 4096])
        load_bc(P, ng, C.ssd_norm_g[0, :], C.ssd_norm_g)
        G = 1
        yb = [P.sb(f"soy{i}", [128, 4096]) for i in range(G)]
        zb = [P.sb(f"soz{i}", [128, 4096]) for i in range(G)]
        yT = [P.sb(f"soyT{i}", [128, 32, 128]) for i in range(G)]
        ht = [P.sb(f"soh{i}", [128, D]) for i in range(G)]
        zt = [P.sb(f"sozt{i}", [128, 256]) for i in range(2)]
        st = P.sb("sost", [128, 4, 8])
        junk = P.sb("sojunk", [128, 512])
        C.wbuf = [P.sb(f"sow{i}", [128, 32, 256]) for i in range(2)]
        for t0 in range(0, C.NL, G):
            nt = min(G, C.NL - t0)
            for i in range(nt):
                lrows = slice((t0 + i) * 128, (t0 + i + 1) * 128)
                rows = slice((C.NCT + t0 + i) * 128, (C.NCT + t0 + i + 1) * 128)
                y, z = yb[i], zb[i]
                P.dma(y[:, :], C.YS[lrows, :], reads=[C.YS], writes=[y])
                P.dma(z[:, :], C.SP[rows, 0:4096], reads=[C.SP], writes=[z])
                P.dma(ht[i][:, :], C.H[rows, :], reads=[C.H], writes=[ht[i]])
                P.act(lambda z=z: nc.scalar.activation(out=z[:, :], in_=z[:, :], func=AF.Silu), [z], [z])
                P.dve(lambda y=y, z=z: nc.vector.tensor_tensor(out=y[:, :], in0=y[:, :], in1=z[:, :], op=ALU.mult), [y, z], [y])
                for g in range(8):
                    P.act(lambda y=y, g=g: (nc.scalar.activation(out=junk[:, :], in_=y[:, g * 512:(g + 1) * 512], func=AF.Square,
                                                                 accum_out=st[:, 0, g:g + 1]),
                                            nc.scalar.copy(out=P.scr[:, 0:1], in_=st[:, 0, g:g + 1]))[1], [y], [junk, st])
                P.dve(lambda: nc.vector.tensor_scalar(out=st[:, 1, :], in0=st[:, 0, :], scalar1=1.0 / 512, scalar2=EPS,
                                                      op0=ALU.mult, op1=ALU.add), [st], [st])
                P.act(lambda: nc.scalar.activation(out=st[:, 2, :], in_=st[:, 1, :], func=AF.Sqrt), [st], [st])
                P.dve(lambda: nc.vector.reciprocal(out=st[:, 3, :], in_=st[:, 2, :]), [st], [st])
                P.dve(lambda y=y: nc.vector.tensor_tensor(out=y[:, :].rearrange("p (g q) -> p g q", q=512),
                                                          in0=y[:, :].rearrange("p (g q) -> p g q", q=512),
                                                          in1=st[:, 3, :].unsqueeze(2).to_broadcast([128, 8, 512]), op=ALU.mult),
                      [y, st], [y])
                P.pool(lambda y=y: nc.gpsimd.tensor_tensor(out=y[:, :], in0=y[:, :], in1=ng[:, :], op=ALU.mult), [y, ng], [y])
                tr_chunks(P, C, y, 32, yT[i])

            def consume_out(n, ti, pb, w):
                z = zt[(n + ti) % 2]
                P.dve(lambda: nc.vector.tensor_tensor(out=z[:, 0:w], in0=pb[:, 0:w], in1=g1[:, n * 256:n * 256 + w], op=ALU.mult),
                      [pb, g1], [z])
                P.dve(lambda: nc.vector.tensor_tensor(out=ht[ti][:, n * 256:n * 256 + w], in0=ht[ti][:, n * 256:n * 256 + w],
                                                      in1=z[:, 0:w], op=ALU.add), [ht[ti], z], [ht[ti]])
            C.wsrc = C.ssd_w_out
            linear(P, C, yT[0:nt], 32, C.ssd_w_out[0], 2048, consume_out, cw=256)
            for i in range(nt):
                rows = slice((C.NCT + t0 + i) * 128, (C.NCT + t0 + i + 1) * 128)
                P.dma(C.H[rows, :], ht[i][:, :], reads=[ht[i]], writes=[C.H])


def phase_final(P, C):
    nc = P.nc
    with P.scope():
        g = P.sb("fng", [128, D])
        load_bc(P, g, C.final_norm_g[:], C.final_norm_g)
        xb = [P.sb(f"fnx{i}", [128, D]) for i in range(2)]
        junk = P.sb("fnjunk", [128, D])
        stb = [P.sb(f"fnst{i}", [128, 4]) for i in range(2)]
        for t in range(C.NO):
            x, st = xb[t % 2], stb[t % 2]
            rows = slice(t * 128, (t + 1) * 128)
            if SPLIT:
                P.dma(x[:, :], C.H2[rows, :], reads=[C.H2], writes=[x])
            else:
                P.dma(x[:, :], C.H[(C.NCT + t) * 128:(C.NCT + t + 1) * 128, :], reads=[C.H], writes=[x])
            P.act(lambda x=x, st=st: (nc.scalar.activation(out=junk[:, :], in_=x[:, :], func=AF.Square, accum_out=st[:, 0:1]),
                                      nc.scalar.copy(out=P.scr[:, 0:1], in_=st[:, 0:1]))[1], [x], [junk, st])
            P.dve(lambda st=st: nc.vector.tensor_scalar(out=st[:, 1:2], in0=st[:, 0:1], scalar1=1.0 / D, scalar2=EPS,
                                                        op0=ALU.mult, op1=ALU.add), [st], [st])
            P.act(lambda st=st: nc.scalar.activation(out=st[:, 3:4], in_=st[:, 1:2], func=AF.Sqrt), [st], [st])
            P.dve(lambda st=st: nc.vector.reciprocal(out=st[:, 2:3], in_=st[:, 3:4]), [st], [st])
            P.dve(lambda x=x, st=st: nc.vector.scalar_tensor_tensor(out=x[:, :], in0=x[:, :], scalar=st[:, 2:3], in1=g[:, :],
                                                                    op0=ALU.mult, op1=ALU.mult), [x, st, g], [x])
            P.dma(C.OUT[t * 128:(t + 1) * 128, :], x[:, :], reads=[x], writes=[C.OUT])


N_CORES = 8
_CACHE = {}


_DBG = {}


def run_model(inputs, NL, NCT, batches, n_cores=None):
    inputs = {k: np.asarray(v) for k, v in inputs.items()}
    P = Prog()
    C = build_program(P, NL, NCT, stage="final")
    P.finish()
    used = set(C.used_inputs)
    shared = make_inputs(inputs, 0, NL, NCT, used=used)
    nb = len(batches)
    n_cores = n_cores or 2 * nb
    NLH = NL // 2
    maps = []
    for core in range(n_cores):
        b, half = batches[core % nb], core // nb
        m = dict(shared)
        m["h0"] = np.ascontiguousarray(np.concatenate([inputs["ctx"][b][:NCT * 128], inputs["x"][b][:NL * 128]], axis=0),
                                       dtype=np.float32)
        m["c_col"] = np.ascontiguousarray(inputs["c"][b].reshape(16, 128).T, dtype=np.float32)
        if "rowidx" in used:
            k = np.arange(NLH, dtype=np.int64)[None, :]
            p = np.arange(128, dtype=np.int64)[:, None]
            m["rowidx"] = np.ascontiguousarray(((NCT + half * NLH + k) * 128 + p).astype(np.int32))
        maps.append(m)
    res = run_bass_kernel_spmd(P.nc, maps, core_ids=list(range(n_cores)))
    out = np.zeros((nb, NL * 128, 2048), np.float32)
    for core in range(n_cores):
        bi, half = core % nb, core // nb
        if SPLIT:
            out[bi, half * NLH * 128:(half + 1) * NLH * 128] = np.asarray(res.results[core]["OUT"])
        elif half == 0:
            out[bi] = np.asarray(res.results[core]["OUT"])
    return out


def kernel(**inputs):
    return run_model(inputs, 32, 2, [0, 1, 2, 3], n_cores=N_CORES)
```

```python
from contextlib import ExitStack
import numpy as np
import concourse.bass as bass
import concourse.mybir as mybir
from concourse.bass_utils import run_bass_kernel_spmd

F32 = mybir.dt.float32
BF16 = mybir.dt.bfloat16
I32 = mybir.dt.int32
U32 = mybir.dt.uint32
AF = mybir.ActivationFunctionType
ALU = mybir.AluOpType
AX = mybir.AxisListType

EPOCH = 30000


class Res:
    __slots__ = ("name", "lw", "rd", "t")

    def __init__(self, name, t=None):
        self.name = name
        self.lw = None
        self.rd = []
        self.t = t

    def __getitem__(self, k):
        return self.t[k]


class Prog:
    ENGS = ("pe", "act", "dve", "pool", "sp")
    NSLOT = {"sp": 24, "act": 6, "pool": 12}

    def __init__(self):
        self.nc = bass.Bass("TRN2", target_bir_lowering=False)
        self.es = ExitStack()
        self.ops = []
        self.psbanks = None
        self.psn = 0
        self.reserved = set()
        self.scopes = [self.es]
        self.last_c = {}
        self.slot_next = {q: 0 for q in self.NSLOT}
        self.slot_last = {q: [None] * k for q, k in self.NSLOT.items()}
        self.pending = {e: set() for e in self.ENGS}

    def sb(self, name, shape, dt=F32):
        self.uid = getattr(self, "uid", 0) + 1
        name = f"{name}_{self.uid}"
        t = self.scopes[-1].enter_context(self.nc.sbuf_tensor(name, list(shape), dt))
        return Res(name, t)

    def scope(self):
        prog = self

        class _S:
            def __enter__(s2):
                prog.scopes.append(ExitStack())

            def __exit__(s2, *a):
                prog.scopes.pop().close()
                prog.barrier()
        return _S()

    def barrier(self):
        s = set(self.last_c.values())
        for q in self.NSLOT:
            s.update(x for x in self.slot_last[q] if x is not None)
        for e in self.ENGS:
            self.pending[e] = set(s)

    def ps(self, name, shape, dt=F32):
        t = self.es.enter_context(self.nc.psum_tensor(name, list(shape), dt))
        return Res(name, t)

    def bank(self):
        if self.psbanks is None:
            self.psall = self.ps("psall", [128, 8, 512], F32)
            self.psbanks = [Res(f"psb{i}", self.psall.t[:, i, :]) for i in range(8)]
        while self.psn % 8 in self.reserved:
            self.psn += 1
        b = self.psbanks[self.psn % 8]
        self.psn += 1
        return b

    def bank2(self):
        self.bank()
        self.psn -= 1
        while self.psn % 2 or (self.psn % 8) in self.reserved or (self.psn % 8 + 1) in self.reserved:
            self.psn += 1
        i = self.psn % 8
        self.psn += 2
        self.last_pair = (i, i + 1)
        return self.psall.t[:, i:i + 2, :].rearrange("p a b -> p (a b)"), self.psbanks[i], self.psbanks[i + 1]

    def dram(self, name, shape, dt=F32, kind="Internal"):
        t = self.nc.dram_tensor(name, list(shape), dt, kind=kind)
        return Res(name, t.ap())

    def op(self, eng, fn, reads=(), writes=(), dma=False, soft=()):
        idx = len(self.ops)
        deps = set()
        softdeps = set()
        for r in reads:
            if r.lw is not None:
                (softdeps if r in soft else deps).add(r.lw)
        for w in writes:
            tgt = softdeps if w in soft else deps
            if w.lw is not None:
                tgt.add(w.lw)
            tgt.update(w.rd)
        for d in softdeps:
            if self.ops[d][0] != eng or self.ops[d][3] or dma:
                deps.add(d)
        for r in reads:
            r.rd.append(idx)
        for w in writes:
            w.lw = idx
            w.rd = []
        if self.pending[eng]:
            deps.update(self.pending[eng])
            self.pending[eng] = set()
        slot = None
        if dma:
            slot = self.slot_next[eng]
            self.slot_next[eng] = (slot + 1) % self.NSLOT[eng]
            prev = self.slot_last[eng][slot]
            if prev is not None:
                deps.add(prev)
            self.slot_last[eng][slot] = idx
        else:
            self.last_c[eng] = idx
        self.ops.append([eng, fn, deps, dma, slot])
        return idx

    def pe(self, fn, reads=(), writes=()):
        return self.op("pe", fn, reads, writes)

    def act(self, fn, reads=(), writes=()):
        return self.op("act", fn, reads, writes)

    def dve(self, fn, reads=(), writes=(), soft=()):
        return self.op("dve", fn, reads, writes, soft=soft)

    def pool(self, fn, reads=(), writes=()):
        return self.op("pool", fn, reads, writes)

    def dma(self, out, in_, reads=(), writes=(), q="sp", **kw):
        nc = self.nc
        e = {"sp": nc.sync, "act": nc.scalar, "pool": nc.gpsimd}[q]
        return self.op(q, lambda: e.dma_start(out=out, in_=in_, **kw), reads, writes, dma=True)

    def dump(self, name, ap, res, shape):
        if not getattr(self, "debug", False):
            return
        d = self.dram("DBG_" + name, shape, F32, "ExternalOutput")
        self.dma(d[tuple(slice(None) for _ in shape)], ap, reads=[res], writes=[d])
        self.dbg = getattr(self, "dbg", []) + [d]

    def final(self, resources):
        nc = self.nc
        self.op("sp", lambda: nc.sync.nop(), reads=list(resources), writes=[])

    def finish(self):
        nc = self.nc
        ops = self.ops
        n = len(ops)

        def skip(d, i):
            return ops[d][0] == "pe" and ops[i][0] == "pe" and not ops[d][3] and not ops[i][3]

        needed = [False] * n
        for i, (eng, fn, deps, dma, slot) in enumerate(ops):
            for d in deps:
                if not skip(d, i):
                    needed[d] = True
        cnt = {e: 0 for e in self.ENGS}
        slot_cnt = {q: [0] * k for q, k in self.NSLOT.items()}
        slot_ep = {q: [0] * k for q, k in self.NSLOT.items()}
        ticket = [None] * n
        for i, (eng, fn, deps, dma, s) in enumerate(ops):
            if dma:
                if slot_cnt[eng][s] + 16 > EPOCH:
                    slot_cnt[eng][s] = 0
                    slot_ep[eng][s] += 1
                slot_cnt[eng][s] += 16
                ticket[i] = (("d", eng, s, slot_ep[eng][s]), slot_cnt[eng][s])
            elif needed[i]:
                cnt[eng] += 1
                ep = (cnt[eng] - 1) // EPOCH
                ticket[i] = (("c", eng, ep), cnt[eng] - ep * EPOCH)
        semkeys = sorted({t[0] for t in ticket if t is not None}, key=str)
        sems = {}
        for k in semkeys:
            sems[k] = self.es.enter_context(nc.semaphore("s_" + "_".join(str(x) for x in k)))
        self.nsem = len(sems)
        per_eng = {e: [] for e in self.ENGS}
        for i, o in enumerate(ops):
            per_eng[o[0]].append(i)
        engobj = {"pe": nc.tensor, "act": nc.scalar, "dve": nc.vector, "pool": nc.gpsimd, "sp": nc.sync}

        def emit(ename):
            e = engobj[ename]
            waited = {}
            for i in per_eng[ename]:
                eng, fn, deps, dma, slot = ops[i]
                dl = deps
                need = {}
                for d in dl:
                    if skip(d, i):
                        continue
                    k, v = ticket[d]
                    if need.get(k, 0) < v:
                        need[k] = v
                for k, v in sorted(need.items(), key=str):
                    if waited.get(k, 0) >= v:
                        continue
                    if any(kk[0] == k[0] and kk[1:-1] == k[1:-1] and kk[-1] > k[-1] for kk in waited):
                        continue
                    e.wait_ge(sems[k], v)
                    waited[k] = v
                ins = fn()
                if ticket[i] is not None:
                    k, v = ticket[i]
                    ins.then_inc(sems[k], 16 if dma else 1)

        with nc.Block() as block:
            @block.tensor
            def _(eng):
                emit("pe")

            @block.scalar
            def _(eng):
                emit("act")

            @block.vector
            def _(eng):
                emit("dve")

            @block.gpsimd
            def _(eng):
                emit("pool")

            @block.sync
            def _(eng):
                emit("sp")
        self.es.close()
        return nc


def phase_mod(P, depth, c_col, cc_col, mod_w, mod_b, MOD):
    nc = P.nc
    cs = P.sb("mod_cs", [128, 32])
    cs2 = P.sb("mod_cs2", [128, 32])
    rep = P.sb("mod_rep", [128, 32, 128])
    P.dma(cs[:, 0:16], c_col[:, :], reads=[c_col], writes=[cs])
    P.dma(cs[:, 16:32], cc_col[:, :], reads=[cc_col], writes=[cs])
    P.act(lambda: nc.scalar.activation(out=cs2[:, :], in_=cs[:, :], func=AF.Silu), [cs], [cs2])
    for kc in range(32):
        P.dve(lambda kc=kc: nc.vector.tensor_copy(out=rep[:, kc, :], in_=cs2[:, kc:kc + 1].to_broadcast([128, 128])),
              [cs2], [rep])
    wb = [P.sb(f"mod_w{i}", [128, 16, 512]) for i in range(2)]
    bb = [P.sb(f"mod_b{i}", [128, 512]) for i in range(2)]
    ob = [P.sb(f"mod_o{i}", [128, 512]) for i in range(4)]
    it = 0
    for l in range(depth):
        for n in range(24):
            wt, bt = wb[it % 2], bb[it % 2]
            P.dma(wt[:, :, :], mod_w[l, :, n * 512:(n + 1) * 512].rearrange("(kc p) n -> p kc n", p=128),
                  reads=[mod_w], writes=[wt])
            P.dma(bt[:, :], mod_b[l, n * 512:(n + 1) * 512].partition_broadcast(128), reads=[mod_b], writes=[bt])
            for kind in range(2):
                pb = P.bank()
                for kc in range(16):
                    P.pe(lambda kc=kc, kind=kind, pb=pb, wt=wt: nc.tensor.matmul(
                        pb[:, :], lhsT=rep[:, kind * 16 + kc, :], rhs=wt[:, kc, :], start=(kc == 0), stop=(kc == 15)),
                        [rep, wt], [pb])
                ot = ob[(it * 2 + kind) % 4]
                P.dve(lambda pb=pb, ot=ot, bt=bt: nc.vector.tensor_tensor(out=ot[:, :], in0=pb[:, :], in1=bt[:, :], op=ALU.add),
                      [pb, bt], [ot])
                P.dma(MOD[l, kind, :, n * 512:(n + 1) * 512], ot[:, :], reads=[ot], writes=[MOD])
            it += 1


D = 2048
EPS = 1e-6


class Ctx:
    pass


def tr_chunks(P, C, src, n, dst, col0=0, rows=128, cw=128):
    nc = P.nc
    per = 512 // rows if rows >= 128 else 4
    j0 = 0
    while j0 < n:
        nb = min(per, n - j0)
        pb = P.bank()
        for j in range(nb):
            P.pe(lambda j=j, j0=j0, pb=pb: nc.tensor.transpose(
                out=pb[0:cw, j * rows:(j + 1) * rows], in_=src[0:rows, col0 + (j0 + j) * cw: col0 + (j0 + j + 1) * cw],
                identity=C.ident[0:rows, 0:rows]), [src, C.ident], [pb])
        C.ev ^= 1
        o = dst[0:cw, j0:j0 + nb, 0:rows]
        i = pb[0:cw, 0:nb * rows].rearrange("p (j t) -> p j t", t=rows)
        if C.ev:
            P.act(lambda o=o, i=i: nc.scalar.copy(out=o, in_=i), [pb], [dst])
        else:
            P.dve(lambda o=o, i=i: nc.vector.tensor_copy(out=o, in_=i), [pb], [dst])
        j0 += nb


def linear(P, C, xT, nk, W, N, consume, cw=512, wtag="lw"):
    nc = P.nc
    wb = C.wbuf
    nch = (N + cw - 1) // cw
    for n in range(nch):
        w = min(cw, N - n * cw)
        wt = wb[C.wi % len(wb)]
        C.wi += 1
        P.dma(wt[:, 0:nk, 0:w], W[:, n * cw:n * cw + w].rearrange("(kc p) n -> p kc n", p=128),
              reads=[C.wsrc], writes=[wt])
        for t, xt in enumerate(xT):
            pb = P.bank()
            for kc in range(nk):
                P.pe(lambda kc=kc, pb=pb, xt=xt, wt=wt, w=w: nc.tensor.matmul(
                    pb[:, 0:w], lhsT=xt[:, kc, :], rhs=wt[:, kc, 0:w], start=(kc == 0), stop=(kc == nk - 1)),
                    [xt, wt], [pb])
            consume(n, t, pb, w)


def load_bc(P, dst, src_ap, srcres):
    P.dma(dst[:, :], src_ap.partition_broadcast(128), reads=[srcres], writes=[dst])


def prep_mod(P, C, l, j_shift, j_scale, g_ap, gres, Gp, Sh):
    nc = P.nc
    with P.scope():
        gt = P.sb("pm_g", [128, D])
        load_bc(P, gt, g_ap, gres)
        for kind in range(2):
            P.dma(Sh[kind][:, :], C.MOD[l, kind, :, j_shift * D:(j_shift + 1) * D], reads=[C.MOD], writes=[Sh[kind]])
            P.dma(Gp[kind][:, :], C.MOD[l, kind, :, j_scale * D:(j_scale + 1) * D], reads=[C.MOD], writes=[Gp[kind]])
            P.dve(lambda kind=kind: nc.vector.scalar_tensor_tensor(
                out=Gp[kind][:, :], in0=Gp[kind][:, :], scalar=1.0, in1=gt[:, :], op0=ALU.add, op1=ALU.mult),
                [Gp[kind], gt], [Gp[kind]])


def norm_mod(P, C, x, out, Gp, Sh, junk, st):
    nc = P.nc
    P.act(lambda: (nc.scalar.activation(out=junk[:, :], in_=x, func=AF.Square, accum_out=st[:, 0:1]),
                   nc.scalar.copy(out=P.scr[:, 0:1], in_=st[:, 0:1]))[1], [C.xres], [junk, st])
    P.dve(lambda: nc.vector.tensor_scalar(out=st[:, 1:2], in0=st[:, 0:1], scalar1=1.0 / D, scalar2=EPS,
                                          op0=ALU.mult, op1=ALU.add), [st], [st])
    P.act(lambda: nc.scalar.activation(out=st[:, 3:4], in_=st[:, 1:2], func=AF.Sqrt), [st], [st])
    P.dve(lambda: nc.vector.reciprocal(out=st[:, 2:3], in_=st[:, 3:4]), [st], [st])
    P.dve(lambda: nc.vector.scalar_tensor_tensor(out=out, in0=x, scalar=st[:, 2:3], in1=Gp[:, :],
                                                 op0=ALU.mult, op1=ALU.mult), [C.xres, st, Gp], [C.ores])
    P.dve(lambda: nc.vector.tensor_tensor(out=out, in0=out, in1=Sh[:, :], op=ALU.add), [C.ores, Sh], [C.ores])


def tile_groups(C, G=2):
    gs = []
    for t0 in range(0, C.NCT, G):
        gs.append((1, t0, min(G, C.NCT - t0)))
    for t0 in range(0, C.NL, G):
        gs.append((0, C.NCT + t0, min(G, C.NL - t0)))
    return gs


def phase_inproj0(P, C, l, hsrc):
    nc = P.nc
    with P.scope():
        Gp = [P.sb(f"ipGp{k}", [128, D]) for k in range(2)]
        Sh = [P.sb(f"ipSh{k}", [128, D]) for k in range(2)]
        prep_mod(P, C, l, 0, 1, C.norm1_g[l, :], C.norm1_g, Gp, Sh)
        G = 2
        xs = [P.sb(f"ipx{i}", [128, D]) for i in range(G)]
        xT = [P.sb(f"ipxT{i}", [128, 16, 128]) for i in range(G)]
        os_ = [P.sb(f"ipo{i}", [128, 2560]) for i in range(G)]
        junk = P.sb("ipjunk", [128, D])
        st = P.sb("ipst", [128, 4])
        cs = P.sb("ipcs", [128, 2, 64])
        tmp = P.sb("iptmp", [128, 4, 10, 64])
        C.wbuf = [P.sb(f"ipw{i}", [128, 16, 512]) for i in range(2)]
        for kind, t0, nt in tile_groups(C, G):
            for i in range(nt):
                t = t0 + i
                P.dma(xs[i][:, :], hsrc[t * 128:(t + 1) * 128, :], reads=[hsrc], writes=[xs[i]])
                C.xres, C.ores = xs[i], xs[i]
                norm_mod(P, C, xs[i][:, :], xs[i][:, :], Gp[kind], Sh[kind], junk, st)
                tr_chunks(P, C, xs[i], 16, xT[i])

            def consume(n, ti, pb, w):
                o = os_[ti]
                C.ev ^= 1
                if C.ev:
                    P.act(lambda: nc.scalar.copy(out=o[:, n * 512:n * 512 + w], in_=pb[:, 0:w]), [pb], [o])
                else:
                    P.dve(lambda: nc.vector.tensor_copy(out=o[:, n * 512:n * 512 + w], in_=pb[:, 0:w]), [pb], [o])
            C.wsrc = C.ab_w_in
            linear(P, C, xT[0:nt], 16, C.ab_w_in[0], 2560, consume)
            for i in range(nt):
                t = t0 + i
                o = os_[i]
                if kind == 0:
                    lt = t - C.NCT
                    P.dma(cs[:, :, :], C.rope[lt * 128:(lt + 1) * 128, :, :], reads=[C.rope], writes=[cs])
                    qk = o[:, 0:1280].rearrange("p (h two d) -> p h two d", two=2, d=64)
                    x1, x2 = qk[:, :, 0, :], qk[:, :, 1, :]
                    cosb = cs[:, 0:1, :].to_broadcast([128, 10, 64])
                    sinb = cs[:, 1:2, :].to_broadcast([128, 10, 64])
                    for j, (a, b) in enumerate([(x1, cosb), (x2, sinb), (x2, cosb), (x1, sinb)]):
                        P.dve(lambda j=j, a=a, b=b: nc.vector.tensor_tensor(out=tmp[:, j, :, :], in0=a, in1=b, op=ALU.mult),
                              [o, cs], [tmp])
                    P.dve(lambda x1=x1: nc.vector.tensor_tensor(out=x1, in0=tmp[:, 0, :, :], in1=tmp[:, 1, :, :], op=ALU.subtract),
                          [tmp], [o])
                    P.dve(lambda x2=x2: nc.vector.tensor_tensor(out=x2, in0=tmp[:, 2, :, :], in1=tmp[:, 3, :, :], op=ALU.add),
                          [tmp], [o])
                P.dma(C.QKVU[t * 128:(t + 1) * 128, :], o[:, :], reads=[o], writes=[C.QKVU])


def sub(res, name):
    return Res(name, res.t)


ATTN_SCALE = 128 ** -0.5


def phase_attn(P, C):
    nc = P.nc
    NCT, NL = C.NCT, C.NL
    T = NCT + NL
    with P.scope():
        KT = [P.sb(f"atKT{h}", [128, T * 128]) for h in range(2)]
        KTr = [sub(KT[0], f"KTr{t}") for t in range(T)]
        V = P.sb("atV", [128, T, 256])
        Vr = [sub(V, f"Vr{t}") for t in range(T)]
        kin = [P.sb(f"atk{i}", [128, 256]) for i in range(2)]
        sink = P.sb("atsink", [128, 8])
        load_bc(P, sink, C.attn_sink[0, :], C.attn_sink)
        mask = P.sb("atmask", [128, 384])
        P.dma(mask[:, :], C.bandmask[:, :], reads=[C.bandmask], writes=[mask])
        for t in range(T):
            ki = kin[t % 2]
            P.dma(ki[:, :], C.QKVU[t * 128:(t + 1) * 128, 1024:1280], reads=[C.QKVU], writes=[ki])
            P.dma(V[:, t, :], C.QKVU[t * 128:(t + 1) * 128, 1280:1536], reads=[C.QKVU], writes=[Vr[t]])
            pb = P.bank()
            for h in range(2):
                P.pe(lambda h=h, pb=pb, ki=ki: nc.tensor.transpose(
                    out=pb[:, h * 128:(h + 1) * 128], in_=ki[:, h * 128:(h + 1) * 128], identity=C.ident[:, :]),
                    [ki, C.ident], [pb])
            for h in range(2):
                P.act(lambda h=h, pb=pb, t=t: nc.scalar.copy(out=KT[h][:, t * 128:(t + 1) * 128],
                                                           in_=pb[:, h * 128:(h + 1) * 128]), [pb], [KTr[t]])
        qb = [P.sb(f"atq{i}", [128, 1024]) for i in range(2)]
        qT = [P.sb(f"atqT{i}", [128, 8, 128]) for i in range(2)]
        ob = [P.sb(f"ato{i}", [128, 1024]) for i in range(2)]
        scb = [P.sb(f"atsc{i}", [128, 640]) for i in range(2)]
        prb = [P.sb(f"atpr{i}", [128, 640]) for i in range(2)]
        ptb = [P.sb(f"atpt{i}", [128, 5, 128]) for i in range(2)]
        mst = [P.sb(f"atm{i}", [128, 8, 8]) for i in range(2)]
        it = 0
        for t in range(T):
            kind = 1 if t < NCT else 0
            q, QT, o_t, m = qb[t % 2], qT[t % 2], ob[t % 2], mst[t % 2]
            P.dma(q[:, :], C.QKVU[t * 128:(t + 1) * 128, 0:1024], reads=[C.QKVU], writes=[q])
            tr_chunks(P, C, q, 8, QT)
            ktiles = list(range(NCT))
            if kind == 0:
                n = t - NCT
                lo, hi = max(0, n - 1), min(NL - 1, n + 1)
                moff = (lo - (n - 1)) * 128
                ktiles += list(range(NCT + lo, NCT + hi + 1))
                nb = hi - lo + 1
            nk = len(ktiles) * 128
            for hq in range(8):
                h = hq // 4
                sc, pr, PT = scb[it % 2], prb[it % 2], ptb[it % 2]
                it += 1
                pbA = P.bank()
                P.pe(lambda pbA=pbA, QT=QT, hq=hq, h=h: nc.tensor.matmul(
                    pbA[:, 0:NCT * 128], lhsT=QT[:, hq, :], rhs=KT[h][:, 0:NCT * 128], start=True, stop=True),
                    [QT] + KTr[0:NCT], [pbA])
                P.act(lambda pbA=pbA, sc=sc: nc.scalar.activation(
                    out=sc[:, 0:NCT * 128], in_=pbA[:, 0:NCT * 128], func=AF.Copy, scale=ATTN_SCALE), [pbA], [sc])
                if kind == 0:
                    pbB = P.bank()
                    c0, c1 = (NCT + lo) * 128, (NCT + hi + 1) * 128
                    P.pe(lambda pbB=pbB, QT=QT, hq=hq, h=h, c0=c0, c1=c1, nb=nb: nc.tensor.matmul(
                        pbB[:, 0:nb * 128], lhsT=QT[:, hq, :], rhs=KT[h][:, c0:c1], start=True, stop=True),
                        [QT] + KTr[NCT + lo:NCT + hi + 1], [pbB])
                    P.dve(lambda pbB=pbB, sc=sc, nb=nb, moff=moff: nc.vector.scalar_tensor_tensor(
                        out=sc[:, NCT * 128:NCT * 128 + nb * 128], in0=pbB[:, 0:nb * 128], scalar=ATTN_SCALE,
                        in1=mask[:, moff:moff + nb * 128], op0=ALU.mult, op1=ALU.add), [pbB, mask], [sc])
                P.dve(lambda sc=sc, m=m, hq=hq, nk=nk: nc.vector.reduce_max(out=m[:, hq, 0:1], in_=sc[:, 0:nk], axis=AX.X),
                      [sc], [m])
                P.dve(lambda m=m, hq=hq: nc.vector.tensor_tensor(out=m[:, hq, 0:1], in0=m[:, hq, 0:1],
                                                                 in1=sink[:, hq:hq + 1], op=ALU.max), [m, sink], [m])
                P.dve(lambda m=m, hq=hq: nc.vector.tensor_scalar(out=m[:, hq, 1:2], in0=m[:, hq, 0:1], scalar1=-1.0,
                                                                 scalar2=None, op0=ALU.mult), [m], [m])
                P.act(lambda sc=sc, pr=pr, m=m, hq=hq, nk=nk: (nc.scalar.activation(
                    out=pr[:, 0:nk], in_=sc[:, 0:nk], func=AF.Exp, bias=m[:, hq, 1:2], scale=1.0,
                    accum_out=m[:, hq, 2:3]), nc.scalar.copy(out=P.scr[:, 0:1], in_=m[:, hq, 2:3]))[1], [sc, m], [pr, m])
                P.act(lambda m=m, hq=hq: nc.scalar.activation(
                    out=m[:, hq, 3:4], in_=sink[:, hq:hq + 1], func=AF.Exp, bias=m[:, hq, 1:2], scale=1.0), [sink, m], [m])
                P.dve(lambda m=m, hq=hq: nc.vector.tensor_tensor(out=m[:, hq, 4:5], in0=m[:, hq, 2:3],
                                                                 in1=m[:, hq, 3:4], op=ALU.add), [m], [m])
                P.dve(lambda m=m, hq=hq: nc.vector.reciprocal(out=m[:, hq, 5:6], in_=m[:, hq, 4:5]), [m], [m])
                tr_chunks(P, C, pr, nk // 128, PT)
                pbO = P.bank()
                for j, kt in enumerate(ktiles):
                    P.pe(lambda j=j, kt=kt, pbO=pbO, PT=PT, h=h, last=(j == len(ktiles) - 1): nc.tensor.matmul(
                        pbO[:, 0:128], lhsT=PT[:, j, :], rhs=V[:, kt, h * 128:(h + 1) * 128], start=(j == 0), stop=last),
                        [PT, Vr[kt]], [pbO])
                P.act(lambda pbO=pbO, o_t=o_t, m=m, hq=hq: nc.scalar.activation(
                    out=o_t[:, hq * 128:(hq + 1) * 128], in_=pbO[:, 0:128], func=AF.Copy, scale=m[:, hq, 5:6]),
                    [pbO, m], [o_t])
            P.dma(C.ATS5[t * 128:(t + 1) * 128, 0:1024], o_t[:, :], reads=[o_t], writes=[C.ATS5])


SPLIT = False


class Ctx:
    def __init__(self, P, NL, NCT, probes=()):
        T = NL + NCT
        self.NO = NL // 2 if SPLIT else NL
        self.P, self.NL, self.NCT, self.T = P, NL, NCT, T
        self.probes = set(probes)
        self.used_inputs = []
        self.ev = 0
        self.wi = 0
        R = T * 128
        self.spec = {
            "h0": ([R, 2048], "in"), "c_col": ([128, 16], "in"), "cc_col": ([128, 16], "in"),
            "mod_w": ([2, 2048, 12288], "in"), "mod_b": ([2, 12288], "in"),
            "norm1_g": ([2, 2048], "in"), "norm2_g": ([2, 2048], "in"),
            "ab_w_in": ([1, 2048, 2560], "in"), "attn_sink": ([1, 8], "in"),
            "s5_a_re": ([1, 2, 64, 64], "in"), "s5_a_im": ([1, 2, 64, 64], "in"), "s5_log_dt": ([1, 2, 64, 64], "in"),
            "s5_b_re": ([1, 2, 64, 64, 16], "in"), "s5_b_im": ([1, 2, 64, 64, 16], "in"),
            "s5_c_re": ([1, 2, 64, 16, 64], "in"), "s5_c_im": ([1, 2, 64, 16, 64], "in"),
            "s5_d": ([1, 1024], "in"), "s5_glu_w": ([1, 1024, 1024], "in"), "s5_glu_b": ([1, 1024], "in"),
            "ab_w_out": ([1, 2048, 2048], "in"),
            "ssd_w_in": ([1, 2048, 10368], "in"), "ssd_conv_w": ([1, 5, 6144], "in"), "ssd_conv_b": ([1, 6144], "in"),
            "ssd_dt_bias": ([1, 128], "in"), "ssd_a_log": ([1, 128], "in"), "ssd_d": ([1, 64], "in"),
            "ssd_norm_g": ([1, 4096], "in"), "ssd_w_out": ([1, 4096, 2048], "in"),
            "peer_wq": ([2, 2048, 1024], "in"), "peer_keys": ([2, 8, 2, 128, 64], "in"),
            "peer_u": ([2, 16384, 2048], "in"), "peer_v": ([2, 16384, 2048], "in"),
            "final_norm_g": ([2048], "in"),
            "ident": ([128, 128], "in"), "rope": ([NL * 128, 2, 64], "in"), "bandmask": ([128, 384], "in"),
            "tri": ([4, 128, 128], "in"), "ncol": ([128, 4], "in"), "iota256": ([128, 256], "in"),
            "MOD": ([2, 2, 128, 12288], "tmp"), "QKVU": ([R, 2560], "tmp"), "ATS5": ([R, 2048], "tmp"),
            "H": ([R, 2048], "tmp"), "Y5": ([R, 1024], "tmp"), "BB": ([2, 64, 16, 128], "tmp"),
            "SP": ([R, 10368], "tmp"), "XBC": ([R, 6144], "tmp"), "YS": ([NL * 128, 4096], "tmp"),
            "ssdmask": ([2, 128, 128], "in"),
            "UVB": ([2 * 16384, 4096], "tmp", BF16),
            "rowidx": ([128, NL // 2], "in", I32), "H2": ([NL // 2 * 128, 2048], "tmp"),
            "OUT": ([self.NO * 128, 2048], "out"),
        }

    def __getattr__(self, name):
        spec = self.__dict__.get("spec", {})
        if name not in spec:
            raise AttributeError(name)
        shape, kind = spec[name][0], spec[name][1]
        dt = spec[name][2] if len(spec[name]) > 2 else F32
        k = {"in": "ExternalInput", "out": "ExternalOutput"}.get(kind)
        if k is None:
            k = "ExternalOutput" if name in self.probes else "Internal"
        r = self.P.dram(name, shape, dt, k)
        if kind == "in":
            self.used_inputs.append(name)
        setattr(self, name, r)
        return r


STAGES = ["mod", "inproj0", "attn", "s5", "outproj0", "peer0", "ssd", "peer1", "final"]


def build_program(P, NL, NCT, stage="final", probes=()):
    C = Ctx(P, NL, NCT, probes)
    nc = P.nc
    si = STAGES.index(stage)
    C.ident_d = C.ident
    idt = P.es.enter_context(nc.sbuf_tensor("ident_sb", [128, 128], F32))
    ident_sb = Res("ident_sb", idt)
    P.dma(ident_sb[:, :], C.ident_d[:, :], reads=[C.ident_d], writes=[ident_sb])
    C.ident = ident_sb
    P.scr = Res("scr", P.es.enter_context(nc.sbuf_tensor("scr_tail", [128, 4], F32)))
    io = Res("iota_sb", P.es.enter_context(nc.sbuf_tensor("iota_sb", [128, 256], F32)))
    P.dma(io[:, :], C.iota256[:, :], reads=[C.iota256], writes=[io])
    C.iota = io
    with P.scope():
        phase_mod(P, 2, C.c_col, C.cc_col, C.mod_w, C.mod_b, C.MOD)
    outs = [C.MOD]
    if si >= 1:
        phase_inproj0(P, C, 0, C.h0)
        outs.append(C.QKVU)
    if si >= 2:
        phase_attn(P, C)
        outs.append(C.ATS5)
    if si >= 3:
        phase_s5(P, C)
        outs.append(C.Y5)
    if si >= 4:
        phase_outproj0(P, C, 0, C.h0)
        outs.append(C.H)
    if si >= 5:
        phase_peer(P, C, 0, list(range(C.T)))
    if si >= 6:
        phase_ssd_in(P, C, 1)
        phase_ssd_conv(P, C)
        for d in range(2):
            ssd_pass(P, C, d)
        phase_ssd_out(P, C, 1)
    if si >= 7:
        if SPLIT:
            phase_peer(P, C, 1, list(range(C.NL // 2)), split=True)
        else:
            phase_peer(P, C, 1, list(range(C.NCT, C.T)))
    if si >= 8:
        phase_final(P, C)
        outs.append(C.OUT)
    P.final(outs + getattr(P, "dbg", []))
    return C


def host_constants(NL):
    t = np.arange(NL * 128)
    row = (t // 64).astype(np.float32)
    col = (t % 64).astype(np.float32)
    inv = (10000.0 ** (-np.arange(32, dtype=np.float32) / 32)).astype(np.float32)
    ang = np.concatenate([row[:, None] * inv, col[:, None] * inv], axis=-1).astype(np.float32)
    rope = np.stack([np.cos(ang), np.sin(ang)], axis=1).astype(np.float32)
    i = np.arange(128)[:, None]
    j = np.arange(128)[None, :]
    neg = np.float32(-30000.0)
    bandmask = np.concatenate([np.where(j >= i, 0, neg), np.zeros((128, 128)), np.where(j <= i, 0, neg)],
                              axis=1).astype(np.float32)
    iota = np.broadcast_to(np.arange(256, dtype=np.float32)[None, :], (128, 256)).copy()
    return {"ident": np.eye(128, dtype=np.float32), "rope": rope, "bandmask": bandmask, "iota256": iota}


def make_inputs(inp, b, NL, NCT, used=None):
    m = {}
    m["h0"] = np.ascontiguousarray(np.concatenate([inp["ctx"][b][:NCT * 128], inp["x"][b][:NL * 128]], axis=0))
    m["c_col"] = np.ascontiguousarray(inp["c"][b].reshape(16, 128).T)
    m["cc_col"] = np.ascontiguousarray(inp["c_ctx"].reshape(16, 128).T)
    for k in ["mod_w", "mod_b", "norm1_g", "norm2_g", "ab_w_in", "attn_sink", "s5_a_re", "s5_a_im", "s5_log_dt",
              "s5_b_re", "s5_b_im", "s5_c_re", "s5_c_im", "s5_glu_w", "s5_glu_b", "ab_w_out", "ssd_w_in",
              "ssd_conv_w", "ssd_conv_b", "ssd_d", "ssd_norm_g", "ssd_w_out", "peer_wq", "peer_keys", "peer_u",
              "peer_v", "final_norm_g"]:
        m[k] = inp[k]
    m["s5_d"] = inp["s5_d"].reshape(1, 1024)
    m["ssd_dt_bias"] = inp["ssd_dt_bias"].reshape(1, 128)
    m["ssd_a_log"] = inp["ssd_a_log"].reshape(1, 128)
    m.update(host_constants(NL))
    m.update(host_s5_constants())
    if used is not None:
        m = {k: np.ascontiguousarray(v, dtype=np.float32) for k, v in m.items() if k in used}
    return m


PI = float(np.pi)


def sin_of(P, out, ang, shift, tmps, rin, rtmp, rout):
    nc = P.nc
    z, kf, ki = tmps
    P.dve(lambda: nc.vector.tensor_scalar(out=z, in0=ang, scalar1=float(shift), scalar2=None, op0=ALU.add), rin, rtmp)
    P.dve(lambda: nc.vector.tensor_scalar(out=ki, in0=z, scalar1=1.0 / (2 * PI), scalar2=None, op0=ALU.mult), rtmp, rtmp)
    P.dve(lambda: nc.vector.tensor_copy(out=kf, in_=ki), rtmp, rtmp)
    P.dve(lambda: nc.vector.scalar_tensor_tensor(out=z, in0=kf, scalar=-2 * PI, in1=z, op0=ALU.mult, op1=ALU.add), rtmp, rtmp)
    P.dve(lambda: nc.vector.tensor_scalar(out=kf, in0=z, scalar1=PI, scalar2=-2 * PI, op0=ALU.is_gt, op1=ALU.mult), rtmp, rtmp)
    P.dve(lambda: nc.vector.tensor_tensor(out=z, in0=z, in1=kf, op=ALU.add), rtmp, rtmp)
    P.dve(lambda: nc.vector.tensor_scalar(out=kf, in0=z, scalar1=-PI, scalar2=2 * PI, op0=ALU.is_lt, op1=ALU.mult), rtmp, rtmp)
    P.dve(lambda: nc.vector.tensor_tensor(out=z, in0=z, in1=kf, op=ALU.add), rtmp, rtmp)
    P.act(lambda: nc.scalar.activation(out=out, in_=z, func=AF.Sin), rtmp, rout)


def cmul(P, a_re, a_im, t_re, t_im, o_re, o_im, tmps, ra, rt, ro, conj=False):
    nc = P.nc
    tA, tB, tC, tD = tmps
    P.dve(lambda: nc.vector.tensor_tensor(out=tA[0], in0=a_re, in1=t_re, op=ALU.mult), ra + rt, [tA[1]])
    P.pool(lambda: nc.gpsimd.tensor_tensor(out=tB[0], in0=a_im, in1=t_im, op=ALU.mult), ra + rt, [tB[1]])
    P.pool(lambda: nc.gpsimd.tensor_tensor(out=tC[0], in0=a_re, in1=t_im, op=ALU.mult), ra + rt, [tC[1]])
    P.dve(lambda: nc.vector.tensor_tensor(out=tD[0], in0=a_im, in1=t_re, op=ALU.mult), ra + rt, [tD[1]])
    P.dve(lambda: nc.vector.tensor_tensor(out=o_re, in0=tA[0], in1=tB[0], op=ALU.subtract), [tA[1], tB[1]], ro)
    P.pool(lambda: nc.gpsimd.tensor_tensor(out=o_im, in0=tC[0], in1=tD[0], op=ALU.add), [tC[1], tD[1]], ro)


def phase_s5(P, C):
    with P.scope():
        for d in range(2):
            s5_prep(P, C, d)
    for d in range(2):
        s5_pass(P, C, d)


def s5_prep(P, C, d):
    nc = P.nc
    if True:
        if True:
            ar = P.sb(f"s5ar{d}", [64, 64]); ai = P.sb(f"s5ai{d}", [64, 64]); ld = P.sb(f"s5ld{d}", [64, 64])
            br = P.sb(f"s5br{d}", [64, 64, 16]); bi = P.sb(f"s5bi{d}", [64, 64, 16])
            P.dma(ar[:, :], C.s5_a_re[0, d], reads=[C.s5_a_re], writes=[ar])
            P.dma(ai[:, :], C.s5_a_im[0, d], reads=[C.s5_a_im], writes=[ai])
            P.dma(ld[:, :], C.s5_log_dt[0, d], reads=[C.s5_log_dt], writes=[ld])
            P.dma(br[:, :, :], C.s5_b_re[0, d], reads=[C.s5_b_re], writes=[br])
            P.dma(bi[:, :, :], C.s5_b_im[0, d], reads=[C.s5_b_im], writes=[bi])
            w = P.sb(f"s5w{d}", [64, 16, 64])
            W = lambda k: w[:, k, :]
            P.act(lambda: nc.scalar.activation(out=W(0), in_=ld[:, :], func=AF.Exp), [ld], [w])
            P.dve(lambda: nc.vector.tensor_tensor(out=W(1), in0=W(0), in1=ar[:, :], op=ALU.mult), [w, ar], [w])
            P.dve(lambda: nc.vector.tensor_tensor(out=W(2), in0=W(0), in1=ai[:, :], op=ALU.mult), [w, ai], [w])
            P.act(lambda: nc.scalar.activation(out=W(3), in_=W(1), func=AF.Exp), [w], [w])
            ki_s = P.sb(f"s5kis{d}", [64, 64], I32)
            tm3 = (W(4), W(14), ki_s[:, :])
            sin_of(P, W(5), W(2), 0.0, tm3, [w], [w, ki_s], [w])
            sin_of(P, W(6), W(2), PI / 2, tm3, [w], [w, ki_s], [w])
            P.dve(lambda: nc.vector.tensor_tensor(out=W(7), in0=W(3), in1=W(6), op=ALU.mult), [w], [w])
            P.dve(lambda: nc.vector.tensor_scalar(out=W(7), in0=W(7), scalar1=-1.0, scalar2=None, op0=ALU.add), [w], [w])
            P.dve(lambda: nc.vector.tensor_tensor(out=W(8), in0=W(3), in1=W(5), op=ALU.mult), [w], [w])
            P.dve(lambda: nc.vector.tensor_tensor(out=W(9), in0=ar[:, :], in1=ar[:, :], op=ALU.mult), [ar], [w])
            P.dve(lambda: nc.vector.tensor_tensor(out=W(12), in0=ai[:, :], in1=ai[:, :], op=ALU.mult), [ai], [w])
            P.dve(lambda: nc.vector.tensor_tensor(out=W(9), in0=W(9), in1=W(12), op=ALU.add), [w], [w])
            P.dve(lambda: nc.vector.reciprocal(out=W(9), in_=W(9)), [w], [w])
            P.dve(lambda: nc.vector.tensor_tensor(out=W(12), in0=W(7), in1=ar[:, :], op=ALU.mult), [w, ar], [w])
            P.dve(lambda: nc.vector.tensor_tensor(out=W(13), in0=W(8), in1=ai[:, :], op=ALU.mult), [w, ai], [w])
            P.dve(lambda: nc.vector.tensor_tensor(out=W(12), in0=W(12), in1=W(13), op=ALU.add), [w], [w])
            P.dve(lambda: nc.vector.tensor_tensor(out=W(10), in0=W(12), in1=W(9), op=ALU.mult), [w], [w])
            P.dve(lambda: nc.vector.tensor_tensor(out=W(12), in0=W(8), in1=ar[:, :], op=ALU.mult), [w, ar], [w])
            P.dve(lambda: nc.vector.tensor_tensor(out=W(13), in0=W(7), in1=ai[:, :], op=ALU.mult), [w, ai], [w])
            P.dve(lambda: nc.vector.tensor_tensor(out=W(12), in0=W(12), in1=W(13), op=ALU.subtract), [w], [w])
            P.dve(lambda: nc.vector.tensor_tensor(out=W(11), in0=W(12), in1=W(9), op=ALU.mult), [w], [w])
            bbT = P.sb(f"s5bbT{d}", [64, 16, 2, 64])
            t1 = P.sb(f"s5t1{d}", [64, 64, 16]); t2 = P.sb(f"s5t2{d}", [64, 64, 16])
            fre = w[:, 10, :].unsqueeze(2).to_broadcast([64, 64, 16])
            fim = w[:, 11, :].unsqueeze(2).to_broadcast([64, 64, 16])
            o_re = bbT[:, :, 0, :].rearrange("g c p -> g p c")
            o_im = bbT[:, :, 1, :].rearrange("g c p -> g p c")
            P.dve(lambda: nc.vector.tensor_tensor(out=t1[:, :, :], in0=br[:, :, :], in1=fre, op=ALU.mult), [br, w], [t1])
            P.dve(lambda: nc.vector.tensor_tensor(out=t2[:, :, :], in0=bi[:, :, :], in1=fim, op=ALU.mult), [bi, w], [t2])
            P.dve(lambda: nc.vector.tensor_tensor(out=o_re, in0=t1[:, :, :], in1=t2[:, :, :], op=ALU.subtract), [t1, t2], [bbT])
            P.dve(lambda: nc.vector.tensor_tensor(out=t1[:, :, :], in0=bi[:, :, :], in1=fre, op=ALU.mult), [bi, w, bbT], [t1])
            P.dve(lambda: nc.vector.tensor_tensor(out=t2[:, :, :], in0=br[:, :, :], in1=fim, op=ALU.mult), [br, w, bbT], [t2])
            P.dve(lambda: nc.vector.tensor_tensor(out=o_im, in0=t1[:, :, :], in1=t2[:, :, :], op=ALU.add), [t1, t2], [bbT])
            P.dma(C.BB[d], bbT[:, :, :, :].rearrange("g c r p -> g c (r p)"), reads=[bbT], writes=[C.BB])


def s5_pass(P, C, d):
    nc = P.nc
    NCT, NL, T = C.NCT, C.NL, C.T
    if True:
        with P.scope():
            tabs = [P.sb(f"s5tab{k}", [128, 4096]) for k in range(4)]
            with P.scope():
                rho = P.sb("s5rho", [128, 4096]); th = P.sb("s5th", [128, 4096]); dtb = P.sb("s5dtb", [128, 4096])
                tmp = P.sb("s5tmp", [128, 4096])
                ncol = P.sb("s5ncol", [128, 4])
                P.dma(ncol[:, :], C.ncol[:, :], reads=[C.ncol], writes=[ncol])
                load_bc(P, dtb, C.s5_log_dt[0, d].rearrange("g p -> (g p)"), C.s5_log_dt)
                load_bc(P, rho, C.s5_a_re[0, d].rearrange("g p -> (g p)"), C.s5_a_re)
                load_bc(P, th, C.s5_a_im[0, d].rearrange("g p -> (g p)"), C.s5_a_im)
                P.act(lambda: nc.scalar.activation(out=dtb[:, :], in_=dtb[:, :], func=AF.Exp), [dtb], [dtb])
                P.dve(lambda: nc.vector.tensor_tensor(out=rho[:, :], in0=rho[:, :], in1=dtb[:, :], op=ALU.mult), [rho, dtb], [rho])
                P.dve(lambda: nc.vector.tensor_tensor(out=th[:, :], in0=th[:, :], in1=dtb[:, :], op=ALU.mult), [th, dtb], [th])
                P.dve(lambda: nc.vector.tensor_scalar(out=th[:, :], in0=th[:, :], scalar1=ncol[:, d:d + 1], scalar2=None,
                                                      op0=ALU.mult), [th, ncol], [th])
                E = dtb
                tmp2 = P.sb("s5tmp2", [128, 4096])
                ki_b = P.sb("s5kib", [128, 4096], I32)
                tm3 = (tmp[:, :], tmp2[:, :], ki_b[:, :])
                sin_of(P, tabs[1][:, :], th[:, :], 0.0, tm3, [th], [tmp, tmp2, ki_b], [tabs[1]])
                sin_of(P, tabs[0][:, :], th[:, :], PI / 2, tm3, [th], [tmp, tmp2, ki_b], [tabs[0]])
                P.act(lambda: nc.scalar.activation(out=E[:, :], in_=rho[:, :], func=AF.Exp, scale=ncol[:, d:d + 1]),
                      [rho, ncol], [E])
                P.dve(lambda: nc.vector.tensor_tensor(out=tabs[2][:, :], in0=tabs[0][:, :], in1=E[:, :], op=ALU.mult),
                      [tabs[0], E], [tabs[2]])
                P.dve(lambda: nc.vector.tensor_tensor(out=tabs[3][:, :], in0=tabs[1][:, :], in1=E[:, :], op=ALU.mult),
                      [tabs[1], E], [tabs[3]])
                P.act(lambda: nc.scalar.activation(out=E[:, :], in_=rho[:, :], func=AF.Exp, scale=ncol[:, 2 + d:3 + d]),
                      [rho, ncol, tabs[2], tabs[3]], [E])
                P.dve(lambda: nc.vector.tensor_tensor(out=tabs[0][:, :], in0=tabs[0][:, :], in1=E[:, :], op=ALU.mult),
                      [tabs[0], E, tabs[2]], [tabs[0]])
                P.dve(lambda: nc.vector.scalar_tensor_tensor(out=tabs[1][:, :], in0=tabs[1][:, :], scalar=-1.0, in1=E[:, :],
                                                             op0=ALU.mult, op1=ALU.mult), [tabs[1], E, tabs[3]], [tabs[1]])
            for k in range(4):
                P.dump(f"tab{d}_{k}", tabs[k][:, :], tabs[k], [128, 4096])
            RB = P.sb("s5RB", [128, 8, 1024])
            P.pool(lambda: nc.gpsimd.memset(RB[:, :, :], 0.0), [], [RB])
            for g in range(64):
                gb, gl = g // 8, g % 8
                P.dma(RB[gl * 16:(gl + 1) * 16, gb, gl * 128:(gl + 1) * 128], C.BB[d, g], reads=[C.BB], writes=[RB])
            CM = P.sb("s5CM", [128, 1024])
            cin = [P.sb(f"s5cin{i}", [128, 128]) for i in range(2)]
            for gb in range(8):
                ci_ = cin[gb % 2]
                P.dma(ci_[:, 0:64], C.s5_c_re[0, d].rearrange("g c p -> (g c) p")[gb * 128:(gb + 1) * 128, :],
                      reads=[C.s5_c_re], writes=[ci_])
                P.dma(ci_[:, 64:128], C.s5_c_im[0, d].rearrange("g c p -> (g c) p")[gb * 128:(gb + 1) * 128, :],
                      reads=[C.s5_c_im], writes=[ci_])
                P.dve(lambda ci_=ci_: nc.vector.tensor_scalar(out=ci_[:, 64:128], in0=ci_[:, 64:128], scalar1=-1.0,
                                                              scalar2=None, op0=ALU.mult), [ci_], [ci_])
                pb = P.bank()
                P.pe(lambda ci_=ci_, pb=pb: nc.tensor.transpose(out=pb[:, 0:128], in_=ci_[:, :], identity=C.ident[:, :]),
                     [ci_, C.ident], [pb])
                P.act(lambda pb=pb, gb=gb: nc.scalar.copy(out=CM[:, gb * 128:(gb + 1) * 128], in_=pb[:, 0:128]), [pb], [CM])
            tri = P.sb("s5tri", [128, 2, 128])
            P.dma(tri[:, 0, :], C.tri[d], reads=[C.tri], writes=[tri])
            P.dma(tri[:, 1, :], C.tri[2 + d], reads=[C.tri], writes=[tri])
            dbc = P.sb("s5dbc", [128, 1024])
            load_bc(P, dbc, C.s5_d[0, :], C.s5_d)
            sbuf = [P.sb(f"s5s{i}", [128, 8192]) for i in range(2)]
            ub = [P.sb(f"s5u{i}", [128, 1024]) for i in range(1)]
            uTb = [P.sb(f"s5uT{i}", [128, 8, 128]) for i in range(1)]
            bsb = [P.sb(f"s5b{i}", [128, 1024]) for i in range(2)]
            sTb = [P.sb(f"s5sT{i}", [128, 4, 128]) for i in range(2)]
            yb = [P.sb(f"s5y{i}", [128, 1024]) for i in range(1)]
            tm = [[P.sb(f"s5tm{i}_{k}", [128, 8, 64]) for k in range(4)] for i in range(1)]
            order = list(range(T)) if d == 0 else list(range(NCT - 1, -1, -1)) + list(range(T - 1, NCT - 1, -1))
            for ci, t in enumerate(order):
                u, uT, snew, sprev, yv = ub[0], uTb[0], sbuf[ci % 2], sbuf[(ci + 1) % 2], yb[0]
                rows = slice(t * 128, (t + 1) * 128)
                P.dma(u[:, :], C.QKVU[rows, 1536:2560], reads=[C.QKVU], writes=[u])
                tr_chunks(P, C, u, 8, uT)
                it = 0
                for gb in range(8):
                    ap2, bA, bB = P.bank2()
                    for half, bk in enumerate((bA, bB)):
                        P.pe(lambda gb=gb, half=half, bk=bk, uT=uT: nc.tensor.matmul(
                            bk[:, :], lhsT=uT[:, gb, :], rhs=RB[:, gb, half * 512:(half + 1) * 512], start=True, stop=True),
                            [uT, RB], [bk])
                    bs = bsb[gb % 2]
                    P.act(lambda ap2=ap2, bs=bs: nc.scalar.copy(out=bs[:, :], in_=ap2), [bA, bB], [bs])
                    v = bs[:, :].rearrange("t (g r p) -> t g r p", g=8, r=2)
                    wv = snew[:, gb * 1024:(gb + 1) * 1024].rearrange("t (g r p) -> t g r p", g=8, r=2)
                    tr_ = tabs[0][:, gb * 512:(gb + 1) * 512].rearrange("t (g p) -> t g p", p=64)
                    ti_ = tabs[1][:, gb * 512:(gb + 1) * 512].rearrange("t (g p) -> t g p", p=64)
                    tmps = [(x[:, :, :], x) for x in tm[0]]
                    cmul(P, v[:, :, 0, :], v[:, :, 1, :], tr_, ti_, wv[:, :, 0, :], wv[:, :, 1, :], tmps,
                         [bs], [tabs[0], tabs[1]], [snew])
                for gb in range(8):
                    ap2, bA, bB = P.bank2()
                    for half, bk in enumerate((bA, bB)):
                        c0 = gb * 1024 + half * 512
                        P.pe(lambda bk=bk, c0=c0, snew=snew, first=(ci == 0): nc.tensor.matmul(
                            bk[:, :], lhsT=tri[:, 0, :], rhs=snew[:, c0:c0 + 512], start=True, stop=first),
                            [tri, snew], [bk])
                        if ci > 0:
                            P.pe(lambda bk=bk, c0=c0, sprev=sprev: nc.tensor.matmul(
                                bk[:, :], lhsT=tri[:, 1, :], rhs=sprev[:, c0:c0 + 512], start=False, stop=True),
                                [tri, sprev], [bk])
                    bs = bsb[gb % 2]
                    P.act(lambda ap2=ap2, bs=bs: nc.scalar.copy(out=bs[:, :], in_=ap2), [bA, bB], [bs])
                    v = bs[:, :].rearrange("t (g r p) -> t g r p", g=8, r=2)
                    wv = snew[:, gb * 1024:(gb + 1) * 1024].rearrange("t (g r p) -> t g r p", g=8, r=2)
                    tr_ = tabs[2][:, gb * 512:(gb + 1) * 512].rearrange("t (g p) -> t g p", p=64)
                    ti_ = tabs[3][:, gb * 512:(gb + 1) * 512].rearrange("t (g p) -> t g p", p=64)
                    tmps = [(x[:, :, :], x) for x in tm[0]]
                    cmul(P, v[:, :, 0, :], v[:, :, 1, :], tr_, ti_, wv[:, :, 0, :], wv[:, :, 1, :], tmps,
                         [bs], [tabs[2], tabs[3]], [snew])
                if ci == 0:
                    P.dump(f"s{d}", snew[:, :], snew, [128, 8192])
                    P.dump(f"CM{d}", CM[:, :], CM, [128, 1024])
                    P.dump(f"RB{d}", RB[:, :, :], RB, [128, 8, 1024])
                yap, yA, yB = P.bank2()
                P.reserved = set(P.last_pair)
                for g4 in range(16):
                    sT = sTb[g4 % 2]
                    tr_chunks(P, C, snew, 4, sT, col0=g4 * 512)
                    for j in range(4):
                        g = g4 * 4 + j
                        yk = yA if g < 32 else yB
                        P.pe(lambda sT=sT, j=j, g=g, yk=yk: nc.tensor.matmul(
                            yk[:, (g % 32) * 16:(g % 32) * 16 + 16], lhsT=sT[:, j, :], rhs=CM[:, g * 16:(g + 1) * 16],
                            start=True, stop=True), [sT, CM], [yk])
                if d == 0:
                    P.dve(lambda yv=yv, u=u: nc.vector.tensor_tensor(out=yv[:, :], in0=u[:, :], in1=dbc[:, :], op=ALU.mult),
                          [u, dbc], [yv])
                else:
                    P.dma(yv[:, :], C.Y5[rows, :], reads=[C.Y5], writes=[yv])
                P.dve(lambda yv=yv, yap=yap: nc.vector.tensor_tensor(out=yv[:, :], in0=yv[:, :], in1=yap, op=ALU.add),
                      [yv, yA, yB], [yv])
                P.reserved = set()
                P.dma(C.Y5[rows, :], yv[:, :], reads=[yv], writes=[C.Y5])


def host_s5_constants():
    i = np.arange(128)[:, None]
    j = np.arange(128)[None, :]
    tri = np.stack([(i <= j), (i >= j), np.broadcast_to(i == 127, (128, 128)), np.broadcast_to(i == 0, (128, 128))]
                   ).astype(np.float32)
    t = np.arange(128, dtype=np.float32)
    ncol = np.stack([t + 1, 128 - t, -(t + 1), -(128 - t)], axis=1).astype(np.float32)
    neg = np.float32(-30000.0)
    ssdmask = np.stack([np.where(i <= j, 0, neg), np.where(i >= j, 0, neg)]).astype(np.float32)
    return {"tri": tri, "ncol": ncol, "ssdmask": ssdmask}


def phase_outproj0(P, C, l, hsrc):
    nc = P.nc
    with P.scope():
        G = 2
        g1 = [P.sb(f"opg1{k}", [128, D]) for k in range(2)]
        for kind in range(2):
            P.dma(g1[kind][:, :], C.MOD[l, kind, :, 2 * D:3 * D], reads=[C.MOD], writes=[g1[kind]])
        gb = P.sb("opglub", [128, 1024])
        load_bc(P, gb, C.s5_glu_b[0, :], C.s5_glu_b)
        cat = [P.sb(f"opcat{i}", [128, D]) for i in range(G)]
        catT = [P.sb(f"opcatT{i}", [128, 16, 128]) for i in range(G)]
        gg = [P.sb(f"opg{i}", [128, 1024]) for i in range(G)]
        gT = [P.sb(f"opgT{i}", [128, 8, 128]) for i in range(G)]
        ht = [P.sb(f"oph{i}", [128, D]) for i in range(G)]
        zt = [P.sb(f"opz{i}", [128, 512]) for i in range(2)]
        C.wbuf = [P.sb(f"opw{i}", [128, 16, 512]) for i in range(2)]
        for kind, t0, nt in tile_groups(C, G):
            for i in range(nt):
                rows = slice((t0 + i) * 128, (t0 + i + 1) * 128)
                P.dma(gg[i][:, :], C.Y5[rows, :], reads=[C.Y5], writes=[gg[i]])
                P.dma(cat[i][:, 0:1024], C.ATS5[rows, 0:1024], reads=[C.ATS5], writes=[cat[i]])
                P.dma(ht[i][:, :], hsrc[rows, :], reads=[hsrc], writes=[ht[i]])
                P.act(lambda i=i: nc.scalar.activation(out=gg[i][:, :], in_=gg[i][:, :], func=AF.Gelu), [gg[i]], [gg[i]])
                tr_chunks(P, C, gg[i], 8, gT[i])

            def consume_glu(n, ti, pb, w):
                z = zt[(n + ti) % 2]
                P.dve(lambda: nc.vector.tensor_tensor(out=z[:, 0:w], in0=pb[:, 0:w], in1=gb[:, n * 512:n * 512 + w], op=ALU.add),
                      [pb, gb], [z])
                P.act(lambda: nc.scalar.activation(out=z[:, 0:w], in_=z[:, 0:w], func=AF.Sigmoid), [z], [z])
                P.dve(lambda: nc.vector.tensor_tensor(out=cat[ti][:, 1024 + n * 512:1024 + n * 512 + w],
                                                      in0=gg[ti][:, n * 512:n * 512 + w], in1=z[:, 0:w], op=ALU.mult),
                      [gg[ti], z], [cat[ti]])
            C.wsrc = C.s5_glu_w
            linear(P, C, gT[0:nt], 8, C.s5_glu_w[0], 1024, consume_glu)
            for i in range(nt):
                tr_chunks(P, C, cat[i], 16, catT[i])

            def consume_out(n, ti, pb, w, kind=kind):
                z = zt[(n + ti) % 2]
                P.dve(lambda: nc.vector.tensor_tensor(out=z[:, 0:w], in0=pb[:, 0:w], in1=g1[kind][:, n * 512:n * 512 + w],
                                                      op=ALU.mult), [pb, g1[kind]], [z])
                P.dve(lambda: nc.vector.tensor_tensor(out=ht[ti][:, n * 512:n * 512 + w], in0=ht[ti][:, n * 512:n * 512 + w],
                                                      in1=z[:, 0:w], op=ALU.add), [ht[ti], z], [ht[ti]])
            C.wsrc = C.ab_w_out
            linear(P, C, catT[0:nt], 16, C.ab_w_out[0], 2048, consume_out)
            for i in range(nt):
                rows = slice((t0 + i) * 128, (t0 + i + 1) * 128)
                P.dma(C.H[rows, :], ht[i][:, :], reads=[ht[i]], writes=[C.H])


def topk16(P, src_ap, srcres, seg2, vals, idx, vres, ires):
    nc = P.nc
    P.dve(lambda: nc.vector.max(out=vals[:, 0:8], in_=src_ap), [srcres], [vres])
    if idx is not None:
        P.dve(lambda: nc.vector.max_index(out=idx[:, 0:8], in_max=vals[:, 0:8], in_values=src_ap), [srcres, vres], [ires])
    P.dve(lambda: nc.vector.match_replace(out=seg2[:, :], in_to_replace=vals[:, 0:8], in_values=src_ap, imm_value=-1e30),
          [srcres, vres], [seg2])
    P.dve(lambda: nc.vector.max(out=vals[:, 8:16], in_=seg2[:, :]), [seg2], [vres])
    if idx is not None:
        P.dve(lambda: nc.vector.max_index(out=idx[:, 8:16], in_max=vals[:, 8:16], in_values=seg2[:, :]), [seg2, vres], [ires])


def phase_peer_convert(P, C, l):
    nc = P.nc
    with P.scope():
        fu = [P.sb(f"pcfu{i}", [128, 4, 2048]) for i in range(2)]
        fv = [P.sb(f"pcfv{i}", [128, 4, 2048]) for i in range(2)]
        bout = [P.sb(f"pcb{i}", [128, 4, 4096], BF16) for i in range(2)]
        for n in range(32):
            a, b, bo = fu[n % 2], fv[n % 2], bout[n % 2]
            P.dma(a[:, :, :], C.peer_u[l, n * 512:(n + 1) * 512, :].rearrange("(p r) d -> p r d", r=4),
                  reads=[C.peer_u], writes=[a])
            P.dma(b[:, :, :], C.peer_v[l, n * 512:(n + 1) * 512, :].rearrange("(p r) d -> p r d", r=4),
                  reads=[C.peer_v], writes=[b])
            P.dve(lambda a=a, bo=bo: nc.vector.tensor_copy(out=bo[:, :, 0:2048], in_=a[:, :, :]), [a], [bo])
            P.act(lambda b=b, bo=bo: nc.scalar.copy(out=bo[:, :, 2048:4096], in_=b[:, :, :]), [b], [bo])
            r0 = l * 16384 + n * 512
            P.dma(C.UVB[r0:r0 + 512, :].rearrange("(p r) d -> p r d", r=4), bo[:, :, :], reads=[bo], writes=[C.UVB])


def phase_peer(P, C, l, tiles, split=False):
    nc = P.nc
    phase_peer_convert(P, C, l)
    with P.scope():
        Gp = [P.sb(f"peGp{k}", [128, D]) for k in range(2)]
        Sh = [P.sb(f"peSh{k}", [128, D]) for k in range(2)]
        prep_mod(P, C, l, 3, 4, C.norm2_g[l, :], C.norm2_g, Gp, Sh)
        g2 = [P.sb(f"peg2{k}", [128, D]) for k in range(2)]
        for kind in range(2):
            P.dma(g2[kind][:, :], C.MOD[l, kind, :, 5 * D:6 * D], reads=[C.MOD], writes=[g2[kind]])
        KM = P.sb("peKM", [128, 8, 256])
        P.pool(lambda: nc.gpsimd.memset(KM[:, :, :], 0.0), [], [KM])
        kin = [P.sb(f"pekin{i}", [128, 2, 64]) for i in range(2)]
        for h in range(8):
            ki = kin[h % 2]
            P.dma(ki[:, :, :], C.peer_keys[l, h].rearrange("half k d -> k half d"), reads=[C.peer_keys], writes=[ki])
            pb = P.bank()
            P.pe(lambda ki=ki, pb=pb: nc.tensor.transpose(out=pb[:, 0:128], in_=ki[:, :, :].rearrange("k a d -> k (a d)"),
                                                         identity=C.ident[:, :]), [ki, C.ident], [pb])
            P.act(lambda pb=pb, h=h: nc.scalar.copy(out=KM[0:64, h, 0:128], in_=pb[0:64, 0:128]), [pb], [KM])
            P.act(lambda pb=pb, h=h: nc.scalar.copy(out=KM[64:128, h, 128:256], in_=pb[64:128, 0:128]), [pb], [KM])
        hb = [P.sb(f"peh{i}", [128, D]) for i in range(1)]
        fb = [P.sb(f"pef{i}", [128, D]) for i in range(1)]
        fT = P.sb("pefT", [128, 16, 128])
        q = P.sb("peq", [128, 1024])
        qT = P.sb("peqT", [128, 8, 128])
        sc = P.sb("pesc", [128, 8, 256])
        seg2 = P.sb("peseg2", [128, 256])
        s2h = P.sb("peseg2h", [128, 128])
        v12 = P.sb("pev12", [128, 8, 2, 16])
        i12 = P.sb("pei12", [128, 8, 2, 16], U32)
        i12f = P.sb("pei12f", [128, 8, 2, 16])
        cand = P.sb("pecand", [128, 8, 256])
        cidx = sc
        score = P.sb("pescore", [128, 8, 16])
        pos = P.sb("pepos", [128, 8, 16], U32)
        posf = P.sb("peposf", [128, 8, 16])
        ef = P.sb("peef", [128, 128])
        ei = P.sb("peei", [128, 128], I32)
        gate = P.sb("pegate", [128, 8, 16])
        gs = P.sb("pegs", [128, 16])
        araw = P.sb("pearaw", [128, 128])
        wgt = P.sb("pewgt", [128, 128])
        junk = P.sb("pejunk", [128, D], BF16)
        gl = P.sb("pegl", [128, 128])
        st = P.sb("pest", [128, 4])
        acc = P.sb("peacc", [128, D])
        NG = 8
        gbuf = [P.sb(f"peg{i}", [128, 2 * D], BF16) for i in range(NG)]
        if split:
            ridx = P.sb("peridx", [128, C.NL // 2], I32)
            P.dma(ridx[:, :], C.rowidx[:, :], reads=[C.rowidx], writes=[ridx])
        C.wbuf = [P.sb(f"pew{i}", [128, 16, 128]) for i in range(2)]
        gi = 0
        for it, t in enumerate(tiles):
            kind = 1 if (t < C.NCT and not split) else 0
            rows = slice(t * 128, (t + 1) * 128)
            hT, f = hb[0], fb[0]
            if split:
                P.op("pool", lambda t=t: nc.gpsimd.indirect_dma_start(
                    out=hT[:, :], out_offset=None, in_=C.H[:, :],
                    in_offset=bass.IndirectOffsetOnAxis(ap=ridx[:, t:t + 1], axis=0)), [ridx, C.H], [hT], dma=True)
            else:
                P.dma(hT[:, :], C.H[rows, :], reads=[C.H], writes=[hT])
            C.xres, C.ores = hT, f
            norm_mod(P, C, hT[:, :], f[:, :], Gp[kind], Sh[kind], junk, st)
            tr_chunks(P, C, f, 16, fT)

            def consume_q(n, ti, pb, w):
                P.act(lambda: nc.scalar.copy(out=q[:, n * 128:n * 128 + w], in_=pb[:, 0:w]), [pb], [q])
            C.wsrc = C.peer_wq
            linear(P, C, [fT], 16, C.peer_wq[l], 1024, consume_q, cw=128)
            tr_chunks(P, C, q, 8, qT)
            for h2 in range(4):
                pb = P.bank()
                for j in range(2):
                    h = h2 * 2 + j
                    P.pe(lambda pb=pb, j=j, h=h: nc.tensor.matmul(pb[:, j * 256:(j + 1) * 256], lhsT=qT[:, h, :], rhs=KM[:, h, :],
                                                                  start=True, stop=True), [qT, KM], [pb])
                P.act(lambda pb=pb, h2=h2: nc.scalar.copy(out=sc[:, 2 * h2:2 * h2 + 2, :],
                                                         in_=pb[:, :].rearrange("p (a b) -> p a b", b=256)), [pb], [sc])
            for h in range(8):
                for half in range(2):
                    topk16(P, sc[:, h, half * 128:(half + 1) * 128], sc, s2h,
                           v12[:, h, half, :], i12[:, h, half, :], v12, i12)
            P.dve(lambda: nc.vector.tensor_copy(out=i12f[:, :, :, :], in_=i12[:, :, :, :]), [i12], [i12f])
            P.dve(lambda: nc.vector.tensor_tensor(
                out=cand[:, :, :].rearrange("p h (a b) -> p h a b", b=16),
                in0=v12[:, :, 0, :].unsqueeze(3).to_broadcast([128, 8, 16, 16]),
                in1=v12[:, :, 1, :].unsqueeze(2).to_broadcast([128, 8, 16, 16]), op=ALU.add), [v12], [cand])
            P.dve(lambda: nc.vector.tensor_scalar(out=i12f[:, :, 0, :], in0=i12f[:, :, 0, :], scalar1=128.0, scalar2=None,
                                                  op0=ALU.mult), [i12f], [i12f])
            P.dve(lambda: nc.vector.tensor_tensor(
                out=cidx[:, :, :].rearrange("p h (a b) -> p h a b", b=16),
                in0=i12f[:, :, 0, :].unsqueeze(3).to_broadcast([128, 8, 16, 16]),
                in1=i12f[:, :, 1, :].unsqueeze(2).to_broadcast([128, 8, 16, 16]), op=ALU.add), [i12f], [cidx])
            for h in range(8):
                topk16(P, cand[:, h, :], cand, seg2, score[:, h, :], pos[:, h, :], score, pos)
            P.dve(lambda: nc.vector.tensor_copy(out=posf[:, :, :], in_=pos[:, :, :]), [pos], [posf])
            for h in range(8):
                for k in range(16):
                    P.dve(lambda h=h, k=k: (nc.vector.scalar_tensor_tensor(
                        out=seg2[:, :], in0=C.iota[:, :], scalar=posf[:, h, k:k + 1], in1=cidx[:, h, :],
                        op0=ALU.is_equal, op1=ALU.mult, accum_out=ef[:, h * 16 + k:h * 16 + k + 1]),
                        nc.vector.tensor_copy(out=P.scr[:, 1:2], in_=ef[:, h * 16 + k:h * 16 + k + 1]))[1],
                        [C.iota, posf, cidx], [seg2, ef])
            P.dve(lambda: nc.vector.tensor_copy(out=ei[:, :], in_=ef[:, :]), [ef], [ei])
            P.dve(lambda: nc.vector.tensor_tensor(out=gate[:, :, :], in0=score[:, :, :],
                                                  in1=score[:, :, 0:1].to_broadcast([128, 8, 16]), op=ALU.subtract), [score], [gate])
            P.act(lambda: nc.scalar.activation(out=gate[:, :, :], in_=gate[:, :, :], func=AF.Exp), [gate], [gate])
            P.dve(lambda: nc.vector.reduce_sum(out=gs[:, 0:8], in_=gate[:, :, :], axis=AX.X), [gate], [gs])
            P.dve(lambda: nc.vector.reciprocal(out=gs[:, 8:16], in_=gs[:, 0:8]), [gs], [gs])
            P.dve(lambda: nc.vector.tensor_tensor(out=gate[:, :, :], in0=gate[:, :, :],
                                                  in1=gs[:, 8:16].unsqueeze(2).to_broadcast([128, 8, 16]), op=ALU.mult), [gate, gs], [gate])
            gate2 = gate[:, :, :].rearrange("p h k -> p (h k)")
            for blk in range(32):
                gs_ = []
                for jj in range(4):
                    j = blk * 4 + jj
                    g = gbuf[gi % NG]
                    gi += 1
                    gs_.append(g)
                    P.op("pool", lambda g=g, j=j: nc.gpsimd.indirect_dma_start(
                        out=g[:, :], out_offset=None, in_=C.UVB[:, :],
                        in_offset=bass.IndirectOffsetOnAxis(ap=ei[:, j:j + 1], axis=0), element_offset=l * 16384 * 4096),
                        [ei, C.UVB], [g], dma=True)
                    P.dve(lambda g=g, j=j, f=f: (nc.vector.scalar_tensor_tensor(
                        out=junk[:, :], in0=g[:, 0:D], scalar=1.0, in1=f[:, :], op0=ALU.mult, op1=ALU.mult,
                        accum_out=araw[:, j:j + 1]), nc.vector.tensor_copy(out=P.scr[:, 1:2], in_=araw[:, j:j + 1]))[1],
                        [g, f], [junk, araw], soft=(junk, araw))
                c0, c1 = blk * 4, blk * 4 + 4
                P.act(lambda c0=c0, c1=c1: nc.scalar.activation(out=gl[:, c0:c1], in_=araw[:, c0:c1], func=AF.Gelu), [araw], [gl])
                P.dve(lambda c0=c0, c1=c1: nc.vector.tensor_tensor(out=wgt[:, c0:c1], in0=gl[:, c0:c1], in1=gate2[:, c0:c1],
                                                                   op=ALU.mult), [gl, gate], [wgt])
                for jj in range(4):
                    j = blk * 4 + jj
                    g = gs_[jj]
                    if j == 0:
                        P.dve(lambda g=g: nc.vector.tensor_scalar(out=acc[:, :], in0=g[:, D:2 * D], scalar1=wgt[:, 0:1], scalar2=None,
                                                                  op0=ALU.mult), [g, wgt], [acc])
                    else:
                        P.dve(lambda g=g, j=j: nc.vector.scalar_tensor_tensor(
                            out=acc[:, :], in0=g[:, D:2 * D], scalar=wgt[:, j:j + 1], in1=acc[:, :], op0=ALU.mult, op1=ALU.add),
                            [g, wgt, acc], [acc], soft=(acc,))
            P.dve(lambda kind=kind: nc.vector.tensor_tensor(out=acc[:, :], in0=acc[:, :], in1=g2[kind][:, :], op=ALU.mult),
                  [acc, g2[kind]], [acc])
            P.dve(lambda hT=hT: nc.vector.tensor_tensor(out=hT[:, :], in0=hT[:, :], in1=acc[:, :], op=ALU.add), [hT, acc], [hT])
            if split:
                P.dma(C.H2[rows, :], hT[:, :], reads=[hT], writes=[C.H2])
            else:
                P.dma(C.H[rows, :], hT[:, :], reads=[hT], writes=[C.H])


def seg2_half(seg2):
    return Res("seg2h", seg2.t[:, 0:128])


def phase_ssd_in(P, C, l):
    nc = P.nc
    with P.scope():
        Gp = [P.sb(f"siGp{k}", [128, D]) for k in range(2)]
        Sh = [P.sb(f"siSh{k}", [128, D]) for k in range(2)]
        prep_mod(P, C, l, 0, 1, C.norm1_g[l, :], C.norm1_g, Gp, Sh)
        G = 2
        xs = [P.sb(f"six{i}", [128, D]) for i in range(G)]
        xT = [P.sb(f"sixT{i}", [128, 16, 128]) for i in range(G)]
        ob = [P.sb(f"sio{i}", [128, 512]) for i in range(4)]
        junk = P.sb("sijunk", [128, D])
        st = P.sb("sist", [128, 4])
        C.wbuf = [P.sb(f"siw{i}", [128, 16, 512]) for i in range(2)]
        cnt = [0]
        for kind, t0, nt in tile_groups(C, G):
            for i in range(nt):
                t = t0 + i
                P.dma(xs[i][:, :], C.H[t * 128:(t + 1) * 128, :], reads=[C.H], writes=[xs[i]])
                C.xres, C.ores = xs[i], xs[i]
                norm_mod(P, C, xs[i][:, :], xs[i][:, :], Gp[kind], Sh[kind], junk, st)
                tr_chunks(P, C, xs[i], 16, xT[i])

            def consume(n, ti, pb, w, t0=t0):
                o = ob[cnt[0] % 4]
                cnt[0] += 1
                t = t0 + ti
                if cnt[0] % 2:
                    P.act(lambda: nc.scalar.copy(out=o[:, 0:w], in_=pb[:, 0:w]), [pb], [o])
                else:
                    P.dve(lambda: nc.vector.tensor_copy(out=o[:, 0:w], in_=pb[:, 0:w]), [pb], [o])
                P.dma(C.SP[t * 128:(t + 1) * 128, n * 512:n * 512 + w], o[:, 0:w], reads=[o], writes=[C.SP])
            C.wsrc = C.ssd_w_in
            linear(P, C, xT[0:nt], 16, C.ssd_w_in[0], 10368, consume)


def phase_ssd_conv(P, C):
    nc = P.nc
    CW = 2048
    with P.scope():
        wk = P.sb("scw", [128, 5, CW])
        bias = P.sb("scb", [128, CW])
        xk = [P.sb(f"scx{k}", [128, CW]) for k in range(5)]
        for c in range(3):
            cols = slice(4096 + c * CW, 4096 + (c + 1) * CW)
            for k in range(5):
                P.dma(wk[:, k, :], C.ssd_conv_w[0, k, c * CW:(c + 1) * CW].partition_broadcast(128),
                      reads=[C.ssd_conv_w], writes=[wk])
            P.dma(bias[:, :], C.ssd_conv_b[0, c * CW:(c + 1) * CW].partition_broadcast(128), reads=[C.ssd_conv_b], writes=[bias])
            for t in range(C.T):
                s0, s1 = (0, C.NCT) if t < C.NCT else (C.NCT, C.T)
                r0 = t * 128
                for k in range(5):
                    lo, hi = r0 + k - 2, r0 + k - 2 + 128
                    vlo, vhi = max(lo, s0 * 128), min(hi, s1 * 128)
                    if vlo > lo or vhi < hi:
                        P.pool(lambda k=k: nc.gpsimd.memset(xk[k][:, :], 0.0), [], [xk[k]])
                    P.dma(xk[k][vlo - lo:vhi - lo, :], C.SP[vlo:vhi, cols], reads=[C.SP], writes=[xk[k]])
                for k in range(5):
                    if k % 2 == 0:
                        P.dve(lambda k=k: nc.vector.tensor_tensor(out=xk[k][:, :], in0=xk[k][:, :], in1=wk[:, k, :], op=ALU.mult),
                              [xk[k], wk], [xk[k]])
                    else:
                        P.pool(lambda k=k: nc.gpsimd.tensor_tensor(out=xk[k][:, :], in0=xk[k][:, :], in1=wk[:, k, :], op=ALU.mult),
                               [xk[k], wk], [xk[k]])
                P.pool(lambda: nc.gpsimd.tensor_tensor(out=xk[1][:, :], in0=xk[1][:, :], in1=xk[3][:, :], op=ALU.add),
                       [xk[1], xk[3]], [xk[1]])
                P.dve(lambda: nc.vector.tensor_tensor(out=xk[0][:, :], in0=xk[0][:, :], in1=xk[2][:, :], op=ALU.add),
                      [xk[0], xk[2]], [xk[0]])
                P.pool(lambda: nc.gpsimd.tensor_tensor(out=xk[4][:, :], in0=xk[4][:, :], in1=bias[:, :], op=ALU.add),
                       [xk[4], bias], [xk[4]])
                P.dve(lambda: nc.vector.tensor_tensor(out=xk[0][:, :], in0=xk[0][:, :], in1=xk[1][:, :], op=ALU.add),
                      [xk[0], xk[1]], [xk[0]])
                P.dve(lambda: nc.vector.tensor_tensor(out=xk[0][:, :], in0=xk[0][:, :], in1=xk[4][:, :], op=ALU.add),
                      [xk[0], xk[4]], [xk[0]])
                P.act(lambda: nc.scalar.activation(out=xk[0][:, :], in_=xk[0][:, :], func=AF.Silu), [xk[0]], [xk[0]])
                P.dma(C.XBC[r0:r0 + 128, c * CW:(c + 1) * CW], xk[0][:, :], reads=[xk[0]], writes=[C.XBC])


def ssd_pass(P, C, d):
    nc = P.nc
    NCT, NL, T = C.NCT, C.NL, C.T
    with P.scope():
        tri = P.sb("sstri", [128, 128])
        P.dma(tri[:, :], C.tri[d], reads=[C.tri], writes=[tri])
        nmask = P.sb("ssnm", [128, 128])
        P.dma(nmask[:, :], C.ssdmask[d], reads=[C.ssdmask], writes=[nmask])
        ones = P.sb("ssones", [128, 128])
        P.pool(lambda: nc.gpsimd.memset(ones[:, :], 1.0), [], [ones])
        abc = P.sb("ssa", [128, 64])
        load_bc(P, abc, C.ssd_a_log[0, d * 64:(d + 1) * 64], C.ssd_a_log)
        P.act(lambda: nc.scalar.activation(out=abc[:, :], in_=abc[:, :], func=AF.Exp), [abc], [abc])
        P.dve(lambda: nc.vector.tensor_scalar(out=abc[:, :], in0=abc[:, :], scalar1=-1.0, scalar2=None, op0=ALU.mult), [abc], [abc])
        dtb = P.sb("ssdtb", [128, 64])
        load_bc(P, dtb, C.ssd_dt_bias[0, d * 64:(d + 1) * 64], C.ssd_dt_bias)
        dsk = P.sb("ssdsk", [128, 64])
        load_bc(P, dsk, C.ssd_d[0, :], C.ssd_d)
        ST = P.sb("ssST", [128, 4096])
        P.pool(lambda: nc.gpsimd.memset(ST[:, :], 0.0), [], [ST])
        xs = P.sb("ssx", [128, 4096]); xdd = P.sb("ssxdd", [128, 4096]); yt = P.sb("ssy", [128, 4096])
        bc = P.sb("ssbc", [128, 2048])
        BT = P.sb("ssBT", [128, 8, 128]); CT = P.sb("ssCT", [128, 8, 128]); cbT = P.sb("sscbT", [128, 8, 128])
        sm = P.sb("sssm", [128, 16, 64])
        LT = [P.sb(f"ssLT{i}", [128, 128]) for i in range(4)]
        dec = [P.sb(f"ssdec{i}", [128, 128]) for i in range(4)]
        yo = [P.sb(f"ssyo{i}", [128, 512]) for i in range(2)]
        yd = [P.sb(f"ssyd{i}", [128, 512]) for i in range(2)]
        if d == 0:
            order = list(range(T))
        else:
            order = list(range(NCT - 1, -1, -1)) + list(range(T - 1, NCT - 1, -1))
        DTR, X_, LA, CS, TOT, DTE, ECS, NCS, TMP, DT, ETOT = range(11)
        S = lambda k: sm[:, k, :]
        for t in order:
            lat = t >= NCT
            rows = slice(t * 128, (t + 1) * 128)
            lrows = slice((t - NCT) * 128, (t - NCT + 1) * 128)
            P.dma(xs[:, :], C.XBC[rows, 0:4096], reads=[C.XBC], writes=[xs])
            P.dma(bc[:, :], C.XBC[rows, 4096:6144], reads=[C.XBC], writes=[bc])
            P.dma(sm[:, DTR, :], C.SP[rows, 10240 + d * 64:10240 + (d + 1) * 64], reads=[C.SP], writes=[sm])
            P.dve(lambda: nc.vector.tensor_tensor(out=S(X_), in0=S(DTR), in1=dtb[:, :], op=ALU.add), [sm, dtb], [sm])
            P.act(lambda: nc.scalar.activation(out=S(TMP), in_=S(X_), func=AF.Abs), [sm], [sm])
            P.act(lambda: nc.scalar.activation(out=S(TMP), in_=S(TMP), func=AF.Exp, scale=-1.0), [sm], [sm])
            P.act(lambda: nc.scalar.activation(out=S(TMP), in_=S(TMP), func=AF.Ln, bias=1.0), [sm], [sm])
            P.dve(lambda: nc.vector.tensor_scalar(out=S(DT), in0=S(X_), scalar1=0.0, scalar2=None, op0=ALU.max), [sm], [sm])
            P.dve(lambda: nc.vector.tensor_tensor(out=S(DT), in0=S(DT), in1=S(TMP), op=ALU.add), [sm], [sm])
            P.dve(lambda: nc.vector.tensor_tensor(out=S(LA), in0=S(DT), in1=abc[:, :], op=ALU.mult), [sm, abc], [sm])
            pb = P.bank()
            P.pe(lambda pb=pb: nc.tensor.matmul(pb[:, 0:64], lhsT=tri[:, :], rhs=S(LA), start=True, stop=True), [tri, sm], [pb])
            P.pe(lambda pb=pb: nc.tensor.matmul(pb[:, 64:128], lhsT=ones[:, :], rhs=S(LA), start=True, stop=True), [ones, sm], [pb])
            P.act(lambda pb=pb: nc.scalar.copy(out=sm[:, CS:CS + 2, :], in_=pb[:, 0:128].rearrange("p (a b) -> p a b", b=64)),
                  [pb], [sm])
            P.dve(lambda: nc.vector.tensor_tensor(out=S(DTE), in0=S(TOT), in1=S(CS), op=ALU.subtract), [sm], [sm])
            P.act(lambda: nc.scalar.activation(out=S(DTE), in_=S(DTE), func=AF.Exp), [sm], [sm])
            P.act(lambda: nc.scalar.activation(out=S(ECS), in_=S(CS), func=AF.Exp), [sm], [sm])
            P.act(lambda: nc.scalar.activation(out=S(ETOT), in_=S(TOT), func=AF.Exp), [sm], [sm])
            P.dve(lambda: nc.vector.tensor_scalar(out=S(NCS), in0=S(CS), scalar1=-1.0, scalar2=None, op0=ALU.mult), [sm], [sm])
            x3 = xs[:, :].rearrange("p (h q) -> p h q", q=64)
            if lat:
                if d == 0:
                    P.pool(lambda: nc.gpsimd.tensor_tensor(out=yt[:, :].rearrange("p (h q) -> p h q", q=64), in0=x3,
                                                           in1=dsk[:, :].unsqueeze(2).to_broadcast([128, 64, 64]), op=ALU.mult),
                           [xs, dsk], [yt])
                else:
                    P.dma(yt[:, :], C.YS[lrows, :], reads=[C.YS], writes=[yt])
            P.dve(lambda: nc.vector.tensor_tensor(out=x3, in0=x3, in1=S(DT).unsqueeze(2).to_broadcast([128, 64, 64]), op=ALU.mult),
                  [xs, sm], [xs])
            P.dve(lambda: nc.vector.tensor_tensor(out=xdd[:, :].rearrange("p (h q) -> p h q", q=64), in0=x3,
                                                  in1=S(DTE).unsqueeze(2).to_broadcast([128, 64, 64]), op=ALU.mult),
                  [xs, sm], [xdd])
            if lat:
                tr_chunks(P, C, bc, 8, BT, col0=0)
                tr_chunks(P, C, bc, 8, CT, col0=1024)
                for g in range(8):
                    pb = P.bank()
                    P.pe(lambda pb=pb, g=g: nc.tensor.matmul(pb[:, 0:128], lhsT=BT[:, g, :], rhs=CT[:, g, :], start=True, stop=True),
                         [BT, CT], [pb])
                    P.act(lambda pb=pb, g=g: nc.scalar.copy(out=cbT[:, g, :], in_=pb[:, 0:128]), [pb], [cbT])
                for g in range(8):
                    pbo = P.bank()
                    P.pe(lambda pbo=pbo, g=g: nc.tensor.matmul(pbo[:, :], lhsT=CT[:, g, :], rhs=ST[:, g * 512:(g + 1) * 512],
                                                               start=True, stop=True), [CT, ST], [pbo])
                    yo_ = yo[g % 2]
                    P.dve(lambda pbo=pbo, yo_=yo_, g=g: nc.vector.tensor_tensor(
                        out=yo_[:, :].rearrange("p (h q) -> p h q", q=64), in0=pbo[:, :].rearrange("p (h q) -> p h q", q=64),
                        in1=sm[:, ECS, g * 8:(g + 1) * 8].unsqueeze(2).to_broadcast([128, 8, 64]), op=ALU.mult), [pbo, sm], [yo_])
                    pby = P.bank()
                    P.reserved = {(P.psn - 1) % 8}
                    for j in range(8):
                        h = g * 8 + j
                        lt, dc = LT[h % 4], dec[h % 4]
                        P.pool(lambda lt=lt, h=h: nc.gpsimd.tensor_scalar(out=lt[:, :], in0=tri[:, :], scalar1=sm[:, LA, h:h + 1],
                                                                         scalar2=None, op0=ALU.mult), [tri, sm], [lt])
                        pbd = P.bank()
                        P.pe(lambda pbd=pbd, lt=lt: nc.tensor.matmul(pbd[:, 0:128], lhsT=ones[:, :], rhs=lt[:, :], start=True, stop=False),
                             [ones, lt], [pbd])
                        P.pe(lambda pbd=pbd: nc.tensor.matmul(pbd[:, 0:128], lhsT=C.ident[:, :], rhs=nmask[:, :], start=False, stop=True),
                             [C.ident, nmask], [pbd])
                        P.act(lambda pbd=pbd, dc=dc, h=h: nc.scalar.activation(out=dc[:, :], in_=pbd[:, 0:128], func=AF.Exp,
                                                                                bias=sm[:, NCS, h:h + 1], scale=1.0), [pbd, sm], [dc])
                        P.dve(lambda dc=dc, g=g: nc.vector.tensor_tensor(out=dc[:, :], in0=dc[:, :], in1=cbT[:, g, :], op=ALU.mult),
                              [dc, cbT], [dc])
                        P.pe(lambda pby=pby, dc=dc, j=j, h=h: nc.tensor.matmul(pby[:, j * 64:(j + 1) * 64], lhsT=dc[:, :],
                                                                               rhs=xs[:, h * 64:(h + 1) * 64], start=True, stop=True),
                             [dc, xs], [pby])
                    P.reserved = set()
                    yd_ = yd[g % 2]
                    P.act(lambda pby=pby, yd_=yd_: nc.scalar.copy(out=yd_[:, :], in_=pby[:, :]), [pby], [yd_])
                    P.pool(lambda yo_=yo_, yd_=yd_: nc.gpsimd.tensor_tensor(out=yo_[:, :], in0=yo_[:, :], in1=yd_[:, :], op=ALU.add),
                           [yo_, yd_], [yo_])
                    P.pool(lambda yo_=yo_, g=g: nc.gpsimd.tensor_tensor(out=yt[:, g * 512:(g + 1) * 512], in0=yt[:, g * 512:(g + 1) * 512],
                                                                      in1=yo_[:, :], op=ALU.add), [yo_, yt], [yt])
                P.dma(C.YS[lrows, :], yt[:, :], reads=[yt], writes=[C.YS])
                if t == NCT:
                    P.dump(f"ssm{d}", sm[:, :, :], sm, [128, 16, 64])
                    P.dump(f"scbT{d}", cbT[:, :, :], cbT, [128, 8, 128])
                    P.dump(f"sdec{d}", dec[3][:, :], dec[3], [128, 128])
                    P.dump(f"sST{d}", ST[:, :], ST, [128, 4096])
                    P.dump(f"sxs{d}", xs[:, :], xs, [128, 4096])
                    P.dump(f"syd{d}", yd[1][:, :], yd[1], [128, 512])
                    P.dump(f"syo{d}", yo[1][:, :], yo[1], [128, 512])
            P.dve(lambda: nc.vector.tensor_tensor(out=ST[:, :].rearrange("p (h q) -> p h q", q=64),
                                                  in0=ST[:, :].rearrange("p (h q) -> p h q", q=64),
                                                  in1=S(ETOT).unsqueeze(2).to_broadcast([128, 64, 64]), op=ALU.mult), [ST, sm], [ST])
            for g in range(8):
                pbs = P.bank()
                P.pe(lambda pbs=pbs, g=g: nc.tensor.matmul(pbs[:, :], lhsT=bc[:, g * 128:(g + 1) * 128], rhs=xdd[:, g * 512:(g + 1) * 512],
                                                           start=True, stop=True), [bc, xdd], [pbs])
                P.dve(lambda pbs=pbs, g=g: nc.vector.tensor_tensor(out=ST[:, g * 512:(g + 1) * 512], in0=ST[:, g * 512:(g + 1) * 512],
                                                                   in1=pbs[:, :], op=ALU.add), [pbs, ST], [ST])


def phase_ssd_out(P, C, l):
    nc = P.nc
    with P.scope():
        g1 = P.sb("sog1", [128, D])
        P.dma(g1[:, :], C.MOD[l, 0, :, 2 * D:3 * D], reads=[C.MOD], writes=[g1])
        ng = P.sb("song", [128, 4096])
        load_bc(P, ng, C.ssd_norm_g[0, :], C.ssd_norm_g)
        G = 1
        yb = [P.sb(f"soy{i}", [128, 4096]) for i in range(G)]
        zb = [P.sb(f"soz{i}", [128, 4096]) for i in range(G)]
        yT = [P.sb(f"soyT{i}", [128, 32, 128]) for i in range(G)]
        ht = [P.sb(f"soh{i}", [128, D]) for i in range(G)]
        zt = [P.sb(f"sozt{i}", [128, 256]) for i in range(2)]
        st = P.sb("sost", [128, 4, 8])
        junk = P.sb("sojunk", [128, 512])
        C.wbuf = [P.sb(f"sow{i}", [128, 32, 256]) for i in range(2)]
        for t0 in range(0, C.NL, G):
            nt = min(G, C.NL - t0)
            for i in range(nt):
                lrows = slice((t0 + i) * 128, (t0 + i + 1) * 128)
                rows = slice((C.NCT + t0 + i) * 128, (C.NCT + t0 + i + 1) * 128)
                y, z = yb[i], zb[i]
                P.dma(y[:, :], C.YS[lrows, :], reads=[C.YS], writes=[y])
                P.dma(z[:, :], C.SP[rows, 0:4096], reads=[C.SP], writes=[z])
                P.dma(ht[i][:, :], C.H[rows, :], reads=[C.H], writes=[ht[i]])
                P.act(lambda z=z: nc.scalar.activation(out=z[:, :], in_=z[:, :], func=AF.Silu), [z], [z])
                P.dve(lambda y=y, z=z: nc.vector.tensor_tensor(out=y[:, :], in0=y[:, :], in1=z[:, :], op=ALU.mult), [y, z], [y])
                for g in range(8):
                    P.act(lambda y=y, g=g: (nc.scalar.activation(out=junk[:, :], in_=y[:, g * 512:(g + 1) * 512], func=AF.Square,
                                                                 accum_out=st[:, 0, g:g + 1]),
                                            nc.scalar.copy(out=P.scr[:, 0:1], in_=st[:, 0, g:g + 1]))[1], [y], [junk, st])
                P.dve(lambda: nc.vector.tensor_scalar(out=st[:, 1, :], in0=st[:, 0, :], scalar1=1.0 / 512, scalar2=EPS,
                                                      op0=ALU.mult, op1=ALU.add), [st], [st])
                P.act(lambda: nc.scalar.activation(out=st[:, 2, :], in_=st[:, 1, :], func=AF.Sqrt), [st], [st])
                P.dve(lambda: nc.vector.reciprocal(out=st[:, 3, :], in_=st[:, 2, :]), [st], [st])
                P.dve(lambda y=y: nc.vector.tensor_tensor(out=y[:, :].rearrange("p (g q) -> p g q", q=512),
                                                          in0=y[:, :].rearrange("p (g q) -> p g q", q=512),
                                                          in1=st[:, 3, :].unsqueeze(2).to_broadcast([128, 8, 512]), op=ALU.mult),
                      [y, st], [y])
                P.pool(lambda y=y: nc.gpsimd.tensor_tensor(out=y[:, :], in0=y[:, :], in1=ng[:, :], op=ALU.mult), [y, ng], [y])
                tr_chunks(P, C, y, 32, yT[i])

            def consume_out(n, ti, pb, w):
                z = zt[(n + ti) % 2]
                P.dve(lambda: nc.vector.tensor_tensor(out=z[:, 0:w], in0=pb[:, 0:w], in1=g1[:, n * 256:n * 256 + w], op=ALU.mult),
                      [pb, g1], [z])
                P.dve(lambda: nc.vector.tensor_tensor(out=ht[ti][:, n * 256:n * 256 + w], in0=ht[ti][:, n * 256:n * 256 + w],
                                                      in1=z[:, 0:w], op=ALU.add), [ht[ti], z], [ht[ti]])
            C.wsrc = C.ssd_w_out
            linear(P, C, yT[0:nt], 32, C.ssd_w_out[0], 2048, consume_out, cw=256)
            for i in range(nt):
                rows = slice((C.NCT + t0 + i) * 128, (C.NCT + t0 + i + 1) * 128)
                P.dma(C.H[rows, :], ht[i][:, :], reads=[ht[i]], writes=[C.H])


def phase_final(P, C):
    nc = P.nc
    with P.scope():
        g = P.sb("fng", [128, D])
        load_bc(P, g, C.final_norm_g[:], C.final_norm_g)
        xb = [P.sb(f"fnx{i}", [128, D]) for i in range(2)]
        junk = P.sb("fnjunk", [128, D])
        stb = [P.sb(f"fnst{i}", [128, 4]) for i in range(2)]
        for t in range(C.NO):
            x, st = xb[t % 2], stb[t % 2]
            rows = slice(t * 128, (t + 1) * 128)
            if SPLIT:
                P.dma(x[:, :], C.H2[rows, :], reads=[C.H2], writes=[x])
            else:
                P.dma(x[:, :], C.H[(C.NCT + t) * 128:(C.NCT + t + 1) * 128, :], reads=[C.H], writes=[x])
            P.act(lambda x=x, st=st: (nc.scalar.activation(out=junk[:, :], in_=x[:, :], func=AF.Square, accum_out=st[:, 0:1]),
                                      nc.scalar.copy(out=P.scr[:, 0:1], in_=st[:, 0:1]))[1], [x], [junk, st])
            P.dve(lambda st=st: nc.vector.tensor_scalar(out=st[:, 1:2], in0=st[:, 0:1], scalar1=1.0 / D, scalar2=EPS,
                                                        op0=ALU.mult, op1=ALU.add), [st], [st])
            P.act(lambda st=st: nc.scalar.activation(out=st[:, 3:4], in_=st[:, 1:2], func=AF.Sqrt), [st], [st])
            P.dve(lambda st=st: nc.vector.reciprocal(out=st[:, 2:3], in_=st[:, 3:4]), [st], [st])
            P.dve(lambda x=x, st=st: nc.vector.scalar_tensor_tensor(out=x[:, :], in0=x[:, :], scalar=st[:, 2:3], in1=g[:, :],
                                                                    op0=ALU.mult, op1=ALU.mult), [x, st, g], [x])
            P.dma(C.OUT[t * 128:(t + 1) * 128, :], x[:, :], reads=[x], writes=[C.OUT])


N_CORES = 8
_CACHE = {}


_DBG = {}


def run_model(inputs, NL, NCT, batches, n_cores=None):
    inputs = {k: np.asarray(v) for k, v in inputs.items()}
    P = Prog()
    C = build_program(P, NL, NCT, stage="final")
    P.finish()
    used = set(C.used_inputs)
    shared = make_inputs(inputs, 0, NL, NCT, used=used)
    nb = len(batches)
    n_cores = n_cores or 2 * nb
    NLH = NL // 2
    maps = []
    for core in range(n_cores):
        b, half = batches[core % nb], core // nb
        m = dict(shared)
        m["h0"] = np.ascontiguousarray(np.concatenate([inputs["ctx"][b][:NCT * 128], inputs["x"][b][:NL * 128]], axis=0),
                                       dtype=np.float32)
        m["c_col"] = np.ascontiguousarray(inputs["c"][b].reshape(16, 128).T, dtype=np.float32)
        if "rowidx" in used:
            k = np.arange(NLH, dtype=np.int64)[None, :]
            p = np.arange(128, dtype=np.int64)[:, None]
            m["rowidx"] = np.ascontiguousarray(((NCT + half * NLH + k) * 128 + p).astype(np.int32))
        maps.append(m)
    res = run_bass_kernel_spmd(P.nc, maps, core_ids=list(range(n_cores)))
    out = np.zeros((nb, NL * 128, 2048), np.float32)
    for core in range(n_cores):
        bi, half = core % nb, core // nb
        if SPLIT:
            out[bi, half * NLH * 128:(half + 1) * NLH * 128] = np.asarray(res.results[core]["OUT"])
        elif half == 0:
            out[bi] = np.asarray(res.results[core]["OUT"])
    return out


def kernel(**inputs):
    return run_model(inputs, 32, 2, [0, 1, 2, 3], n_cores=N_CORES)
```

```python
from contextlib import ExitStack
import numpy as np
import concourse.bass as bass
import concourse.mybir as mybir
from concourse.bass_utils import run_bass_kernel_spmd

F32 = mybir.dt.float32
BF16 = mybir.dt.bfloat16
I32 = mybir.dt.int32
U32 = mybir.dt.uint32
AF = mybir.ActivationFunctionType
ALU = mybir.AluOpType
AX = mybir.AxisListType

EPOCH = 30000


class Res:
    __slots__ = ("name", "lw", "rd", "t")

    def __init__(self, name, t=None):
        self.name = name
        self.lw = None
        self.rd = []
        self.t = t

    def __getitem__(self, k):
        return self.t[k]


class Prog:
    ENGS = ("pe", "act", "dve", "pool", "sp")
    NSLOT = {"sp": 24, "act": 6, "pool": 12}

    def __init__(self):
        self.nc = bass.Bass("TRN2", target_bir_lowering=False)
        self.es = ExitStack()
        self.ops = []
        self.psbanks = None
        self.psn = 0
        self.reserved = set()
        self.scopes = [self.es]
        self.last_c = {}
        self.slot_next = {q: 0 for q in self.NSLOT}
        self.slot_last = {q: [None] * k for q, k in self.NSLOT.items()}
        self.pending = {e: set() for e in self.ENGS}

    def sb(self, name, shape, dt=F32):
        self.uid = getattr(self, "uid", 0) + 1
        name = f"{name}_{self.uid}"
        t = self.scopes[-1].enter_context(self.nc.sbuf_tensor(name, list(shape), dt))
        return Res(name, t)

    def scope(self):
        prog = self

        class _S:
            def __enter__(s2):
                prog.scopes.append(ExitStack())

            def __exit__(s2, *a):
                prog.scopes.pop().close()
                prog.barrier()
        return _S()

    def barrier(self):
        s = set(self.last_c.values())
        for q in self.NSLOT:
            s.update(x for x in self.slot_last[q] if x is not None)
        for e in self.ENGS:
            self.pending[e] = set(s)

    def ps(self, name, shape, dt=F32):
        t = self.es.enter_context(self.nc.psum_tensor(name, list(shape), dt))
        return Res(name, t)

    def bank(self):
        if self.psbanks is None:
            self.psall = self.ps("psall", [128, 8, 512], F32)
            self.psbanks = [Res(f"psb{i}", self.psall.t[:, i, :]) for i in range(8)]
        while self.psn % 8 in self.reserved:
            self.psn += 1
        b = self.psbanks[self.psn % 8]
        self.psn += 1
        return b

    def bank2(self):
        self.bank()
        self.psn -= 1
        while self.psn % 2 or (self.psn % 8) in self.reserved or (self.psn % 8 + 1) in self.reserved:
            self.psn += 1
        i = self.psn % 8
        self.psn += 2
        self.last_pair = (i, i + 1)
        return self.psall.t[:, i:i + 2, :].rearrange("p a b -> p (a b)"), self.psbanks[i], self.psbanks[i + 1]

    def dram(self, name, shape, dt=F32, kind="Internal"):
        t = self.nc.dram_tensor(name, list(shape), dt, kind=kind)
        return Res(name, t.ap())

    def op(self, eng, fn, reads=(), writes=(), dma=False, soft=()):
        idx = len(self.ops)
        deps = set()
        softdeps = set()
        for r in reads:
            if r.lw is not None:
                (softdeps if r in soft else deps).add(r.lw)
        for w in writes:
            tgt = softdeps if w in soft else deps
            if w.lw is not None:
                tgt.add(w.lw)
            tgt.update(w.rd)
        for d in softdeps:
            if self.ops[d][0] != eng or self.ops[d][3] or dma:
                deps.add(d)
        for r in reads:
            r.rd.append(idx)
        for w in writes:
            w.lw = idx
            w.rd = []
        if self.pending[eng]:
            deps.update(self.pending[eng])
            self.pending[eng] = set()
        slot = None
        if dma:
            slot = self.slot_next[eng]
            self.slot_next[eng] = (slot + 1) % self.NSLOT[eng]
            prev = self.slot_last[eng][slot]
            if prev is not None:
                deps.add(prev)
            self.slot_last[eng][slot] = idx
        else:
            self.last_c[eng] = idx
        self.ops.append([eng, fn, deps, dma, slot])
        return idx

    def pe(self, fn, reads=(), writes=()):
        return self.op("pe", fn, reads, writes)

    def act(self, fn, reads=(), writes=()):
        return self.op("act", fn, reads, writes)

    def dve(self, fn, reads=(), writes=(), soft=()):
        return self.op("dve", fn, reads, writes, soft=soft)

    def pool(self, fn, reads=(), writes=()):
        return self.op("pool", fn, reads, writes)

    def dma(self, out, in_, reads=(), writes=(), q="sp", **kw):
        nc = self.nc
        e = {"sp": nc.sync, "act": nc.scalar, "pool": nc.gpsimd}[q]
        return self.op(q, lambda: e.dma_start(out=out, in_=in_, **kw), reads, writes, dma=True)

    def dump(self, name, ap, res, shape):
        if not getattr(self, "debug", False):
            return
        d = self.dram("DBG_" + name, shape, F32, "ExternalOutput")
        self.dma(d[tuple(slice(None) for _ in shape)], ap, reads=[res], writes=[d])
        self.dbg = getattr(self, "dbg", []) + [d]

    def final(self, resources):
        nc = self.nc
        self.op("sp", lambda: nc.sync.nop(), reads=list(resources), writes=[])

    def finish(self):
        nc = self.nc
        ops = self.ops
        n = len(ops)

        def skip(d, i):
            return ops[d][0] == "pe" and ops[i][0] == "pe" and not ops[d][3] and not ops[i][3]

        needed = [False] * n
        for i, (eng, fn, deps, dma, slot) in enumerate(ops):
            for d in deps:
                if not skip(d, i):
                    needed[d] = True
        cnt = {e: 0 for e in self.ENGS}
        slot_cnt = {q: [0] * k for q, k in self.NSLOT.items()}
        slot_ep = {q: [0] * k for q, k in self.NSLOT.items()}
        ticket = [None] * n
        for i, (eng, fn, deps, dma, s) in enumerate(ops):
            if dma:
                if slot_cnt[eng][s] + 16 > EPOCH:
                    slot_cnt[eng][s] = 0
                    slot_ep[eng][s] += 1
                slot_cnt[eng][s] += 16
                ticket[i] = (("d", eng, s, slot_ep[eng][s]), slot_cnt[eng][s])
            elif needed[i]:
                cnt[eng] += 1
                ep = (cnt[eng] - 1) // EPOCH
                ticket[i] = (("c", eng, ep), cnt[eng] - ep * EPOCH)
        semkeys = sorted({t[0] for t in ticket if t is not None}, key=str)
        sems = {}
        for k in semkeys:
            sems[k] = self.es.enter_context(nc.semaphore("s_" + "_".join(str(x) for x in k)))
        self.nsem = len(sems)
        per_eng = {e: [] for e in self.ENGS}
        for i, o in enumerate(ops):
            per_eng[o[0]].append(i)
        engobj = {"pe": nc.tensor, "act": nc.scalar, "dve": nc.vector, "pool": nc.gpsimd, "sp": nc.sync}

        def emit(ename):
            e = engobj[ename]
            waited = {}
            for i in per_eng[ename]:
                eng, fn, deps, dma, slot = ops[i]
                dl = deps
                need = {}
                for d in dl:
                    if skip(d, i):
                        continue
                    k, v = ticket[d]
                    if need.get(k, 0) < v:
                        need[k] = v
                for k, v in sorted(need.items(), key=str):
                    if waited.get(k, 0) >= v:
                        continue
                    if any(kk[0] == k[0] and kk[1:-1] == k[1:-1] and kk[-1] > k[-1] for kk in waited):
                        continue
                    e.wait_ge(sems[k], v)
                    waited[k] = v
                ins = fn()
                if ticket[i] is not None:
                    k, v = ticket[i]
                    ins.then_inc(sems[k], 16 if dma else 1)

        with nc.Block() as block:
            @block.tensor
            def _(eng):
                emit("pe")

            @block.scalar
            def _(eng):
                emit("act")

            @block.vector
            def _(eng):
                emit("dve")

            @block.gpsimd
            def _(eng):
                emit("pool")

            @block.sync
            def _(eng):
                emit("sp")
        self.es.close()
        return nc


def phase_mod(P, depth, c_col, cc_col, mod_w, mod_b, MOD):
    nc = P.nc
    cs = P.sb("mod_cs", [128, 32])
    cs2 = P.sb("mod_cs2", [128, 32])
    rep = P.sb("mod_rep", [128, 32, 128])
    P.dma(cs[:, 0:16], c_col[:, :], reads=[c_col], writes=[cs])
    P.dma(cs[:, 16:32], cc_col[:, :], reads=[cc_col], writes=[cs])
    P.act(lambda: nc.scalar.activation(out=cs2[:, :], in_=cs[:, :], func=AF.Silu), [cs], [cs2])
    for kc in range(32):
        P.dve(lambda kc=kc: nc.vector.tensor_copy(out=rep[:, kc, :], in_=cs2[:, kc:kc + 1].to_broadcast([128, 128])),
              [cs2], [rep])
    wb = [P.sb(f"mod_w{i}", [128, 16, 512]) for i in range(2)]
    bb = [P.sb(f"mod_b{i}", [128, 512]) for i in range(2)]
    ob = [P.sb(f"mod_o{i}", [128, 512]) for i in range(4)]
    it = 0
    for l in range(depth):
        for n in range(24):
            wt, bt = wb[it % 2], bb[it % 2]
            P.dma(wt[:, :, :], mod_w[l, :, n * 512:(n + 1) * 512].rearrange("(kc p) n -> p kc n", p=128),
                  reads=[mod_w], writes=[wt])
            P.dma(bt[:, :], mod_b[l, n * 512:(n + 1) * 512].partition_broadcast(128), reads=[mod_b], writes=[bt])
            for kind in range(2):
                pb = P.bank()
                for kc in range(16):
                    P.pe(lambda kc=kc, kind=kind, pb=pb, wt=wt: nc.tensor.matmul(
                        pb[:, :], lhsT=rep[:, kind * 16 + kc, :], rhs=wt[:, kc, :], start=(kc == 0), stop=(kc == 15)),
                        [rep, wt], [pb])
                ot = ob[(it * 2 + kind) % 4]
                P.dve(lambda pb=pb, ot=ot, bt=bt: nc.vector.tensor_tensor(out=ot[:, :], in0=pb[:, :], in1=bt[:, :], op=ALU.add),
                      [pb, bt], [ot])
                P.dma(MOD[l, kind, :, n * 512:(n + 1) * 512], ot[:, :], reads=[ot], writes=[MOD])
            it += 1


D = 2048
EPS = 1e-6


class Ctx:
    pass


def tr_chunks(P, C, src, n, dst, col0=0, rows=128, cw=128):
    nc = P.nc
    per = 512 // rows if rows >= 128 else 4
    j0 = 0
    while j0 < n:
        nb = min(per, n - j0)
        pb = P.bank()
        for j in range(nb):
            P.pe(lambda j=j, j0=j0, pb=pb: nc.tensor.transpose(
                out=pb[0:cw, j * rows:(j + 1) * rows], in_=src[0:rows, col0 + (j0 + j) * cw: col0 + (j0 + j + 1) * cw],
                identity=C.ident[0:rows, 0:rows]), [src, C.ident], [pb])
        C.ev ^= 1
        o = dst[0:cw, j0:j0 + nb, 0:rows]
        i = pb[0:cw, 0:nb * rows].rearrange("p (j t) -> p j t", t=rows)
        if C.ev:
            P.act(lambda o=o, i=i: nc.scalar.copy(out=o, in_=i), [pb], [dst])
        else:
            P.dve(lambda o=o, i=i: nc.vector.tensor_copy(out=o, in_=i), [pb], [dst])
        j0 += nb


def linear(P, C, xT, nk, W, N, consume, cw=512, wtag="lw", bf16=False):
    nc = P.nc
    wb = C.wbuf
    nch = (N + cw - 1) // cw
    for n in range(nch):
        w = min(cw, N - n * cw)
        wt = wb[C.wi % len(wb)]
        C.wi += 1
        P.dma(wt[:, 0:nk, 0:w], W[:, n * cw:n * cw + w].rearrange("(kc p) n -> p kc n", p=128),
              reads=[C.wsrc], writes=[wt])
        if bf16:
            w16 = C.wbuf16[C.wi % len(C.wbuf16)]
            P.act(lambda wt=wt, w16=w16, w=w: nc.scalar.copy(out=w16[:, 0:nk, 0:w], in_=wt[:, 0:nk, 0:w]), [wt], [w16])
            wt = w16
        for t, xt in enumerate(xT):
            pb = P.bank()
            for kc in range(nk):
                P.pe(lambda kc=kc, pb=pb, xt=xt, wt=wt, w=w: nc.tensor.matmul(
                    pb[:, 0:w], lhsT=xt[:, kc, :], rhs=wt[:, kc, 0:w], start=(kc == 0), stop=(kc == nk - 1)),
                    [xt, wt], [pb])
            consume(n, t, pb, w)


def load_bc(P, dst, src_ap, srcres):
    P.dma(dst[:, :], src_ap.partition_broadcast(128), reads=[srcres], writes=[dst])


def prep_mod(P, C, l, j_shift, j_scale, g_ap, gres, Gp, Sh):
    nc = P.nc
    with P.scope():
        gt = P.sb("pm_g", [128, D])
        load_bc(P, gt, g_ap, gres)
        for kind in range(2):
            P.dma(Sh[kind][:, :], C.MOD[l, kind, :, j_shift * D:(j_shift + 1) * D], reads=[C.MOD], writes=[Sh[kind]])
            P.dma(Gp[kind][:, :], C.MOD[l, kind, :, j_scale * D:(j_scale + 1) * D], reads=[C.MOD], writes=[Gp[kind]])
            P.dve(lambda kind=kind: nc.vector.scalar_tensor_tensor(
                out=Gp[kind][:, :], in0=Gp[kind][:, :], scalar=1.0, in1=gt[:, :], op0=ALU.add, op1=ALU.mult),
                [Gp[kind], gt], [Gp[kind]])


def norm_mod(P, C, x, out, Gp, Sh, junk, st):
    nc = P.nc
    P.act(lambda: (nc.scalar.activation(out=junk[:, :], in_=x, func=AF.Square, accum_out=st[:, 0:1]),
                   nc.scalar.copy(out=P.scr[:, 0:1], in_=st[:, 0:1]))[1], [C.xres], [junk, st])
    P.dve(lambda: nc.vector.tensor_scalar(out=st[:, 1:2], in0=st[:, 0:1], scalar1=1.0 / D, scalar2=EPS,
                                          op0=ALU.mult, op1=ALU.add), [st], [st])
    P.act(lambda: nc.scalar.activation(out=st[:, 3:4], in_=st[:, 1:2], func=AF.Sqrt), [st], [st])
    P.dve(lambda: nc.vector.reciprocal(out=st[:, 2:3], in_=st[:, 3:4]), [st], [st])
    P.dve(lambda: nc.vector.scalar_tensor_tensor(out=out, in0=x, scalar=st[:, 2:3], in1=Gp[:, :],
                                                 op0=ALU.mult, op1=ALU.mult), [C.xres, st, Gp], [C.ores])
    P.dve(lambda: nc.vector.tensor_tensor(out=out, in0=out, in1=Sh[:, :], op=ALU.add), [C.ores, Sh], [C.ores])


def tile_groups(C, G=2):
    gs = []
    for t0 in range(0, C.NCT, G):
        gs.append((1, t0, min(G, C.NCT - t0)))
    for t0 in range(0, C.NL, G):
        gs.append((0, C.NCT + t0, min(G, C.NL - t0)))
    return gs


def phase_inproj0(P, C, l, hsrc):
    nc = P.nc
    with P.scope():
        Gp = [P.sb(f"ipGp{k}", [128, D]) for k in range(2)]
        Sh = [P.sb(f"ipSh{k}", [128, D]) for k in range(2)]
        prep_mod(P, C, l, 0, 1, C.norm1_g[l, :], C.norm1_g, Gp, Sh)
        G = 2
        xs = [P.sb(f"ipx{i}", [128, D]) for i in range(G)]
        xT = [P.sb(f"ipxT{i}", [128, 16, 128], BF16) for i in range(G)]
        C.wbuf16 = [P.sb(f"ipw16_{i}", [128, 16, 512], BF16) for i in range(2)]
        os_ = [P.sb(f"ipo{i}", [128, 2560]) for i in range(G)]
        junk = P.sb("ipjunk", [128, D])
        st = P.sb("ipst", [128, 4])
        cs = P.sb("ipcs", [128, 2, 64])
        tmp = P.sb("iptmp", [128, 4, 10, 64])
        C.wbuf = [P.sb(f"ipw{i}", [128, 16, 512]) for i in range(2)]
        for kind, t0, nt in tile_groups(C, G):
            for i in range(nt):
                t = t0 + i
                P.dma(xs[i][:, :], hsrc[t * 128:(t + 1) * 128, :], reads=[hsrc], writes=[xs[i]])
                C.xres, C.ores = xs[i], xs[i]
                norm_mod(P, C, xs[i][:, :], xs[i][:, :], Gp[kind], Sh[kind], junk, st)
                tr_chunks(P, C, xs[i], 16, xT[i])

            def consume(n, ti, pb, w):
                o = os_[ti]
                C.ev ^= 1
                if C.ev:
                    P.act(lambda: nc.scalar.copy(out=o[:, n * 512:n * 512 + w], in_=pb[:, 0:w]), [pb], [o])
                else:
                    P.dve(lambda: nc.vector.tensor_copy(out=o[:, n * 512:n * 512 + w], in_=pb[:, 0:w]), [pb], [o])
            C.wsrc = C.ab_w_in
            linear(P, C, xT[0:nt], 16, C.ab_w_in[0], 2560, consume, bf16=True)
            for i in range(nt):
                t = t0 + i
                o = os_[i]
                if kind == 0:
                    lt = t - C.NCT
                    P.dma(cs[:, :, :], C.rope[lt * 128:(lt + 1) * 128, :, :], reads=[C.rope], writes=[cs])
                    qk = o[:, 0:1280].rearrange("p (h two d) -> p h two d", two=2, d=64)
                    x1, x2 = qk[:, :, 0, :], qk[:, :, 1, :]
                    cosb = cs[:, 0:1, :].to_broadcast([128, 10, 64])
                    sinb = cs[:, 1:2, :].to_broadcast([128, 10, 64])
                    for j, (a, b) in enumerate([(x1, cosb), (x2, sinb), (x2, cosb), (x1, sinb)]):
                        P.dve(lambda j=j, a=a, b=b: nc.vector.tensor_tensor(out=tmp[:, j, :, :], in0=a, in1=b, op=ALU.mult),
                              [o, cs], [tmp])
                    P.dve(lambda x1=x1: nc.vector.tensor_tensor(out=x1, in0=tmp[:, 0, :, :], in1=tmp[:, 1, :, :], op=ALU.subtract),
                          [tmp], [o])
                    P.dve(lambda x2=x2: nc.vector.tensor_tensor(out=x2, in0=tmp[:, 2, :, :], in1=tmp[:, 3, :, :], op=ALU.add),
                          [tmp], [o])
                P.dma(C.QKVU[t * 128:(t + 1) * 128, :], o[:, :], reads=[o], writes=[C.QKVU])


def sub(res, name):
    return Res(name, res.t)


ATTN_SCALE = 128 ** -0.5


def phase_attn(P, C):
    nc = P.nc
    NCT, NL = C.NCT, C.NL
    T = NCT + NL
    with P.scope():
        KT = [P.sb(f"atKT{h}", [128, T * 128]) for h in range(2)]
        KTr = [sub(KT[0], f"KTr{t}") for t in range(T)]
        V = P.sb("atV", [128, T, 256])
        Vr = [sub(V, f"Vr{t}") for t in range(T)]
        kin = [P.sb(f"atk{i}", [128, 256]) for i in range(2)]
        sink = P.sb("atsink", [128, 8])
        load_bc(P, sink, C.attn_sink[0, :], C.attn_sink)
        mask = P.sb("atmask", [128, 384])
        P.dma(mask[:, :], C.bandmask[:, :], reads=[C.bandmask], writes=[mask])
        for t in range(T):
            ki = kin[t % 2]
            P.dma(ki[:, :], C.QKVU[t * 128:(t + 1) * 128, 1024:1280], reads=[C.QKVU], writes=[ki])
            P.dma(V[:, t, :], C.QKVU[t * 128:(t + 1) * 128, 1280:1536], reads=[C.QKVU], writes=[Vr[t]])
            pb = P.bank()
            for h in range(2):
                P.pe(lambda h=h, pb=pb, ki=ki: nc.tensor.transpose(
                    out=pb[:, h * 128:(h + 1) * 128], in_=ki[:, h * 128:(h + 1) * 128], identity=C.ident[:, :]),
                    [ki, C.ident], [pb])
            for h in range(2):
                P.act(lambda h=h, pb=pb, t=t: nc.scalar.copy(out=KT[h][:, t * 128:(t + 1) * 128],
                                                           in_=pb[:, h * 128:(h + 1) * 128]), [pb], [KTr[t]])
        qb = [P.sb(f"atq{i}", [128, 1024]) for i in range(2)]
        qT = [P.sb(f"atqT{i}", [128, 8, 128]) for i in range(2)]
        ob = [P.sb(f"ato{i}", [128, 1024]) for i in range(2)]
        scb = [P.sb(f"atsc{i}", [128, 640]) for i in range(2)]
        prb = [P.sb(f"atpr{i}", [128, 640]) for i in range(2)]
        ptb = [P.sb(f"atpt{i}", [128, 5, 128]) for i in range(2)]
        mst = [P.sb(f"atm{i}", [128, 8, 8]) for i in range(2)]
        it = 0
        for t in range(T):
            kind = 1 if t < NCT else 0
            q, QT, o_t, m = qb[t % 2], qT[t % 2], ob[t % 2], mst[t % 2]
            P.dma(q[:, :], C.QKVU[t * 128:(t + 1) * 128, 0:1024], reads=[C.QKVU], writes=[q])
            tr_chunks(P, C, q, 8, QT)
            ktiles = list(range(NCT))
            if kind == 0:
                n = t - NCT
                lo, hi = max(0, n - 1), min(NL - 1, n + 1)
                moff = (lo - (n - 1)) * 128
                ktiles += list(range(NCT + lo, NCT + hi + 1))
                nb = hi - lo + 1
            nk = len(ktiles) * 128
            for hq in range(8):
                h = hq // 4
                sc, pr, PT = scb[it % 2], prb[it % 2], ptb[it % 2]
                it += 1
                pbA = P.bank()
                P.pe(lambda pbA=pbA, QT=QT, hq=hq, h=h: nc.tensor.matmul(
                    pbA[:, 0:NCT * 128], lhsT=QT[:, hq, :], rhs=KT[h][:, 0:NCT * 128], start=True, stop=True),
                    [QT] + KTr[0:NCT], [pbA])
                P.act(lambda pbA=pbA, sc=sc: nc.scalar.activation(
                    out=sc[:, 0:NCT * 128], in_=pbA[:, 0:NCT * 128], func=AF.Copy, scale=ATTN_SCALE), [pbA], [sc])
                if kind == 0:
                    pbB = P.bank()
                    c0, c1 = (NCT + lo) * 128, (NCT + hi + 1) * 128
                    P.pe(lambda pbB=pbB, QT=QT, hq=hq, h=h, c0=c0, c1=c1, nb=nb: nc.tensor.matmul(
                        pbB[:, 0:nb * 128], lhsT=QT[:, hq, :], rhs=KT[h][:, c0:c1], start=True, stop=True),
                        [QT] + KTr[NCT + lo:NCT + hi + 1], [pbB])
                    P.dve(lambda pbB=pbB, sc=sc, nb=nb, moff=moff: nc.vector.scalar_tensor_tensor(
                        out=sc[:, NCT * 128:NCT * 128 + nb * 128], in0=pbB[:, 0:nb * 128], scalar=ATTN_SCALE,
                        in1=mask[:, moff:moff + nb * 128], op0=ALU.mult, op1=ALU.add), [pbB, mask], [sc])
                P.dve(lambda sc=sc, m=m, hq=hq, nk=nk: nc.vector.reduce_max(out=m[:, hq, 0:1], in_=sc[:, 0:nk], axis=AX.X),
                      [sc], [m])
                P.dve(lambda m=m, hq=hq: nc.vector.tensor_tensor(out=m[:, hq, 0:1], in0=m[:, hq, 0:1],
                                                                 in1=sink[:, hq:hq + 1], op=ALU.max), [m, sink], [m])
                P.dve(lambda m=m, hq=hq: nc.vector.tensor_scalar(out=m[:, hq, 1:2], in0=m[:, hq, 0:1], scalar1=-1.0,
                                                                 scalar2=None, op0=ALU.mult), [m], [m])
                P.act(lambda sc=sc, pr=pr, m=m, hq=hq, nk=nk: (nc.scalar.activation(
                    out=pr[:, 0:nk], in_=sc[:, 0:nk], func=AF.Exp, bias=m[:, hq, 1:2], scale=1.0,
                    accum_out=m[:, hq, 2:3]), nc.scalar.copy(out=P.scr[:, 0:1], in_=m[:, hq, 2:3]))[1], [sc, m], [pr, m])
                P.act(lambda m=m, hq=hq: nc.scalar.activation(
                    out=m[:, hq, 3:4], in_=sink[:, hq:hq + 1], func=AF.Exp, bias=m[:, hq, 1:2], scale=1.0), [sink, m], [m])
                P.dve(lambda m=m, hq=hq: nc.vector.tensor_tensor(out=m[:, hq, 4:5], in0=m[:, hq, 2:3],
                                                                 in1=m[:, hq, 3:4], op=ALU.add), [m], [m])
                P.dve(lambda m=m, hq=hq: nc.vector.reciprocal(out=m[:, hq, 5:6], in_=m[:, hq, 4:5]), [m], [m])
                tr_chunks(P, C, pr, nk // 128, PT)
                pbO = P.bank()
                for j, kt in enumerate(ktiles):
                    P.pe(lambda j=j, kt=kt, pbO=pbO, PT=PT, h=h, last=(j == len(ktiles) - 1): nc.tensor.matmul(
                        pbO[:, 0:128], lhsT=PT[:, j, :], rhs=V[:, kt, h * 128:(h + 1) * 128], start=(j == 0), stop=last),
                        [PT, Vr[kt]], [pbO])
                P.act(lambda pbO=pbO, o_t=o_t, m=m, hq=hq: nc.scalar.activation(
                    out=o_t[:, hq * 128:(hq + 1) * 128], in_=pbO[:, 0:128], func=AF.Copy, scale=m[:, hq, 5:6]),
                    [pbO, m], [o_t])
            P.dma(C.ATS5[t * 128:(t + 1) * 128, 0:1024], o_t[:, :], reads=[o_t], writes=[C.ATS5])


SPLIT = False


class Ctx:
    def __init__(self, P, NL, NCT, probes=()):
        T = NL + NCT
        self.NO = NL // 2 if SPLIT else NL
        self.P, self.NL, self.NCT, self.T = P, NL, NCT, T
        self.probes = set(probes)
        self.used_inputs = []
        self.ev = 0
        self.wi = 0
        R = T * 128
        self.spec = {
            "h0": ([R, 2048], "in"), "c_col": ([128, 16], "in"), "cc_col": ([128, 16], "in"),
            "mod_w": ([2, 2048, 12288], "in"), "mod_b": ([2, 12288], "in"),
            "norm1_g": ([2, 2048], "in"), "norm2_g": ([2, 2048], "in"),
            "ab_w_in": ([1, 2048, 2560], "in"), "attn_sink": ([1, 8], "in"),
            "s5_a_re": ([1, 2, 64, 64], "in"), "s5_a_im": ([1, 2, 64, 64], "in"), "s5_log_dt": ([1, 2, 64, 64], "in"),
            "s5_b_re": ([1, 2, 64, 64, 16], "in"), "s5_b_im": ([1, 2, 64, 64, 16], "in"),
            "s5_c_re": ([1, 2, 64, 16, 64], "in"), "s5_c_im": ([1, 2, 64, 16, 64], "in"),
            "s5_d": ([1, 1024], "in"), "s5_glu_w": ([1, 1024, 1024], "in"), "s5_glu_b": ([1, 1024], "in"),
            "ab_w_out": ([1, 2048, 2048], "in"),
            "ssd_w_in": ([1, 2048, 10368], "in"), "ssd_conv_w": ([1, 5, 6144], "in"), "ssd_conv_b": ([1, 6144], "in"),
            "ssd_dt_bias": ([1, 128], "in"), "ssd_a_log": ([1, 128], "in"), "ssd_d": ([1, 64], "in"),
            "ssd_norm_g": ([1, 4096], "in"), "ssd_w_out": ([1, 4096, 2048], "in"),
            "peer_wq": ([2, 2048, 1024], "in"), "peer_keys": ([2, 8, 2, 128, 64], "in"),
            "peer_u": ([2, 16384, 2048], "in"), "peer_v": ([2, 16384, 2048], "in"),
            "final_norm_g": ([2048], "in"),
            "ident": ([128, 128], "in"), "rope": ([NL * 128, 2, 64], "in"), "bandmask": ([128, 384], "in"),
            "tri": ([4, 128, 128], "in"), "ncol": ([128, 4], "in"), "iota256": ([128, 256], "in"),
            "MOD": ([2, 2, 128, 12288], "tmp"), "QKVU": ([R, 2560], "tmp"), "ATS5": ([R, 2048], "tmp"),
            "H": ([R, 2048], "tmp"), "Y5": ([R, 1024], "tmp"), "BB": ([2, 64, 16, 128], "tmp"),
            "SP": ([R, 10368], "tmp"), "XBC": ([R, 6144], "tmp"), "YS": ([NL * 128, 4096], "tmp"),
            "ssdmask": ([2, 128, 128], "in"),
            "UVB": ([2 * 16384, 4096], "tmp", BF16),
            "rowidx": ([128, NL // 2], "in", I32), "H2": ([NL // 2 * 128, 2048], "tmp"),
            "OUT": ([self.NO * 128, 2048], "out"),
        }

    def __getattr__(self, name):
        spec = self.__dict__.get("spec", {})
        if name not in spec:
            raise AttributeError(name)
        shape, kind = spec[name][0], spec[name][1]
        dt = spec[name][2] if len(spec[name]) > 2 else F32
        k = {"in": "ExternalInput", "out": "ExternalOutput"}.get(kind)
        if k is None:
            k = "ExternalOutput" if name in self.probes else "Internal"
        r = self.P.dram(name, shape, dt, k)
        if kind == "in":
            self.used_inputs.append(name)
        setattr(self, name, r)
        return r


STAGES = ["mod", "inproj0", "attn", "s5", "outproj0", "peer0", "ssd", "peer1", "final"]


def build_program(P, NL, NCT, stage="final", probes=()):
    C = Ctx(P, NL, NCT, probes)
    nc = P.nc
    si = STAGES.index(stage)
    C.ident_d = C.ident
    idt = P.es.enter_context(nc.sbuf_tensor("ident_sb", [128, 128], F32))
    ident_sb = Res("ident_sb", idt)
    P.dma(ident_sb[:, :], C.ident_d[:, :], reads=[C.ident_d], writes=[ident_sb])
    C.ident = ident_sb
    P.scr = Res("scr", P.es.enter_context(nc.sbuf_tensor("scr_tail", [128, 4], F32)))
    io = Res("iota_sb", P.es.enter_context(nc.sbuf_tensor("iota_sb", [128, 256], F32)))
    P.dma(io[:, :], C.iota256[:, :], reads=[C.iota256], writes=[io])
    C.iota = io
    with P.scope():
        phase_mod(P, 2, C.c_col, C.cc_col, C.mod_w, C.mod_b, C.MOD)
    outs = [C.MOD]
    if si >= 1:
        phase_inproj0(P, C, 0, C.h0)
        outs.append(C.QKVU)
    if si >= 2:
        phase_attn(P, C)
        outs.append(C.ATS5)
    if si >= 3:
        phase_s5(P, C)
        outs.append(C.Y5)
    if si >= 4:
        phase_outproj0(P, C, 0, C.h0)
        outs.append(C.H)
    if si >= 5:
        phase_peer(P, C, 0, list(range(C.T)))
    if si >= 6:
        phase_ssd_in(P, C, 1)
        phase_ssd_conv(P, C)
        for d in range(2):
            ssd_pass(P, C, d)
        phase_ssd_out(P, C, 1)
    if si >= 7:
        if SPLIT:
            phase_peer(P, C, 1, list(range(C.NL // 2)), split=True)
        else:
            phase_peer(P, C, 1, list(range(C.NCT, C.T)))
    if si >= 8:
        phase_final(P, C)
        outs.append(C.OUT)
    P.final(outs + getattr(P, "dbg", []))
    return C


def host_constants(NL):
    t = np.arange(NL * 128)
    row = (t // 64).astype(np.float32)
    col = (t % 64).astype(np.float32)
    inv = (10000.0 ** (-np.arange(32, dtype=np.float32) / 32)).astype(np.float32)
    ang = np.concatenate([row[:, None] * inv, col[:, None] * inv], axis=-1).astype(np.float32)
    rope = np.stack([np.cos(ang), np.sin(ang)], axis=1).astype(np.float32)
    i = np.arange(128)[:, None]
    j = np.arange(128)[None, :]
    neg = np.float32(-30000.0)
    bandmask = np.concatenate([np.where(j >= i, 0, neg), np.zeros((128, 128)), np.where(j <= i, 0, neg)],
                              axis=1).astype(np.float32)
    iota = np.broadcast_to(np.arange(256, dtype=np.float32)[None, :], (128, 256)).copy()
    return {"ident": np.eye(128, dtype=np.float32), "rope": rope, "bandmask": bandmask, "iota256": iota}


def make_inputs(inp, b, NL, NCT, used=None):
    m = {}
    m["h0"] = np.ascontiguousarray(np.concatenate([inp["ctx"][b][:NCT * 128], inp["x"][b][:NL * 128]], axis=0))
    m["c_col"] = np.ascontiguousarray(inp["c"][b].reshape(16, 128).T)
    m["cc_col"] = np.ascontiguousarray(inp["c_ctx"].reshape(16, 128).T)
    for k in ["mod_w", "mod_b", "norm1_g", "norm2_g", "ab_w_in", "attn_sink", "s5_a_re", "s5_a_im", "s5_log_dt",
              "s5_b_re", "s5_b_im", "s5_c_re", "s5_c_im", "s5_glu_w", "s5_glu_b", "ab_w_out", "ssd_w_in",
              "ssd_conv_w", "ssd_conv_b", "ssd_d", "ssd_norm_g", "ssd_w_out", "peer_wq", "peer_keys", "peer_u",
              "peer_v", "final_norm_g"]:
        m[k] = inp[k]
    m["s5_d"] = inp["s5_d"].reshape(1, 1024)
    m["ssd_dt_bias"] = inp["ssd_dt_bias"].reshape(1, 128)
    m["ssd_a_log"] = inp["ssd_a_log"].reshape(1, 128)
    m.update(host_constants(NL))
    m.update(host_s5_constants())
    if used is not None:
        m = {k: np.ascontiguousarray(v, dtype=np.float32) for k, v in m.items() if k in used}
    return m


PI = float(np.pi)


def sin_of(P, out, ang, shift, tmps, rin, rtmp, rout):
    nc = P.nc
    z, kf, ki = tmps
    P.dve(lambda: nc.vector.tensor_scalar(out=z, in0=ang, scalar1=float(shift), scalar2=None, op0=ALU.add), rin, rtmp)
    P.dve(lambda: nc.vector.tensor_scalar(out=ki, in0=z, scalar1=1.0 / (2 * PI), scalar2=None, op0=ALU.mult), rtmp, rtmp)
    P.dve(lambda: nc.vector.tensor_copy(out=kf, in_=ki), rtmp, rtmp)
    P.dve(lambda: nc.vector.scalar_tensor_tensor(out=z, in0=kf, scalar=-2 * PI, in1=z, op0=ALU.mult, op1=ALU.add), rtmp, rtmp)
    P.dve(lambda: nc.vector.tensor_scalar(out=kf, in0=z, scalar1=PI, scalar2=-2 * PI, op0=ALU.is_gt, op1=ALU.mult), rtmp, rtmp)
    P.dve(lambda: nc.vector.tensor_tensor(out=z, in0=z, in1=kf, op=ALU.add), rtmp, rtmp)
    P.dve(lambda: nc.vector.tensor_scalar(out=kf, in0=z, scalar1=-PI, scalar2=2 * PI, op0=ALU.is_lt, op1=ALU.mult), rtmp, rtmp)
    P.dve(lambda: nc.vector.tensor_tensor(out=z, in0=z, in1=kf, op=ALU.add), rtmp, rtmp)
    P.act(lambda: nc.scalar.activation(out=out, in_=z, func=AF.Sin), rtmp, rout)


def cmul(P, a_re, a_im, t_re, t_im, o_re, o_im, tmps, ra, rt, ro, conj=False):
    nc = P.nc
    tA, tB, tC, tD = tmps
    P.dve(lambda: nc.vector.tensor_tensor(out=tA[0], in0=a_re, in1=t_re, op=ALU.mult), ra + rt, [tA[1]])
    P.pool(lambda: nc.gpsimd.tensor_tensor(out=tB[0], in0=a_im, in1=t_im, op=ALU.mult), ra + rt, [tB[1]])
    P.pool(lambda: nc.gpsimd.tensor_tensor(out=tC[0], in0=a_re, in1=t_im, op=ALU.mult), ra + rt, [tC[1]])
    P.dve(lambda: nc.vector.tensor_tensor(out=tD[0], in0=a_im, in1=t_re, op=ALU.mult), ra + rt, [tD[1]])
    P.dve(lambda: nc.vector.tensor_tensor(out=o_re, in0=tA[0], in1=tB[0], op=ALU.subtract), [tA[1], tB[1]], ro)
    P.pool(lambda: nc.gpsimd.tensor_tensor(out=o_im, in0=tC[0], in1=tD[0], op=ALU.add), [tC[1], tD[1]], ro)


def phase_s5(P, C):
    with P.scope():
        for d in range(2):
            s5_prep(P, C, d)
    for d in range(2):
        s5_pass(P, C, d)


def s5_prep(P, C, d):
    nc = P.nc
    if True:
        if True:
            ar = P.sb(f"s5ar{d}", [64, 64]); ai = P.sb(f"s5ai{d}", [64, 64]); ld = P.sb(f"s5ld{d}", [64, 64])
            br = P.sb(f"s5br{d}", [64, 64, 16]); bi = P.sb(f"s5bi{d}", [64, 64, 16])
            P.dma(ar[:, :], C.s5_a_re[0, d], reads=[C.s5_a_re], writes=[ar])
            P.dma(ai[:, :], C.s5_a_im[0, d], reads=[C.s5_a_im], writes=[ai])
            P.dma(ld[:, :], C.s5_log_dt[0, d], reads=[C.s5_log_dt], writes=[ld])
            P.dma(br[:, :, :], C.s5_b_re[0, d], reads=[C.s5_b_re], writes=[br])
            P.dma(bi[:, :, :], C.s5_b_im[0, d], reads=[C.s5_b_im], writes=[bi])
            w = P.sb(f"s5w{d}", [64, 16, 64])
            W = lambda k: w[:, k, :]
            P.act(lambda: nc.scalar.activation(out=W(0), in_=ld[:, :], func=AF.Exp), [ld], [w])
            P.dve(lambda: nc.vector.tensor_tensor(out=W(1), in0=W(0), in1=ar[:, :], op=ALU.mult), [w, ar], [w])
            P.dve(lambda: nc.vector.tensor_tensor(out=W(2), in0=W(0), in1=ai[:, :], op=ALU.mult), [w, ai], [w])
            P.act(lambda: nc.scalar.activation(out=W(3), in_=W(1), func=AF.Exp), [w], [w])
            ki_s = P.sb(f"s5kis{d}", [64, 64], I32)
            tm3 = (W(4), W(14), ki_s[:, :])
            sin_of(P, W(5), W(2), 0.0, tm3, [w], [w, ki_s], [w])
            sin_of(P, W(6), W(2), PI / 2, tm3, [w], [w, ki_s], [w])
            P.dve(lambda: nc.vector.tensor_tensor(out=W(7), in0=W(3), in1=W(6), op=ALU.mult), [w], [w])
            P.dve(lambda: nc.vector.tensor_scalar(out=W(7), in0=W(7), scalar1=-1.0, scalar2=None, op0=ALU.add), [w], [w])
            P.dve(lambda: nc.vector.tensor_tensor(out=W(8), in0=W(3), in1=W(5), op=ALU.mult), [w], [w])
            P.dve(lambda: nc.vector.tensor_tensor(out=W(9), in0=ar[:, :], in1=ar[:, :], op=ALU.mult), [ar], [w])
            P.dve(lambda: nc.vector.tensor_tensor(out=W(12), in0=ai[:, :], in1=ai[:, :], op=ALU.mult), [ai], [w])
            P.dve(lambda: nc.vector.tensor_tensor(out=W(9), in0=W(9), in1=W(12), op=ALU.add), [w], [w])
            P.dve(lambda: nc.vector.reciprocal(out=W(9), in_=W(9)), [w], [w])
            P.dve(lambda: nc.vector.tensor_tensor(out=W(12), in0=W(7), in1=ar[:, :], op=ALU.mult), [w, ar], [w])
            P.dve(lambda: nc.vector.tensor_tensor(out=W(13), in0=W(8), in1=ai[:, :], op=ALU.mult), [w, ai], [w])
            P.dve(lambda: nc.vector.tensor_tensor(out=W(12), in0=W(12), in1=W(13), op=ALU.add), [w], [w])
            P.dve(lambda: nc.vector.tensor_tensor(out=W(10), in0=W(12), in1=W(9), op=ALU.mult), [w], [w])
            P.dve(lambda: nc.vector.tensor_tensor(out=W(12), in0=W(8), in1=ar[:, :], op=ALU.mult), [w, ar], [w])
            P.dve(lambda: nc.vector.tensor_tensor(out=W(13), in0=W(7), in1=ai[:, :], op=ALU.mult), [w, ai], [w])
            P.dve(lambda: nc.vector.tensor_tensor(out=W(12), in0=W(12), in1=W(13), op=ALU.subtract), [w], [w])
            P.dve(lambda: nc.vector.tensor_tensor(out=W(11), in0=W(12), in1=W(9), op=ALU.mult), [w], [w])
            bbT = P.sb(f"s5bbT{d}", [64, 16, 2, 64])
            t1 = P.sb(f"s5t1{d}", [64, 64, 16]); t2 = P.sb(f"s5t2{d}", [64, 64, 16])
            fre = w[:, 10, :].unsqueeze(2).to_broadcast([64, 64, 16])
            fim = w[:, 11, :].unsqueeze(2).to_broadcast([64, 64, 16])
            o_re = bbT[:, :, 0, :].rearrange("g c p -> g p c")
            o_im = bbT[:, :, 1, :].rearrange("g c p -> g p c")
            P.dve(lambda: nc.vector.tensor_tensor(out=t1[:, :, :], in0=br[:, :, :], in1=fre, op=ALU.mult), [br, w], [t1])
            P.dve(lambda: nc.vector.tensor_tensor(out=t2[:, :, :], in0=bi[:, :, :], in1=fim, op=ALU.mult), [bi, w], [t2])
            P.dve(lambda: nc.vector.tensor_tensor(out=o_re, in0=t1[:, :, :], in1=t2[:, :, :], op=ALU.subtract), [t1, t2], [bbT])
            P.dve(lambda: nc.vector.tensor_tensor(out=t1[:, :, :], in0=bi[:, :, :], in1=fre, op=ALU.mult), [bi, w, bbT], [t1])
            P.dve(lambda: nc.vector.tensor_tensor(out=t2[:, :, :], in0=br[:, :, :], in1=fim, op=ALU.mult), [br, w, bbT], [t2])
            P.dve(lambda: nc.vector.tensor_tensor(out=o_im, in0=t1[:, :, :], in1=t2[:, :, :], op=ALU.add), [t1, t2], [bbT])
            P.dma(C.BB[d], bbT[:, :, :, :].rearrange("g c r p -> g c (r p)"), reads=[bbT], writes=[C.BB])


def s5_pass(P, C, d):
    nc = P.nc
    NCT, NL, T = C.NCT, C.NL, C.T
    if True:
        with P.scope():
            tabs = [P.sb(f"s5tab{k}", [128, 4096]) for k in range(4)]
            with P.scope():
                rho = P.sb("s5rho", [128, 4096]); th = P.sb("s5th", [128, 4096]); dtb = P.sb("s5dtb", [128, 4096])
                tmp = P.sb("s5tmp", [128, 4096])
                ncol = P.sb("s5ncol", [128, 4])
                P.dma(ncol[:, :], C.ncol[:, :], reads=[C.ncol], writes=[ncol])
                load_bc(P, dtb, C.s5_log_dt[0, d].rearrange("g p -> (g p)"), C.s5_log_dt)
                load_bc(P, rho, C.s5_a_re[0, d].rearrange("g p -> (g p)"), C.s5_a_re)
                load_bc(P, th, C.s5_a_im[0, d].rearrange("g p -> (g p)"), C.s5_a_im)
                P.act(lambda: nc.scalar.activation(out=dtb[:, :], in_=dtb[:, :], func=AF.Exp), [dtb], [dtb])
                P.dve(lambda: nc.vector.tensor_tensor(out=rho[:, :], in0=rho[:, :], in1=dtb[:, :], op=ALU.mult), [rho, dtb], [rho])
                P.dve(lambda: nc.vector.tensor_tensor(out=th[:, :], in0=th[:, :], in1=dtb[:, :], op=ALU.mult), [th, dtb], [th])
                P.dve(lambda: nc.vector.tensor_scalar(out=th[:, :], in0=th[:, :], scalar1=ncol[:, d:d + 1], scalar2=None,
                                                      op0=ALU.mult), [th, ncol], [th])
                E = dtb
                tmp2 = P.sb("s5tmp2", [128, 4096])
                ki_b = P.sb("s5kib", [128, 4096], I32)
                tm3 = (tmp[:, :], tmp2[:, :], ki_b[:, :])
                sin_of(P, tabs[1][:, :], th[:, :], 0.0, tm3, [th], [tmp, tmp2, ki_b], [tabs[1]])
                sin_of(P, tabs[0][:, :], th[:, :], PI / 2, tm3, [th], [tmp, tmp2, ki_b], [tabs[0]])
                P.act(lambda: nc.scalar.activation(out=E[:, :], in_=rho[:, :], func=AF.Exp, scale=ncol[:, d:d + 1]),
                      [rho, ncol], [E])
                P.dve(lambda: nc.vector.tensor_tensor(out=tabs[2][:, :], in0=tabs[0][:, :], in1=E[:, :], op=ALU.mult),
                      [tabs[0], E], [tabs[2]])
                P.dve(lambda: nc.vector.tensor_tensor(out=tabs[3][:, :], in0=tabs[1][:, :], in1=E[:, :], op=ALU.mult),
                      [tabs[1], E], [tabs[3]])
                P.act(lambda: nc.scalar.activation(out=E[:, :], in_=rho[:, :], func=AF.Exp, scale=ncol[:, 2 + d:3 + d]),
                      [rho, ncol, tabs[2], tabs[3]], [E])
                P.dve(lambda: nc.vector.tensor_tensor(out=tabs[0][:, :], in0=tabs[0][:, :], in1=E[:, :], op=ALU.mult),
                      [tabs[0], E, tabs[2]], [tabs[0]])
                P.dve(lambda: nc.vector.scalar_tensor_tensor(out=tabs[1][:, :], in0=tabs[1][:, :], scalar=-1.0, in1=E[:, :],
                                                             op0=ALU.mult, op1=ALU.mult), [tabs[1], E, tabs[3]], [tabs[1]])
            for k in range(4):
                P.dump(f"tab{d}_{k}", tabs[k][:, :], tabs[k], [128, 4096])
            RB = P.sb("s5RB", [128, 8, 1024])
            P.pool(lambda: nc.gpsimd.memset(RB[:, :, :], 0.0), [], [RB])
            for g in range(64):
                gb, gl = g // 8, g % 8
                P.dma(RB[gl * 16:(gl + 1) * 16, gb, gl * 128:(gl + 1) * 128], C.BB[d, g], reads=[C.BB], writes=[RB])
            CM = P.sb("s5CM", [128, 1024])
            cin = [P.sb(f"s5cin{i}", [128, 128]) for i in range(2)]
            for gb in range(8):
                ci_ = cin[gb % 2]
                P.dma(ci_[:, 0:64], C.s5_c_re[0, d].rearrange("g c p -> (g c) p")[gb * 128:(gb + 1) * 128, :],
                      reads=[C.s5_c_re], writes=[ci_])
                P.dma(ci_[:, 64:128], C.s5_c_im[0, d].rearrange("g c p -> (g c) p")[gb * 128:(gb + 1) * 128, :],
                      reads=[C.s5_c_im], writes=[ci_])
                P.dve(lambda ci_=ci_: nc.vector.tensor_scalar(out=ci_[:, 64:128], in0=ci_[:, 64:128], scalar1=-1.0,
                                                              scalar2=None, op0=ALU.mult), [ci_], [ci_])
                pb = P.bank()
                P.pe(lambda ci_=ci_, pb=pb: nc.tensor.transpose(out=pb[:, 0:128], in_=ci_[:, :], identity=C.ident[:, :]),
                     [ci_, C.ident], [pb])
                P.act(lambda pb=pb, gb=gb: nc.scalar.copy(out=CM[:, gb * 128:(gb + 1) * 128], in_=pb[:, 0:128]), [pb], [CM])
            tri = P.sb("s5tri", [128, 2, 128])
            P.dma(tri[:, 0, :], C.tri[d], reads=[C.tri], writes=[tri])
            P.dma(tri[:, 1, :], C.tri[2 + d], reads=[C.tri], writes=[tri])
            dbc = P.sb("s5dbc", [128, 1024])
            load_bc(P, dbc, C.s5_d[0, :], C.s5_d)
            sbuf = [P.sb(f"s5s{i}", [128, 8192]) for i in range(2)]
            ub = [P.sb(f"s5u{i}", [128, 1024]) for i in range(1)]
            uTb = [P.sb(f"s5uT{i}", [128, 8, 128]) for i in range(1)]
            bsb = [P.sb(f"s5b{i}", [128, 1024]) for i in range(2)]
            sTb = [P.sb(f"s5sT{i}", [128, 4, 128]) for i in range(2)]
            yb = [P.sb(f"s5y{i}", [128, 1024]) for i in range(1)]
            tm = [[P.sb(f"s5tm{i}_{k}", [128, 8, 64]) for k in range(4)] for i in range(1)]
            order = list(range(T)) if d == 0 else list(range(NCT - 1, -1, -1)) + list(range(T - 1, NCT - 1, -1))
            for ci, t in enumerate(order):
                u, uT, snew, sprev, yv = ub[0], uTb[0], sbuf[ci % 2], sbuf[(ci + 1) % 2], yb[0]
                rows = slice(t * 128, (t + 1) * 128)
                P.dma(u[:, :], C.QKVU[rows, 1536:2560], reads=[C.QKVU], writes=[u])
                tr_chunks(P, C, u, 8, uT)
                it = 0
                for gb in range(8):
                    ap2, bA, bB = P.bank2()
                    for half, bk in enumerate((bA, bB)):
                        P.pe(lambda gb=gb, half=half, bk=bk, uT=uT: nc.tensor.matmul(
                            bk[:, :], lhsT=uT[:, gb, :], rhs=RB[:, gb, half * 512:(half + 1) * 512], start=True, stop=True),
                            [uT, RB], [bk])
                    bs = bsb[gb % 2]
                    P.act(lambda ap2=ap2, bs=bs: nc.scalar.copy(out=bs[:, :], in_=ap2), [bA, bB], [bs])
                    v = bs[:, :].rearrange("t (g r p) -> t g r p", g=8, r=2)
                    wv = snew[:, gb * 1024:(gb + 1) * 1024].rearrange("t (g r p) -> t g r p", g=8, r=2)
                    tr_ = tabs[0][:, gb * 512:(gb + 1) * 512].rearrange("t (g p) -> t g p", p=64)
                    ti_ = tabs[1][:, gb * 512:(gb + 1) * 512].rearrange("t (g p) -> t g p", p=64)
                    tmps = [(x[:, :, :], x) for x in tm[0]]
                    cmul(P, v[:, :, 0, :], v[:, :, 1, :], tr_, ti_, wv[:, :, 0, :], wv[:, :, 1, :], tmps,
                         [bs], [tabs[0], tabs[1]], [snew])
                for gb in range(8):
                    ap2, bA, bB = P.bank2()
                    for half, bk in enumerate((bA, bB)):
                        c0 = gb * 1024 + half * 512
                        P.pe(lambda bk=bk, c0=c0, snew=snew, first=(ci == 0): nc.tensor.matmul(
                            bk[:, :], lhsT=tri[:, 0, :], rhs=snew[:, c0:c0 + 512], start=True, stop=first),
                            [tri, snew], [bk])
                        if ci > 0:
                            P.pe(lambda bk=bk, c0=c0, sprev=sprev: nc.tensor.matmul(
                                bk[:, :], lhsT=tri[:, 1, :], rhs=sprev[:, c0:c0 + 512], start=False, stop=True),
                                [tri, sprev], [bk])
                    bs = bsb[gb % 2]
                    P.act(lambda ap2=ap2, bs=bs: nc.scalar.copy(out=bs[:, :], in_=ap2), [bA, bB], [bs])
                    v = bs[:, :].rearrange("t (g r p) -> t g r p", g=8, r=2)
                    wv = snew[:, gb * 1024:(gb + 1) * 1024].rearrange("t (g r p) -> t g r p", g=8, r=2)
                    tr_ = tabs[2][:, gb * 512:(gb + 1) * 512].rearrange("t (g p) -> t g p", p=64)
                    ti_ = tabs[3][:, gb * 512:(gb + 1) * 512].rearrange("t (g p) -> t g p", p=64)
                    tmps = [(x[:, :, :], x) for x in tm[0]]
                    cmul(P, v[:, :, 0, :], v[:, :, 1, :], tr_, ti_, wv[:, :, 0, :], wv[:, :, 1, :], tmps,
                         [bs], [tabs[2], tabs[3]], [snew])
                if ci == 0:
                    P.dump(f"s{d}", snew[:, :], snew, [128, 8192])
                    P.dump(f"CM{d}", CM[:, :], CM, [128, 1024])
                    P.dump(f"RB{d}", RB[:, :, :], RB, [128, 8, 1024])
                yap, yA, yB = P.bank2()
                P.reserved = set(P.last_pair)
                for g4 in range(16):
                    sT = sTb[g4 % 2]
                    tr_chunks(P, C, snew, 4, sT, col0=g4 * 512)
                    for j in range(4):
                        g = g4 * 4 + j
                        yk = yA if g < 32 else yB
                        P.pe(lambda sT=sT, j=j, g=g, yk=yk: nc.tensor.matmul(
                            yk[:, (g % 32) * 16:(g % 32) * 16 + 16], lhsT=sT[:, j, :], rhs=CM[:, g * 16:(g + 1) * 16],
                            start=True, stop=True), [sT, CM], [yk])
                if d == 0:
                    P.dve(lambda yv=yv, u=u: nc.vector.tensor_tensor(out=yv[:, :], in0=u[:, :], in1=dbc[:, :], op=ALU.mult),
                          [u, dbc], [yv])
                else:
                    P.dma(yv[:, :], C.Y5[rows, :], reads=[C.Y5], writes=[yv])
                P.dve(lambda yv=yv, yap=yap: nc.vector.tensor_tensor(out=yv[:, :], in0=yv[:, :], in1=yap, op=ALU.add),
                      [yv, yA, yB], [yv])
                P.reserved = set()
                P.dma(C.Y5[rows, :], yv[:, :], reads=[yv], writes=[C.Y5])


def host_s5_constants():
    i = np.arange(128)[:, None]
    j = np.arange(128)[None, :]
    tri = np.stack([(i <= j), (i >= j), np.broadcast_to(i == 127, (128, 128)), np.broadcast_to(i == 0, (128, 128))]
                   ).astype(np.float32)
    t = np.arange(128, dtype=np.float32)
    ncol = np.stack([t + 1, 128 - t, -(t + 1), -(128 - t)], axis=1).astype(np.float32)
    neg = np.float32(-30000.0)
    ssdmask = np.stack([np.where(i <= j, 0, neg), np.where(i >= j, 0, neg)]).astype(np.float32)
    return {"tri": tri, "ncol": ncol, "ssdmask": ssdmask}


def phase_outproj0(P, C, l, hsrc):
    nc = P.nc
    with P.scope():
        G = 2
        g1 = [P.sb(f"opg1{k}", [128, D]) for k in range(2)]
        for kind in range(2):
            P.dma(g1[kind][:, :], C.MOD[l, kind, :, 2 * D:3 * D], reads=[C.MOD], writes=[g1[kind]])
        gb = P.sb("opglub", [128, 1024])
        load_bc(P, gb, C.s5_glu_b[0, :], C.s5_glu_b)
        cat = [P.sb(f"opcat{i}", [128, D]) for i in range(G)]
        catT = [P.sb(f"opcatT{i}", [128, 16, 128], BF16) for i in range(G)]
        C.wbuf16 = [P.sb(f"opw16_{i}", [128, 16, 512], BF16) for i in range(2)]
        gg = [P.sb(f"opg{i}", [128, 1024]) for i in range(G)]
        gT = [P.sb(f"opgT{i}", [128, 8, 128], BF16) for i in range(G)]
        ht = [P.sb(f"oph{i}", [128, D]) for i in range(G)]
        zt = [P.sb(f"opz{i}", [128, 512]) for i in range(2)]
        C.wbuf = [P.sb(f"opw{i}", [128, 16, 512]) for i in range(2)]
        for kind, t0, nt in tile_groups(C, G):
            for i in range(nt):
                rows = slice((t0 + i) * 128, (t0 + i + 1) * 128)
                P.dma(gg[i][:, :], C.Y5[rows, :], reads=[C.Y5], writes=[gg[i]])
                P.dma(cat[i][:, 0:1024], C.ATS5[rows, 0:1024], reads=[C.ATS5], writes=[cat[i]])
                P.dma(ht[i][:, :], hsrc[rows, :], reads=[hsrc], writes=[ht[i]])
                P.act(lambda i=i: nc.scalar.activation(out=gg[i][:, :], in_=gg[i][:, :], func=AF.Gelu), [gg[i]], [gg[i]])
                tr_chunks(P, C, gg[i], 8, gT[i])

            def consume_glu(n, ti, pb, w):
                z = zt[(n + ti) % 2]
                P.dve(lambda: nc.vector.tensor_tensor(out=z[:, 0:w], in0=pb[:, 0:w], in1=gb[:, n * 512:n * 512 + w], op=ALU.add),
                      [pb, gb], [z])
                P.act(lambda: nc.scalar.activation(out=z[:, 0:w], in_=z[:, 0:w], func=AF.Sigmoid), [z], [z])
                P.dve(lambda: nc.vector.tensor_tensor(out=cat[ti][:, 1024 + n * 512:1024 + n * 512 + w],
                                                      in0=gg[ti][:, n * 512:n * 512 + w], in1=z[:, 0:w], op=ALU.mult),
                      [gg[ti], z], [cat[ti]])
            C.wsrc = C.s5_glu_w
            linear(P, C, gT[0:nt], 8, C.s5_glu_w[0], 1024, consume_glu, bf16=True)
            for i in range(nt):
                tr_chunks(P, C, cat[i], 16, catT[i])

            def consume_out(n, ti, pb, w, kind=kind):
                z = zt[(n + ti) % 2]
                P.dve(lambda: nc.vector.tensor_tensor(out=z[:, 0:w], in0=pb[:, 0:w], in1=g1[kind][:, n * 512:n * 512 + w],
                                                      op=ALU.mult), [pb, g1[kind]], [z])
                P.dve(lambda: nc.vector.tensor_tensor(out=ht[ti][:, n * 512:n * 512 + w], in0=ht[ti][:, n * 512:n * 512 + w],
                                                      in1=z[:, 0:w], op=ALU.add), [ht[ti], z], [ht[ti]])
            C.wsrc = C.ab_w_out
            linear(P, C, catT[0:nt], 16, C.ab_w_out[0], 2048, consume_out, bf16=True)
            for i in range(nt):
                rows = slice((t0 + i) * 128, (t0 + i + 1) * 128)
                P.dma(C.H[rows, :], ht[i][:, :], reads=[ht[i]], writes=[C.H])


def topk16(P, src_ap, srcres, seg2, vals, idx, vres, ires):
    nc = P.nc
    P.dve(lambda: nc.vector.max(out=vals[:, 0:8], in_=src_ap), [srcres], [vres])
    if idx is not None:
        P.dve(lambda: nc.vector.max_index(out=idx[:, 0:8], in_max=vals[:, 0:8], in_values=src_ap), [srcres, vres], [ires])
    P.dve(lambda: nc.vector.match_replace(out=seg2[:, :], in_to_replace=vals[:, 0:8], in_values=src_ap, imm_value=-1e30),
          [srcres, vres], [seg2])
    P.dve(lambda: nc.vector.max(out=vals[:, 8:16], in_=seg2[:, :]), [seg2], [vres])
    if idx is not None:
        P.dve(lambda: nc.vector.max_index(out=idx[:, 8:16], in_max=vals[:, 8:16], in_values=seg2[:, :]), [seg2, vres], [ires])


def phase_peer_convert(P, C, l):
    nc = P.nc
    with P.scope():
        fu = [P.sb(f"pcfu{i}", [128, 4, 2048]) for i in range(2)]
        fv = [P.sb(f"pcfv{i}", [128, 4, 2048]) for i in range(2)]
        bout = [P.sb(f"pcb{i}", [128, 4, 4096], BF16) for i in range(2)]
        for n in range(32):
            a, b, bo = fu[n % 2], fv[n % 2], bout[n % 2]
            P.dma(a[:, :, :], C.peer_u[l, n * 512:(n + 1) * 512, :].rearrange("(p r) d -> p r d", r=4),
                  reads=[C.peer_u], writes=[a])
            P.dma(b[:, :, :], C.peer_v[l, n * 512:(n + 1) * 512, :].rearrange("(p r) d -> p r d", r=4),
                  reads=[C.peer_v], writes=[b])
            P.dve(lambda a=a, bo=bo: nc.vector.tensor_copy(out=bo[:, :, 0:2048], in_=a[:, :, :]), [a], [bo])
            P.act(lambda b=b, bo=bo: nc.scalar.copy(out=bo[:, :, 2048:4096], in_=b[:, :, :]), [b], [bo])
            r0 = l * 16384 + n * 512
            P.dma(C.UVB[r0:r0 + 512, :].rearrange("(p r) d -> p r d", r=4), bo[:, :, :], reads=[bo], writes=[C.UVB])


def phase_peer(P, C, l, tiles, split=False):
    nc = P.nc
    phase_peer_convert(P, C, l)
    with P.scope():
        Gp = [P.sb(f"peGp{k}", [128, D]) for k in range(2)]
        Sh = [P.sb(f"peSh{k}", [128, D]) for k in range(2)]
        prep_mod(P, C, l, 3, 4, C.norm2_g[l, :], C.norm2_g, Gp, Sh)
        g2 = [P.sb(f"peg2{k}", [128, D]) for k in range(2)]
        for kind in range(2):
            P.dma(g2[kind][:, :], C.MOD[l, kind, :, 5 * D:6 * D], reads=[C.MOD], writes=[g2[kind]])
        KM = P.sb("peKM", [128, 8, 256])
        P.pool(lambda: nc.gpsimd.memset(KM[:, :, :], 0.0), [], [KM])
        kin = [P.sb(f"pekin{i}", [128, 2, 64]) for i in range(2)]
        for h in range(8):
            ki = kin[h % 2]
            P.dma(ki[:, :, :], C.peer_keys[l, h].rearrange("half k d -> k half d"), reads=[C.peer_keys], writes=[ki])
            pb = P.bank()
            P.pe(lambda ki=ki, pb=pb: nc.tensor.transpose(out=pb[:, 0:128], in_=ki[:, :, :].rearrange("k a d -> k (a d)"),
                                                         identity=C.ident[:, :]), [ki, C.ident], [pb])
            P.act(lambda pb=pb, h=h: nc.scalar.copy(out=KM[0:64, h, 0:128], in_=pb[0:64, 0:128]), [pb], [KM])
            P.act(lambda pb=pb, h=h: nc.scalar.copy(out=KM[64:128, h, 128:256], in_=pb[64:128, 0:128]), [pb], [KM])
        hb = [P.sb(f"peh{i}", [128, D]) for i in range(1)]
        fb = [P.sb(f"pef{i}", [128, D]) for i in range(1)]
        fT = P.sb("pefT", [128, 16, 128])
        q = P.sb("peq", [128, 1024])
        qT = P.sb("peqT", [128, 8, 128])
        sc = P.sb("pesc", [128, 8, 256])
        seg2 = P.sb("peseg2", [128, 256])
        s2h = P.sb("peseg2h", [128, 128])
        v12 = P.sb("pev12", [128, 8, 2, 16])
        i12 = P.sb("pei12", [128, 8, 2, 16], U32)
        i12f = P.sb("pei12f", [128, 8, 2, 16])
        cand = P.sb("pecand", [128, 8, 256])
        cidx = sc
        score = P.sb("pescore", [128, 8, 16])
        pos = P.sb("pepos", [128, 8, 16], U32)
        posf = P.sb("peposf", [128, 8, 16])
        ef = P.sb("peef", [128, 128])
        ei = P.sb("peei", [128, 128], I32)
        gate = P.sb("pegate", [128, 8, 16])
        gs = P.sb("pegs", [128, 16])
        araw = P.sb("pearaw", [128, 128])
        wgt = P.sb("pewgt", [128, 128])
        junk = P.sb("pejunk", [128, D], BF16)
        gl = P.sb("pegl", [128, 128])
        st = P.sb("pest", [128, 4])
        acc = P.sb("peacc", [128, D])
        NG = 8
        gbuf = [P.sb(f"peg{i}", [128, 2 * D], BF16) for i in range(NG)]
        if split:
            ridx = P.sb("peridx", [128, C.NL // 2], I32)
            P.dma(ridx[:, :], C.rowidx[:, :], reads=[C.rowidx], writes=[ridx])
        C.wbuf = [P.sb(f"pew{i}", [128, 16, 128]) for i in range(2)]
        gi = 0
        for it, t in enumerate(tiles):
            kind = 1 if (t < C.NCT and not split) else 0
            rows = slice(t * 128, (t + 1) * 128)
            hT, f = hb[0], fb[0]
            if split:
                P.op("pool", lambda t=t: nc.gpsimd.indirect_dma_start(
                    out=hT[:, :], out_offset=None, in_=C.H[:, :],
                    in_offset=bass.IndirectOffsetOnAxis(ap=ridx[:, t:t + 1], axis=0)), [ridx, C.H], [hT], dma=True)
            else:
                P.dma(hT[:, :], C.H[rows, :], reads=[C.H], writes=[hT])
            C.xres, C.ores = hT, f
            norm_mod(P, C, hT[:, :], f[:, :], Gp[kind], Sh[kind], junk, st)
            tr_chunks(P, C, f, 16, fT)

            def consume_q(n, ti, pb, w):
                P.act(lambda: nc.scalar.copy(out=q[:, n * 128:n * 128 + w], in_=pb[:, 0:w]), [pb], [q])
            C.wsrc = C.peer_wq
            linear(P, C, [fT], 16, C.peer_wq[l], 1024, consume_q, cw=128)
            tr_chunks(P, C, q, 8, qT)
            for h2 in range(4):
                pb = P.bank()
                for j in range(2):
                    h = h2 * 2 + j
                    P.pe(lambda pb=pb, j=j, h=h: nc.tensor.matmul(pb[:, j * 256:(j + 1) * 256], lhsT=qT[:, h, :], rhs=KM[:, h, :],
                                                                  start=True, stop=True), [qT, KM], [pb])
                P.act(lambda pb=pb, h2=h2: nc.scalar.copy(out=sc[:, 2 * h2:2 * h2 + 2, :],
                                                         in_=pb[:, :].rearrange("p (a b) -> p a b", b=256)), [pb], [sc])
            for h in range(8):
                for half in range(2):
                    topk16(P, sc[:, h, half * 128:(half + 1) * 128], sc, s2h,
                           v12[:, h, half, :], i12[:, h, half, :], v12, i12)
            P.dve(lambda: nc.vector.tensor_copy(out=i12f[:, :, :, :], in_=i12[:, :, :, :]), [i12], [i12f])
            P.dve(lambda: nc.vector.tensor_tensor(
                out=cand[:, :, :].rearrange("p h (a b) -> p h a b", b=16),
                in0=v12[:, :, 0, :].unsqueeze(3).to_broadcast([128, 8, 16, 16]),
                in1=v12[:, :, 1, :].unsqueeze(2).to_broadcast([128, 8, 16, 16]), op=ALU.add), [v12], [cand])
            P.dve(lambda: nc.vector.tensor_scalar(out=i12f[:, :, 0, :], in0=i12f[:, :, 0, :], scalar1=128.0, scalar2=None,
                                                  op0=ALU.mult), [i12f], [i12f])
            P.dve(lambda: nc.vector.tensor_tensor(
                out=cidx[:, :, :].rearrange("p h (a b) -> p h a b", b=16),
                in0=i12f[:, :, 0, :].unsqueeze(3).to_broadcast([128, 8, 16, 16]),
                in1=i12f[:, :, 1, :].unsqueeze(2).to_broadcast([128, 8, 16, 16]), op=ALU.add), [i12f], [cidx])
            for h in range(8):
                topk16(P, cand[:, h, :], cand, seg2, score[:, h, :], pos[:, h, :], score, pos)
            P.dve(lambda: nc.vector.tensor_copy(out=posf[:, :, :], in_=pos[:, :, :]), [pos], [posf])
            for h in range(8):
                for k in range(16):
                    P.dve(lambda h=h, k=k: (nc.vector.scalar_tensor_tensor(
                        out=seg2[:, :], in0=C.iota[:, :], scalar=posf[:, h, k:k + 1], in1=cidx[:, h, :],
                        op0=ALU.is_equal, op1=ALU.mult, accum_out=ef[:, h * 16 + k:h * 16 + k + 1]),
                        nc.vector.tensor_copy(out=P.scr[:, 1:2], in_=ef[:, h * 16 + k:h * 16 + k + 1]))[1],
                        [C.iota, posf, cidx], [seg2, ef])
            P.dve(lambda: nc.vector.tensor_copy(out=ei[:, :], in_=ef[:, :]), [ef], [ei])
            P.dve(lambda: nc.vector.tensor_tensor(out=gate[:, :, :], in0=score[:, :, :],
                                                  in1=score[:, :, 0:1].to_broadcast([128, 8, 16]), op=ALU.subtract), [score], [gate])
            P.act(lambda: nc.scalar.activation(out=gate[:, :, :], in_=gate[:, :, :], func=AF.Exp), [gate], [gate])
            P.dve(lambda: nc.vector.reduce_sum(out=gs[:, 0:8], in_=gate[:, :, :], axis=AX.X), [gate], [gs])
            P.dve(lambda: nc.vector.reciprocal(out=gs[:, 8:16], in_=gs[:, 0:8]), [gs], [gs])
            P.dve(lambda: nc.vector.tensor_tensor(out=gate[:, :, :], in0=gate[:, :, :],
                                                  in1=gs[:, 8:16].unsqueeze(2).to_broadcast([128, 8, 16]), op=ALU.mult), [gate, gs], [gate])
            gate2 = gate[:, :, :].rearrange("p h k -> p (h k)")
            for blk in range(32):
                gs_ = []
                for jj in range(4):
                    j = blk * 4 + jj
                    g = gbuf[gi % NG]
                    gi += 1
                    gs_.append(g)
                    P.op("pool", lambda g=g, j=j: nc.gpsimd.indirect_dma_start(
                        out=g[:, :], out_offset=None, in_=C.UVB[:, :],
                        in_offset=bass.IndirectOffsetOnAxis(ap=ei[:, j:j + 1], axis=0), element_offset=l * 16384 * 4096),
                        [ei, C.UVB], [g], dma=True)
                    P.dve(lambda g=g, j=j, f=f: (nc.vector.scalar_tensor_tensor(
                        out=junk[:, :], in0=g[:, 0:D], scalar=1.0, in1=f[:, :], op0=ALU.mult, op1=ALU.mult,
                        accum_out=araw[:, j:j + 1]), nc.vector.tensor_copy(out=P.scr[:, 1:2], in_=araw[:, j:j + 1]))[1],
                        [g, f], [junk, araw], soft=(junk, araw))
                c0, c1 = blk * 4, blk * 4 + 4
                P.act(lambda c0=c0, c1=c1: nc.scalar.activation(out=gl[:, c0:c1], in_=araw[:, c0:c1], func=AF.Gelu), [araw], [gl])
                P.dve(lambda c0=c0, c1=c1: nc.vector.tensor_tensor(out=wgt[:, c0:c1], in0=gl[:, c0:c1], in1=gate2[:, c0:c1],
                                                                   op=ALU.mult), [gl, gate], [wgt])
                for jj in range(4):
                    j = blk * 4 + jj
                    g = gs_[jj]
                    if j == 0:
                        P.dve(lambda g=g: nc.vector.tensor_scalar(out=acc[:, :], in0=g[:, D:2 * D], scalar1=wgt[:, 0:1], scalar2=None,
                                                                  op0=ALU.mult), [g, wgt], [acc])
                    else:
                        P.dve(lambda g=g, j=j: nc.vector.scalar_tensor_tensor(
                            out=acc[:, :], in0=g[:, D:2 * D], scalar=wgt[:, j:j + 1], in1=acc[:, :], op0=ALU.mult, op1=ALU.add),
                            [g, wgt, acc], [acc], soft=(acc,))
            P.dve(lambda kind=kind: nc.vector.tensor_tensor(out=acc[:, :], in0=acc[:, :], in1=g2[kind][:, :], op=ALU.mult),
                  [acc, g2[kind]], [acc])
            P.dve(lambda hT=hT: nc.vector.tensor_tensor(out=hT[:, :], in0=hT[:, :], in1=acc[:, :], op=ALU.add), [hT, acc], [hT])
            if split:
                P.dma(C.H2[rows, :], hT[:, :], reads=[hT], writes=[C.H2])
            else:
                P.dma(C.H[rows, :], hT[:, :], reads=[hT], writes=[C.H])


def seg2_half(seg2):
    return Res("seg2h", seg2.t[:, 0:128])


def phase_ssd_in(P, C, l):
    nc = P.nc
    with P.scope():
        Gp = [P.sb(f"siGp{k}", [128, D]) for k in range(2)]
        Sh = [P.sb(f"siSh{k}", [128, D]) for k in range(2)]
        prep_mod(P, C, l, 0, 1, C.norm1_g[l, :], C.norm1_g, Gp, Sh)
        G = 4
        xs = [P.sb(f"six{i}", [128, D]) for i in range(G)]
        xT = [P.sb(f"sixT{i}", [128, 16, 128], BF16) for i in range(G)]
        C.wbuf16 = [P.sb(f"siw16_{i}", [128, 16, 512], BF16) for i in range(2)]
        ob = [P.sb(f"sio{i}", [128, 512]) for i in range(4)]
        junk = P.sb("sijunk", [128, D])
        st = P.sb("sist", [128, 4])
        C.wbuf = [P.sb(f"siw{i}", [128, 16, 512]) for i in range(2)]
        cnt = [0]
        for kind, t0, nt in tile_groups(C, G):
            for i in range(nt):
                t = t0 + i
                P.dma(xs[i][:, :], C.H[t * 128:(t + 1) * 128, :], reads=[C.H], writes=[xs[i]])
                C.xres, C.ores = xs[i], xs[i]
                norm_mod(P, C, xs[i][:, :], xs[i][:, :], Gp[kind], Sh[kind], junk, st)
                tr_chunks(P, C, xs[i], 16, xT[i])

            def consume(n, ti, pb, w, t0=t0):
                o = ob[cnt[0] % 4]
                cnt[0] += 1
                t = t0 + ti
                if cnt[0] % 2:
                    P.act(lambda: nc.scalar.copy(out=o[:, 0:w], in_=pb[:, 0:w]), [pb], [o])
                else:
                    P.dve(lambda: nc.vector.tensor_copy(out=o[:, 0:w], in_=pb[:, 0:w]), [pb], [o])
                P.dma(C.SP[t * 128:(t + 1) * 128, n * 512:n * 512 + w], o[:, 0:w], reads=[o], writes=[C.SP])
            C.wsrc = C.ssd_w_in
            linear(P, C, xT[0:nt], 16, C.ssd_w_in[0], 10368, consume, bf16=True)


def phase_ssd_conv(P, C):
    nc = P.nc
    CW = 2048
    with P.scope():
        wk = P.sb("scw", [128, 5, CW])
        bias = P.sb("scb", [128, CW])
        xk = [P.sb(f"scx{k}", [128, CW]) for k in range(5)]
        for c in range(3):
            cols = slice(4096 + c * CW, 4096 + (c + 1) * CW)
            for k in range(5):
                P.dma(wk[:, k, :], C.ssd_conv_w[0, k, c * CW:(c + 1) * CW].partition_broadcast(128),
                      reads=[C.ssd_conv_w], writes=[wk])
            P.dma(bias[:, :], C.ssd_conv_b[0, c * CW:(c + 1) * CW].partition_broadcast(128), reads=[C.ssd_conv_b], writes=[bias])
            for t in range(C.T):
                s0, s1 = (0, C.NCT) if t < C.NCT else (C.NCT, C.T)
                r0 = t * 128
                for k in range(5):
                    lo, hi = r0 + k - 2, r0 + k - 2 + 128
                    vlo, vhi = max(lo, s0 * 128), min(hi, s1 * 128)
                    if vlo > lo or vhi < hi:
                        P.pool(lambda k=k: nc.gpsimd.memset(xk[k][:, :], 0.0), [], [xk[k]])
                    P.dma(xk[k][vlo - lo:vhi - lo, :], C.SP[vlo:vhi, cols], reads=[C.SP], writes=[xk[k]])
                for k in range(5):
                    if k % 2 == 0:
                        P.dve(lambda k=k: nc.vector.tensor_tensor(out=xk[k][:, :], in0=xk[k][:, :], in1=wk[:, k, :], op=ALU.mult),
                              [xk[k], wk], [xk[k]])
                    else:
                        P.pool(lambda k=k: nc.gpsimd.tensor_tensor(out=xk[k][:, :], in0=xk[k][:, :], in1=wk[:, k, :], op=ALU.mult),
                               [xk[k], wk], [xk[k]])
                P.pool(lambda: nc.gpsimd.tensor_tensor(out=xk[1][:, :], in0=xk[1][:, :], in1=xk[3][:, :], op=ALU.add),
                       [xk[1], xk[3]], [xk[1]])
                P.dve(lambda: nc.vector.tensor_tensor(out=xk[0][:, :], in0=xk[0][:, :], in1=xk[2][:, :], op=ALU.add),
                      [xk[0], xk[2]], [xk[0]])
                P.pool(lambda: nc.gpsimd.tensor_tensor(out=xk[4][:, :], in0=xk[4][:, :], in1=bias[:, :], op=ALU.add),
                       [xk[4], bias], [xk[4]])
                P.dve(lambda: nc.vector.tensor_tensor(out=xk[0][:, :], in0=xk[0][:, :], in1=xk[1][:, :], op=ALU.add),
                      [xk[0], xk[1]], [xk[0]])
                P.dve(lambda: nc.vector.tensor_tensor(out=xk[0][:, :], in0=xk[0][:, :], in1=xk[4][:, :], op=ALU.add),
                      [xk[0], xk[4]], [xk[0]])
                P.act(lambda: nc.scalar.activation(out=xk[0][:, :], in_=xk[0][:, :], func=AF.Silu), [xk[0]], [xk[0]])
                P.dma(C.XBC[r0:r0 + 128, c * CW:(c + 1) * CW], xk[0][:, :], reads=[xk[0]], writes=[C.XBC])


def ssd_pass(P, C, d):
    nc = P.nc
    NCT, NL, T = C.NCT, C.NL, C.T
    with P.scope():
        tri = P.sb("sstri", [128, 128])
        P.dma(tri[:, :], C.tri[d], reads=[C.tri], writes=[tri])
        nmask = P.sb("ssnm", [128, 128])
        P.dma(nmask[:, :], C.ssdmask[d], reads=[C.ssdmask], writes=[nmask])
        ones = P.sb("ssones", [128, 128])
        P.pool(lambda: nc.gpsimd.memset(ones[:, :], 1.0), [], [ones])
        abc = P.sb("ssa", [128, 64])
        load_bc(P, abc, C.ssd_a_log[0, d * 64:(d + 1) * 64], C.ssd_a_log)
        P.act(lambda: nc.scalar.activation(out=abc[:, :], in_=abc[:, :], func=AF.Exp), [abc], [abc])
        P.dve(lambda: nc.vector.tensor_scalar(out=abc[:, :], in0=abc[:, :], scalar1=-1.0, scalar2=None, op0=ALU.mult), [abc], [abc])
        dtb = P.sb("ssdtb", [128, 64])
        load_bc(P, dtb, C.ssd_dt_bias[0, d * 64:(d + 1) * 64], C.ssd_dt_bias)
        dsk = P.sb("ssdsk", [128, 64])
        load_bc(P, dsk, C.ssd_d[0, :], C.ssd_d)
        ST = P.sb("ssST", [128, 4096])
        P.pool(lambda: nc.gpsimd.memset(ST[:, :], 0.0), [], [ST])
        xs = P.sb("ssx", [128, 4096]); xdd = P.sb("ssxdd", [128, 4096]); yt = P.sb("ssy", [128, 4096])
        bc = P.sb("ssbc", [128, 2048])
        BT = P.sb("ssBT", [128, 8, 128]); CT = P.sb("ssCT", [128, 8, 128]); cbT = P.sb("sscbT", [128, 8, 128])
        sm = P.sb("sssm", [128, 16, 64])
        LT = [P.sb(f"ssLT{i}", [128, 128]) for i in range(4)]
        dec = [P.sb(f"ssdec{i}", [128, 128]) for i in range(4)]
        yo = [P.sb(f"ssyo{i}", [128, 512]) for i in range(2)]
        yd = [P.sb(f"ssyd{i}", [128, 512]) for i in range(2)]
        if d == 0:
            order = list(range(T))
        else:
            order = list(range(NCT - 1, -1, -1)) + list(range(T - 1, NCT - 1, -1))
        DTR, X_, LA, CS, TOT, DTE, ECS, NCS, TMP, DT, ETOT = range(11)
        S = lambda k: sm[:, k, :]
        for t in order:
            lat = t >= NCT
            rows = slice(t * 128, (t + 1) * 128)
            lrows = slice((t - NCT) * 128, (t - NCT + 1) * 128)
            P.dma(xs[:, :], C.XBC[rows, 0:4096], reads=[C.XBC], writes=[xs])
            P.dma(bc[:, :], C.XBC[rows, 4096:6144], reads=[C.XBC], writes=[bc])
            P.dma(sm[:, DTR, :], C.SP[rows, 10240 + d * 64:10240 + (d + 1) * 64], reads=[C.SP], writes=[sm])
            P.dve(lambda: nc.vector.tensor_tensor(out=S(X_), in0=S(DTR), in1=dtb[:, :], op=ALU.add), [sm, dtb], [sm])
            P.act(lambda: nc.scalar.activation(out=S(TMP), in_=S(X_), func=AF.Abs), [sm], [sm])
            P.act(lambda: nc.scalar.activation(out=S(TMP), in_=S(TMP), func=AF.Exp, scale=-1.0), [sm], [sm])
            P.act(lambda: nc.scalar.activation(out=S(TMP), in_=S(TMP), func=AF.Ln, bias=1.0), [sm], [sm])
            P.dve(lambda: nc.vector.tensor_scalar(out=S(DT), in0=S(X_), scalar1=0.0, scalar2=None, op0=ALU.max), [sm], [sm])
            P.dve(lambda: nc.vector.tensor_tensor(out=S(DT), in0=S(DT), in1=S(TMP), op=ALU.add), [sm], [sm])
            P.dve(lambda: nc.vector.tensor_tensor(out=S(LA), in0=S(DT), in1=abc[:, :], op=ALU.mult), [sm, abc], [sm])
            pb = P.bank()
            P.pe(lambda pb=pb: nc.tensor.matmul(pb[:, 0:64], lhsT=tri[:, :], rhs=S(LA), start=True, stop=True), [tri, sm], [pb])
            P.pe(lambda pb=pb: nc.tensor.matmul(pb[:, 64:128], lhsT=ones[:, :], rhs=S(LA), start=True, stop=True), [ones, sm], [pb])
            P.act(lambda pb=pb: nc.scalar.copy(out=sm[:, CS:CS + 2, :], in_=pb[:, 0:128].rearrange("p (a b) -> p a b", b=64)),
                  [pb], [sm])
            P.dve(lambda: nc.vector.tensor_tensor(out=S(DTE), in0=S(TOT), in1=S(CS), op=ALU.subtract), [sm], [sm])
            P.act(lambda: nc.scalar.activation(out=S(DTE), in_=S(DTE), func=AF.Exp), [sm], [sm])
            P.act(lambda: nc.scalar.activation(out=S(ECS), in_=S(CS), func=AF.Exp), [sm], [sm])
            P.act(lambda: nc.scalar.activation(out=S(ETOT), in_=S(TOT), func=AF.Exp), [sm], [sm])
            P.dve(lambda: nc.vector.tensor_scalar(out=S(NCS), in0=S(CS), scalar1=-1.0, scalar2=None, op0=ALU.mult), [sm], [sm])
            x3 = xs[:, :].rearrange("p (h q) -> p h q", q=64)
            if lat:
                if d == 0:
                    P.pool(lambda: nc.gpsimd.tensor_tensor(out=yt[:, :].rearrange("p (h q) -> p h q", q=64), in0=x3,
                                                           in1=dsk[:, :].unsqueeze(2).to_broadcast([128, 64, 64]), op=ALU.mult),
                           [xs, dsk], [yt])
                else:
                    P.dma(yt[:, :], C.YS[lrows, :], reads=[C.YS], writes=[yt])
            P.dve(lambda: nc.vector.tensor_tensor(out=x3, in0=x3, in1=S(DT).unsqueeze(2).to_broadcast([128, 64, 64]), op=ALU.mult),
                  [xs, sm], [xs])
            P.dve(lambda: nc.vector.tensor_tensor(out=xdd[:, :].rearrange("p (h q) -> p h q", q=64), in0=x3,
                                                  in1=S(DTE).unsqueeze(2).to_broadcast([128, 64, 64]), op=ALU.mult),
                  [xs, sm], [xdd])
            if lat:
                tr_chunks(P, C, bc, 8, BT, col0=0)
                tr_chunks(P, C, bc, 8, CT, col0=1024)
                for g in range(8):
                    pb = P.bank()
                    P.pe(lambda pb=pb, g=g: nc.tensor.matmul(pb[:, 0:128], lhsT=BT[:, g, :], rhs=CT[:, g, :], start=True, stop=True),
                         [BT, CT], [pb])
                    P.act(lambda pb=pb, g=g: nc.scalar.copy(out=cbT[:, g, :], in_=pb[:, 0:128]), [pb], [cbT])
                for g in range(8):
                    pbo = P.bank()
                    P.pe(lambda pbo=pbo, g=g: nc.tensor.matmul(pbo[:, :], lhsT=CT[:, g, :], rhs=ST[:, g * 512:(g + 1) * 512],
                                                               start=True, stop=True), [CT, ST], [pbo])
                    yo_ = yo[g % 2]
                    P.dve(lambda pbo=pbo, yo_=yo_, g=g: nc.vector.tensor_tensor(
                        out=yo_[:, :].rearrange("p (h q) -> p h q", q=64), in0=pbo[:, :].rearrange("p (h q) -> p h q", q=64),
                        in1=sm[:, ECS, g * 8:(g + 1) * 8].unsqueeze(2).to_broadcast([128, 8, 64]), op=ALU.mult), [pbo, sm], [yo_])
                    pby = P.bank()
                    P.reserved = {(P.psn - 1) % 8}
                    for j in range(8):
                        h = g * 8 + j
                        lt, dc = LT[h % 4], dec[h % 4]
                        if h % 2:
                            P.pool(lambda lt=lt, h=h: nc.gpsimd.tensor_scalar(out=lt[:, :], in0=tri[:, :], scalar1=sm[:, LA, h:h + 1],
                                                                             scalar2=None, op0=ALU.mult), [tri, sm], [lt])
                        else:
                            P.act(lambda lt=lt, h=h: nc.scalar.activation(out=lt[:, :], in_=tri[:, :], func=AF.Copy,
                                                                          scale=sm[:, LA, h:h + 1]), [tri, sm], [lt])
                        pbd = P.bank()
                        P.pe(lambda pbd=pbd, lt=lt: nc.tensor.matmul(pbd[:, 0:128], lhsT=ones[:, :], rhs=lt[:, :], start=True, stop=False),
                             [ones, lt], [pbd])
                        P.pe(lambda pbd=pbd: nc.tensor.matmul(pbd[:, 0:128], lhsT=C.ident[:, :], rhs=nmask[:, :], start=False, stop=True),
                             [C.ident, nmask], [pbd])
                        P.act(lambda pbd=pbd, dc=dc, h=h: nc.scalar.activation(out=dc[:, :], in_=pbd[:, 0:128], func=AF.Exp,
                                                                                bias=sm[:, NCS, h:h + 1], scale=1.0), [pbd, sm], [dc])
                        P.dve(lambda dc=dc, g=g: nc.vector.tensor_tensor(out=dc[:, :], in0=dc[:, :], in1=cbT[:, g, :], op=ALU.mult),
                              [dc, cbT], [dc])
                        P.pe(lambda pby=pby, dc=dc, j=j, h=h: nc.tensor.matmul(pby[:, j * 64:(j + 1) * 64], lhsT=dc[:, :],
                                                                               rhs=xs[:, h * 64:(h + 1) * 64], start=True, stop=True),
                             [dc, xs], [pby])
                    P.reserved = set()
                    yd_ = yd[g % 2]
                    P.act(lambda pby=pby, yd_=yd_: nc.scalar.copy(out=yd_[:, :], in_=pby[:, :]), [pby], [yd_])
                    P.dve(lambda yo_=yo_, yd_=yd_: nc.vector.tensor_tensor(out=yo_[:, :], in0=yo_[:, :], in1=yd_[:, :], op=ALU.add),
                          [yo_, yd_], [yo_])
                    P.pool(lambda yo_=yo_, g=g: nc.gpsimd.tensor_tensor(out=yt[:, g * 512:(g + 1) * 512], in0=yt[:, g * 512:(g + 1) * 512],
                                                                      in1=yo_[:, :], op=ALU.add), [yo_, yt], [yt])
                P.dma(C.YS[lrows, :], yt[:, :], reads=[yt], writes=[C.YS])
                if t == NCT:
                    P.dump(f"ssm{d}", sm[:, :, :], sm, [128, 16, 64])
                    P.dump(f"scbT{d}", cbT[:, :, :], cbT, [128, 8, 128])
                    P.dump(f"sdec{d}", dec[3][:, :], dec[3], [128, 128])
                    P.dump(f"sST{d}", ST[:, :], ST, [128, 4096])
                    P.dump(f"sxs{d}", xs[:, :], xs, [128, 4096])
                    P.dump(f"syd{d}", yd[1][:, :], yd[1], [128, 512])
                    P.dump(f"syo{d}", yo[1][:, :], yo[1], [128, 512])
            P.dve(lambda: nc.vector.tensor_tensor(out=ST[:, :].rearrange("p (h q) -> p h q", q=64),
                                                  in0=ST[:, :].rearrange("p (h q) -> p h q", q=64),
                                                  in1=S(ETOT).unsqueeze(2).to_broadcast([128, 64, 64]), op=ALU.mult), [ST, sm], [ST])
            for g in range(8):
                pbs = P.bank()
                P.pe(lambda pbs=pbs, g=g: nc.tensor.matmul(pbs[:, :], lhsT=bc[:, g * 128:(g + 1) * 128], rhs=xdd[:, g * 512:(g + 1) * 512],
                                                           start=True, stop=True), [bc, xdd], [pbs])
                P.dve(lambda pbs=pbs, g=g: nc.vector.tensor_tensor(out=ST[:, g * 512:(g + 1) * 512], in0=ST[:, g * 512:(g + 1) * 512],
                                                                   in1=pbs[:, :], op=ALU.add), [pbs, ST], [ST])


def phase_ssd_out(P, C, l):
    nc = P.nc
    with P.scope():
        g1 = P.sb("sog1", [128, D])
        P.dma(g1[:, :], C.MOD[l, 0, :, 2 * D:3 * D], reads=[C.MOD], writes=[g1])
        ng = P.sb("song", [128, 4096])
        load_bc(P, ng, C.ssd_norm_g[0, :], C.ssd_norm_g)
        G = 1
        yb = [P.sb(f"soy{i}", [128, 4096]) for i in range(G)]
        zb = [P.sb(f"soz{i}", [128, 4096]) for i in range(G)]
        yT = [P.sb(f"soyT{i}", [128, 32, 128], BF16) for i in range(G)]
        C.wbuf16 = [P.sb(f"sow16_{i}", [128, 32, 256], BF16) for i in range(2)]
        ht = [P.sb(f"soh{i}", [128, D]) for i in range(G)]
        zt = [P.sb(f"sozt{i}", [128, 256]) for i in range(2)]
        st = P.sb("sost", [128, 4, 8])
        junk = P.sb("sojunk", [128, 512])
        C.wbuf = [P.sb(f"sow{i}", [128, 32, 256]) for i in range(2)]
        for t0 in range(0, C.NL, G):
            nt = min(G, C.NL - t0)
            for i in range(nt):
                lrows = slice((t0 + i) * 128, (t0 + i + 1) * 128)
                rows = slice((C.NCT + t0 + i) * 128, (C.NCT + t0 + i + 1) * 128)
                y, z = yb[i], zb[i]
                P.dma(y[:, :], C.YS[lrows, :], reads=[C.YS], writes=[y])
                P.dma(z[:, :], C.SP[rows, 0:4096], reads=[C.SP], writes=[z])
                P.dma(ht[i][:, :], C.H[rows, :], reads=[C.H], writes=[ht[i]])
                P.act(lambda z=z: nc.scalar.activation(out=z[:, :], in_=z[:, :], func=AF.Silu), [z], [z])
                P.dve(lambda y=y, z=z: nc.vector.tensor_tensor(out=y[:, :], in0=y[:, :], in1=z[:, :], op=ALU.mult), [y, z], [y])
                for g in range(8):
                    P.act(lambda y=y, g=g: (nc.scalar.activation(out=junk[:, :], in_=y[:, g * 512:(g + 1) * 512], func=AF.Square,
                                                                 accum_out=st[:, 0, g:g + 1]),
                                            nc.scalar.copy(out=P.scr[:, 0:1], in_=st[:, 0, g:g + 1]))[1], [y], [junk, st])
                P.dve(lambda: nc.vector.tensor_scalar(out=st[:, 1, :], in0=st[:, 0, :], scalar1=1.0 / 512, scalar2=EPS,
                                                      op0=ALU.mult, op1=ALU.add), [st], [st])
                P.act(lambda: nc.scalar.activation(out=st[:, 2, :], in_=st[:, 1, :], func=AF.Sqrt), [st], [st])
                P.dve(lambda: nc.vector.reciprocal(out=st[:, 3, :], in_=st[:, 2, :]), [st], [st])
                P.dve(lambda y=y: nc.vector.tensor_tensor(out=y[:, :].rearrange("p (g q) -> p g q", q=512),
                                                          in0=y[:, :].rearrange("p (g q) -> p g q", q=512),
                                                          in1=st[:, 3, :].unsqueeze(2).to_broadcast([128, 8, 512]), op=ALU.mult),
                      [y, st], [y])
                P.pool(lambda y=y: nc.gpsimd.tensor_tensor(out=y[:, :], in0=y[:, :], in1=ng[:, :], op=ALU.mult), [y, ng], [y])
                tr_chunks(P, C, y, 32, yT[i])

            def consume_out(n, ti, pb, w):
                z = zt[(n + ti) % 2]
                P.dve(lambda: nc.vector.tensor_tensor(out=z[:, 0:w], in0=pb[:, 0:w], in1=g1[:, n * 256:n * 256 + w], op=ALU.mult),
                      [pb, g1], [z])
                P.dve(lambda: nc.vector.tensor_tensor(out=ht[ti][:, n * 256:n * 256 + w], in0=ht[ti][:, n * 256:n * 256 + w],
                                                      in1=z[:, 0:w], op=ALU.add), [ht[ti], z], [ht[ti]])
            C.wsrc = C.ssd_w_out
            linear(P, C, yT[0:nt], 32, C.ssd_w_out[0], 2048, consume_out, cw=256, bf16=True)
            for i in range(nt):
                rows = slice((C.NCT + t0 + i) * 128, (C.NCT + t0 + i + 1) * 128)
                P.dma(C.H[rows, :], ht[i][:, :], reads=[ht[i]], writes=[C.H])


def phase_final(P, C):
    nc = P.nc
    with P.scope():
        g = P.sb("fng", [128, D])
        load_bc(P, g, C.final_norm_g[:], C.final_norm_g)
        xb = [P.sb(f"fnx{i}", [128, D]) for i in range(2)]
        junk = P.sb("fnjunk", [128, D])
        stb = [P.sb(f"fnst{i}", [128, 4]) for i in range(2)]
        for t in range(C.NO):
            x, st = xb[t % 2], stb[t % 2]
            rows = slice(t * 128, (t + 1) * 128)
            if SPLIT:
                P.dma(x[:, :], C.H2[rows, :], reads=[C.H2], writes=[x])
            else:
                P.dma(x[:, :], C.H[(C.NCT + t) * 128:(C.NCT + t + 1) * 128, :], reads=[C.H], writes=[x])
            P.act(lambda x=x, st=st: (nc.scalar.activation(out=junk[:, :], in_=x[:, :], func=AF.Square, accum_out=st[:, 0:1]),
                                      nc.scalar.copy(out=P.scr[:, 0:1], in_=st[:, 0:1]))[1], [x], [junk, st])
            P.dve(lambda st=st: nc.vector.tensor_scalar(out=st[:, 1:2], in0=st[:, 0:1], scalar1=1.0 / D, scalar2=EPS,
                                                        op0=ALU.mult, op1=ALU.add), [st], [st])
            P.act(lambda st=st: nc.scalar.activation(out=st[:, 3:4], in_=st[:, 1:2], func=AF.Sqrt), [st], [st])
            P.dve(lambda st=st: nc.vector.reciprocal(out=st[:, 2:3], in_=st[:, 3:4]), [st], [st])
            P.dve(lambda x=x, st=st: nc.vector.scalar_tensor_tensor(out=x[:, :], in0=x[:, :], scalar=st[:, 2:3], in1=g[:, :],
                                                                    op0=ALU.mult, op1=ALU.mult), [x, st, g], [x])
            P.dma(C.OUT[t * 128:(t + 1) * 128, :], x[:, :], reads=[x], writes=[C.OUT])


N_CORES = 8
_CACHE = {}


_DBG = {}


def run_model(inputs, NL, NCT, batches, n_cores=None):
    inputs = {k: np.asarray(v) for k, v in inputs.items()}
    P = Prog()
    C = build_program(P, NL, NCT, stage="final")
    P.finish()
    used = set(C.used_inputs)
    shared = make_inputs(inputs, 0, NL, NCT, used=used)
    nb = len(batches)
    n_cores = n_cores or 2 * nb
    NLH = NL // 2
    maps = []
    for core in range(n_cores):
        b, half = batches[core % nb], core // nb
        m = dict(shared)
        m["h0"] = np.ascontiguousarray(np.concatenate([inputs["ctx"][b][:NCT * 128], inputs["x"][b][:NL * 128]], axis=0),
                                       dtype=np.float32)
        m["c_col"] = np.ascontiguousarray(inputs["c"][b].reshape(16, 128).T, dtype=np.float32)
        if "rowidx" in used:
            k = np.arange(NLH, dtype=np.int64)[None, :]
            p = np.arange(128, dtype=np.int64)[:, None]
            m["rowidx"] = np.ascontiguousarray(((NCT + half * NLH + k) * 128 + p).astype(np.int32))
        maps.append(m)
    res = run_bass_kernel_spmd(P.nc, maps, core_ids=list(range(n_cores)))
    out = np.zeros((nb, NL * 128, 2048), np.float32)
    for core in range(n_cores):
        bi, half = core % nb, core // nb
        if SPLIT:
            out[bi, half * NLH * 128:(half + 1) * NLH * 128] = np.asarray(res.results[core]["OUT"])
        elif half == 0:
            out[bi] = np.asarray(res.results[core]["OUT"])
    return out


def kernel(**inputs):
    return run_model(inputs, 32, 2, [0, 1, 2, 3], n_cores=N_CORES)
```

```python
from contextlib import ExitStack
import numpy as np
import concourse.bass as bass
import concourse.mybir as mybir
from concourse.bass_utils import run_bass_kernel_spmd

F32 = mybir.dt.float32
BF16 = mybir.dt.bfloat16
I32 = mybir.dt.int32
U32 = mybir.dt.uint32
AF = mybir.ActivationFunctionType
ALU = mybir.AluOpType
AX = mybir.AxisListType

EPOCH = 30000


class Res:
    __slots__ = ("name", "lw", "rd", "t")

    def __init__(self, name, t=None):
        self.name = name
        self.lw = None
        self.rd = []
        self.t = t

    def __getitem__(self, k):
        return self.t[k]


class Prog:
    ENGS = ("pe", "act", "dve", "pool", "sp")
    NSLOT = {"sp": 24, "act": 6, "pool": 12}

    def __init__(self):
        self.nc = bass.Bass("TRN2", target_bir_lowering=False)
        self.es = ExitStack()
        self.ops = []
        self.psbanks = None
        self.psn = 0
        self.reserved = set()
        self.scopes = [self.es]
        self.last_c = {}
        self.slot_next = {q: 0 for q in self.NSLOT}
        self.slot_last = {q: [None] * k for q, k in self.NSLOT.items()}
        self.pending = {e: set() for e in self.ENGS}

    def sb(self, name, shape, dt=F32):
        self.uid = getattr(self, "uid", 0) + 1
        name = f"{name}_{self.uid}"
        t = self.scopes[-1].enter_context(self.nc.sbuf_tensor(name, list(shape), dt))
        return Res(name, t)

    def scope(self):
        prog = self

        class _S:
            def __enter__(s2):
                prog.scopes.append(ExitStack())

            def __exit__(s2, *a):
                prog.scopes.pop().close()
                prog.barrier()
        return _S()

    def barrier(self):
        s = set(self.last_c.values())
        for q in self.NSLOT:
            s.update(x for x in self.slot_last[q] if x is not None)
        for e in self.ENGS:
            self.pending[e] = set(s)

    def ps(self, name, shape, dt=F32):
        t = self.es.enter_context(self.nc.psum_tensor(name, list(shape), dt))
        return Res(name, t)

    def bank(self):
        if self.psbanks is None:
            self.psall = self.ps("psall", [128, 8, 512], F32)
            self.psbanks = [Res(f"psb{i}", self.psall.t[:, i, :]) for i in range(8)]
        while self.psn % 8 in self.reserved:
            self.psn += 1
        b = self.psbanks[self.psn % 8]
        self.psn += 1
        return b

    def bank2(self):
        self.bank()
        self.psn -= 1
        while self.psn % 2 or (self.psn % 8) in self.reserved or (self.psn % 8 + 1) in self.reserved:
            self.psn += 1
        i = self.psn % 8
        self.psn += 2
        self.last_pair = (i, i + 1)
        return self.psall.t[:, i:i + 2, :].rearrange("p a b -> p (a b)"), self.psbanks[i], self.psbanks[i + 1]

    def dram(self, name, shape, dt=F32, kind="Internal"):
        t = self.nc.dram_tensor(name, list(shape), dt, kind=kind)
        return Res(name, t.ap())

    def op(self, eng, fn, reads=(), writes=(), dma=False, soft=()):
        idx = len(self.ops)
        deps = set()
        softdeps = set()
        for r in reads:
            if r.lw is not None:
                (softdeps if r in soft else deps).add(r.lw)
        for w in writes:
            tgt = softdeps if w in soft else deps
            if w.lw is not None:
                tgt.add(w.lw)
            tgt.update(w.rd)
        for d in softdeps:
            if self.ops[d][0] != eng or self.ops[d][3] or dma:
                deps.add(d)
        for r in reads:
            r.rd.append(idx)
        for w in writes:
            w.lw = idx
            w.rd = []
        if self.pending[eng]:
            deps.update(self.pending[eng])
            self.pending[eng] = set()
        slot = None
        if dma:
            slot = self.slot_next[eng]
            self.slot_next[eng] = (slot + 1) % self.NSLOT[eng]
            prev = self.slot_last[eng][slot]
            if prev is not None:
                deps.add(prev)
            self.slot_last[eng][slot] = idx
        else:
            self.last_c[eng] = idx
        self.ops.append([eng, fn, deps, dma, slot])
        return idx

    def pe(self, fn, reads=(), writes=()):
        return self.op("pe", fn, reads, writes)

    def act(self, fn, reads=(), writes=()):
        return self.op("act", fn, reads, writes)

    def dve(self, fn, reads=(), writes=(), soft=()):
        return self.op("dve", fn, reads, writes, soft=soft)

    def pool(self, fn, reads=(), writes=()):
        return self.op("pool", fn, reads, writes)

    def dma(self, out, in_, reads=(), writes=(), q="sp", **kw):
        nc = self.nc
        e = {"sp": nc.sync, "act": nc.scalar, "pool": nc.gpsimd}[q]
        return self.op(q, lambda: e.dma_start(out=out, in_=in_, **kw), reads, writes, dma=True)

    def dump(self, name, ap, res, shape):
        if not getattr(self, "debug", False):
            return
        d = self.dram("DBG_" + name, shape, F32, "ExternalOutput")
        self.dma(d[tuple(slice(None) for _ in shape)], ap, reads=[res], writes=[d])
        self.dbg = getattr(self, "dbg", []) + [d]

    def final(self, resources):
        nc = self.nc
        self.op("sp", lambda: nc.sync.nop(), reads=list(resources), writes=[])

    def finish(self):
        nc = self.nc
        ops = self.ops
        n = len(ops)

        def skip(d, i):
            return ops[d][0] == "pe" and ops[i][0] == "pe" and not ops[d][3] and not ops[i][3]

        needed = [False] * n
        for i, (eng, fn, deps, dma, slot) in enumerate(ops):
            for d in deps:
                if not skip(d, i):
                    needed[d] = True
        cnt = {e: 0 for e in self.ENGS}
        slot_cnt = {q: [0] * k for q, k in self.NSLOT.items()}
        slot_ep = {q: [0] * k for q, k in self.NSLOT.items()}
        ticket = [None] * n
        for i, (eng, fn, deps, dma, s) in enumerate(ops):
            if dma:
                if slot_cnt[eng][s] + 16 > EPOCH:
                    slot_cnt[eng][s] = 0
                    slot_ep[eng][s] += 1
                slot_cnt[eng][s] += 16
                ticket[i] = (("d", eng, s, slot_ep[eng][s]), slot_cnt[eng][s])
            elif needed[i]:
                cnt[eng] += 1
                ep = (cnt[eng] - 1) // EPOCH
                ticket[i] = (("c", eng, ep), cnt[eng] - ep * EPOCH)
        semkeys = sorted({t[0] for t in ticket if t is not None}, key=str)
        sems = {}
        for k in semkeys:
            sems[k] = self.es.enter_context(nc.semaphore("s_" + "_".join(str(x) for x in k)))
        self.nsem = len(sems)
        per_eng = {e: [] for e in self.ENGS}
        for i, o in enumerate(ops):
            per_eng[o[0]].append(i)
        engobj = {"pe": nc.tensor, "act": nc.scalar, "dve": nc.vector, "pool": nc.gpsimd, "sp": nc.sync}

        def emit(ename):
            e = engobj[ename]
            waited = {}
            for i in per_eng[ename]:
                eng, fn, deps, dma, slot = ops[i]
                dl = deps
                need = {}
                for d in dl:
                    if skip(d, i):
                        continue
                    k, v = ticket[d]
                    if need.get(k, 0) < v:
                        need[k] = v
                for k, v in sorted(need.items(), key=str):
                    if waited.get(k, 0) >= v:
                        continue
                    if any(kk[0] == k[0] and kk[1:-1] == k[1:-1] and kk[-1] > k[-1] for kk in waited):
                        continue
                    e.wait_ge(sems[k], v)
                    waited[k] = v
                ins = fn()
                if ticket[i] is not None:
                    k, v = ticket[i]
                    ins.then_inc(sems[k], 16 if dma else 1)

        with nc.Block() as block:
            @block.tensor
            def _(eng):
                emit("pe")

            @block.scalar
            def _(eng):
                emit("act")

            @block.vector
            def _(eng):
                emit("dve")

            @block.gpsimd
            def _(eng):
                emit("pool")

            @block.sync
            def _(eng):
                emit("sp")
        self.es.close()
        return nc


def phase_mod(P, depth, c_col, cc_col, mod_w, mod_b, MOD):
    nc = P.nc
    cs = P.sb("mod_cs", [128, 32])
    cs2 = P.sb("mod_cs2", [128, 32])
    rep = P.sb("mod_rep", [128, 32, 128])
    P.dma(cs[:, 0:16], c_col[:, :], reads=[c_col], writes=[cs])
    P.dma(cs[:, 16:32], cc_col[:, :], reads=[cc_col], writes=[cs])
    P.act(lambda: nc.scalar.activation(out=cs2[:, :], in_=cs[:, :], func=AF.Silu), [cs], [cs2])
    for kc in range(32):
        P.dve(lambda kc=kc: nc.vector.tensor_copy(out=rep[:, kc, :], in_=cs2[:, kc:kc + 1].to_broadcast([128, 128])),
              [cs2], [rep])
    wb = [P.sb(f"mod_w{i}", [128, 16, 512]) for i in range(2)]
    bb = [P.sb(f"mod_b{i}", [128, 512]) for i in range(2)]
    ob = [P.sb(f"mod_o{i}", [128, 512]) for i in range(4)]
    it = 0
    for l in range(depth):
        for n in range(24):
            wt, bt = wb[it % 2], bb[it % 2]
            P.dma(wt[:, :, :], mod_w[l, :, n * 512:(n + 1) * 512].rearrange("(kc p) n -> p kc n", p=128),
                  reads=[mod_w], writes=[wt])
            P.dma(bt[:, :], mod_b[l, n * 512:(n + 1) * 512].partition_broadcast(128), reads=[mod_b], writes=[bt])
            for kind in range(2):
                pb = P.bank()
                for kc in range(16):
                    P.pe(lambda kc=kc, kind=kind, pb=pb, wt=wt: nc.tensor.matmul(
                        pb[:, :], lhsT=rep[:, kind * 16 + kc, :], rhs=wt[:, kc, :], start=(kc == 0), stop=(kc == 15)),
                        [rep, wt], [pb])
                ot = ob[(it * 2 + kind) % 4]
                P.dve(lambda pb=pb, ot=ot, bt=bt: nc.vector.tensor_tensor(out=ot[:, :], in0=pb[:, :], in1=bt[:, :], op=ALU.add),
                      [pb, bt], [ot])
                P.dma(MOD[l, kind, :, n * 512:(n + 1) * 512], ot[:, :], reads=[ot], writes=[MOD])
            it += 1


D = 2048
EPS = 1e-6


class Ctx:
    pass


def tr_chunks(P, C, src, n, dst, col0=0, rows=128, cw=128):
    nc = P.nc
    per = 512 // rows if rows >= 128 else 4
    j0 = 0
    while j0 < n:
        nb = min(per, n - j0)
        pb = P.bank()
        for j in range(nb):
            P.pe(lambda j=j, j0=j0, pb=pb: nc.tensor.transpose(
                out=pb[0:cw, j * rows:(j + 1) * rows], in_=src[0:rows, col0 + (j0 + j) * cw: col0 + (j0 + j + 1) * cw],
                identity=C.ident[0:rows, 0:rows]), [src, C.ident], [pb])
        C.ev ^= 1
        o = dst[0:cw, j0:j0 + nb, 0:rows]
        i = pb[0:cw, 0:nb * rows].rearrange("p (j t) -> p j t", t=rows)
        if C.ev:
            P.act(lambda o=o, i=i: nc.scalar.copy(out=o, in_=i), [pb], [dst])
        else:
            P.dve(lambda o=o, i=i: nc.vector.tensor_copy(out=o, in_=i), [pb], [dst])
        j0 += nb


def linear(P, C, xT, nk, W, N, consume, cw=512, wtag="lw", bf16=False):
    nc = P.nc
    wb = C.wbuf
    nch = (N + cw - 1) // cw
    for n in range(nch):
        w = min(cw, N - n * cw)
        wt = wb[C.wi % len(wb)]
        C.wi += 1
        P.dma(wt[:, 0:nk, 0:w], W[:, n * cw:n * cw + w].rearrange("(kc p) n -> p kc n", p=128),
              reads=[C.wsrc], writes=[wt])
        if bf16:
            w16 = C.wbuf16[C.wi % len(C.wbuf16)]
            P.act(lambda wt=wt, w16=w16, w=w: nc.scalar.copy(out=w16[:, 0:nk, 0:w], in_=wt[:, 0:nk, 0:w]), [wt], [w16])
            wt = w16
        for t, xt in enumerate(xT):
            pb = P.bank()
            for kc in range(nk):
                P.pe(lambda kc=kc, pb=pb, xt=xt, wt=wt, w=w: nc.tensor.matmul(
                    pb[:, 0:w], lhsT=xt[:, kc, :], rhs=wt[:, kc, 0:w], start=(kc == 0), stop=(kc == nk - 1)),
                    [xt, wt], [pb])
            consume(n, t, pb, w)


def load_bc(P, dst, src_ap, srcres):
    P.dma(dst[:, :], src_ap.partition_broadcast(128), reads=[srcres], writes=[dst])


def prep_mod(P, C, l, j_shift, j_scale, g_ap, gres, Gp, Sh):
    nc = P.nc
    with P.scope():
        gt = P.sb("pm_g", [128, D])
        load_bc(P, gt, g_ap, gres)
        for kind in range(2):
            P.dma(Sh[kind][:, :], C.MOD[l, kind, :, j_shift * D:(j_shift + 1) * D], reads=[C.MOD], writes=[Sh[kind]])
            P.dma(Gp[kind][:, :], C.MOD[l, kind, :, j_scale * D:(j_scale + 1) * D], reads=[C.MOD], writes=[Gp[kind]])
            P.dve(lambda kind=kind: nc.vector.scalar_tensor_tensor(
                out=Gp[kind][:, :], in0=Gp[kind][:, :], scalar=1.0, in1=gt[:, :], op0=ALU.add, op1=ALU.mult),
                [Gp[kind], gt], [Gp[kind]])


def norm_mod(P, C, x, out, Gp, Sh, junk, st):
    nc = P.nc
    P.act(lambda: (nc.scalar.activation(out=junk[:, :], in_=x, func=AF.Square, accum_out=st[:, 0:1]),
                   nc.scalar.copy(out=P.scr[:, 0:1], in_=st[:, 0:1]))[1], [C.xres], [junk, st])
    P.dve(lambda: nc.vector.tensor_scalar(out=st[:, 1:2], in0=st[:, 0:1], scalar1=1.0 / D, scalar2=EPS,
                                          op0=ALU.mult, op1=ALU.add), [st], [st])
    P.act(lambda: nc.scalar.activation(out=st[:, 3:4], in_=st[:, 1:2], func=AF.Sqrt), [st], [st])
    P.dve(lambda: nc.vector.reciprocal(out=st[:, 2:3], in_=st[:, 3:4]), [st], [st])
    P.dve(lambda: nc.vector.scalar_tensor_tensor(out=out, in0=x, scalar=st[:, 2:3], in1=Gp[:, :],
                                                 op0=ALU.mult, op1=ALU.mult), [C.xres, st, Gp], [C.ores])
    P.dve(lambda: nc.vector.tensor_tensor(out=out, in0=out, in1=Sh[:, :], op=ALU.add), [C.ores, Sh], [C.ores])


def tile_groups(C, G=2):
    gs = []
    for t0 in range(0, C.NCT, G):
        gs.append((1, t0, min(G, C.NCT - t0)))
    for t0 in range(0, C.NL, G):
        gs.append((0, C.NCT + t0, min(G, C.NL - t0)))
    return gs


def phase_inproj0(P, C, l, hsrc):
    nc = P.nc
    with P.scope():
        Gp = [P.sb(f"ipGp{k}", [128, D]) for k in range(2)]
        Sh = [P.sb(f"ipSh{k}", [128, D]) for k in range(2)]
        prep_mod(P, C, l, 0, 1, C.norm1_g[l, :], C.norm1_g, Gp, Sh)
        G = 2
        xs = [P.sb(f"ipx{i}", [128, D]) for i in range(G)]
        xT = [P.sb(f"ipxT{i}", [128, 16, 128], BF16) for i in range(G)]
        C.wbuf16 = [P.sb(f"ipw16_{i}", [128, 16, 512], BF16) for i in range(2)]
        os_ = [P.sb(f"ipo{i}", [128, 2560]) for i in range(G)]
        junk = P.sb("ipjunk", [128, D])
        st = P.sb("ipst", [128, 4])
        cs = P.sb("ipcs", [128, 2, 64])
        tmp = P.sb("iptmp", [128, 4, 10, 64])
        C.wbuf = [P.sb(f"ipw{i}", [128, 16, 512]) for i in range(2)]
        for kind, t0, nt in tile_groups(C, G):
            for i in range(nt):
                t = t0 + i
                P.dma(xs[i][:, :], hsrc[t * 128:(t + 1) * 128, :], reads=[hsrc], writes=[xs[i]])
                C.xres, C.ores = xs[i], xs[i]
                norm_mod(P, C, xs[i][:, :], xs[i][:, :], Gp[kind], Sh[kind], junk, st)
                tr_chunks(P, C, xs[i], 16, xT[i])

            def consume(n, ti, pb, w):
                o = os_[ti]
                C.ev ^= 1
                if C.ev:
                    P.act(lambda: nc.scalar.copy(out=o[:, n * 512:n * 512 + w], in_=pb[:, 0:w]), [pb], [o])
                else:
                    P.dve(lambda: nc.vector.tensor_copy(out=o[:, n * 512:n * 512 + w], in_=pb[:, 0:w]), [pb], [o])
            C.wsrc = C.ab_w_in
            linear(P, C, xT[0:nt], 16, C.ab_w_in[0], 2560, consume, bf16=True)
            for i in range(nt):
                t = t0 + i
                o = os_[i]
                if kind == 0:
                    lt = t - C.NCT
                    P.dma(cs[:, :, :], C.rope[lt * 128:(lt + 1) * 128, :, :], reads=[C.rope], writes=[cs])
                    qk = o[:, 0:1280].rearrange("p (h two d) -> p h two d", two=2, d=64)
                    x1, x2 = qk[:, :, 0, :], qk[:, :, 1, :]
                    cosb = cs[:, 0:1, :].to_broadcast([128, 10, 64])
                    sinb = cs[:, 1:2, :].to_broadcast([128, 10, 64])
                    for j, (a, b) in enumerate([(x1, cosb), (x2, sinb), (x2, cosb), (x1, sinb)]):
                        P.dve(lambda j=j, a=a, b=b: nc.vector.tensor_tensor(out=tmp[:, j, :, :], in0=a, in1=b, op=ALU.mult),
                              [o, cs], [tmp])
                    P.dve(lambda x1=x1: nc.vector.tensor_tensor(out=x1, in0=tmp[:, 0, :, :], in1=tmp[:, 1, :, :], op=ALU.subtract),
                          [tmp], [o])
                    P.dve(lambda x2=x2: nc.vector.tensor_tensor(out=x2, in0=tmp[:, 2, :, :], in1=tmp[:, 3, :, :], op=ALU.add),
                          [tmp], [o])
                P.dma(C.QKVU[t * 128:(t + 1) * 128, :], o[:, :], reads=[o], writes=[C.QKVU])


def sub(res, name):
    return Res(name, res.t)


ATTN_SCALE = 128 ** -0.5


def phase_attn(P, C):
    nc = P.nc
    NCT, NL = C.NCT, C.NL
    T = NCT + NL
    with P.scope():
        KT = [P.sb(f"atKT{h}", [128, T * 128]) for h in range(2)]
        KTr = [sub(KT[0], f"KTr{t}") for t in range(T)]
        V = P.sb("atV", [128, T, 256])
        Vr = [sub(V, f"Vr{t}") for t in range(T)]
        kin = [P.sb(f"atk{i}", [128, 256]) for i in range(2)]
        sink = P.sb("atsink", [128, 8])
        load_bc(P, sink, C.attn_sink[0, :], C.attn_sink)
        mask = P.sb("atmask", [128, 384])
        P.dma(mask[:, :], C.bandmask[:, :], reads=[C.bandmask], writes=[mask])
        for t in range(T):
            ki = kin[t % 2]
            P.dma(ki[:, :], C.QKVU[t * 128:(t + 1) * 128, 1024:1280], reads=[C.QKVU], writes=[ki])
            P.dma(V[:, t, :], C.QKVU[t * 128:(t + 1) * 128, 1280:1536], reads=[C.QKVU], writes=[Vr[t]])
            pb = P.bank()
            for h in range(2):
                P.pe(lambda h=h, pb=pb, ki=ki: nc.tensor.transpose(
                    out=pb[:, h * 128:(h + 1) * 128], in_=ki[:, h * 128:(h + 1) * 128], identity=C.ident[:, :]),
                    [ki, C.ident], [pb])
            for h in range(2):
                P.act(lambda h=h, pb=pb, t=t: nc.scalar.copy(out=KT[h][:, t * 128:(t + 1) * 128],
                                                           in_=pb[:, h * 128:(h + 1) * 128]), [pb], [KTr[t]])
        qb = [P.sb(f"atq{i}", [128, 1024]) for i in range(2)]
        qT = [P.sb(f"atqT{i}", [128, 8, 128]) for i in range(2)]
        ob = [P.sb(f"ato{i}", [128, 1024]) for i in range(2)]
        scb = [P.sb(f"atsc{i}", [128, 640]) for i in range(2)]
        prb = [P.sb(f"atpr{i}", [128, 640]) for i in range(2)]
        ptb = [P.sb(f"atpt{i}", [128, 5, 128]) for i in range(2)]
        mst = [P.sb(f"atm{i}", [128, 8, 8]) for i in range(2)]
        it = 0
        for t in range(T):
            kind = 1 if t < NCT else 0
            q, QT, o_t, m = qb[t % 2], qT[t % 2], ob[t % 2], mst[t % 2]
            P.dma(q[:, :], C.QKVU[t * 128:(t + 1) * 128, 0:1024], reads=[C.QKVU], writes=[q])
            tr_chunks(P, C, q, 8, QT)
            ktiles = list(range(NCT))
            if kind == 0:
                n = t - NCT
                lo, hi = max(0, n - 1), min(NL - 1, n + 1)
                moff = (lo - (n - 1)) * 128
                ktiles += list(range(NCT + lo, NCT + hi + 1))
                nb = hi - lo + 1
            nk = len(ktiles) * 128
            for hq in range(8):
                h = hq // 4
                sc, pr, PT = scb[it % 2], prb[it % 2], ptb[it % 2]
                it += 1
                pbA = P.bank()
                P.pe(lambda pbA=pbA, QT=QT, hq=hq, h=h: nc.tensor.matmul(
                    pbA[:, 0:NCT * 128], lhsT=QT[:, hq, :], rhs=KT[h][:, 0:NCT * 128], start=True, stop=True),
                    [QT] + KTr[0:NCT], [pbA])
                P.act(lambda pbA=pbA, sc=sc: nc.scalar.activation(
                    out=sc[:, 0:NCT * 128], in_=pbA[:, 0:NCT * 128], func=AF.Copy, scale=ATTN_SCALE), [pbA], [sc])
                if kind == 0:
                    pbB = P.bank()
                    c0, c1 = (NCT + lo) * 128, (NCT + hi + 1) * 128
                    P.pe(lambda pbB=pbB, QT=QT, hq=hq, h=h, c0=c0, c1=c1, nb=nb: nc.tensor.matmul(
                        pbB[:, 0:nb * 128], lhsT=QT[:, hq, :], rhs=KT[h][:, c0:c1], start=True, stop=True),
                        [QT] + KTr[NCT + lo:NCT + hi + 1], [pbB])
                    P.dve(lambda pbB=pbB, sc=sc, nb=nb, moff=moff: nc.vector.scalar_tensor_tensor(
                        out=sc[:, NCT * 128:NCT * 128 + nb * 128], in0=pbB[:, 0:nb * 128], scalar=ATTN_SCALE,
                        in1=mask[:, moff:moff + nb * 128], op0=ALU.mult, op1=ALU.add), [pbB, mask], [sc])
                P.dve(lambda sc=sc, m=m, hq=hq, nk=nk: nc.vector.reduce_max(out=m[:, hq, 0:1], in_=sc[:, 0:nk], axis=AX.X),
                      [sc], [m])
                P.dve(lambda m=m, hq=hq: nc.vector.tensor_tensor(out=m[:, hq, 0:1], in0=m[:, hq, 0:1],
                                                                 in1=sink[:, hq:hq + 1], op=ALU.max), [m, sink], [m])
                P.dve(lambda m=m, hq=hq: nc.vector.tensor_scalar(out=m[:, hq, 1:2], in0=m[:, hq, 0:1], scalar1=-1.0,
                                                                 scalar2=None, op0=ALU.mult), [m], [m])
                P.act(lambda sc=sc, pr=pr, m=m, hq=hq, nk=nk: (nc.scalar.activation(
                    out=pr[:, 0:nk], in_=sc[:, 0:nk], func=AF.Exp, bias=m[:, hq, 1:2], scale=1.0,
                    accum_out=m[:, hq, 2:3]), nc.scalar.copy(out=P.scr[:, 0:1], in_=m[:, hq, 2:3]))[1], [sc, m], [pr, m])
                P.act(lambda m=m, hq=hq: nc.scalar.activation(
                    out=m[:, hq, 3:4], in_=sink[:, hq:hq + 1], func=AF.Exp, bias=m[:, hq, 1:2], scale=1.0), [sink, m], [m])
                P.dve(lambda m=m, hq=hq: nc.vector.tensor_tensor(out=m[:, hq, 4:5], in0=m[:, hq, 2:3],
                                                                 in1=m[:, hq, 3:4], op=ALU.add), [m], [m])
                P.dve(lambda m=m, hq=hq: nc.vector.reciprocal(out=m[:, hq, 5:6], in_=m[:, hq, 4:5]), [m], [m])
                tr_chunks(P, C, pr, nk // 128, PT)
                pbO = P.bank()
                for j, kt in enumerate(ktiles):
                    P.pe(lambda j=j, kt=kt, pbO=pbO, PT=PT, h=h, last=(j == len(ktiles) - 1): nc.tensor.matmul(
                        pbO[:, 0:128], lhsT=PT[:, j, :], rhs=V[:, kt, h * 128:(h + 1) * 128], start=(j == 0), stop=last),
                        [PT, Vr[kt]], [pbO])
                P.act(lambda pbO=pbO, o_t=o_t, m=m, hq=hq: nc.scalar.activation(
                    out=o_t[:, hq * 128:(hq + 1) * 128], in_=pbO[:, 0:128], func=AF.Copy, scale=m[:, hq, 5:6]),
                    [pbO, m], [o_t])
            P.dma(C.ATS5[t * 128:(t + 1) * 128, 0:1024], o_t[:, :], reads=[o_t], writes=[C.ATS5])


SPLIT = False


class Ctx:
    def __init__(self, P, NL, NCT, probes=()):
        T = NL + NCT
        self.NO = NL // 2 if SPLIT else NL
        self.P, self.NL, self.NCT, self.T = P, NL, NCT, T
        self.probes = set(probes)
        self.used_inputs = []
        self.ev = 0
        self.wi = 0
        R = T * 128
        self.spec = {
            "h0": ([R, 2048], "in"), "c_col": ([128, 16], "in"), "cc_col": ([128, 16], "in"),
            "mod_w": ([2, 2048, 12288], "in"), "mod_b": ([2, 12288], "in"),
            "norm1_g": ([2, 2048], "in"), "norm2_g": ([2, 2048], "in"),
            "ab_w_in": ([1, 2048, 2560], "in"), "attn_sink": ([1, 8], "in"),
            "s5_a_re": ([1, 2, 64, 64], "in"), "s5_a_im": ([1, 2, 64, 64], "in"), "s5_log_dt": ([1, 2, 64, 64], "in"),
            "s5_b_re": ([1, 2, 64, 64, 16], "in"), "s5_b_im": ([1, 2, 64, 64, 16], "in"),
            "s5_c_re": ([1, 2, 64, 16, 64], "in"), "s5_c_im": ([1, 2, 64, 16, 64], "in"),
            "s5_d": ([1, 1024], "in"), "s5_glu_w": ([1, 1024, 1024], "in"), "s5_glu_b": ([1, 1024], "in"),
            "ab_w_out": ([1, 2048, 2048], "in"),
            "ssd_w_in": ([1, 2048, 10368], "in"), "ssd_conv_w": ([1, 5, 6144], "in"), "ssd_conv_b": ([1, 6144], "in"),
            "ssd_dt_bias": ([1, 128], "in"), "ssd_a_log": ([1, 128], "in"), "ssd_d": ([1, 64], "in"),
            "ssd_norm_g": ([1, 4096], "in"), "ssd_w_out": ([1, 4096, 2048], "in"),
            "peer_wq": ([2, 2048, 1024], "in"), "peer_keys": ([2, 8, 2, 128, 64], "in"),
            "peer_u": ([2, 16384, 2048], "in"), "peer_v": ([2, 16384, 2048], "in"),
            "final_norm_g": ([2048], "in"),
            "ident": ([128, 128], "in"), "rope": ([NL * 128, 2, 64], "in"), "bandmask": ([128, 384], "in"),
            "tri": ([4, 128, 128], "in"), "ncol": ([128, 4], "in"), "iota256": ([128, 256], "in"),
            "MOD": ([2, 2, 128, 12288], "tmp"), "QKVU": ([R, 2560], "tmp"), "ATS5": ([R, 2048], "tmp"),
            "H": ([R, 2048], "tmp"), "Y5": ([R, 1024], "tmp"), "BB": ([2, 64, 16, 128], "tmp"),
            "SP": ([R, 10368], "tmp"), "XBC": ([R, 6144], "tmp"), "YS": ([NL * 128, 4096], "tmp"),
            "ssdmask": ([2, 128, 128], "in"),
            "UVB": ([2 * 16384, 4096], "tmp", BF16),
            "rowidx": ([128, NL // 2], "in", I32), "H2": ([NL // 2 * 128, 2048], "tmp"),
            "OUT": ([self.NO * 128, 2048], "out"),
        }

    def __getattr__(self, name):
        spec = self.__dict__.get("spec", {})
        if name not in spec:
            raise AttributeError(name)
        shape, kind = spec[name][0], spec[name][1]
        dt = spec[name][2] if len(spec[name]) > 2 else F32
        k = {"in": "ExternalInput", "out": "ExternalOutput"}.get(kind)
        if k is None:
            k = "ExternalOutput" if name in self.probes else "Internal"
        r = self.P.dram(name, shape, dt, k)
        if kind == "in":
            self.used_inputs.append(name)
        setattr(self, name, r)
        return r


STAGES = ["mod", "inproj0", "attn", "s5", "outproj0", "peer0", "ssd", "peer1", "final"]


def build_program(P, NL, NCT, stage="final", probes=()):
    C = Ctx(P, NL, NCT, probes)
    nc = P.nc
    si = STAGES.index(stage)
    C.ident_d = C.ident
    idt = P.es.enter_context(nc.sbuf_tensor("ident_sb", [128, 128], F32))
    ident_sb = Res("ident_sb", idt)
    P.dma(ident_sb[:, :], C.ident_d[:, :], reads=[C.ident_d], writes=[ident_sb])
    C.ident = ident_sb
    P.scr = Res("scr", P.es.enter_context(nc.sbuf_tensor("scr_tail", [128, 4], F32)))
    io = Res("iota_sb", P.es.enter_context(nc.sbuf_tensor("iota_sb", [128, 256], F32)))
    P.dma(io[:, :], C.iota256[:, :], reads=[C.iota256], writes=[io])
    C.iota = io
    with P.scope():
        phase_mod(P, 2, C.c_col, C.cc_col, C.mod_w, C.mod_b, C.MOD)
    outs = [C.MOD]
    if si >= 1:
        phase_inproj0(P, C, 0, C.h0)
        outs.append(C.QKVU)
    if si >= 2:
        phase_attn(P, C)
        outs.append(C.ATS5)
    if si >= 3:
        phase_s5(P, C)
        outs.append(C.Y5)
    if si >= 4:
        phase_outproj0(P, C, 0, C.h0)
        outs.append(C.H)
    if si >= 5:
        phase_peer(P, C, 0, list(range(C.T)))
    if si >= 6:
        phase_ssd_in(P, C, 1)
        phase_ssd_conv(P, C)
        for d in range(2):
            ssd_pass(P, C, d)
        phase_ssd_out(P, C, 1)
    if si >= 7:
        if SPLIT:
            phase_peer(P, C, 1, list(range(C.NL // 2)), split=True)
        else:
            phase_peer(P, C, 1, list(range(C.NCT, C.T)))
    if si >= 8:
        phase_final(P, C)
        outs.append(C.OUT)
    P.final(outs + getattr(P, "dbg", []))
    return C


def host_constants(NL):
    t = np.arange(NL * 128)
    row = (t // 64).astype(np.float32)
    col = (t % 64).astype(np.float32)
    inv = (10000.0 ** (-np.arange(32, dtype=np.float32) / 32)).astype(np.float32)
    ang = np.concatenate([row[:, None] * inv, col[:, None] * inv], axis=-1).astype(np.float32)
    rope = np.stack([np.cos(ang), np.sin(ang)], axis=1).astype(np.float32)
    i = np.arange(128)[:, None]
    j = np.arange(128)[None, :]
    neg = np.float32(-30000.0)
    bandmask = np.concatenate([np.where(j >= i, 0, neg), np.zeros((128, 128)), np.where(j <= i, 0, neg)],
                              axis=1).astype(np.float32)
    iota = np.broadcast_to(np.arange(256, dtype=np.float32)[None, :], (128, 256)).copy()
    return {"ident": np.eye(128, dtype=np.float32), "rope": rope, "bandmask": bandmask, "iota256": iota}


def make_inputs(inp, b, NL, NCT, used=None):
    m = {}
    m["h0"] = np.ascontiguousarray(np.concatenate([inp["ctx"][b][:NCT * 128], inp["x"][b][:NL * 128]], axis=0))
    m["c_col"] = np.ascontiguousarray(inp["c"][b].reshape(16, 128).T)
    m["cc_col"] = np.ascontiguousarray(inp["c_ctx"].reshape(16, 128).T)
    for k in ["mod_w", "mod_b", "norm1_g", "norm2_g", "ab_w_in", "attn_sink", "s5_a_re", "s5_a_im", "s5_log_dt",
              "s5_b_re", "s5_b_im", "s5_c_re", "s5_c_im", "s5_glu_w", "s5_glu_b", "ab_w_out", "ssd_w_in",
              "ssd_conv_w", "ssd_conv_b", "ssd_d", "ssd_norm_g", "ssd_w_out", "peer_wq", "peer_keys", "peer_u",
              "peer_v", "final_norm_g"]:
        m[k] = inp[k]
    m["s5_d"] = inp["s5_d"].reshape(1, 1024)
    m["ssd_dt_bias"] = inp["ssd_dt_bias"].reshape(1, 128)
    m["ssd_a_log"] = inp["ssd_a_log"].reshape(1, 128)
    m.update(host_constants(NL))
    m.update(host_s5_constants())
    if used is not None:
        m = {k: np.ascontiguousarray(v, dtype=np.float32) for k, v in m.items() if k in used}
    return m


PI = float(np.pi)


def sin_of(P, out, ang, shift, tmps, rin, rtmp, rout):
    nc = P.nc
    z, kf, ki = tmps
    P.dve(lambda: nc.vector.tensor_scalar(out=z, in0=ang, scalar1=float(shift), scalar2=None, op0=ALU.add), rin, rtmp)
    P.dve(lambda: nc.vector.tensor_scalar(out=ki, in0=z, scalar1=1.0 / (2 * PI), scalar2=None, op0=ALU.mult), rtmp, rtmp)
    P.dve(lambda: nc.vector.tensor_copy(out=kf, in_=ki), rtmp, rtmp)
    P.dve(lambda: nc.vector.scalar_tensor_tensor(out=z, in0=kf, scalar=-2 * PI, in1=z, op0=ALU.mult, op1=ALU.add), rtmp, rtmp)
    P.dve(lambda: nc.vector.tensor_scalar(out=kf, in0=z, scalar1=PI, scalar2=-2 * PI, op0=ALU.is_gt, op1=ALU.mult), rtmp, rtmp)
    P.dve(lambda: nc.vector.tensor_tensor(out=z, in0=z, in1=kf, op=ALU.add), rtmp, rtmp)
    P.dve(lambda: nc.vector.tensor_scalar(out=kf, in0=z, scalar1=-PI, scalar2=2 * PI, op0=ALU.is_lt, op1=ALU.mult), rtmp, rtmp)
    P.dve(lambda: nc.vector.tensor_tensor(out=z, in0=z, in1=kf, op=ALU.add), rtmp, rtmp)
    P.act(lambda: nc.scalar.activation(out=out, in_=z, func=AF.Sin), rtmp, rout)


def cmul(P, a_re, a_im, t_re, t_im, o_re, o_im, tmps, ra, rt, ro, conj=False):
    nc = P.nc
    tA, tB, tC, tD = tmps
    P.dve(lambda: nc.vector.tensor_tensor(out=tA[0], in0=a_re, in1=t_re, op=ALU.mult), ra + rt, [tA[1]])
    P.pool(lambda: nc.gpsimd.tensor_tensor(out=tB[0], in0=a_im, in1=t_im, op=ALU.mult), ra + rt, [tB[1]])
    P.pool(lambda: nc.gpsimd.tensor_tensor(out=tC[0], in0=a_re, in1=t_im, op=ALU.mult), ra + rt, [tC[1]])
    P.dve(lambda: nc.vector.tensor_tensor(out=tD[0], in0=a_im, in1=t_re, op=ALU.mult), ra + rt, [tD[1]])
    P.dve(lambda: nc.vector.tensor_tensor(out=o_re, in0=tA[0], in1=tB[0], op=ALU.subtract), [tA[1], tB[1]], ro)
    P.pool(lambda: nc.gpsimd.tensor_tensor(out=o_im, in0=tC[0], in1=tD[0], op=ALU.add), [tC[1], tD[1]], ro)


def phase_s5(P, C):
    with P.scope():
        for d in range(2):
            s5_prep(P, C, d)
    for d in range(2):
        s5_pass(P, C, d)


def s5_prep(P, C, d):
    nc = P.nc
    if True:
        if True:
            ar = P.sb(f"s5ar{d}", [64, 64]); ai = P.sb(f"s5ai{d}", [64, 64]); ld = P.sb(f"s5ld{d}", [64, 64])
            br = P.sb(f"s5br{d}", [64, 64, 16]); bi = P.sb(f"s5bi{d}", [64, 64, 16])
            P.dma(ar[:, :], C.s5_a_re[0, d], reads=[C.s5_a_re], writes=[ar])
            P.dma(ai[:, :], C.s5_a_im[0, d], reads=[C.s5_a_im], writes=[ai])
            P.dma(ld[:, :], C.s5_log_dt[0, d], reads=[C.s5_log_dt], writes=[ld])
            P.dma(br[:, :, :], C.s5_b_re[0, d], reads=[C.s5_b_re], writes=[br])
            P.dma(bi[:, :, :], C.s5_b_im[0, d], reads=[C.s5_b_im], writes=[bi])
            w = P.sb(f"s5w{d}", [64, 16, 64])
            W = lambda k: w[:, k, :]
            P.act(lambda: nc.scalar.activation(out=W(0), in_=ld[:, :], func=AF.Exp), [ld], [w])
            P.dve(lambda: nc.vector.tensor_tensor(out=W(1), in0=W(0), in1=ar[:, :], op=ALU.mult), [w, ar], [w])
            P.dve(lambda: nc.vector.tensor_tensor(out=W(2), in0=W(0), in1=ai[:, :], op=ALU.mult), [w, ai], [w])
            P.act(lambda: nc.scalar.activation(out=W(3), in_=W(1), func=AF.Exp), [w], [w])
            ki_s = P.sb(f"s5kis{d}", [64, 64], I32)
            tm3 = (W(4), W(14), ki_s[:, :])
            sin_of(P, W(5), W(2), 0.0, tm3, [w], [w, ki_s], [w])
            sin_of(P, W(6), W(2), PI / 2, tm3, [w], [w, ki_s], [w])
            P.dve(lambda: nc.vector.tensor_tensor(out=W(7), in0=W(3), in1=W(6), op=ALU.mult), [w], [w])
            P.dve(lambda: nc.vector.tensor_scalar(out=W(7), in0=W(7), scalar1=-1.0, scalar2=None, op0=ALU.add), [w], [w])
            P.dve(lambda: nc.vector.tensor_tensor(out=W(8), in0=W(3), in1=W(5), op=ALU.mult), [w], [w])
            P.dve(lambda: nc.vector.tensor_tensor(out=W(9), in0=ar[:, :], in1=ar[:, :], op=ALU.mult), [ar], [w])
            P.dve(lambda: nc.vector.tensor_tensor(out=W(12), in0=ai[:, :], in1=ai[:, :], op=ALU.mult), [ai], [w])
            P.dve(lambda: nc.vector.tensor_tensor(out=W(9), in0=W(9), in1=W(12), op=ALU.add), [w], [w])
            P.dve(lambda: nc.vector.reciprocal(out=W(9), in_=W(9)), [w], [w])
            P.dve(lambda: nc.vector.tensor_tensor(out=W(12), in0=W(7), in1=ar[:, :], op=ALU.mult), [w, ar], [w])
            P.dve(lambda: nc.vector.tensor_tensor(out=W(13), in0=W(8), in1=ai[:, :], op=ALU.mult), [w, ai], [w])
            P.dve(lambda: nc.vector.tensor_tensor(out=W(12), in0=W(12), in1=W(13), op=ALU.add), [w], [w])
            P.dve(lambda: nc.vector.tensor_tensor(out=W(10), in0=W(12), in1=W(9), op=ALU.mult), [w], [w])
            P.dve(lambda: nc.vector.tensor_tensor(out=W(12), in0=W(8), in1=ar[:, :], op=ALU.mult), [w, ar], [w])
            P.dve(lambda: nc.vector.tensor_tensor(out=W(13), in0=W(7), in1=ai[:, :], op=ALU.mult), [w, ai], [w])
            P.dve(lambda: nc.vector.tensor_tensor(out=W(12), in0=W(12), in1=W(13), op=ALU.subtract), [w], [w])
            P.dve(lambda: nc.vector.tensor_tensor(out=W(11), in0=W(12), in1=W(9), op=ALU.mult), [w], [w])
            bbT = P.sb(f"s5bbT{d}", [64, 16, 2, 64])
            t1 = P.sb(f"s5t1{d}", [64, 64, 16]); t2 = P.sb(f"s5t2{d}", [64, 64, 16])
            fre = w[:, 10, :].unsqueeze(2).to_broadcast([64, 64, 16])
            fim = w[:, 11, :].unsqueeze(2).to_broadcast([64, 64, 16])
            o_re = bbT[:, :, 0, :].rearrange("g c p -> g p c")
            o_im = bbT[:, :, 1, :].rearrange("g c p -> g p c")
            P.dve(lambda: nc.vector.tensor_tensor(out=t1[:, :, :], in0=br[:, :, :], in1=fre, op=ALU.mult), [br, w], [t1])
            P.dve(lambda: nc.vector.tensor_tensor(out=t2[:, :, :], in0=bi[:, :, :], in1=fim, op=ALU.mult), [bi, w], [t2])
            P.dve(lambda: nc.vector.tensor_tensor(out=o_re, in0=t1[:, :, :], in1=t2[:, :, :], op=ALU.subtract), [t1, t2], [bbT])
            P.dve(lambda: nc.vector.tensor_tensor(out=t1[:, :, :], in0=bi[:, :, :], in1=fre, op=ALU.mult), [bi, w, bbT], [t1])
            P.dve(lambda: nc.vector.tensor_tensor(out=t2[:, :, :], in0=br[:, :, :], in1=fim, op=ALU.mult), [br, w, bbT], [t2])
            P.dve(lambda: nc.vector.tensor_tensor(out=o_im, in0=t1[:, :, :], in1=t2[:, :, :], op=ALU.add), [t1, t2], [bbT])
            P.dma(C.BB[d], bbT[:, :, :, :].rearrange("g c r p -> g c (r p)"), reads=[bbT], writes=[C.BB])


def s5_pass(P, C, d):
    nc = P.nc
    NCT, NL, T = C.NCT, C.NL, C.T
    if True:
        with P.scope():
            tabs = [P.sb(f"s5tab{k}", [128, 4096]) for k in range(4)]
            with P.scope():
                rho = P.sb("s5rho", [128, 4096]); th = P.sb("s5th", [128, 4096]); dtb = P.sb("s5dtb", [128, 4096])
                tmp = P.sb("s5tmp", [128, 4096])
                ncol = P.sb("s5ncol", [128, 4])
                P.dma(ncol[:, :], C.ncol[:, :], reads=[C.ncol], writes=[ncol])
                load_bc(P, dtb, C.s5_log_dt[0, d].rearrange("g p -> (g p)"), C.s5_log_dt)
                load_bc(P, rho, C.s5_a_re[0, d].rearrange("g p -> (g p)"), C.s5_a_re)
                load_bc(P, th, C.s5_a_im[0, d].rearrange("g p -> (g p)"), C.s5_a_im)
                P.act(lambda: nc.scalar.activation(out=dtb[:, :], in_=dtb[:, :], func=AF.Exp), [dtb], [dtb])
                P.dve(lambda: nc.vector.tensor_tensor(out=rho[:, :], in0=rho[:, :], in1=dtb[:, :], op=ALU.mult), [rho, dtb], [rho])
                P.dve(lambda: nc.vector.tensor_tensor(out=th[:, :], in0=th[:, :], in1=dtb[:, :], op=ALU.mult), [th, dtb], [th])
                P.dve(lambda: nc.vector.tensor_scalar(out=th[:, :], in0=th[:, :], scalar1=ncol[:, d:d + 1], scalar2=None,
                                                      op0=ALU.mult), [th, ncol], [th])
                E = dtb
                tmp2 = P.sb("s5tmp2", [128, 4096])
                ki_b = P.sb("s5kib", [128, 4096], I32)
                tm3 = (tmp[:, :], tmp2[:, :], ki_b[:, :])
                sin_of(P, tabs[1][:, :], th[:, :], 0.0, tm3, [th], [tmp, tmp2, ki_b], [tabs[1]])
                sin_of(P, tabs[0][:, :], th[:, :], PI / 2, tm3, [th], [tmp, tmp2, ki_b], [tabs[0]])
                P.act(lambda: nc.scalar.activation(out=E[:, :], in_=rho[:, :], func=AF.Exp, scale=ncol[:, d:d + 1]),
                      [rho, ncol], [E])
                P.dve(lambda: nc.vector.tensor_tensor(out=tabs[2][:, :], in0=tabs[0][:, :], in1=E[:, :], op=ALU.mult),
                      [tabs[0], E], [tabs[2]])
                P.dve(lambda: nc.vector.tensor_tensor(out=tabs[3][:, :], in0=tabs[1][:, :], in1=E[:, :], op=ALU.mult),
                      [tabs[1], E], [tabs[3]])
                P.act(lambda: nc.scalar.activation(out=E[:, :], in_=rho[:, :], func=AF.Exp, scale=ncol[:, 2 + d:3 + d]),
                      [rho, ncol, tabs[2], tabs[3]], [E])
                P.dve(lambda: nc.vector.tensor_tensor(out=tabs[0][:, :], in0=tabs[0][:, :], in1=E[:, :], op=ALU.mult),
                      [tabs[0], E, tabs[2]], [tabs[0]])
                P.dve(lambda: nc.vector.scalar_tensor_tensor(out=tabs[1][:, :], in0=tabs[1][:, :], scalar=-1.0, in1=E[:, :],
                                                             op0=ALU.mult, op1=ALU.mult), [tabs[1], E, tabs[3]], [tabs[1]])
            for k in range(4):
                P.dump(f"tab{d}_{k}", tabs[k][:, :], tabs[k], [128, 4096])
            RB = P.sb("s5RB", [128, 8, 1024])
            P.pool(lambda: nc.gpsimd.memset(RB[:, :, :], 0.0), [], [RB])
            for g in range(64):
                gb, gl = g // 8, g % 8
                P.dma(RB[gl * 16:(gl + 1) * 16, gb, gl * 128:(gl + 1) * 128], C.BB[d, g], reads=[C.BB], writes=[RB])
            CM = P.sb("s5CM", [128, 1024])
            cin = [P.sb(f"s5cin{i}", [128, 128]) for i in range(2)]
            for gb in range(8):
                ci_ = cin[gb % 2]
                P.dma(ci_[:, 0:64], C.s5_c_re[0, d].rearrange("g c p -> (g c) p")[gb * 128:(gb + 1) * 128, :],
                      reads=[C.s5_c_re], writes=[ci_])
                P.dma(ci_[:, 64:128], C.s5_c_im[0, d].rearrange("g c p -> (g c) p")[gb * 128:(gb + 1) * 128, :],
                      reads=[C.s5_c_im], writes=[ci_])
                P.dve(lambda ci_=ci_: nc.vector.tensor_scalar(out=ci_[:, 64:128], in0=ci_[:, 64:128], scalar1=-1.0,
                                                              scalar2=None, op0=ALU.mult), [ci_], [ci_])
                pb = P.bank()
                P.pe(lambda ci_=ci_, pb=pb: nc.tensor.transpose(out=pb[:, 0:128], in_=ci_[:, :], identity=C.ident[:, :]),
                     [ci_, C.ident], [pb])
                P.act(lambda pb=pb, gb=gb: nc.scalar.copy(out=CM[:, gb * 128:(gb + 1) * 128], in_=pb[:, 0:128]), [pb], [CM])
            tri = P.sb("s5tri", [128, 2, 128])
            P.dma(tri[:, 0, :], C.tri[d], reads=[C.tri], writes=[tri])
            P.dma(tri[:, 1, :], C.tri[2 + d], reads=[C.tri], writes=[tri])
            dbc = P.sb("s5dbc", [128, 1024])
            load_bc(P, dbc, C.s5_d[0, :], C.s5_d)
            sbuf = [P.sb(f"s5s{i}", [128, 8192]) for i in range(2)]
            ub = [P.sb(f"s5u{i}", [128, 1024]) for i in range(1)]
            uTb = [P.sb(f"s5uT{i}", [128, 8, 128]) for i in range(1)]
            bsb = [P.sb(f"s5b{i}", [128, 1024]) for i in range(2)]
            sTb = [P.sb(f"s5sT{i}", [128, 4, 128]) for i in range(2)]
            yb = [P.sb(f"s5y{i}", [128, 1024]) for i in range(1)]
            tm = [[P.sb(f"s5tm{i}_{k}", [128, 8, 64]) for k in range(4)] for i in range(1)]
            order = list(range(T)) if d == 0 else list(range(NCT - 1, -1, -1)) + list(range(T - 1, NCT - 1, -1))
            for ci, t in enumerate(order):
                u, uT, snew, sprev, yv = ub[0], uTb[0], sbuf[ci % 2], sbuf[(ci + 1) % 2], yb[0]
                rows = slice(t * 128, (t + 1) * 128)
                P.dma(u[:, :], C.QKVU[rows, 1536:2560], reads=[C.QKVU], writes=[u])
                tr_chunks(P, C, u, 8, uT)
                it = 0
                for gb in range(8):
                    ap2, bA, bB = P.bank2()
                    for half, bk in enumerate((bA, bB)):
                        P.pe(lambda gb=gb, half=half, bk=bk, uT=uT: nc.tensor.matmul(
                            bk[:, :], lhsT=uT[:, gb, :], rhs=RB[:, gb, half * 512:(half + 1) * 512], start=True, stop=True),
                            [uT, RB], [bk])
                    bs = bsb[gb % 2]
                    P.act(lambda ap2=ap2, bs=bs: nc.scalar.copy(out=bs[:, :], in_=ap2), [bA, bB], [bs])
                    v = bs[:, :].rearrange("t (g r p) -> t g r p", g=8, r=2)
                    wv = snew[:, gb * 1024:(gb + 1) * 1024].rearrange("t (g r p) -> t g r p", g=8, r=2)
                    tr_ = tabs[0][:, gb * 512:(gb + 1) * 512].rearrange("t (g p) -> t g p", p=64)
                    ti_ = tabs[1][:, gb * 512:(gb + 1) * 512].rearrange("t (g p) -> t g p", p=64)
                    tmps = [(x[:, :, :], x) for x in tm[0]]
                    cmul(P, v[:, :, 0, :], v[:, :, 1, :], tr_, ti_, wv[:, :, 0, :], wv[:, :, 1, :], tmps,
                         [bs], [tabs[0], tabs[1]], [snew])
                for gb in range(8):
                    ap2, bA, bB = P.bank2()
                    for half, bk in enumerate((bA, bB)):
                        c0 = gb * 1024 + half * 512
                        P.pe(lambda bk=bk, c0=c0, snew=snew, first=(ci == 0): nc.tensor.matmul(
                            bk[:, :], lhsT=tri[:, 0, :], rhs=snew[:, c0:c0 + 512], start=True, stop=first),
                            [tri, snew], [bk])
                        if ci > 0:
                            P.pe(lambda bk=bk, c0=c0, sprev=sprev: nc.tensor.matmul(
                                bk[:, :], lhsT=tri[:, 1, :], rhs=sprev[:, c0:c0 + 512], start=False, stop=True),
                                [tri, sprev], [bk])
                    bs = bsb[gb % 2]
                    P.act(lambda ap2=ap2, bs=bs: nc.scalar.copy(out=bs[:, :], in_=ap2), [bA, bB], [bs])
                    v = bs[:, :].rearrange("t (g r p) -> t g r p", g=8, r=2)
                    wv = snew[:, gb * 1024:(gb + 1) * 1024].rearrange("t (g r p) -> t g r p", g=8, r=2)
                    tr_ = tabs[2][:, gb * 512:(gb + 1) * 512].rearrange("t (g p) -> t g p", p=64)
                    ti_ = tabs[3][:, gb * 512:(gb + 1) * 512].rearrange("t (g p) -> t g p", p=64)
                    tmps = [(x[:, :, :], x) for x in tm[0]]
                    cmul(P, v[:, :, 0, :], v[:, :, 1, :], tr_, ti_, wv[:, :, 0, :], wv[:, :, 1, :], tmps,
                         [bs], [tabs[2], tabs[3]], [snew])
                if ci == 0:
                    P.dump(f"s{d}", snew[:, :], snew, [128, 8192])
                    P.dump(f"CM{d}", CM[:, :], CM, [128, 1024])
                    P.dump(f"RB{d}", RB[:, :, :], RB, [128, 8, 1024])
                yap, yA, yB = P.bank2()
                P.reserved = set(P.last_pair)
                for g4 in range(16):
                    sT = sTb[g4 % 2]
                    tr_chunks(P, C, snew, 4, sT, col0=g4 * 512)
                    for j in range(4):
                        g = g4 * 4 + j
                        yk = yA if g < 32 else yB
                        P.pe(lambda sT=sT, j=j, g=g, yk=yk: nc.tensor.matmul(
                            yk[:, (g % 32) * 16:(g % 32) * 16 + 16], lhsT=sT[:, j, :], rhs=CM[:, g * 16:(g + 1) * 16],
                            start=True, stop=True), [sT, CM], [yk])
                if d == 0:
                    P.dve(lambda yv=yv, u=u: nc.vector.tensor_tensor(out=yv[:, :], in0=u[:, :], in1=dbc[:, :], op=ALU.mult),
                          [u, dbc], [yv])
                else:
                    P.dma(yv[:, :], C.Y5[rows, :], reads=[C.Y5], writes=[yv])
                P.dve(lambda yv=yv, yap=yap: nc.vector.tensor_tensor(out=yv[:, :], in0=yv[:, :], in1=yap, op=ALU.add),
                      [yv, yA, yB], [yv])
                P.reserved = set()
                P.dma(C.Y5[rows, :], yv[:, :], reads=[yv], writes=[C.Y5])


def host_s5_constants():
    i = np.arange(128)[:, None]
    j = np.arange(128)[None, :]
    tri = np.stack([(i <= j), (i >= j), np.broadcast_to(i == 127, (128, 128)), np.broadcast_to(i == 0, (128, 128))]
                   ).astype(np.float32)
    t = np.arange(128, dtype=np.float32)
    ncol = np.stack([t + 1, 128 - t, -(t + 1), -(128 - t)], axis=1).astype(np.float32)
    neg = np.float32(-30000.0)
    ssdmask = np.stack([np.where(i <= j, 0, neg), np.where(i >= j, 0, neg)]).astype(np.float32)
    return {"tri": tri, "ncol": ncol, "ssdmask": ssdmask}


def phase_outproj0(P, C, l, hsrc):
    nc = P.nc
    with P.scope():
        G = 2
        g1 = [P.sb(f"opg1{k}", [128, D]) for k in range(2)]
        for kind in range(2):
            P.dma(g1[kind][:, :], C.MOD[l, kind, :, 2 * D:3 * D], reads=[C.MOD], writes=[g1[kind]])
        gb = P.sb("opglub", [128, 1024])
        load_bc(P, gb, C.s5_glu_b[0, :], C.s5_glu_b)
        cat = [P.sb(f"opcat{i}", [128, D]) for i in range(G)]
        catT = [P.sb(f"opcatT{i}", [128, 16, 128], BF16) for i in range(G)]
        C.wbuf16 = [P.sb(f"opw16_{i}", [128, 16, 512], BF16) for i in range(2)]
        gg = [P.sb(f"opg{i}", [128, 1024]) for i in range(G)]
        gT = [P.sb(f"opgT{i}", [128, 8, 128], BF16) for i in range(G)]
        ht = [P.sb(f"oph{i}", [128, D]) for i in range(G)]
        zt = [P.sb(f"opz{i}", [128, 512]) for i in range(2)]
        C.wbuf = [P.sb(f"opw{i}", [128, 16, 512]) for i in range(2)]
        for kind, t0, nt in tile_groups(C, G):
            for i in range(nt):
                rows = slice((t0 + i) * 128, (t0 + i + 1) * 128)
                P.dma(gg[i][:, :], C.Y5[rows, :], reads=[C.Y5], writes=[gg[i]])
                P.dma(cat[i][:, 0:1024], C.ATS5[rows, 0:1024], reads=[C.ATS5], writes=[cat[i]])
                P.dma(ht[i][:, :], hsrc[rows, :], reads=[hsrc], writes=[ht[i]])
                P.act(lambda i=i: nc.scalar.activation(out=gg[i][:, :], in_=gg[i][:, :], func=AF.Gelu), [gg[i]], [gg[i]])
                tr_chunks(P, C, gg[i], 8, gT[i])

            def consume_glu(n, ti, pb, w):
                z = zt[(n + ti) % 2]
                P.dve(lambda: nc.vector.tensor_tensor(out=z[:, 0:w], in0=pb[:, 0:w], in1=gb[:, n * 512:n * 512 + w], op=ALU.add),
                      [pb, gb], [z])
                P.act(lambda: nc.scalar.activation(out=z[:, 0:w], in_=z[:, 0:w], func=AF.Sigmoid), [z], [z])
                P.dve(lambda: nc.vector.tensor_tensor(out=cat[ti][:, 1024 + n * 512:1024 + n * 512 + w],
                                                      in0=gg[ti][:, n * 512:n * 512 + w], in1=z[:, 0:w], op=ALU.mult),
                      [gg[ti], z], [cat[ti]])
            C.wsrc = C.s5_glu_w
            linear(P, C, gT[0:nt], 8, C.s5_glu_w[0], 1024, consume_glu, bf16=True)
            for i in range(nt):
                tr_chunks(P, C, cat[i], 16, catT[i])

            def consume_out(n, ti, pb, w, kind=kind):
                z = zt[(n + ti) % 2]
                P.dve(lambda: nc.vector.tensor_tensor(out=z[:, 0:w], in0=pb[:, 0:w], in1=g1[kind][:, n * 512:n * 512 + w],
                                                      op=ALU.mult), [pb, g1[kind]], [z])
                P.dve(lambda: nc.vector.tensor_tensor(out=ht[ti][:, n * 512:n * 512 + w], in0=ht[ti][:, n * 512:n * 512 + w],
                                                      in1=z[:, 0:w], op=ALU.add), [ht[ti], z], [ht[ti]])
            C.wsrc = C.ab_w_out
            linear(P, C, catT[0:nt], 16, C.ab_w_out[0], 2048, consume_out, bf16=True)
            for i in range(nt):
                rows = slice((t0 + i) * 128, (t0 + i + 1) * 128)
                P.dma(C.H[rows, :], ht[i][:, :], reads=[ht[i]], writes=[C.H])


def topk16(P, src_ap, srcres, seg2, vals, idx, vres, ires):
    nc = P.nc
    P.dve(lambda: nc.vector.max(out=vals[:, 0:8], in_=src_ap), [srcres], [vres])
    if idx is not None:
        P.dve(lambda: nc.vector.max_index(out=idx[:, 0:8], in_max=vals[:, 0:8], in_values=src_ap), [srcres, vres], [ires])
    P.dve(lambda: nc.vector.match_replace(out=seg2[:, :], in_to_replace=vals[:, 0:8], in_values=src_ap, imm_value=-1e30),
          [srcres, vres], [seg2])
    P.dve(lambda: nc.vector.max(out=vals[:, 8:16], in_=seg2[:, :]), [seg2], [vres])
    if idx is not None:
        P.dve(lambda: nc.vector.max_index(out=idx[:, 8:16], in_max=vals[:, 8:16], in_values=seg2[:, :]), [seg2, vres], [ires])


def phase_peer_convert(P, C, l):
    nc = P.nc
    with P.scope():
        fu = [P.sb(f"pcfu{i}", [128, 4, 2048]) for i in range(2)]
        fv = [P.sb(f"pcfv{i}", [128, 4, 2048]) for i in range(2)]
        bout = [P.sb(f"pcb{i}", [128, 4, 4096], BF16) for i in range(2)]
        for n in range(32):
            a, b, bo = fu[n % 2], fv[n % 2], bout[n % 2]
            P.dma(a[:, :, :], C.peer_u[l, n * 512:(n + 1) * 512, :].rearrange("(p r) d -> p r d", r=4),
                  reads=[C.peer_u], writes=[a])
            P.dma(b[:, :, :], C.peer_v[l, n * 512:(n + 1) * 512, :].rearrange("(p r) d -> p r d", r=4),
                  reads=[C.peer_v], writes=[b])
            P.dve(lambda a=a, bo=bo: nc.vector.tensor_copy(out=bo[:, :, 0:2048], in_=a[:, :, :]), [a], [bo])
            P.act(lambda b=b, bo=bo: nc.scalar.copy(out=bo[:, :, 2048:4096], in_=b[:, :, :]), [b], [bo])
            r0 = l * 16384 + n * 512
            P.dma(C.UVB[r0:r0 + 512, :].rearrange("(p r) d -> p r d", r=4), bo[:, :, :], reads=[bo], writes=[C.UVB])


def phase_peer(P, C, l, tiles, split=False):
    nc = P.nc
    phase_peer_convert(P, C, l)
    with P.scope():
        Gp = [P.sb(f"peGp{k}", [128, D]) for k in range(2)]
        Sh = [P.sb(f"peSh{k}", [128, D]) for k in range(2)]
        prep_mod(P, C, l, 3, 4, C.norm2_g[l, :], C.norm2_g, Gp, Sh)
        g2 = [P.sb(f"peg2{k}", [128, D]) for k in range(2)]
        for kind in range(2):
            P.dma(g2[kind][:, :], C.MOD[l, kind, :, 5 * D:6 * D], reads=[C.MOD], writes=[g2[kind]])
        KM = P.sb("peKM", [128, 8, 256])
        P.pool(lambda: nc.gpsimd.memset(KM[:, :, :], 0.0), [], [KM])
        kin = [P.sb(f"pekin{i}", [128, 2, 64]) for i in range(2)]
        for h in range(8):
            ki = kin[h % 2]
            P.dma(ki[:, :, :], C.peer_keys[l, h].rearrange("half k d -> k half d"), reads=[C.peer_keys], writes=[ki])
            pb = P.bank()
            P.pe(lambda ki=ki, pb=pb: nc.tensor.transpose(out=pb[:, 0:128], in_=ki[:, :, :].rearrange("k a d -> k (a d)"),
                                                         identity=C.ident[:, :]), [ki, C.ident], [pb])
            P.act(lambda pb=pb, h=h: nc.scalar.copy(out=KM[0:64, h, 0:128], in_=pb[0:64, 0:128]), [pb], [KM])
            P.act(lambda pb=pb, h=h: nc.scalar.copy(out=KM[64:128, h, 128:256], in_=pb[64:128, 0:128]), [pb], [KM])
        hb = [P.sb(f"peh{i}", [128, D]) for i in range(1)]
        fb = [P.sb(f"pef{i}", [128, D]) for i in range(1)]
        fT = P.sb("pefT", [128, 16, 128])
        q = P.sb("peq", [128, 1024])
        qT = P.sb("peqT", [128, 8, 128])
        sc = P.sb("pesc", [128, 8, 256])
        seg2 = P.sb("peseg2", [128, 256])
        s2h = P.sb("peseg2h", [128, 128])
        v12 = P.sb("pev12", [128, 8, 2, 16])
        i12 = P.sb("pei12", [128, 8, 2, 16], U32)
        i12f = P.sb("pei12f", [128, 8, 2, 16])
        cand = P.sb("pecand", [128, 8, 256])
        cidx = sc
        score = P.sb("pescore", [128, 8, 16])
        pos = P.sb("pepos", [128, 8, 16], U32)
        posf = P.sb("peposf", [128, 8, 16])
        ef = P.sb("peef", [128, 128])
        ei = P.sb("peei", [128, 128], I32)
        gate = P.sb("pegate", [128, 8, 16])
        gs = P.sb("pegs", [128, 16])
        araw = P.sb("pearaw", [128, 128])
        wgt = P.sb("pewgt", [128, 128])
        junk = P.sb("pejunk", [128, D], BF16)
        gl = P.sb("pegl", [128, 128])
        st = P.sb("pest", [128, 4])
        acc = P.sb("peacc", [128, D])
        NG = 8
        gbuf = [P.sb(f"peg{i}", [128, 2 * D], BF16) for i in range(NG)]
        if split:
            ridx = P.sb("peridx", [128, C.NL // 2], I32)
            P.dma(ridx[:, :], C.rowidx[:, :], reads=[C.rowidx], writes=[ridx])
        C.wbuf = [P.sb(f"pew{i}", [128, 16, 128]) for i in range(2)]
        gi = 0
        for it, t in enumerate(tiles):
            kind = 1 if (t < C.NCT and not split) else 0
            rows = slice(t * 128, (t + 1) * 128)
            hT, f = hb[0], fb[0]
            if split:
                P.op("pool", lambda t=t: nc.gpsimd.indirect_dma_start(
                    out=hT[:, :], out_offset=None, in_=C.H[:, :],
                    in_offset=bass.IndirectOffsetOnAxis(ap=ridx[:, t:t + 1], axis=0)), [ridx, C.H], [hT], dma=True)
            else:
                P.dma(hT[:, :], C.H[rows, :], reads=[C.H], writes=[hT])
            C.xres, C.ores = hT, f
            norm_mod(P, C, hT[:, :], f[:, :], Gp[kind], Sh[kind], junk, st)
            tr_chunks(P, C, f, 16, fT)

            def consume_q(n, ti, pb, w):
                P.act(lambda: nc.scalar.copy(out=q[:, n * 128:n * 128 + w], in_=pb[:, 0:w]), [pb], [q])
            C.wsrc = C.peer_wq
            linear(P, C, [fT], 16, C.peer_wq[l], 1024, consume_q, cw=128)
            tr_chunks(P, C, q, 8, qT)
            for h2 in range(4):
                pb = P.bank()
                for j in range(2):
                    h = h2 * 2 + j
                    P.pe(lambda pb=pb, j=j, h=h: nc.tensor.matmul(pb[:, j * 256:(j + 1) * 256], lhsT=qT[:, h, :], rhs=KM[:, h, :],
                                                                  start=True, stop=True), [qT, KM], [pb])
                P.act(lambda pb=pb, h2=h2: nc.scalar.copy(out=sc[:, 2 * h2:2 * h2 + 2, :],
                                                         in_=pb[:, :].rearrange("p (a b) -> p a b", b=256)), [pb], [sc])
            for h in range(8):
                for half in range(2):
                    topk16(P, sc[:, h, half * 128:(half + 1) * 128], sc, s2h,
                           v12[:, h, half, :], i12[:, h, half, :], v12, i12)
            P.dve(lambda: nc.vector.tensor_copy(out=i12f[:, :, :, :], in_=i12[:, :, :, :]), [i12], [i12f])
            P.dve(lambda: nc.vector.tensor_tensor(
                out=cand[:, :, :].rearrange("p h (a b) -> p h a b", b=16),
                in0=v12[:, :, 0, :].unsqueeze(3).to_broadcast([128, 8, 16, 16]),
                in1=v12[:, :, 1, :].unsqueeze(2).to_broadcast([128, 8, 16, 16]), op=ALU.add), [v12], [cand])
            P.dve(lambda: nc.vector.tensor_scalar(out=i12f[:, :, 0, :], in0=i12f[:, :, 0, :], scalar1=128.0, scalar2=None,
                                                  op0=ALU.mult), [i12f], [i12f])
            P.dve(lambda: nc.vector.tensor_tensor(
                out=cidx[:, :, :].rearrange("p h (a b) -> p h a b", b=16),
                in0=i12f[:, :, 0, :].unsqueeze(3).to_broadcast([128, 8, 16, 16]),
                in1=i12f[:, :, 1, :].unsqueeze(2).to_broadcast([128, 8, 16, 16]), op=ALU.add), [i12f], [cidx])
            for h in range(8):
                topk16(P, cand[:, h, :], cand, seg2, score[:, h, :], pos[:, h, :], score, pos)
            P.dve(lambda: nc.vector.tensor_copy(out=posf[:, :, :], in_=pos[:, :, :]), [pos], [posf])
            for h in range(8):
                for k in range(16):
                    P.dve(lambda h=h, k=k, last=(h == 7 and k == 15): (nc.vector.scalar_tensor_tensor(
                        out=junk[:, 0:256], in0=C.iota[:, :], scalar=posf[:, h, k:k + 1], in1=cidx[:, h, :],
                        op0=ALU.is_equal, op1=ALU.mult, accum_out=ef[:, h * 16 + k:h * 16 + k + 1]),
                        nc.vector.tensor_copy(out=P.scr[:, 1:2], in_=ef[:, h * 16 + k:h * 16 + k + 1]) if last else None)[1 if last else 0],
                        [C.iota, posf, cidx], [junk, ef], soft=(junk, ef))
            P.dve(lambda: nc.vector.tensor_copy(out=ei[:, :], in_=ef[:, :]), [ef], [ei])
            P.dve(lambda: nc.vector.tensor_tensor(out=gate[:, :, :], in0=score[:, :, :],
                                                  in1=score[:, :, 0:1].to_broadcast([128, 8, 16]), op=ALU.subtract), [score], [gate])
            P.act(lambda: nc.scalar.activation(out=gate[:, :, :], in_=gate[:, :, :], func=AF.Exp), [gate], [gate])
            P.dve(lambda: nc.vector.reduce_sum(out=gs[:, 0:8], in_=gate[:, :, :], axis=AX.X), [gate], [gs])
            P.dve(lambda: nc.vector.reciprocal(out=gs[:, 8:16], in_=gs[:, 0:8]), [gs], [gs])
            P.dve(lambda: nc.vector.tensor_tensor(out=gate[:, :, :], in0=gate[:, :, :],
                                                  in1=gs[:, 8:16].unsqueeze(2).to_broadcast([128, 8, 16]), op=ALU.mult), [gate, gs], [gate])
            gate2 = gate[:, :, :].rearrange("p h k -> p (h k)")
            for blk in range(32):
                gs_ = []
                for jj in range(4):
                    j = blk * 4 + jj
                    g = gbuf[gi % NG]
                    gi += 1
                    gs_.append(g)
                    P.op("pool", lambda g=g, j=j: nc.gpsimd.indirect_dma_start(
                        out=g[:, :], out_offset=None, in_=C.UVB[:, :],
                        in_offset=bass.IndirectOffsetOnAxis(ap=ei[:, j:j + 1], axis=0), element_offset=l * 16384 * 4096),
                        [ei, C.UVB], [g], dma=True)
                    P.dve(lambda g=g, j=j, f=f, last=(jj == 3): (nc.vector.scalar_tensor_tensor(
                        out=junk[:, :], in0=g[:, 0:D], scalar=1.0, in1=f[:, :], op0=ALU.mult, op1=ALU.mult,
                        accum_out=araw[:, j:j + 1]),
                        nc.vector.tensor_copy(out=P.scr[:, 1:2], in_=araw[:, j:j + 1]) if last else None)[1 if last else 0],
                        [g, f], [junk, araw], soft=(junk, araw))
                c0, c1 = blk * 4, blk * 4 + 4
                P.act(lambda c0=c0, c1=c1: nc.scalar.activation(out=gl[:, c0:c1], in_=araw[:, c0:c1], func=AF.Gelu), [araw], [gl])
                P.dve(lambda c0=c0, c1=c1: nc.vector.tensor_tensor(out=wgt[:, c0:c1], in0=gl[:, c0:c1], in1=gate2[:, c0:c1],
                                                                   op=ALU.mult), [gl, gate], [wgt])
                for jj in range(4):
                    j = blk * 4 + jj
                    g = gs_[jj]
                    if j == 0:
                        P.dve(lambda g=g: nc.vector.tensor_scalar(out=acc[:, :], in0=g[:, D:2 * D], scalar1=wgt[:, 0:1], scalar2=None,
                                                                  op0=ALU.mult), [g, wgt], [acc])
                    else:
                        P.dve(lambda g=g, j=j: nc.vector.scalar_tensor_tensor(
                            out=acc[:, :], in0=g[:, D:2 * D], scalar=wgt[:, j:j + 1], in1=acc[:, :], op0=ALU.mult, op1=ALU.add),
                            [g, wgt, acc], [acc], soft=(acc,))
            P.dve(lambda kind=kind: nc.vector.tensor_tensor(out=acc[:, :], in0=acc[:, :], in1=g2[kind][:, :], op=ALU.mult),
                  [acc, g2[kind]], [acc])
            P.dve(lambda hT=hT: nc.vector.tensor_tensor(out=hT[:, :], in0=hT[:, :], in1=acc[:, :], op=ALU.add), [hT, acc], [hT])
            if split:
                P.dma(C.H2[rows, :], hT[:, :], reads=[hT], writes=[C.H2])
            else:
                P.dma(C.H[rows, :], hT[:, :], reads=[hT], writes=[C.H])


def seg2_half(seg2):
    return Res("seg2h", seg2.t[:, 0:128])


def phase_ssd_in(P, C, l):
    nc = P.nc
    with P.scope():
        Gp = [P.sb(f"siGp{k}", [128, D]) for k in range(2)]
        Sh = [P.sb(f"siSh{k}", [128, D]) for k in range(2)]
        prep_mod(P, C, l, 0, 1, C.norm1_g[l, :], C.norm1_g, Gp, Sh)
        G = 4
        xs = [P.sb(f"six{i}", [128, D]) for i in range(G)]
        xT = [P.sb(f"sixT{i}", [128, 16, 128], BF16) for i in range(G)]
        C.wbuf16 = [P.sb(f"siw16_{i}", [128, 16, 512], BF16) for i in range(2)]
        ob = [P.sb(f"sio{i}", [128, 512]) for i in range(4)]
        junk = P.sb("sijunk", [128, D])
        st = P.sb("sist", [128, 4])
        C.wbuf = [P.sb(f"siw{i}", [128, 16, 512]) for i in range(2)]
        cnt = [0]
        for kind, t0, nt in tile_groups(C, G):
            for i in range(nt):
                t = t0 + i
                P.dma(xs[i][:, :], C.H[t * 128:(t + 1) * 128, :], reads=[C.H], writes=[xs[i]])
                C.xres, C.ores = xs[i], xs[i]
                norm_mod(P, C, xs[i][:, :], xs[i][:, :], Gp[kind], Sh[kind], junk, st)
                tr_chunks(P, C, xs[i], 16, xT[i])

            def consume(n, ti, pb, w, t0=t0):
                o = ob[cnt[0] % 4]
                cnt[0] += 1
                t = t0 + ti
                if cnt[0] % 2:
                    P.act(lambda: nc.scalar.copy(out=o[:, 0:w], in_=pb[:, 0:w]), [pb], [o])
                else:
                    P.dve(lambda: nc.vector.tensor_copy(out=o[:, 0:w], in_=pb[:, 0:w]), [pb], [o])
                P.dma(C.SP[t * 128:(t + 1) * 128, n * 512:n * 512 + w], o[:, 0:w], reads=[o], writes=[C.SP])
            C.wsrc = C.ssd_w_in
            linear(P, C, xT[0:nt], 16, C.ssd_w_in[0], 10368, consume, bf16=True)


def phase_ssd_conv(P, C):
    nc = P.nc
    CW = 2048
    with P.scope():
        wk = P.sb("scw", [128, 5, CW])
        bias = P.sb("scb", [128, CW])
        xk = [P.sb(f"scx{k}", [128, CW]) for k in range(5)]
        for c in range(3):
            cols = slice(4096 + c * CW, 4096 + (c + 1) * CW)
            for k in range(5):
                P.dma(wk[:, k, :], C.ssd_conv_w[0, k, c * CW:(c + 1) * CW].partition_broadcast(128),
                      reads=[C.ssd_conv_w], writes=[wk])
            P.dma(bias[:, :], C.ssd_conv_b[0, c * CW:(c + 1) * CW].partition_broadcast(128), reads=[C.ssd_conv_b], writes=[bias])
            for t in range(C.T):
                s0, s1 = (0, C.NCT) if t < C.NCT else (C.NCT, C.T)
                r0 = t * 128
                for k in range(5):
                    lo, hi = r0 + k - 2, r0 + k - 2 + 128
                    vlo, vhi = max(lo, s0 * 128), min(hi, s1 * 128)
                    if vlo > lo or vhi < hi:
                        P.pool(lambda k=k: nc.gpsimd.memset(xk[k][:, :], 0.0), [], [xk[k]])
                    P.dma(xk[k][vlo - lo:vhi - lo, :], C.SP[vlo:vhi, cols], reads=[C.SP], writes=[xk[k]])
                for k in range(5):
                    if k % 2 == 0:
                        P.dve(lambda k=k: nc.vector.tensor_tensor(out=xk[k][:, :], in0=xk[k][:, :], in1=wk[:, k, :], op=ALU.mult),
                              [xk[k], wk], [xk[k]])
                    else:
                        P.pool(lambda k=k: nc.gpsimd.tensor_tensor(out=xk[k][:, :], in0=xk[k][:, :], in1=wk[:, k, :], op=ALU.mult),
                               [xk[k], wk], [xk[k]])
                P.pool(lambda: nc.gpsimd.tensor_tensor(out=xk[1][:, :], in0=xk[1][:, :], in1=xk[3][:, :], op=ALU.add),
                       [xk[1], xk[3]], [xk[1]])
                P.dve(lambda: nc.vector.tensor_tensor(out=xk[0][:, :], in0=xk[0][:, :], in1=xk[2][:, :], op=ALU.add),
                      [xk[0], xk[2]], [xk[0]])
                P.pool(lambda: nc.gpsimd.tensor_tensor(out=xk[4][:, :], in0=xk[4][:, :], in1=bias[:, :], op=ALU.add),
                       [xk[4], bias], [xk[4]])
                P.dve(lambda: nc.vector.tensor_tensor(out=xk[0][:, :], in0=xk[0][:, :], in1=xk[1][:, :], op=ALU.add),
                      [xk[0], xk[1]], [xk[0]])
                P.dve(lambda: nc.vector.tensor_tensor(out=xk[0][:, :], in0=xk[0][:, :], in1=xk[4][:, :], op=ALU.add),
                      [xk[0], xk[4]], [xk[0]])
                P.act(lambda: nc.scalar.activation(out=xk[0][:, :], in_=xk[0][:, :], func=AF.Silu), [xk[0]], [xk[0]])
                P.dma(C.XBC[r0:r0 + 128, c * CW:(c + 1) * CW], xk[0][:, :], reads=[xk[0]], writes=[C.XBC])


def ssd_pass(P, C, d):
    nc = P.nc
    NCT, NL, T = C.NCT, C.NL, C.T
    with P.scope():
        tri = P.sb("sstri", [128, 128])
        P.dma(tri[:, :], C.tri[d], reads=[C.tri], writes=[tri])
        nmask = P.sb("ssnm", [128, 128])
        P.dma(nmask[:, :], C.ssdmask[d], reads=[C.ssdmask], writes=[nmask])
        ones = P.sb("ssones", [128, 128])
        P.pool(lambda: nc.gpsimd.memset(ones[:, :], 1.0), [], [ones])
        abc = P.sb("ssa", [128, 64])
        load_bc(P, abc, C.ssd_a_log[0, d * 64:(d + 1) * 64], C.ssd_a_log)
        P.act(lambda: nc.scalar.activation(out=abc[:, :], in_=abc[:, :], func=AF.Exp), [abc], [abc])
        P.dve(lambda: nc.vector.tensor_scalar(out=abc[:, :], in0=abc[:, :], scalar1=-1.0, scalar2=None, op0=ALU.mult), [abc], [abc])
        dtb = P.sb("ssdtb", [128, 64])
        load_bc(P, dtb, C.ssd_dt_bias[0, d * 64:(d + 1) * 64], C.ssd_dt_bias)
        dsk = P.sb("ssdsk", [128, 64])
        load_bc(P, dsk, C.ssd_d[0, :], C.ssd_d)
        ST = P.sb("ssST", [128, 4096])
        P.pool(lambda: nc.gpsimd.memset(ST[:, :], 0.0), [], [ST])
        xs = P.sb("ssx", [128, 4096]); xdd = P.sb("ssxdd", [128, 4096]); yt = P.sb("ssy", [128, 4096])
        bc = P.sb("ssbc", [128, 2048])
        BT = P.sb("ssBT", [128, 8, 128]); CT = P.sb("ssCT", [128, 8, 128]); cbT = P.sb("sscbT", [128, 8, 128])
        sm = P.sb("sssm", [128, 16, 64])
        LT = [P.sb(f"ssLT{i}", [128, 128]) for i in range(4)]
        dec = [P.sb(f"ssdec{i}", [128, 128]) for i in range(4)]
        yo = [P.sb(f"ssyo{i}", [128, 512]) for i in range(2)]
        yd = [P.sb(f"ssyd{i}", [128, 512]) for i in range(2)]
        if d == 0:
            order = list(range(T))
        else:
            order = list(range(NCT - 1, -1, -1)) + list(range(T - 1, NCT - 1, -1))
        DTR, X_, LA, CS, TOT, DTE, ECS, NCS, TMP, DT, ETOT = range(11)
        S = lambda k: sm[:, k, :]
        for t in order:
            lat = t >= NCT
            rows = slice(t * 128, (t + 1) * 128)
            lrows = slice((t - NCT) * 128, (t - NCT + 1) * 128)
            P.dma(xs[:, :], C.XBC[rows, 0:4096], reads=[C.XBC], writes=[xs])
            P.dma(bc[:, :], C.XBC[rows, 4096:6144], reads=[C.XBC], writes=[bc])
            P.dma(sm[:, DTR, :], C.SP[rows, 10240 + d * 64:10240 + (d + 1) * 64], reads=[C.SP], writes=[sm])
            P.dve(lambda: nc.vector.tensor_tensor(out=S(X_), in0=S(DTR), in1=dtb[:, :], op=ALU.add), [sm, dtb], [sm])
            P.act(lambda: nc.scalar.activation(out=S(TMP), in_=S(X_), func=AF.Abs), [sm], [sm])
            P.act(lambda: nc.scalar.activation(out=S(TMP), in_=S(TMP), func=AF.Exp, scale=-1.0), [sm], [sm])
            P.act(lambda: nc.scalar.activation(out=S(TMP), in_=S(TMP), func=AF.Ln, bias=1.0), [sm], [sm])
            P.dve(lambda: nc.vector.tensor_scalar(out=S(DT), in0=S(X_), scalar1=0.0, scalar2=None, op0=ALU.max), [sm], [sm])
            P.dve(lambda: nc.vector.tensor_tensor(out=S(DT), in0=S(DT), in1=S(TMP), op=ALU.add), [sm], [sm])
            P.dve(lambda: nc.vector.tensor_tensor(out=S(LA), in0=S(DT), in1=abc[:, :], op=ALU.mult), [sm, abc], [sm])
            pb = P.bank()
            P.pe(lambda pb=pb: nc.tensor.matmul(pb[:, 0:64], lhsT=tri[:, :], rhs=S(LA), start=True, stop=True), [tri, sm], [pb])
            P.pe(lambda pb=pb: nc.tensor.matmul(pb[:, 64:128], lhsT=ones[:, :], rhs=S(LA), start=True, stop=True), [ones, sm], [pb])
            P.act(lambda pb=pb: nc.scalar.copy(out=sm[:, CS:CS + 2, :], in_=pb[:, 0:128].rearrange("p (a b) -> p a b", b=64)),
                  [pb], [sm])
            P.dve(lambda: nc.vector.tensor_tensor(out=S(DTE), in0=S(TOT), in1=S(CS), op=ALU.subtract), [sm], [sm])
            P.act(lambda: nc.scalar.activation(out=S(DTE), in_=S(DTE), func=AF.Exp), [sm], [sm])
            P.act(lambda: nc.scalar.activation(out=S(ECS), in_=S(CS), func=AF.Exp), [sm], [sm])
            P.act(lambda: nc.scalar.activation(out=S(ETOT), in_=S(TOT), func=AF.Exp), [sm], [sm])
            P.dve(lambda: nc.vector.tensor_scalar(out=S(NCS), in0=S(CS), scalar1=-1.0, scalar2=None, op0=ALU.mult), [sm], [sm])
            x3 = xs[:, :].rearrange("p (h q) -> p h q", q=64)
            if lat:
                if d == 0:
                    P.pool(lambda: nc.gpsimd.tensor_tensor(out=yt[:, :].rearrange("p (h q) -> p h q", q=64), in0=x3,
                                                           in1=dsk[:, :].unsqueeze(2).to_broadcast([128, 64, 64]), op=ALU.mult),
                           [xs, dsk], [yt])
                else:
                    P.dma(yt[:, :], C.YS[lrows, :], reads=[C.YS], writes=[yt])
            P.dve(lambda: nc.vector.tensor_tensor(out=x3, in0=x3, in1=S(DT).unsqueeze(2).to_broadcast([128, 64, 64]), op=ALU.mult),
                  [xs, sm], [xs])
            P.dve(lambda: nc.vector.tensor_tensor(out=xdd[:, :].rearrange("p (h q) -> p h q", q=64), in0=x3,
                                                  in1=S(DTE).unsqueeze(2).to_broadcast([128, 64, 64]), op=ALU.mult),
                  [xs, sm], [xdd])
            if lat:
                tr_chunks(P, C, bc, 8, BT, col0=0)
                tr_chunks(P, C, bc, 8, CT, col0=1024)
                for g in range(8):
                    pb = P.bank()
                    P.pe(lambda pb=pb, g=g: nc.tensor.matmul(pb[:, 0:128], lhsT=BT[:, g, :], rhs=CT[:, g, :], start=True, stop=True),
                         [BT, CT], [pb])
                    P.act(lambda pb=pb, g=g: nc.scalar.copy(out=cbT[:, g, :], in_=pb[:, 0:128]), [pb], [cbT])
                for g in range(8):
                    pbo = P.bank()
                    P.pe(lambda pbo=pbo, g=g: nc.tensor.matmul(pbo[:, :], lhsT=CT[:, g, :], rhs=ST[:, g * 512:(g + 1) * 512],
                                                               start=True, stop=True), [CT, ST], [pbo])
                    yo_ = yo[g % 2]
                    P.dve(lambda pbo=pbo, yo_=yo_, g=g: nc.vector.tensor_tensor(
                        out=yo_[:, :].rearrange("p (h q) -> p h q", q=64), in0=pbo[:, :].rearrange("p (h q) -> p h q", q=64),
                        in1=sm[:, ECS, g * 8:(g + 1) * 8].unsqueeze(2).to_broadcast([128, 8, 64]), op=ALU.mult), [pbo, sm], [yo_])
                    pby = P.bank()
                    P.reserved = {(P.psn - 1) % 8}
                    for j in range(8):
                        h = g * 8 + j
                        lt, dc = LT[h % 4], dec[h % 4]
                        if h % 2:
                            P.pool(lambda lt=lt, h=h: nc.gpsimd.tensor_scalar(out=lt[:, :], in0=tri[:, :], scalar1=sm[:, LA, h:h + 1],
                                                                             scalar2=None, op0=ALU.mult), [tri, sm], [lt])
                        else:
                            P.act(lambda lt=lt, h=h: nc.scalar.activation(out=lt[:, :], in_=tri[:, :], func=AF.Copy,
                                                                          scale=sm[:, LA, h:h + 1]), [tri, sm], [lt])
                        pbd = P.bank()
                        P.pe(lambda pbd=pbd, lt=lt: nc.tensor.matmul(pbd[:, 0:128], lhsT=ones[:, :], rhs=lt[:, :], start=True, stop=False),
                             [ones, lt], [pbd])
                        P.pe(lambda pbd=pbd: nc.tensor.matmul(pbd[:, 0:128], lhsT=C.ident[:, :], rhs=nmask[:, :], start=False, stop=True),
                             [C.ident, nmask], [pbd])
                        P.act(lambda pbd=pbd, dc=dc, h=h: nc.scalar.activation(out=dc[:, :], in_=pbd[:, 0:128], func=AF.Exp,
                                                                                bias=sm[:, NCS, h:h + 1], scale=1.0), [pbd, sm], [dc])
                        P.dve(lambda dc=dc, g=g: nc.vector.tensor_tensor(out=dc[:, :], in0=dc[:, :], in1=cbT[:, g, :], op=ALU.mult),
                              [dc, cbT], [dc])
                        P.pe(lambda pby=pby, dc=dc, j=j, h=h: nc.tensor.matmul(pby[:, j * 64:(j + 1) * 64], lhsT=dc[:, :],
                                                                               rhs=xs[:, h * 64:(h + 1) * 64], start=True, stop=True),
                             [dc, xs], [pby])
                    P.reserved = set()
                    yd_ = yd[g % 2]
                    P.act(lambda pby=pby, yd_=yd_: nc.scalar.copy(out=yd_[:, :], in_=pby[:, :]), [pby], [yd_])
                    P.dve(lambda yo_=yo_, yd_=yd_: nc.vector.tensor_tensor(out=yo_[:, :], in0=yo_[:, :], in1=yd_[:, :], op=ALU.add),
                          [yo_, yd_], [yo_])
                    P.pool(lambda yo_=yo_, g=g: nc.gpsimd.tensor_tensor(out=yt[:, g * 512:(g + 1) * 512], in0=yt[:, g * 512:(g + 1) * 512],
                                                                      in1=yo_[:, :], op=ALU.add), [yo_, yt], [yt])
                P.dma(C.YS[lrows, :], yt[:, :], reads=[yt], writes=[C.YS])
                if t == NCT:
                    P.dump(f"ssm{d}", sm[:, :, :], sm, [128, 16, 64])
                    P.dump(f"scbT{d}", cbT[:, :, :], cbT, [128, 8, 128])
                    P.dump(f"sdec{d}", dec[3][:, :], dec[3], [128, 128])
                    P.dump(f"sST{d}", ST[:, :], ST, [128, 4096])
                    P.dump(f"sxs{d}", xs[:, :], xs, [128, 4096])
                    P.dump(f"syd{d}", yd[1][:, :], yd[1], [128, 512])
                    P.dump(f"syo{d}", yo[1][:, :], yo[1], [128, 512])
            P.dve(lambda: nc.vector.tensor_tensor(out=ST[:, :].rearrange("p (h q) -> p h q", q=64),
                                                  in0=ST[:, :].rearrange("p (h q) -> p h q", q=64),
                                                  in1=S(ETOT).unsqueeze(2).to_broadcast([128, 64, 64]), op=ALU.mult), [ST, sm], [ST])
            for g in range(8):
                pbs = P.bank()
                P.pe(lambda pbs=pbs, g=g: nc.tensor.matmul(pbs[:, :], lhsT=bc[:, g * 128:(g + 1) * 128], rhs=xdd[:, g * 512:(g + 1) * 512],
                                                           start=True, stop=True), [bc, xdd], [pbs])
                P.dve(lambda pbs=pbs, g=g: nc.vector.tensor_tensor(out=ST[:, g * 512:(g + 1) * 512], in0=ST[:, g * 512:(g + 1) * 512],
                                                                   in1=pbs[:, :], op=ALU.add), [pbs, ST], [ST])


def phase_ssd_out(P, C, l):
    nc = P.nc
    with P.scope():
        g1 = P.sb("sog1", [128, D])
        P.dma(g1[:, :], C.MOD[l, 0, :, 2 * D:3 * D], reads=[C.MOD], writes=[g1])
        ng = P.sb("song", [128, 4096])
        load_bc(P, ng, C.ssd_norm_g[0, :], C.ssd_norm_g)
        G = 1
        yb = [P.sb(f"soy{i}", [128, 4096]) for i in range(G)]
        zb = [P.sb(f"soz{i}", [128, 4096]) for i in range(G)]
        yT = [P.sb(f"soyT{i}", [128, 32, 128], BF16) for i in range(G)]
        C.wbuf16 = [P.sb(f"sow16_{i}", [128, 32, 256], BF16) for i in range(2)]
        ht = [P.sb(f"soh{i}", [128, D]) for i in range(G)]
        zt = [P.sb(f"sozt{i}", [128, 256]) for i in range(2)]
        st = P.sb("sost", [128, 4, 8])
        junk = P.sb("sojunk", [128, 512])
        C.wbuf = [P.sb(f"sow{i}", [128, 32, 256]) for i in range(2)]
        for t0 in range(0, C.NL, G):
            nt = min(G, C.NL - t0)
            for i in range(nt):
                lrows = slice((t0 + i) * 128, (t0 + i + 1) * 128)
                rows = slice((C.NCT + t0 + i) * 128, (C.NCT + t0 + i + 1) * 128)
                y, z = yb[i], zb[i]
                P.dma(y[:, :], C.YS[lrows, :], reads=[C.YS], writes=[y])
                P.dma(z[:, :], C.SP[rows, 0:4096], reads=[C.SP], writes=[z])
                P.dma(ht[i][:, :], C.H[rows, :], reads=[C.H], writes=[ht[i]])
                P.act(lambda z=z: nc.scalar.activation(out=z[:, :], in_=z[:, :], func=AF.Silu), [z], [z])
                P.dve(lambda y=y, z=z: nc.vector.tensor_tensor(out=y[:, :], in0=y[:, :], in1=z[:, :], op=ALU.mult), [y, z], [y])
                for g in range(8):
                    P.act(lambda y=y, g=g: (nc.scalar.activation(out=junk[:, :], in_=y[:, g * 512:(g + 1) * 512], func=AF.Square,
                                                                 accum_out=st[:, 0, g:g + 1]),
                                            nc.scalar.copy(out=P.scr[:, 0:1], in_=st[:, 0, g:g + 1]))[1], [y], [junk, st])
                P.dve(lambda: nc.vector.tensor_scalar(out=st[:, 1, :], in0=st[:, 0, :], scalar1=1.0 / 512, scalar2=EPS,
                                                      op0=ALU.mult, op1=ALU.add), [st], [st])
                P.act(lambda: nc.scalar.activation(out=st[:, 2, :], in_=st[:, 1, :], func=AF.Sqrt), [st], [st])
                P.dve(lambda: nc.vector.reciprocal(out=st[:, 3, :], in_=st[:, 2, :]), [st], [st])
                P.dve(lambda y=y: nc.vector.tensor_tensor(out=y[:, :].rearrange("p (g q) -> p g q", q=512),
                                                          in0=y[:, :].rearrange("p (g q) -> p g q", q=512),
                                                          in1=st[:, 3, :].unsqueeze(2).to_broadcast([128, 8, 512]), op=ALU.mult),
                      [y, st], [y])
                P.pool(lambda y=y: nc.gpsimd.tensor_tensor(out=y[:, :], in0=y[:, :], in1=ng[:, :], op=ALU.mult), [y, ng], [y])
                tr_chunks(P, C, y, 32, yT[i])

            def consume_out(n, ti, pb, w):
                z = zt[(n + ti) % 2]
                P.dve(lambda: nc.vector.tensor_tensor(out=z[:, 0:w], in0=pb[:, 0:w], in1=g1[:, n * 256:n * 256 + w], op=ALU.mult),
                      [pb, g1], [z])
                P.dve(lambda: nc.vector.tensor_tensor(out=ht[ti][:, n * 256:n * 256 + w], in0=ht[ti][:, n * 256:n * 256 + w],
                                                      in1=z[:, 0:w], op=ALU.add), [ht[ti], z], [ht[ti]])
            C.wsrc = C.ssd_w_out
            linear(P, C, yT[0:nt], 32, C.ssd_w_out[0], 2048, consume_out, cw=256, bf16=True)
            for i in range(nt):
                rows = slice((C.NCT + t0 + i) * 128, (C.NCT + t0 + i + 1) * 128)
                P.dma(C.H[rows, :], ht[i][:, :], reads=[ht[i]], writes=[C.H])


def phase_final(P, C):
    nc = P.nc
    with P.scope():
        g = P.sb("fng", [128, D])
        load_bc(P, g, C.final_norm_g[:], C.final_norm_g)
        xb = [P.sb(f"fnx{i}", [128, D]) for i in range(2)]
        junk = P.sb("fnjunk", [128, D])
        stb = [P.sb(f"fnst{i}", [128, 4]) for i in range(2)]
        for t in range(C.NO):
            x, st = xb[t % 2], stb[t % 2]
            rows = slice(t * 128, (t + 1) * 128)
            if SPLIT:
                P.dma(x[:, :], C.H2[rows, :], reads=[C.H2], writes=[x])
            else:
                P.dma(x[:, :], C.H[(C.NCT + t) * 128:(C.NCT + t + 1) * 128, :], reads=[C.H], writes=[x])
            P.act(lambda x=x, st=st: (nc.scalar.activation(out=junk[:, :], in_=x[:, :], func=AF.Square, accum_out=st[:, 0:1]),
                                      nc.scalar.copy(out=P.scr[:, 0:1], in_=st[:, 0:1]))[1], [x], [junk, st])
            P.dve(lambda st=st: nc.vector.tensor_scalar(out=st[:, 1:2], in0=st[:, 0:1], scalar1=1.0 / D, scalar2=EPS,
                                                        op0=ALU.mult, op1=ALU.add), [st], [st])
            P.act(lambda st=st: nc.scalar.activation(out=st[:, 3:4], in_=st[:, 1:2], func=AF.Sqrt), [st], [st])
            P.dve(lambda st=st: nc.vector.reciprocal(out=st[:, 2:3], in_=st[:, 3:4]), [st], [st])
            P.dve(lambda x=x, st=st: nc.vector.scalar_tensor_tensor(out=x[:, :], in0=x[:, :], scalar=st[:, 2:3], in1=g[:, :],
                                                                    op0=ALU.mult, op1=ALU.mult), [x, st, g], [x])
            P.dma(C.OUT[t * 128:(t + 1) * 128, :], x[:, :], reads=[x], writes=[C.OUT])


N_CORES = 8
_CACHE = {}


_DBG = {}


def run_model(inputs, NL, NCT, batches, n_cores=None):
    inputs = {k: np.asarray(v) for k, v in inputs.items()}
    P = Prog()
    C = build_program(P, NL, NCT, stage="final")
    P.finish()
    used = set(C.used_inputs)
    shared = make_inputs(inputs, 0, NL, NCT, used=used)
    nb = len(batches)
    n_cores = n_cores or 2 * nb
    NLH = NL // 2
    maps = []
    for core in range(n_cores):
        b, half = batches[core % nb], core // nb
        m = dict(shared)
        m["h0"] = np.ascontiguousarray(np.concatenate([inputs["ctx"][b][:NCT * 128], inputs["x"][b][:NL * 128]], axis=0),
                                       dtype=np.float32)
        m["c_col"] = np.ascontiguousarray(inputs["c"][b].reshape(16, 128).T, dtype=np.float32)
        if "rowidx" in used:
            k = np.arange(NLH, dtype=np.int64)[None, :]
            p = np.arange(128, dtype=np.int64)[:, None]
            m["rowidx"] = np.ascontiguousarray(((NCT + half * NLH + k) * 128 + p).astype(np.int32))
        maps.append(m)
    res = run_bass_kernel_spmd(P.nc, maps, core_ids=list(range(n_cores)))
    out = np.zeros((nb, NL * 128, 2048), np.float32)
    for core in range(n_cores):
        bi, half = core % nb, core // nb
        if SPLIT:
            out[bi, half * NLH * 128:(half + 1) * NLH * 128] = np.asarray(res.results[core]["OUT"])
        elif half == 0:
            out[bi] = np.asarray(res.results[core]["OUT"])
    return out


def kernel(**inputs):
    return run_model(inputs, 32, 2, [0, 1, 2, 3], n_cores=N_CORES)
```
